# Optimizing a Trainium2 kernel written in Bass

```python
import math
import jax, jax.numpy as jnp
from jax import lax
import numpy as np

D_MODEL = 1024
BATCH = 8
SEQ = 2048
DEPTH = 1

SSM_GROUP_CH = 16
SSM_WIDTH = D_MODEL // 2
SSM_GROUPS = SSM_WIDTH // SSM_GROUP_CH
SSM_STATE = 64
DT_MIN = 1e-3
DT_MAX = 1e-1
GMLP_WIDTH = D_MODEL // 2
GMLP_HEADS = 8
GMLP_HEAD_DIM = GMLP_WIDTH // GMLP_HEADS
CHUNK = 128
PROJ_WIDTH = SSM_WIDTH + 2 * GMLP_WIDTH + 2 * D_MODEL
N_EXPERTS = 32
TOP_K = 4
D_EXPERT = D_MODEL
SWIGLU_LIMIT = 7.0
SWIGLU_ALPHA = 1.702
ROUTE_BLOCK = 128
N_MOD = 6
EPS = 1e-6

kernel_name = 'hybrid_s5_gmlp_moe_block'


def rms_norm(x, g):
    xf = x.astype(jnp.float32)
    y = xf * lax.rsqrt(jnp.mean(xf * xf, axis=-1, keepdims=True) + EPS)
    return (y * g.astype(jnp.float32)).astype(x.dtype)


def modulate(h, shift, scale):
    return h * (1 + scale[:, None, :]) + shift[:, None, :]


def s5_branch(u, a_re, a_im, log_dt, b_re, b_im, c_re, c_im, d_skip, glu_w, glu_b):
    bsz, seq, _ = u.shape
    uf = u.astype(jnp.float32).reshape(bsz, seq, SSM_GROUPS, SSM_GROUP_CH)
    lam = lax.complex(a_re.astype(jnp.float32), a_im.astype(jnp.float32))
    dt = jnp.exp(log_dt.astype(jnp.float32))[:, None]
    lam_bar = jnp.exp(lam * dt)
    b = lax.complex(b_re.astype(jnp.float32), b_im.astype(jnp.float32))
    b_bar = ((lam_bar - 1) / lam)[..., None] * b
    bu = jnp.einsum('bsgh,gph->bsgp', uf, b_bar)
    decay = jnp.broadcast_to(lam_bar, bu.shape)

    def combine(left, right):
        a_l, s_l = left
        a_r, s_r = right
        return a_r * a_l, a_r * s_l + s_r

    _, states = lax.associative_scan(combine, (decay, bu), axis=1)
    cm = lax.complex(c_re.astype(jnp.float32), c_im.astype(jnp.float32))
    y = jnp.einsum('bsgp,ghp->bsgh', states, cm).real
    y = y + d_skip.astype(jnp.float32).reshape(SSM_GROUPS, SSM_GROUP_CH) * uf
    y = y.reshape(bsz, seq, SSM_WIDTH)
    z = jax.nn.gelu(y)
    out = z * jax.nn.sigmoid(z @ glu_w.astype(jnp.float32) + glu_b.astype(jnp.float32))
    return out.astype(u.dtype)


def gmlp_branch(zuv, ln_g, ln_b, ws, bs):
    bsz, seq, _ = zuv.shape
    z = jax.nn.gelu(zuv)
    u, v = jnp.split(z, 2, axis=-1)
    vf = v.astype(jnp.float32)
    mu = jnp.mean(vf, axis=-1, keepdims=True)
    var = jnp.mean(jnp.square(vf - mu), axis=-1, keepdims=True)
    vn = (vf - mu) * lax.rsqrt(var + EPS) * ln_g.astype(jnp.float32) + ln_b.astype(jnp.float32)
    vn = vn.reshape(bsz, seq // CHUNK, CHUNK, GMLP_HEADS, GMLP_HEAD_DIM)
    causal = jnp.tril(jnp.ones((CHUNK, CHUNK), jnp.float32))
    w = ws.astype(jnp.float32) * causal
    mixed = jnp.einsum('hts,bnshc->bnthc', w, vn) + bs.astype(jnp.float32).T[:, :, None]
    return u * mixed.reshape(bsz, seq, GMLP_WIDTH).astype(u.dtype)


def moe(h, router_w, router_b, w_in, b_in, w_out, b_out):
    bsz, seq, d = h.shape
    tok = h.reshape(-1, d)
    n_tok = tok.shape[0]
    logits = (tok @ router_w + router_b).astype(jnp.float32)
    top_val, top_idx = lax.top_k(logits, TOP_K)
    weights = jax.nn.softmax(top_val, axis=-1)
    n_assign = n_tok * TOP_K
    flat_e = top_idx.reshape(-1)
    flat_tok = jnp.repeat(jnp.arange(n_tok, dtype=jnp.int32), TOP_K)
    flat_w = weights.reshape(-1)
    order = jnp.argsort(flat_e)
    sorted_e = flat_e[order]
    counts = jnp.bincount(flat_e, length=N_EXPERTS)
    start = jnp.cumsum(counts) - counts
    padded = (counts + ROUTE_BLOCK - 1) // ROUTE_BLOCK * ROUTE_BLOCK
    pad_end = jnp.cumsum(padded)
    pad_start = pad_end - padded
    rank = jnp.arange(n_assign, dtype=jnp.int32) - start[sorted_e]
    dest = pad_start[sorted_e] + rank
    n_rows = (n_assign + ROUTE_BLOCK - 1) // ROUTE_BLOCK * ROUTE_BLOCK + N_EXPERTS * ROUTE_BLOCK
    n_blocks = n_rows // ROUTE_BLOCK
    row_tok = jnp.zeros((n_rows,), jnp.int32).at[dest].set(flat_tok[order])
    row_w = jnp.zeros((n_rows,), jnp.float32).at[dest].set(flat_w[order])
    blk_start = jnp.arange(n_blocks, dtype=jnp.int32) * ROUTE_BLOCK
    blk_e = jnp.minimum(jnp.searchsorted(pad_end, blk_start, side='right'), N_EXPERTS - 1)

    def expert_block(args):
        rows, e = args
        xb = tok[rows]
        gu = xb @ w_in[e] + b_in[e]
        gate, up = gu[:, :D_EXPERT], gu[:, D_EXPERT:]
        gate = jnp.minimum(gate, SWIGLU_LIMIT)
        up = jnp.clip(up, -SWIGLU_LIMIT, SWIGLU_LIMIT)
        act = (up + 1) * (gate * jax.nn.sigmoid(SWIGLU_ALPHA * gate))
        return act @ w_out[e] + b_out[e]

    ys = lax.map(expert_block, (row_tok.reshape(n_blocks, ROUTE_BLOCK), blk_e))
    ys = ys.reshape(n_rows, d)
    ys = ys * row_w[:, None].astype(ys.dtype)
    out = jnp.zeros_like(tok).at[row_tok].add(ys.astype(tok.dtype))
    return out.reshape(bsz, seq, d)


def setup_inputs(seed: int = 0) -> dict:
    key = jax.random.key(seed)
    ks = jax.random.split(key, 32)
    f32 = jnp.float32
    nrm = lambda k, shape, s: jax.random.normal(k, shape, f32) * s
    L, D, G, P, H = DEPTH, D_MODEL, SSM_GROUPS, SSM_STATE, SSM_GROUP_CH
    n_idx = jnp.arange(P, dtype=f32)
    return {
        'x': nrm(ks[0], (BATCH, SEQ, D), 1.0),
        'c': nrm(ks[1], (BATCH, D), 1.0),
        'ada_w': nrm(ks[2], (L, D, N_MOD * D), 0.5 * D ** -0.5),
        'ada_b': nrm(ks[3], (L, N_MOD * D), 0.02),
        'norm1_g': 1.0 + nrm(ks[4], (L, D), 0.02),
        'w_in': nrm(ks[5], (L, D, PROJ_WIDTH), D ** -0.5),
        'ssm_a_re': -0.5 + nrm(ks[6], (L, G, P), 0.01),
        'ssm_a_im': math.pi * n_idx + nrm(ks[7], (L, G, P), 0.01),
        'ssm_log_dt': jax.random.uniform(ks[8], (L, G), f32, math.log(DT_MIN), math.log(DT_MAX)),
        'ssm_b_re': nrm(ks[9], (L, G, P, H), (2 * H) ** -0.5),
        'ssm_b_im': nrm(ks[10], (L, G, P, H), (2 * H) ** -0.5),
        'ssm_c_re': nrm(ks[11], (L, G, H, P), P ** -0.5),
        'ssm_c_im': nrm(ks[12], (L, G, H, P), P ** -0.5),
        'ssm_d': nrm(ks[13], (L, SSM_WIDTH), 1.0),
        'ssm_glu_w': nrm(ks[14], (L, SSM_WIDTH, SSM_WIDTH), SSM_WIDTH ** -0.5),
        'ssm_glu_b': nrm(ks[15], (L, SSM_WIDTH), 0.02),
        'w_branch_a': nrm(ks[16], (L, SSM_WIDTH, D), SSM_WIDTH ** -0.5),
        'gmlp_ln_g': 1.0 + nrm(ks[17], (L, GMLP_WIDTH), 0.02),
        'gmlp_ln_b': nrm(ks[18], (L, GMLP_WIDTH), 0.02),
        'gmlp_ws': nrm(ks[19], (L, GMLP_HEADS, CHUNK, CHUNK), CHUNK ** -0.5),
        'gmlp_bs': 1.0 + nrm(ks[20], (L, GMLP_HEADS, CHUNK), 0.02),
        'w_branch_b': nrm(ks[21], (L, GMLP_WIDTH, D), GMLP_WIDTH ** -0.5),
        'w_out': nrm(ks[22], (L, D, D), D ** -0.5),
        'norm2_g': 1.0 + nrm(ks[23], (L, D), 0.02),
        'router_w': nrm(ks[24], (L, D, N_EXPERTS), D ** -0.5),
        'router_b': nrm(ks[25], (L, N_EXPERTS), 0.01),
        'moe_w_in': nrm(ks[26], (L, N_EXPERTS, D, 2 * D_EXPERT), D ** -0.5),
        'moe_b_in': nrm(ks[27], (L, N_EXPERTS, 2 * D_EXPERT), 0.02),
        'moe_w_out': nrm(ks[28], (L, N_EXPERTS, D_EXPERT, D), D_EXPERT ** -0.5),
        'moe_b_out': nrm(ks[29], (L, N_EXPERTS, D), 0.02),
        'final_g': 1.0 + nrm(ks[30], (D,), 0.02),
    }


def reference(x, c, ada_w, ada_b, norm1_g, w_in, ssm_a_re, ssm_a_im, ssm_log_dt, ssm_b_re, ssm_b_im, ssm_c_re, ssm_c_im, ssm_d, ssm_glu_w, ssm_glu_b, w_branch_a, gmlp_ln_g, gmlp_ln_b, gmlp_ws, gmlp_bs, w_branch_b, w_out, norm2_g, router_w, router_b, moe_w_in, moe_b_in, moe_w_out, moe_b_out, final_g):
    split_at = [SSM_WIDTH, SSM_WIDTH + 2 * GMLP_WIDTH, SSM_WIDTH + 2 * GMLP_WIDTH + D_MODEL]
    for layer in range(DEPTH):
        mod = jax.nn.silu(c) @ ada_w[layer] + ada_b[layer]
        shift1, scale1, gate1, shift2, scale2, gate2 = jnp.split(mod, N_MOD, axis=-1)
        h = modulate(rms_norm(x, norm1_g[layer]), shift1, scale1)
        proj = h @ w_in[layer]
        u_ssm, zuv, g_a, g_b = jnp.split(proj, split_at, axis=-1)
        y_a = s5_branch(u_ssm, ssm_a_re[layer], ssm_a_im[layer], ssm_log_dt[layer], ssm_b_re[layer], ssm_b_im[layer], ssm_c_re[layer], ssm_c_im[layer], ssm_d[layer], ssm_glu_w[layer], ssm_glu_b[layer]) @ w_branch_a[layer]
        y_b = gmlp_branch(zuv, gmlp_ln_g[layer], gmlp_ln_b[layer], gmlp_ws[layer], gmlp_bs[layer]) @ w_branch_b[layer]
        merged = jax.nn.sigmoid(g_a) * y_a + jax.nn.sigmoid(g_b) * y_b
        x = x + gate1[:, None, :] * (merged @ w_out[layer])
        h = modulate(rms_norm(x, norm2_g[layer]), shift2, scale2)
        x = x + gate2[:, None, :] * moe(h, router_w[layer], router_b[layer], moe_w_in[layer], moe_b_in[layer], moe_w_out[layer], moe_b_out[layer])
    return rms_norm(x, final_g)
```

```python
import numpy as np
from contextlib import ExitStack
import concourse.bass as bass
import concourse.mybir as mybir
from concourse.bass_utils import run_bass_kernel_spmd

F32 = mybir.dt.float32
BF16 = mybir.dt.bfloat16
I32 = mybir.dt.int32
AF = mybir.ActivationFunctionType
ALU = mybir.AluOpType

T = 2048
D = 1024
NT = 16
ST = 256
NST = T // ST
NE = 32
EPS = 1e-6
PI = float(np.pi)
TWO_PI = float(2 * np.pi)


class Op:
    __slots__ = ("eng", "emit", "deps", "is_dma", "slot", "val", "signal", "mark")


class Prog:
    ENGS = ("pe", "act", "dve", "pool", "sp")

    def __init__(self, nc, es, tag):
        self.nc = nc
        self.es = es
        self.tag = tag
        self.q = {e: [] for e in self.ENGS}
        self.lastw = {}
        self.readers = {}
        self.slot_ops = {}
        self.group_slots = set()

    def _deps(self, reads, writes, op):
        deps = []
        for r in reads:
            w = self.lastw.get(r)
            if w is not None:
                deps.append(w)
        for r in writes:
            w = self.lastw.get(r)
            if w is not None:
                deps.append(w)
            deps.extend(self.readers.get(r, ()))
        for r in reads:
            self.readers.setdefault(r, []).append(op)
        for r in writes:
            self.lastw[r] = op
            self.readers[r] = []
        return [d for d in deps if d is not op]

    def op(self, eng, emit, reads=(), writes=()):
        o = Op()
        o.eng, o.emit, o.is_dma, o.slot, o.signal = eng, emit, False, None, False
        o.mark = None
        o.deps = self._deps(reads, writes, o)
        self.q[eng].append(o)
        return o

    def marker(self, mark):
        for e in self.ENGS:
            o = Op()
            o.eng, o.emit, o.is_dma, o.slot, o.signal, o.mark, o.deps = e, None, False, None, False, mark, []
            self.q[e].append(o)

    def regload(self, regs, ap, reads):
        for e in self.ENGS:
            self.op(e, (lambda eng, e=e: eng.reg_load(regs[e], ap)), reads, ())

    def dma(self, eng, out, in_, reads=(), writes=(), slot=None, group=False, custom=None):
        o = Op()
        o.eng, o.is_dma, o.slot, o.signal = eng, True, slot, True
        o.mark = None
        o.emit = (lambda e: e.dma_start(out=out, in_=in_)) if custom is None else custom
        o.deps = self._deps(reads, writes, o)
        if group:
            o.deps = [d for d in o.deps if not (d.is_dma and d.slot == slot)]
            self.group_slots.add(slot)
        self.q[eng].append(o)
        self.slot_ops.setdefault(slot, []).append(o)
        return o

    def barrier(self):
        pend = []
        for e in self.ENGS:
            for o in reversed(self.q[e]):
                if not o.is_dma and o.emit is not None:
                    pend.append(o)
                    break
        for s, ops in self.slot_ops.items():
            pend.append(ops[-1])
        for e in self.ENGS:
            o = Op()
            o.eng, o.emit, o.is_dma, o.slot, o.signal = e, None, False, None, False
            o.mark = None
            o.deps = list(pend)
            self.q[e].append(o)
        self.lastw = {}
        self.readers = {}

    def emit(self):
        nc = self.nc
        for e in self.ENGS:
            for o in self.q[e]:
                for d in o.deps:
                    d.signal = True
        esem = {}
        for e in self.ENGS:
            esem[e] = self.es.enter_context(nc.semaphore("s%s_%s" % (self.tag, e)))
            c = 0
            for o in self.q[e]:
                if o.is_dma or o.emit is None:
                    continue
                if o.signal:
                    c += 1
                    o.val = c
        ssem = {}
        for i, (s, ops) in enumerate(self.slot_ops.items()):
            ssem[s] = self.es.enter_context(nc.semaphore("d%s_%d" % (self.tag, i)))
            if s in self.group_slots:
                for o in ops:
                    o.val = 16 * len(ops)
            else:
                for j, o in enumerate(ops):
                    o.val = 16 * (j + 1)

        regs = getattr(self, "regs", None)

        def emit_q(e, eng):
            seen = {}

            def emit_one(o):
                need = {}
                for d in o.deps:
                    if d.is_dma:
                        sem = ssem[d.slot]
                    else:
                        if d.emit is None or (d.eng == "pe" and e == "pe"):
                            continue
                        sem = esem[d.eng]
                    k = id(sem)
                    if need.get(k, (None, 0))[1] < d.val:
                        need[k] = (sem, d.val)
                for k, (sem, v) in need.items():
                    if seen.get(k, 0) < v:
                        eng.wait_ge(sem, v)
                        seen[k] = v
                if o.emit is None:
                    return
                ins = o.emit(eng)
                if o.is_dma:
                    ins.then_inc(ssem[o.slot], 16)
                elif o.signal:
                    ins.then_inc(esem[e], 1)

            def emit_range(ops):
                i = 0
                while i < len(ops):
                    o = ops[i]
                    if o.mark is not None and o.mark[0] == "rb":
                        depth = 1
                        j = i + 1
                        while True:
                            m = ops[j].mark
                            if m is not None and m[0] == "rb":
                                depth += 1
                            elif m is not None and m[0] == "re":
                                depth -= 1
                                if depth == 0:
                                    break
                            j += 1
                        body = ops[i + 1:j]
                        real = [b for b in body if b.emit is not None]
                        if real:
                            c = sum(1 for b in real if (not b.is_dma) and b.signal)
                            dcnt = {}
                            for b in real:
                                if b.is_dma:
                                    dcnt[b.slot] = dcnt.get(b.slot, 0) + 1
                            saved = dict(seen)
                            g = eng.If_lt(regs[e], o.mark[1])
                            g.__enter__()
                            if c > 0:
                                eng.sem_inc(esem[e], c)
                            for sl, n in dcnt.items():
                                eng.sem_inc(ssem[sl], 16 * n)
                            g.__exit__(None, None, None)
                            g2 = eng.Else()
                            g2.__enter__()
                            emit_range(body)
                            g2.__exit__(None, None, None)
                            seen.clear()
                            seen.update(saved)
                        i = j + 1
                        continue
                    if o.mark is None:
                        emit_one(o)
                    i += 1

            emit_range(self.q[e])

        with nc.Block() as block:

            @block.tensor
            def _(eng):
                emit_q("pe", eng)

            @block.scalar
            def _(eng):
                emit_q("act", eng)

            @block.vector
            def _(eng):
                emit_q("dve", eng)

            @block.gpsimd
            def _(eng):
                emit_q("pool", eng)

            @block.sync
            def _(eng):
                emit_q("sp", eng)

    def mm(self, out, pairs, r, w, start=True, stop=True):
        pairs = list(pairs)

        def emit(pe):
            n = len(pairs)
            ins = None
            for i, (l, rr) in enumerate(pairs):
                ins = pe.matmul(out, l, rr, start=(start and i == 0), stop=(stop and i == n - 1))
            return ins

        return self.op("pe", emit, r, w)

    def tr(self, out, in_, ident, r, w):
        return self.op("pe", lambda e: e.transpose(out, in_, ident), r, w)

    def tt(self, out, a, b, op, r, w, eng="dve"):
        return self.op(eng, lambda e: e.tensor_tensor(out, a, b, op), r, w)

    def ts(self, out, a, s1, op0, r, w, s2=None, op1=None, eng="dve"):
        if op1 is None:
            return self.op(eng, lambda e: e.tensor_scalar(out, a, s1, None, op0), r, w)
        return self.op(eng, lambda e: e.tensor_scalar(out, a, s1, s2, op0, op1), r, w)

    def stt(self, out, a, s, b, op0, op1, r, w, eng="dve"):
        return self.op(eng, lambda e: e.scalar_tensor_tensor(out, a, s, b, op0, op1), r, w)

    def cp(self, out, a, r, w, eng="dve"):
        return self.op(eng, lambda e: e.tensor_copy(out, a), r, w)

    def memset(self, out, v, w, eng="dve"):
        return self.op(eng, lambda e: e.memset(out, v), (), w)

    def act(self, out, a, func, r, w, bias=None, scale=None, accum=None):
        kw = {}
        if bias is not None:
            kw["bias"] = bias
        if scale is not None:
            kw["scale"] = scale
        if accum is not None:
            kw["accum_out"] = accum
        return self.op("act", lambda e: e.activation(out, a, func, **kw), r, w)

    def scan(self, out, d0, d1, init, r, w):
        return self.op("dve", lambda e: e.tensor_tensor_scan(out, d0, d1, init, ALU.mult, ALU.add), r, w)


def build(stage=None):
    nc = bass.Bass("TRN2", target_bir_lowering=False)
    top = ExitStack()

    def din(name, shape):
        return nc.dram_tensor(name, list(shape), F32, kind="ExternalInput").ap()

    x_d = din("x", [T, D])
    cT_d = din("cT", [128, 8])
    ada_w_d = din("ada_w", [D, 6 * D])
    ada_b_d = din("ada_b", [1, 6 * D])
    g1_d = din("g1", [1, D])
    g2_d = din("g2", [1, D])
    fg_d = din("fg", [1, D])
    w_in_d = din("w_in", [D, 3584])
    ssm_sm_d = din("ssm_sm", [128, 3, 16])
    ssm_lam_d = din("ssm_lam", [3, 2048])
    ssm_B_d = din("ssm_B", [2, 128, 2048])
    ssm_C_d = din("ssm_C", [2, 128, 2048])
    ssm_d_d = din("ssm_d", [128, 4])
    glu_w_d = din("glu_w", [512, 512])
    glu_b_d = din("glu_b", [128, 4])
    wa_d = din("wa", [512, D])
    wb_d = din("wb", [512, D])
    wout_d = din("wout", [D, D])
    lng_d = din("lng", [1, 512])
    lnb_d = din("lnb", [1, 512])
    wsT_d = din("wsT", [128, 8, 128])
    bs_d = din("bs", [1, 1024])
    rw_d = din("router_w", [D, NE])
    rb_d = din("router_b", [1, NE])
    mwi_d = din("moe_w_in", [NE, D, 2 * D])
    mbi_d = din("moe_b_in", [128, NE, 16])
    mwo_d = din("moe_w_out", [NE, D, D])
    mbo_d = din("moe_b_out", [NE, D])
    ident_d = din("ident", [128, 128])
    tri_d = din("tri", [128, 128])
    jj_d = din("jj", [128, ST])
    stri_d = din("stri", [128, 128])
    iota_d = din("iota256", [128, 256])
    eoff_d = din("eoff", [128, NE])
    out_d = nc.dram_tensor("out", [T, D], F32, kind="ExternalOutput").ap()
    x1s_d = nc.dram_tensor("x1s", [T, D], F32, kind=("ExternalOutput" if stage == "M" else "Internal")).ap()

    def dbg_out(Pg, name, ap, shape, dt=F32):
        o = nc.dram_tensor("dbg_" + name, list(shape), dt, kind="ExternalOutput").ap()
        Pg.dma("sp", o, ap, reads=[], writes=["dbg_" + name], slot="dbg_" + name)

    def sbuf(es, name, shape, dt=F32):
        return es.enter_context(nc.sbuf_tensor("sb_" + name, list(shape), dt))

    pst = [top.enter_context(nc.psum_tensor("pst%d" % i, [128, 1024], BF16)) for i in range(2)]
    psf = [top.enter_context(nc.psum_tensor("psf%d" % i, [128, 512], F32)) for i in range(6)]
    rot = {"f": 0, "t": 0, "n": 6}

    def nxt():
        i = rot["f"] % rot["n"]
        rot["f"] += 1
        return psf[i], "psf%d" % i

    def nxt_t():
        i = rot["t"] % 2
        rot["t"] += 1
        return pst[i], "pst%d" % i

    ident_bf = sbuf(top, "ident_bf", [128, 128], BF16)
    ident_f = sbuf(top, "ident_f", [128, 128], F32)
    ones_row = sbuf(top, "ones_row", [1, 128], F32)
    epsc = sbuf(top, "epsc", [128, 1], F32)
    cols = sbuf(top, "cols", [128, 32], F32)
    gate2b = sbuf(top, "gate2b", [128, D], F32)
    modscr_d = nc.dram_tensor("modscr", [2, D], F32, kind="Internal").ap()
    regs = {"pe": top.enter_context(nc.tensor.register("r_pe")), "act": top.enter_context(nc.scalar.register("r_act")),
            "dve": top.enter_context(nc.vector.register("r_dve")), "pool": top.enter_context(nc.gpsimd.register("r_pool")),
            "sp": top.enter_context(nc.sync.register("r_sp"))}
    a1c, s1c, a2c, s2c = (cols[:, 0:8], cols[:, 8:16], cols[:, 16:24], cols[:, 24:32])

    p1 = ExitStack()
    gate1b = sbuf(p1, "gate1b", [128, D], F32)
    BT_bf = sbuf(p1, "BT_bf", [128, 16, 2, 128], BF16)
    CT_bf = sbuf(p1, "CT_bf", [128, 16, 2, 128], BF16)
    cosT = sbuf(p1, "cosT", [128, 16, ST], BF16)
    sinT = sbuf(p1, "sinT", [128, 16, ST], BF16)
    rcol = sbuf(p1, "rcol", [128, 16], F32)
    carry = sbuf(p1, "carry", [128, 16, 2], F32)
    dcol = sbuf(p1, "dcol", [128, 4], F32)
    glubc = sbuf(p1, "glubc", [128, 4], F32)

    sA = ExitStack()
    PA = Prog(nc, top, "A")
    L3 = sbuf(sA, "L3", [128, 3, 2048], F32)
    Bz = sbuf(sA, "Bz", [128, 2, 2048], F32)
    tA = sbuf(sA, "tA", [128, 4096], F32)
    tB = sbuf(sA, "tB", [128, 4096], F32)
    tC = sbuf(sA, "tC", [128, 4096], F32)
    tI = sbuf(sA, "tI", [128, 4096], I32)
    sm = sbuf(sA, "sm", [128, 3, 16], F32)
    smw = sbuf(sA, "smw", [128, 2, 16], F32)
    jj = sbuf(sA, "jj", [128, ST], F32)
    idl = sbuf(sA, "idl", [128, 128], F32)

    PA.dma("sp", idl[:], ident_d, writes=["idl"], slot="ld", group=True)
    PA.dma("sp", ident_f[:], ident_d, writes=["ident_f"], slot="ld", group=True)
    for i in range(3):
        PA.dma("sp", L3[:, i, :], ssm_lam_d[i:i + 1, :].partition_broadcast(128), writes=["L3"], slot="ld", group=True)
    for i in range(2):
        PA.dma("sp", Bz[:, i, :], ssm_B_d[i], writes=["Bz"], slot="ld", group=True)
    PA.dma("sp", sm[:], ssm_sm_d, writes=["sm"], slot="ld", group=True)
    PA.dma("sp", jj[:], jj_d, writes=["jj"], slot="ld", group=True)
    PA.dma("sp", dcol[:], ssm_d_d, writes=["dcol"], slot="ld", group=True)
    PA.dma("sp", glubc[:], glu_b_d, writes=["glubc"], slot="ld", group=True)
    PA.cp(ident_bf[:], idl[:], ["idl"], ["ident_bf"])
    PA.memset(ones_row[:], 1.0, ["ones_row"])
    PA.memset(epsc[:], EPS, ["epsc"])
    PA.memset(carry[:], 0.0, ["carry"])

    def range_reduce(Pg, y, x, shift, ntmp, itmp, rx, ry, rn, ri):
        Pg.ts(ntmp, x, 1.0 / TWO_PI, ALU.mult, [rx], [rn], s2=shift / TWO_PI, op1=ALU.add)
        Pg.cp(itmp, ntmp, [rn], [ri])
        Pg.cp(ntmp, itmp, [ri], [rn])
        Pg.ts(y, x, shift, ALU.add, [rx], [ry])
        Pg.stt(y, ntmp, -TWO_PI, y, ALU.mult, ALU.add, [rn, ry], [ry])
        Pg.op("dve", lambda e: e.tensor_single_scalar(ntmp, y, PI, ALU.is_gt), [ry], [rn])
        Pg.stt(y, ntmp, -TWO_PI, y, ALU.mult, ALU.add, [rn, ry], [ry])
        Pg.op("dve", lambda e: e.tensor_single_scalar(ntmp, y, -PI, ALU.is_lt), [ry], [rn])
        Pg.stt(y, ntmp, TWO_PI, y, ALU.mult, ALU.add, [rn, ry], [ry])

    A0, A1 = tA[:, 0:2048], tA[:, 2048:4096]
    B0, B1 = tB[:, 0:2048], tB[:, 2048:4096]
    C0, C1 = tC[:, 0:2048], tC[:, 2048:4096]
    I0 = tI[:, 0:2048]
    are, aim, ldt = L3[:, 0, :], L3[:, 1, :], L3[:, 2, :]
    PA.act(A0, ldt, AF.Exp, ["L3"], ["A0"])
    PA.tt(A1, aim, A0, ALU.mult, ["L3", "A0"], ["A1"])
    PA.tt(B0, are, A0, ALU.mult, ["L3", "A0"], ["B0"])
    PA.act(B0, B0, AF.Exp, ["B0"], ["B0"])
    range_reduce(PA, C0, A1, 0.0, C1, I0, "A1", "C0", "C1", "I0")
    PA.act(B1, C0, AF.Sin, ["C0"], ["B1"])
    range_reduce(PA, C0, A1, PI / 2, C1, I0, "A1", "C0", "C1", "I0")
    PA.act(A0, C0, AF.Sin, ["C0", "A0"], ["A0"])
    PA.tt(A0, B0, A0, ALU.mult, ["B0", "A0"], ["A0"])
    PA.tt(B1, B0, B1, ALU.mult, ["B0", "B1"], ["B1"])
    PA.ts(A0, A0, -1.0, ALU.add, ["A0"], ["A0"])
    PA.tt(C0, are, are, ALU.mult, ["L3"], ["C0"])
    PA.tt(C1, aim, aim, ALU.mult, ["L3"], ["C1"])
    PA.tt(C0, C0, C1, ALU.add, ["C0", "C1"], ["C0"])
    PA.op("dve", lambda e: e.reciprocal(C0, C0), ["C0"], ["C0"])
    PA.tt(C1, A0, are, ALU.mult, ["A0", "L3"], ["C1"])
    PA.tt(B0, B1, aim, ALU.mult, ["B1", "L3"], ["B0"])
    PA.tt(C1, C1, B0, ALU.add, ["C1", "B0"], ["C1"])
    PA.tt(C1, C1, C0, ALU.mult, ["C1", "C0"], ["C1"])
    PA.tt(B0, B1, are, ALU.mult, ["B1", "L3"], ["B0"])
    PA.tt(A1, A0, aim, ALU.mult, ["A0", "L3"], ["A1"])
    PA.tt(B0, B0, A1, ALU.subtract, ["B0", "A1"], ["B0"])
    PA.tt(B0, B0, C0, ALU.mult, ["B0", "C0"], ["B0"])
    Bre, Bim = Bz[:, 0, :], Bz[:, 1, :]
    v3 = lambda ap: ap.rearrange("p (k s) -> p k s", k=16)
    PA.tt(A0, C1, Bre, ALU.mult, ["C1", "Bz"], ["A0"])
    PA.tt(A1, B0, Bim, ALU.mult, ["B0", "Bz"], ["A1"])
    PA.tt(BT_bf[:, :, 0, :], v3(A0), v3(A1), ALU.subtract, ["A0", "A1"], ["BT"])
    PA.tt(A0, C1, Bim, ALU.mult, ["C1", "Bz", "BT"], ["A0"])
    PA.tt(A1, B0, Bre, ALU.mult, ["B0", "Bz", "BT"], ["A1"])
    PA.tt(BT_bf[:, :, 1, :], v3(A0), v3(A1), ALU.add, ["A0", "A1"], ["BT"])
    PA.dma("sp", Bz[:, 0, :], ssm_C_d[0], reads=["BT"], writes=["Bz"], slot="ld2", group=True)
    PA.dma("sp", Bz[:, 1, :], ssm_C_d[1], reads=["BT"], writes=["Bz"], slot="ld2", group=True)
    PA.cp(CT_bf[:, :, 0, :], v3(Bz[:, 0, :]), ["Bz"], ["CT"])
    PA.ts(CT_bf[:, :, 1, :], v3(Bz[:, 1, :]), -1.0, ALU.mult, ["Bz"], ["CT"])
    PA.act(smw[:, 0, :], sm[:, 2, :], AF.Exp, ["sm"], ["smw0"])
    PA.tt(smw[:, 1, :], sm[:, 1, :], smw[:, 0, :], ALU.mult, ["sm", "smw0"], ["smw1"])
    PA.tt(rcol[:], sm[:, 0, :], smw[:, 0, :], ALU.mult, ["sm", "smw0"], ["rcol"])
    PA.act(rcol[:], rcol[:], AF.Exp, ["rcol"], ["rcol"])
    ang = tA[:, :].rearrange("p (k j) -> p k j", k=16)
    PA.tt(ang, jj[:].unsqueeze(1).to_broadcast([128, 16, ST]), smw[:, 1, :].unsqueeze(2).to_broadcast([128, 16, ST]),
          ALU.mult, ["jj", "smw1", "A0", "A1", "BT"], ["tA"])
    range_reduce(PA, tB[:, :], tA[:, :], 0.0, tC[:, :], tI[:, :], "tA", "tB", "tC", "tI")
    PA.act(sinT[:].rearrange("p k j -> p (k j)"), tB[:, :], AF.Sin, ["tB", "B0", "B1", "C0", "C1"], ["sinT"])
    range_reduce(PA, tB[:, :], tA[:, :], PI / 2, tC[:, :], tI[:, :], "tA", "tB", "tC", "tI")
    PA.act(cosT[:].rearrange("p k j -> p (k j)"), tB[:, :], AF.Sin, ["tB"], ["cosT"])
    PA.barrier()
    if stage == "A":
        dbg_out(PA, "BT", BT_bf[:], [128, 16, 2, 128], BF16)
        dbg_out(PA, "CT", CT_bf[:], [128, 16, 2, 128], BF16)
        dbg_out(PA, "cosT", cosT[:], [128, 16, ST], BF16)
        dbg_out(PA, "sinT", sinT[:], [128, 16, ST], BF16)
        dbg_out(PA, "rcol", rcol[:], [128, 16], F32)
        PA.barrier()
        PA.emit()
        return nc
    PA.emit()
    nc.all_engine_barrier()
    sA.close()

    sB = ExitStack()
    PB = Prog(nc, top, "B")
    modrow = sbuf(sB, "modrow", [1, 6 * D], F32)
    grow = sbuf(sB, "grow", [1, 2 * D], F32)
    arow = sbuf(sB, "arow", [1, 2 * D], F32)
    cTt = sbuf(sB, "cTt", [128, 8], F32)
    adaR = [sbuf(sB, "adaR%d" % i, [128, 1536], F32) for i in range(3)]
    PB.dma("sp", modrow[:], ada_b_d, writes=["modrow"], slot="ld", group=True)
    PB.dma("sp", grow[:, 0:D], g1_d, writes=["grow"], slot="ld", group=True)
    PB.dma("sp", grow[:, D:2 * D], g2_d, writes=["grow"], slot="ld", group=True)
    PB.dma("sp", cTt[:], cT_d, writes=["cTt"], slot="ld", group=True)
    PB.act(cTt[:], cTt[:], AF.Silu, ["cTt"], ["cTt"])
    n_ada = 0
    for qd in range(4):
        banks = [nxt() for _ in range(3)]
        for kc in range(8):
            rb = n_ada % 3
            n_ada += 1
            PB.dma("sp", adaR[rb][:], ada_w_d[kc * 128:(kc + 1) * 128, qd * 1536:(qd + 1) * 1536],
                   writes=["adaR%d" % rb], slot="ada%d" % rb)
            for n in range(3):
                PB.mm(banks[n][0][0:1, :], [(cTt[:, kc:kc + 1], adaR[rb][:, n * 512:(n + 1) * 512])],
                      ["cTt", "adaR%d" % rb], [banks[n][1]], start=(kc == 0), stop=(kc == 7))
        for n in range(3):
            sl = modrow[:, qd * 1536 + n * 512: qd * 1536 + (n + 1) * 512]
            PB.tt(sl, banks[n][0][0:1, :], sl, ALU.add, [banks[n][1], "modrow"], ["modrow"])
    PB.stt(arow[:, 0:D], modrow[:, D:2 * D], 1.0, grow[:, 0:D], ALU.add, ALU.mult, ["modrow", "grow"], ["arow"])
    PB.stt(arow[:, D:2 * D], modrow[:, 4 * D:5 * D], 1.0, grow[:, D:2 * D], ALU.add, ALU.mult, ["modrow", "grow"], ["arow"])
    pc, pcn = nxt()
    vecs = [arow[:, 0:D], modrow[:, 0:D], arow[:, D:2 * D], modrow[:, 3 * D:4 * D]]
    for vi, vec in enumerate(vecs):
        for fc in range(8):
            PB.mm(pc[:, vi * 8 + fc: vi * 8 + fc + 1], [(vec[0:1, fc * 128:(fc + 1) * 128], ones_row[0:1, 0:1])],
                  ["arow", "modrow"], [pcn])
    PB.cp(cols[:], pc[:, 0:32], [pcn], ["cols"])
    for gi, (gsrc, gdst, gname) in enumerate([(modrow[:, 2 * D:3 * D], gate1b, "gate1b"), (modrow[:, 5 * D:6 * D], gate2b, "gate2b"),
                                              ]):
        for h in range(2):
            pb_, pbn = nxt()
            PB.mm(pb_[:, :], [(ones_row[0:1, :], gsrc[0:1, h * 512:(h + 1) * 512])], ["modrow", "arow"], [pbn])
            PB.cp(gdst[:, h * 512:(h + 1) * 512], pb_[:, :], [pbn], [gname])
    PB.dma("sp", modscr_d[0:1, :], arow[:, D:2 * D], reads=["arow"], writes=["modscr0"], slot="ms", group=True)
    PB.dma("sp", modscr_d[1:2, :], modrow[:, 3 * D:4 * D], reads=["modrow"], writes=["modscr1"], slot="ms", group=True)
    PB.barrier()
    if stage == "B":
        dbg_out(PB, "cols", cols[:], [128, 32], F32)
        dbg_out(PB, "gate1b", gate1b[:], [128, D], F32)
        dbg_out(PB, "gate2b", gate2b[:], [128, D], F32)
        PB.barrier()
        PB.emit()
        return nc
    PB.emit()
    nc.all_engine_barrier()
    sB.close()

    PM = Prog(nc, top, "M")
    w_in_bf = sbuf(p1, "w_in_bf", [128, 8, 3584], BF16)
    glu_bf = sbuf(p1, "glu_bf", [128, 4, 512], BF16)
    wa_bf = sbuf(p1, "wa_bf", [128, 4, D], BF16)
    wb_bf = sbuf(p1, "wb_bf", [128, 4, D], BF16)
    wout_bf = sbuf(p1, "wout_bf", [128, 8, D], BF16)
    wsT_bf = sbuf(p1, "wsT_bf", [128, 8, 128], BF16)
    lngb = sbuf(p1, "lngb", [128, 512], F32)
    lnbb = sbuf(p1, "lnbb", [128, 512], F32)
    bs_row = sbuf(p1, "bs_row", [1, 1024], F32)
    xt = [sbuf(p1, "xt%d" % i, [128, D], F32) for i in range(2)]
    xn = sbuf(p1, "xn", [128, D], BF16)
    st = sbuf(p1, "st", [128, 16], F32)
    hT = sbuf(p1, "hT", [128, 8, ST], BF16)
    us32 = sbuf(p1, "us32", [128, 4, ST], F32)
    usbf = sbuf(p1, "usbf", [128, 4, ST], BF16)
    guT = sbuf(p1, "guT", [128, 4, ST], BF16)
    gv = sbuf(p1, "gv", [128, 512], F32)
    gv2 = sbuf(p1, "gv2", [128, 512], F32)
    vn = sbuf(p1, "vn", [128, 2, 512], BF16)
    sga = sbuf(p1, "sga", [128, 8, ST], BF16)
    sgb = sbuf(p1, "sgb", [128, 8, ST], BF16)
    zT = sbuf(p1, "zT", [128, 4, ST], BF16)
    sg = sbuf(p1, "sg", [128, 4, ST], BF16)
    oT = sbuf(p1, "oT", [128, 4, ST], BF16)
    pT = sbuf(p1, "pT", [128, 4, ST], BF16)
    ta = sbuf(p1, "ta", [128, 2, ST], F32)
    tb = sbuf(p1, "tb", [128, 2, ST], F32)
    mT = sbuf(p1, "mT", [128, 8, ST], BF16)
    t1 = sbuf(p1, "t1", [128, ST], F32)
    t2 = sbuf(p1, "t2", [128, ST], F32)
    wri = sbuf(p1, "wri", [128, 2, ST], F32)
    qri = sbuf(p1, "qri", [128, 2, ST], F32)
    s32 = sbuf(p1, "s32", [128, 2, ST], F32)
    sT = sbuf(p1, "sT", [128, 2, ST], BF16)
    yv = sbuf(p1, "yv", [128, ST], F32)

    for kc in range(8):
        PM.dma("pool", w_in_bf[:, kc, :], w_in_d[kc * 128:(kc + 1) * 128, :], writes=["w_in%d" % kc], slot="wl", group=True)
    PM.dma("pool", glu_bf[:], glu_w_d.rearrange("(c p) n -> p c n", p=128), writes=["glu"], slot="wl", group=True)
    PM.dma("pool", wa_bf[:], wa_d.rearrange("(c p) n -> p c n", p=128), writes=["wa"], slot="wl", group=True)
    PM.dma("pool", wb_bf[:], wb_d.rearrange("(c p) n -> p c n", p=128), writes=["wb"], slot="wl", group=True)
    PM.dma("pool", wout_bf[:], wout_d.rearrange("(c p) n -> p c n", p=128), writes=["wout"], slot="wl", group=True)
    PM.dma("sp", lngb[:], lng_d.partition_broadcast(128), writes=["lngb"], slot="wl2", group=True)
    PM.dma("sp", lnbb[:], lnb_d.partition_broadcast(128), writes=["lnbb"], slot="wl2", group=True)
    PM.dma("sp", bs_row[:], bs_d, writes=["bs_row"], slot="wl2", group=True)
    wsst = us32[:].rearrange("p a n -> p (a n)").rearrange("p (h t) -> p h t", h=8)
    PM.dma("sp", wsst, wsT_d, writes=["us32"], slot="wl2", group=True)
    PM.dma("sp", gv[:, 0:128], tri_d, writes=["gv"], slot="wl2", group=True)
    PM.tt(wsT_bf[:], wsst, gv[:, 0:128].unsqueeze(1).to_broadcast([128, 8, 128]), ALU.mult, ["us32", "gv"], ["wsT_bf"])
    W_IN = ["w_in%d" % kc for kc in range(8)]

    def pairview(ps_):
        return ps_[:, :].rearrange("p (a n) -> p a n", a=2)

    xq = [0]
    rot["n"] = 5
    for s in range(NST):
        def front_norm():
            for i in range(2):
                tix = 2 * s + i
                xb = xt[xq[0] % 2]
                xbn = "xt%d" % (xq[0] % 2)
                xq[0] += 1
                PM.dma("sp", xb[:], x_d[tix * 128:(tix + 1) * 128, :], writes=[xbn], slot=xbn)
                PM.memset(st[:, 0:1], 0.0, ["st0"])
                PM.act(xn[:], xb[:], AF.Square, [xbn, "st0"], ["xn", "st0"], accum=st[:, 0:1])
                PM.act(st[:, 1:2], st[:, 0:1], AF.Sqrt, ["st0"], ["st1"], bias=epsc[:, 0:1], scale=1.0 / D)
                PM.op("dve", lambda e: e.reciprocal(st[:, 2:3], st[:, 1:2]), ["st1"], ["st2"])
                PM.ts(xn[:], xb[:], st[:, 2:3], ALU.mult, [xbn, "st2"], ["xn"])
                pt_, ptn = nxt_t()
                for fc in range(8):
                    PM.tr(pt_[:, fc * 128:(fc + 1) * 128], xn[:, fc * 128:(fc + 1) * 128], ident_bf[:], ["xn"], [ptn])
                for fc in range(8):
                    PM.act(hT[:, fc, i * 128:(i + 1) * 128], pt_[:, fc * 128:(fc + 1) * 128], AF.Identity,
                           [ptn], ["hT"], bias=s1c[:, fc:fc + 1], scale=a1c[:, fc:fc + 1])

        def proj_u():
            for pr in range(2):
                ps_, psn = nxt()
                for a in range(2):
                    cc = 2 * pr + a
                    PM.mm(ps_[:, a * ST:(a + 1) * ST], [(w_in_bf[:, kc, cc * 128:(cc + 1) * 128], hT[:, kc, :]) for kc in range(8)],
                          W_IN + ["hT"], [psn])
                PM.act(us32[:, 2 * pr:2 * pr + 2, :], pairview(ps_), AF.Copy, [psn], ["us32"])
            PM.cp(usbf[:], us32[:], ["us32"], ["usbf"], eng="pool")

        def proj_zu():
            for pr in range(2):
                ps_, psn = nxt()
                for a in range(2):
                    cc = 2 * pr + a
                    PM.mm(ps_[:, a * ST:(a + 1) * ST], [(w_in_bf[:, kc, 512 + cc * 128:512 + (cc + 1) * 128], hT[:, kc, :]) for kc in range(8)],
                          W_IN + ["hT"], [psn])
                PM.act(guT[:, 2 * pr:2 * pr + 2, :], pairview(ps_), AF.Gelu_apprx_tanh, [psn], ["guT"])

        def proj_v(i):
            ps_, psn = nxt()
            PM.mm(ps_[:, :], [(hT[:, kc, i * 128:(i + 1) * 128], w_in_bf[:, kc, 1024:1536]) for kc in range(8)],
                  W_IN + ["hT"], [psn])
            PM.memset(st[:, 4:6], 0.0, ["st4"])
            PM.act(gv[:], ps_[:, :], AF.Gelu_apprx_tanh, [psn, "st4"], ["gv", "st4"], accum=st[:, 4:5])
            PM.act(gv2[:], gv[:], AF.Square, ["gv", "st4"], ["gv2", "st4"], accum=st[:, 5:6])
            PM.ts(st[:, 6:7], st[:, 4:5], 1.0 / 512, ALU.mult, ["st4"], ["st6"])
            PM.tt(st[:, 7:8], st[:, 6:7], st[:, 6:7], ALU.mult, ["st6"], ["st7"])
            PM.stt(st[:, 8:9], st[:, 5:6], 1.0 / 512, st[:, 7:8], ALU.mult, ALU.subtract, ["st4", "st7"], ["st8"])
            PM.act(st[:, 9:10], st[:, 8:9], AF.Sqrt, ["st8"], ["st9"], bias=epsc[:, 0:1], scale=1.0)
            PM.op("dve", lambda e: e.reciprocal(st[:, 10:11], st[:, 9:10]), ["st9"], ["st10"])
            PM.stt(st[:, 11:12], st[:, 6:7], -1.0, st[:, 10:11], ALU.mult, ALU.mult, ["st6", "st10"], ["st11"])
            PM.act(gv2[:], gv[:], AF.Identity, ["gv", "st10", "st11"], ["gv2"], bias=st[:, 11:12], scale=st[:, 10:11])
            PM.tt(gv[:], gv2[:], lngb[:], ALU.mult, ["gv2", "lngb"], ["gv"])
            PM.tt(vn[:, i, :], gv[:], lnbb[:], ALU.add, ["gv", "lnbb"], ["vn"])

        def mix(cc):
            ps_, psn = nxt()
            for i in range(2):
                for hh in range(2):
                    h = 2 * cc + hh
                    o_ = ps_[hh * 64:(hh + 1) * 64, i * 128:(i + 1) * 128]

                    def emit(pe, o_=o_, i=i, cc=cc, hh=hh, h=h):
                        pe.matmul(o_, vn[:, i, cc * 128 + hh * 64: cc * 128 + (hh + 1) * 64], wsT_bf[:, h, :], start=True, stop=False)
                        return pe.matmul(o_, ones_row[0:1, 0:64], bs_row[0:1, h * 128:(h + 1) * 128], start=False, stop=True)

                    PM.op("pe", emit, ["vn", "wsT_bf", "bs_row"], [psn])
            PM.tt(pT[:, cc, :], guT[:, cc, :], ps_[:, 0:ST], ALU.mult, ["guT", psn], ["pT"])

        def gates(pr):
            pga, pgan = nxt()
            pgb, pgbn = nxt()
            for a in range(2):
                dc = 2 * pr + a
                sl = slice(a * ST, (a + 1) * ST)
                PM.mm(pga[:, sl], [(w_in_bf[:, kc, 1536 + dc * 128:1536 + (dc + 1) * 128], hT[:, kc, :]) for kc in range(8)], W_IN + ["hT"], [pgan])
                PM.mm(pgb[:, sl], [(w_in_bf[:, kc, 2560 + dc * 128:2560 + (dc + 1) * 128], hT[:, kc, :]) for kc in range(8)], W_IN + ["hT"], [pgbn])
            PM.act(sga[:, 2 * pr:2 * pr + 2, :], pairview(pga), AF.Sigmoid, [pgan], ["sga"])
            PM.act(sgb[:, 2 * pr:2 * pr + 2, :], pairview(pgb), AF.Sigmoid, [pgbn], ["sgb"])

        bu_banks = {}

        def bu(k):
            cc = k // 4
            pb_, pbn = nxt()
            PM.mm(pb_[:, 0:ST], [(BT_bf[:, k, 0, :], usbf[:, cc, :])], ["usbf"], [pbn])
            PM.mm(pb_[:, ST:2 * ST], [(BT_bf[:, k, 1, :], usbf[:, cc, :])], ["usbf"], [pbn])
            bu_banks[k] = (pb_, pbn)

        def ssm_dve(k):
            pb_, pbn = bu_banks[k]
            bre, bim = pb_[:, 0:ST], pb_[:, ST:2 * ST]
            ck, sk = cosT[:, k, :], sinT[:, k, :]
            PM.tt(t1[:], bre, ck, ALU.mult, [pbn], ["t1"])
            PM.tt(t2[:], bim, sk, ALU.mult, [pbn], ["t2"])
            PM.tt(wri[:, 0, :], t1[:], t2[:], ALU.add, ["t1", "t2"], ["wri0"])
            PM.tt(t1[:], bim, ck, ALU.mult, [pbn], ["t1"])
            PM.tt(t2[:], bre, sk, ALU.mult, [pbn], ["t2"])
            PM.tt(wri[:, 1, :], t1[:], t2[:], ALU.subtract, ["t1", "t2"], ["wri1"])
            rb_ = rcol[:, k:k + 1].to_broadcast([128, ST])
            PM.scan(qri[:, 0, :], rb_, wri[:, 0, :], carry[:, k, 0:1], ["wri0", "carry%d" % k], ["qri0"])
            PM.scan(qri[:, 1, :], rb_, wri[:, 1, :], carry[:, k, 1:2], ["wri1", "carry%d" % k], ["qri1"])
            PM.tt(t1[:], qri[:, 0, :], ck, ALU.mult, ["qri0"], ["t1"])
            PM.tt(t2[:], qri[:, 1, :], sk, ALU.mult, ["qri1"], ["t2"])
            PM.tt(s32[:, 0, :], t1[:], t2[:], ALU.subtract, ["t1", "t2"], ["s32a"])
            PM.tt(t1[:], qri[:, 1, :], ck, ALU.mult, ["qri1"], ["t1"])
            PM.tt(t2[:], qri[:, 0, :], sk, ALU.mult, ["qri0"], ["t2"])
            PM.tt(s32[:, 1, :], t1[:], t2[:], ALU.add, ["t1", "t2"], ["s32b"])
            PM.act(sT[:], s32[:], AF.Copy, ["s32a", "s32b"], ["sT"])
            PM.cp(carry[:, k, :], s32[:, :, ST - 1], ["s32a", "s32b"], ["carry%d" % k], eng="pool")

        def ssm_c(k):
            cc = k // 4
            yps, ypn = psf[5], "psf5"
            PM.mm(yps[:, 0:ST], [(CT_bf[:, k, 0, :], sT[:, 0, :]), (CT_bf[:, k, 1, :], sT[:, 1, :])], ["sT"], [ypn],
                  start=(k % 4 == 0), stop=(k % 4 == 3))
            if k % 4 == 3:
                PM.stt(yv[:], us32[:, cc, :], dcol[:, cc:cc + 1], yps[:, 0:ST], ALU.mult, ALU.add, ["us32", ypn], ["yv"])
                PM.act(zT[:, cc, :], yv[:], AF.Gelu_apprx_tanh, ["yv"], ["zT"])

        extras = {0: proj_zu, 1: (lambda: proj_v(0)), 2: (lambda: proj_v(1)),
                  4: (lambda: mix(0)), 5: (lambda: mix(1)), 6: (lambda: mix(2)), 7: (lambda: mix(3)),
                  8: (lambda: gates(0)), 9: (lambda: gates(1)), 10: (lambda: gates(2)), 11: (lambda: gates(3))}
        front_norm()
        proj_u()
        bu(0)
        for k in range(16):
            if k + 1 < 16:
                bu(k + 1)
            ssm_dve(k)
            if k in extras:
                extras[k]()
            ssm_c(k)
        for pr in range(2):
            ps_, psn = nxt()
            for a in range(2):
                n = 2 * pr + a
                PM.mm(ps_[:, a * ST:(a + 1) * ST], [(glu_bf[:, cc, n * 128:(n + 1) * 128], zT[:, cc, :]) for cc in range(4)],
                      ["glu", "zT"], [psn])
                PM.act(sg[:, n, :], ps_[:, a * ST:(a + 1) * ST], AF.Sigmoid, [psn], ["sg"], bias=glubc[:, n:n + 1], scale=1.0)
        PM.tt(oT[:], zT[:], sg[:], ALU.mult, ["zT", "sg"], ["oT"])
        for pr in range(4):
            pya, pyan = nxt()
            pyb, pybn = nxt()
            for a in range(2):
                dc = 2 * pr + a
                sl = slice(a * ST, (a + 1) * ST)
                PM.mm(pya[:, sl], [(wa_bf[:, cc, dc * 128:(dc + 1) * 128], oT[:, cc, :]) for cc in range(4)], ["wa", "oT"], [pyan])
                PM.mm(pyb[:, sl], [(wb_bf[:, cc, dc * 128:(dc + 1) * 128], pT[:, cc, :]) for cc in range(4)], ["wb", "pT"], [pybn])
            PM.tt(ta[:], sga[:, 2 * pr:2 * pr + 2, :], pairview(pya), ALU.mult, ["sga", pyan], ["ta"])
            PM.tt(tb[:], sgb[:, 2 * pr:2 * pr + 2, :], pairview(pyb), ALU.mult, ["sgb", pybn], ["tb"])
            PM.tt(mT[:, 2 * pr:2 * pr + 2, :], ta[:], tb[:], ALU.add, ["ta", "tb"], ["mT"])
        for i in range(2):
            tix = 2 * s + i
            xb = xt[xq[0] % 2]
            xbn = "xt%d" % (xq[0] % 2)
            xq[0] += 1
            PM.dma("sp", xb[:], x_d[tix * 128:(tix + 1) * 128, :], writes=[xbn], slot=xbn)
            for dh in range(2):
                ps_, psn = nxt()
                PM.mm(ps_[:, :], [(mT[:, kc, i * 128:(i + 1) * 128], wout_bf[:, kc, dh * 512:(dh + 1) * 512]) for kc in range(8)],
                      ["mT", "wout"], [psn])
                PM.tt(gv2[:], ps_[:, :], gate1b[:, dh * 512:(dh + 1) * 512], ALU.mult, [psn], ["gv2"])
                PM.tt(xb[:, dh * 512:(dh + 1) * 512], gv2[:], xb[:, dh * 512:(dh + 1) * 512], ALU.add, ["gv2", xbn], [xbn])
            PM.dma("sp", x1s_d[tix * 128:(tix + 1) * 128, :], xb[:], reads=[xbn], writes=["x1s%d" % tix], slot=xbn)
    rot["n"] = 6
    PM.barrier()
    PM.emit()
    if stage == "M":
        return nc
    nc.all_engine_barrier()
    p1.close()

    p2 = ExitStack()
    PX = Prog(nc, top, "X")
    PX.regs = regs
    UW = 256
    NU = T // UW
    x1 = sbuf(p2, "x1", [128, NT, D], F32)
    h2tm = sbuf(p2, "h2tm", [128, NT, D], BF16)
    bgc = sbuf(p2, "bgc", [128, NE, 16], F32)
    posm = sbuf(p2, "posm", [128, NT, NE], F32)
    rwhl = sbuf(p2, "rwhl", [128, NT, NE, 2], BF16)
    iota = sbuf(p2, "iota", [128, UW], F32)
    ne_i = sbuf(p2, "ne_i", [1, NE], I32)
    p2r = ExitStack()
    rwb_bf = sbuf(p2r, "rwb_bf", [128, 8, NE], BF16)
    rbb = sbuf(p2r, "rbb", [128, NE], F32)
    bo_g = sbuf(p2r, "bo_g", [NE, D], F32)
    maskf = sbuf(p2r, "maskf", [128, NT, NE], F32)
    maskb = sbuf(p2r, "maskb", [128, NT, NE], BF16)
    rw = sbuf(p2r, "rw", [128, NT, NE], F32)
    stri_bf = sbuf(p2r, "stri_bf", [128, 128], BF16)
    ones_bf = sbuf(p2r, "ones_bf", [128, 128], BF16)
    cnt = sbuf(p2r, "cnt", [1, 3, NE], F32)
    acc2_ = [sbuf(p2r, "acc2_%d" % i, [128, D], F32) for i in range(2)]
    xn2_ = [sbuf(p2r, "xn2_%d" % i, [128, D], BF16) for i in range(2)]
    h2Tt_ = [sbuf(p2r, "h2Tt_%d" % i, [128, 8, 128], BF16) for i in range(2)]
    rwT_ = [sbuf(p2r, "rwT_%d" % i, [NE, 128], F32) for i in range(2)]
    rt_ = [sbuf(p2r, "rt_%d" % i, [128, 4, NE], F32) for i in range(2)]
    rs_ = [sbuf(p2r, "rs_%d" % i, [128, 24], F32) for i in range(2)]
    rtp = sbuf(p2r, "rtp", [128, 4, NE], F32)
    a2b = sbuf(p2r, "a2b", [128, D], F32)
    s2b = sbuf(p2r, "s2b", [128, D], F32)
    ldf = sbuf(p2r, "ldf", [128, 256], F32)

    PX.dma("sp", rbb[:], rb_d.partition_broadcast(128), writes=["rbb"], slot="ld", group=True)
    PX.dma("sp", a2b[:], modscr_d[0:1, :].partition_broadcast(128), writes=["a2b"], slot="ld", group=True)
    PX.dma("sp", s2b[:], modscr_d[1:2, :].partition_broadcast(128), writes=["s2b"], slot="ld", group=True)
    PX.dma("sp", bgc[:], mbi_d, writes=["bgc"], slot="ld", group=True)
    PX.dma("sp", bo_g[:], mbo_d, writes=["bo_g"], slot="ld", group=True)
    PX.dma("sp", iota[:], iota_d, writes=["iota"], slot="ld", group=True)
    PX.dma("sp", ldf[:, 0:128], stri_d, writes=["ldf"], slot="ld", group=True)
    PX.dma("pool", rwb_bf[:], rw_d.rearrange("(c p) e -> p c e", p=128), writes=["rwb"], slot="ldp")
    PX.cp(stri_bf[:], ldf[:, 0:128], ["ldf"], ["stri_bf"])
    PX.memset(ones_bf[:], 1.0, ["ones_bf"])
    PX.ts(bgc[:, :, 8:16], bgc[:, :, 8:16], 1.0, ALU.add, ["bgc"], ["bgc"])
    PX.tt(bo_g[:], bo_g[:], gate2b[0:NE, :], ALU.mult, ["bo_g"], ["bo_g"])

    for tix in range(NT):
        pb2 = tix % 2
        rs, rt, rwT, h2Tt, xn2, acc2 = rs_[pb2], rt_[pb2], rwT_[pb2], h2Tt_[pb2], xn2_[pb2], acc2_[pb2]
        xb = x1[:, tix, :]
        xbn = "x1_%d" % tix
        PX.dma("sp", xb, x1s_d[tix * 128:(tix + 1) * 128, :], writes=[xbn], slot="x1l%d" % tix)
        PX.memset(rs[:, 0:1], 0.0, ["rs0_%d" % pb2])
        PX.act(xn2[:], xb, AF.Square, [xbn, "rs0_%d" % pb2], ["xn2_%d" % pb2, "rs0_%d" % pb2], accum=rs[:, 0:1])
        PX.act(rs[:, 1:2], rs[:, 0:1], AF.Sqrt, ["rs0_%d" % pb2], ["rs1_%d" % pb2], bias=epsc[:, 0:1], scale=1.0 / D)
        PX.op("dve", lambda e, rs=rs: e.reciprocal(rs[:, 2:3], rs[:, 1:2]), ["rs1_%d" % pb2], ["rs2_%d" % pb2])
        PX.stt(acc2[:], xb, rs[:, 2:3], a2b[:], ALU.mult, ALU.mult, [xbn, "rs2_%d" % pb2], ["acc2_%d" % pb2])
        PX.tt(h2tm[:, tix, :], acc2[:], s2b[:], ALU.add, ["acc2_%d" % pb2], ["h2tm%d" % tix])
        pt_, ptn = nxt_t()
        for fc in range(8):
            PX.tr(pt_[:, fc * 128:(fc + 1) * 128], h2tm[:, tix, fc * 128:(fc + 1) * 128], ident_bf[:], ["h2tm%d" % tix], [ptn])
        PX.act(h2Tt[:].rearrange("p a b -> p (a b)"), pt_[:, :], AF.Copy, [ptn], ["h2Tt_%d" % pb2])
        pl, pln = nxt()
        PX.mm(pl[:, 0:NE], [(h2Tt[:, kc, :], rwb_bf[:, kc, :]) for kc in range(8)], ["h2Tt_%d" % pb2, "rwb"], [pln])
        lg, ex = rt[:, 0, :], rt[:, 2, :]
        PX.tt(lg, pl[:, 0:NE], rbb[:], ALU.add, [pln, "rbb"], ["lg_%d" % pb2])
        PX.op("dve", lambda e, lg=lg, rs=rs: e.max(rs[:, 4:12], lg), ["lg_%d" % pb2], ["rs4_%d" % pb2])
        PX.op("dve", lambda e, lg=lg, tix=tix, rs=rs: e.tensor_single_scalar(maskf[:, tix, :], lg, rs[:, 7:8], ALU.is_ge), ["lg_%d" % pb2, "rs4_%d" % pb2], ["maskf%d" % tix])
        PX.ts(rs[:, 12:13], rs[:, 4:5], -1.0, ALU.mult, ["rs4_%d" % pb2], ["rs12_%d" % pb2])
        PX.act(ex, lg, AF.Exp, ["lg_%d" % pb2, "rs12_%d" % pb2], ["ex_%d" % pb2], bias=rs[:, 12:13], scale=1.0)
        PX.tt(ex, ex, maskf[:, tix, :], ALU.mult, ["ex_%d" % pb2, "maskf%d" % tix], ["ex_%d" % pb2])
        PX.op("dve", lambda e, ex=ex, rs=rs: e.reduce_sum(rs[:, 13:14], ex, mybir.AxisListType.X), ["ex_%d" % pb2], ["rs13_%d" % pb2])
        PX.op("dve", lambda e, rs=rs: e.reciprocal(rs[:, 14:15], rs[:, 13:14]), ["rs13_%d" % pb2], ["rs14_%d" % pb2])
        PX.ts(rw[:, tix, :], ex, rs[:, 14:15], ALU.mult, ["ex_%d" % pb2, "rs14_%d" % pb2], ["rw%d" % tix])
        PX.cp(maskb[:, tix, :], maskf[:, tix, :], ["maskf%d" % tix], ["maskb%d" % tix])
        pr_, prn = nxt()
        PX.op("pe", lambda e, pr_=pr_, tix=tix: e.transpose(pr_[0:NE, 0:128], rw[:, tix, :], ident_f[:]), ["rw%d" % tix], [prn])
        PX.cp(rwT[:], pr_[0:NE, 0:128], [prn], ["rwT_%d" % pb2])
        for dh in range(2):
            pb_, pbn = nxt()
            PX.mm(pb_[:, :], [(rwT[:], bo_g[:, dh * 512:(dh + 1) * 512])], ["rwT_%d" % pb2, "bo_g"], [pbn])
            PX.tt(x1[:, tix, dh * 512:(dh + 1) * 512], pb_[:, :], x1[:, tix, dh * 512:(dh + 1) * 512], ALU.add, [pbn, xbn], [xbn])
        PX.cp(rwhl[:, tix, :, 0], rw[:, tix, :], ["rw%d" % tix], ["rwhl%d" % tix])
        PX.cp(rt[:, 1, :], rwhl[:, tix, :, 0], ["rwhl%d" % tix], ["rt1_%d" % pb2])
        PX.tt(rt[:, 1, :], rw[:, tix, :], rt[:, 1, :], ALU.subtract, ["rw%d" % tix, "rt1_%d" % pb2], ["rt1_%d" % pb2])
        PX.cp(rwhl[:, tix, :, 1], rt[:, 1, :], ["rt1_%d" % pb2], ["rwhl%d" % tix])

    rt = rtp

    MB = ["maskb%d" % t for t in range(NT)]
    for tix in range(NT):
        pp_, ppn = nxt()
        prs = [(ones_bf[:], maskb[:, t2, :]) for t2 in range(tix)] + [(stri_bf[:], maskb[:, tix, :])]
        PX.mm(pp_[:, 0:NE], prs, MB[:tix + 1] + ["ones_bf", "stri_bf"], [ppn])
        PX.ts(rt[:, 1, :], maskf[:, tix, :], -1.0, ALU.add, ["maskf%d" % tix], ["rt1"], s2=1.0e6, op1=ALU.mult)
        PX.tt(rt[:, 3, :], pp_[:, 0:NE], maskf[:, tix, :], ALU.mult, [ppn, "maskf%d" % tix], ["rt3"])
        PX.tt(posm[:, tix, :], rt[:, 3, :], rt[:, 1, :], ALU.add, ["rt3", "rt1"], ["posm"])
    pc_, pcn_ = nxt()
    PX.mm(pc_[0:1, 0:NE], [(ones_bf[:, 0:1], maskb[:, t2, :]) for t2 in range(NT)], MB + ["ones_bf"], [pcn_])
    PX.cp(cnt[:, 0, :], pc_[0:1, 0:NE], [pcn_], ["cnt0"])
    PX.memset(cnt[:, 1, :], 0.0, ["cnt1"])
    for u in range(NU):
        PX.stt(cnt[:, 1, :], cnt[:, 0, :], float(UW * u), cnt[:, 1, :], ALU.is_gt, ALU.add, ["cnt0", "cnt1"], ["cnt1"])
    PX.cp(ne_i[:], cnt[:, 1, :], ["cnt1"], ["ne_i"])
    PX.barrier()
    if stage == "XP":
        dbg_out(PX, "posm", posm[:], [128, NT, NE], F32)
        dbg_out(PX, "rw", rw[:], [128, NT, NE], F32)
        dbg_out(PX, "cnt", cnt[:], [1, 3, NE], F32)
        dbg_out(PX, "x1b", x1[:], [128, NT, D], F32)
        PX.barrier()
        PX.emit()
        return nc
    PX.emit()
    nc.all_engine_barrier()
    p2r.close()

    PX = Prog(nc, top, "E")
    PX.regs = regs
    p2e = ExitStack()
    NWP = 5
    wq = [sbuf(p2e, "wq%d" % i, [128, 8, 512], BF16) for i in range(NWP)]
    wo = sbuf(p2e, "wo", [128, 8, D], BF16)
    SelAll = sbuf(p2e, "SelAll", [128, NT, UW], BF16)
    SelT = sbuf(p2e, "SelT", [128, 2, T], BF16)
    pmu = sbuf(p2e, "pmu", [128, NT], F32)
    h2g = sbuf(p2e, "h2g", [128, 8, UW], BF16)
    actT = sbuf(p2e, "actT", [128, 8, UW], BF16)
    g_sb = sbuf(p2e, "g_sb", [128, 2, UW], BF16)
    sg2 = sbuf(p2e, "sg2", [128, 2, UW], BF16)
    u_sb = sbuf(p2e, "u_sb", [128, 2, UW], BF16)
    pp = sbuf(p2e, "pp", [128, 2, UW], BF16)
    yw = sbuf(p2e, "yw", [128, 2, D], BF16)
    rws = sbuf(p2e, "rws", [128, 2], F32)
    rws4 = sbuf(p2e, "rws4", [128, 4], F32)

    NEX = NE

    def piece_slot(e, q):
        return (4 * e + q) % NWP

    def load_piece(e, q):
        sl = piece_slot(e, q)
        src = mwi_d[e].rearrange("(c p) n -> p c n", p=128)
        PX.dma("pool", wq[sl][:, :, 0:256], src[:, :, 256 * q:256 * (q + 1)], writes=["wg%d" % sl], slot="wg%d" % sl)
        PX.dma("pool", wq[sl][:, :, 256:512], src[:, :, D + 256 * q:D + 256 * (q + 1)], writes=["wu%d" % sl], slot="wu%d" % sl)

    def load_wo(e):
        PX.dma("pool", wo[:], mwo_d[e].rearrange("(c p) n -> p c n", p=128), writes=["wo"], slot="wo")

    for q in range(4):
        load_piece(0, q)
    H2 = ["h2tm%d" % t for t in range(NT)]
    X1 = ["x1_%d" % t for t in range(NT)]
    for e in range(NEX):
        if e + 1 < NE:
            load_piece(e + 1, 0)
        load_wo(e)
        PX.regload(regs, ne_i[0:1, e:e + 1], ["ne_i"])
        for u in range(NU):
            PX.marker(("rb", u + 1))
            PX.ts(pmu[:], posm[:, :, e], float(-UW * u), ALU.add, ["posm"], ["pmu"])
            for t2 in range(NT):
                PX.op("dve", lambda eng, t2=t2: eng.tensor_single_scalar(SelAll[:, t2, :], iota[:], pmu[:, t2:t2 + 1], ALU.is_equal),
                      ["pmu", "iota"], ["Sel%d" % t2])
            SEL = ["Sel%d" % t2 for t2 in range(NT)]
            for fp in range(4):
                ps_, psn = nxt()
                for a in range(2):
                    fc = 2 * fp + a
                    PX.mm(ps_[:, a * UW:(a + 1) * UW], [(h2tm[:, t2, fc * 128:(fc + 1) * 128], SelAll[:, t2, :]) for t2 in range(NT)],
                          H2 + SEL, [psn])
                PX.act(h2g[:, 2 * fp:2 * fp + 2, :], ps_[:, :].rearrange("p (a n) -> p a n", a=2), AF.Copy, [psn], ["h2g"])
            prw, prwn = nxt()
            for st_ in range(2):
                PX.mm(prw[:, 2 * st_:2 * st_ + 2], [(SelAll[:, t2, st_ * 128:(st_ + 1) * 128], rwhl[:, t2, e, :]) for t2 in range(NT)],
                      SEL + ["rwhl"], [prwn])
            PX.act(rws4[:], prw[:, 0:4], AF.Copy, [prwn], ["rws4"])
            prv = rws4[:].rearrange("p (s h) -> p s h", s=2)
            PX.tt(rws[:], prv[:, :, 0], prv[:, :, 1], ALU.add, ["rws4"], ["rws"])
            for jp in range(4):
                sl = piece_slot(e, jp)
                pg_, pgn = nxt()
                pu_, pun = nxt()
                for a in range(2):
                    PX.mm(pg_[:, a * UW:(a + 1) * UW], [(wq[sl][:, kc, a * 128:(a + 1) * 128], h2g[:, kc, :]) for kc in range(8)], ["wg%d" % sl, "h2g"], [pgn])
                    PX.mm(pu_[:, a * UW:(a + 1) * UW], [(wq[sl][:, kc, 256 + a * 128:256 + (a + 1) * 128], h2g[:, kc, :]) for kc in range(8)], ["wu%d" % sl, "h2g"], [pun])
                for a in range(2):
                    j = 2 * jp + a
                    PX.ts(g_sb[:, a, :], pg_[:, a * UW:(a + 1) * UW], bgc[:, e, j:j + 1], ALU.add, [pgn, "bgc"], ["g_sb"], s2=7.0, op1=ALU.min)
                    PX.ts(u_sb[:, a, :], pu_[:, a * UW:(a + 1) * UW], bgc[:, e, 8 + j:9 + j], ALU.add, [pun, "bgc"], ["u_sb"], s2=8.0, op1=ALU.min)
                PX.act(sg2[:], g_sb[:], AF.Sigmoid, ["g_sb"], ["sg2"], scale=1.702)
                PX.tt(pp[:], g_sb[:], sg2[:], ALU.mult, ["g_sb", "sg2"], ["pp"])
                PX.stt(actT[:, 2 * jp:2 * jp + 2, :], u_sb[:], -6.0, pp[:], ALU.max, ALU.mult, ["u_sb", "pp"], ["actT%d" % jp])
            for st_ in range(2):
                for dh in range(2):
                    po, pon = nxt()
                    PX.mm(po[:, :], [(actT[:, jc, st_ * 128:(st_ + 1) * 128], wo[:, jc, dh * 512:(dh + 1) * 512]) for jc in range(8)],
                          ["actT%d" % jp for jp in range(4)] + ["wo"], [pon])
                    PX.stt(yw[:, st_, dh * 512:(dh + 1) * 512], po[:, :], rws[:, st_:st_ + 1], gate2b[:, dh * 512:(dh + 1) * 512],
                           ALU.mult, ALU.mult, [pon, "rws"], ["yw"])
            for st_ in range(2):
                for h8 in range(2):
                    pt_, ptn = nxt_t()
                    for i8 in range(8):
                        t2 = 8 * h8 + i8
                        PX.tr(pt_[:, i8 * 128:(i8 + 1) * 128], SelAll[:, t2, st_ * 128:(st_ + 1) * 128], ident_bf[:], ["Sel%d" % t2], [ptn])
                    PX.act(SelT[:, st_, h8 * 1024:(h8 + 1) * 1024], pt_[:, :], AF.Copy, [ptn], ["SelT"])
            for t2 in range(NT):
                for dh in range(2):
                    po, pon = nxt()
                    PX.mm(po[:, :], [(SelT[:, st_, t2 * 128:(t2 + 1) * 128], yw[:, st_, dh * 512:(dh + 1) * 512]) for st_ in range(2)],
                          ["SelT", "yw"], [pon])
                    PX.tt(x1[:, t2, dh * 512:(dh + 1) * 512], po[:, :], x1[:, t2, dh * 512:(dh + 1) * 512], ALU.add, [pon, X1[t2]], [X1[t2]])
        for u in range(NU):
            PX.marker(("re",))
        if e + 1 < NE:
            for q in range(1, 4):
                load_piece(e + 1, q)
    PX.barrier()
    if stage == "XE":
        dbg_out(PX, "x2", x1[:], [128, NT, D], F32)
        PX.barrier()
        PX.emit()
        return nc
    PX.emit()
    nc.all_engine_barrier()
    p2e.close()

    PX = Prog(nc, top, "F")
    fgb = sbuf(p2, "fgb", [128, D], F32)
    xn2 = sbuf(p2, "xn2f", [128, D], BF16)
    rs = sbuf(p2, "rsf", [128, 8], F32)
    PX.dma("sp", fgb[:], fg_d.partition_broadcast(128), writes=["fgb"], slot="ld", group=True)
    for tix in range(NT):
        r_ = "x1_%d" % tix
        PX.memset(rs[:, 0:1], 0.0, ["rs0"])
        PX.act(xn2[:], x1[:, tix, :], AF.Square, [r_, "rs0"], ["xn2", "rs0"], accum=rs[:, 0:1])
        PX.act(rs[:, 1:2], rs[:, 0:1], AF.Sqrt, ["rs0"], ["rs1"], bias=epsc[:, 0:1], scale=1.0 / D)
        PX.op("dve", lambda e: e.reciprocal(rs[:, 2:3], rs[:, 1:2]), ["rs1"], ["rs2"])
        PX.stt(x1[:, tix, :], x1[:, tix, :], rs[:, 2:3], fgb[:], ALU.mult, ALU.mult, [r_, "rs2", "fgb"], [r_])
        PX.dma("sp", out_d[tix * 128:(tix + 1) * 128, :], x1[:, tix, :], reads=[r_], writes=["out%d" % tix], slot="outs", group=True)
    PX.barrier()
    PX.emit()
    p2.close()
    top.close()
    return nc


def _prep(inputs):
    f = lambda a: np.ascontiguousarray(np.asarray(a), dtype=np.float32)
    sh = {}
    sh["ada_w"] = f(inputs["ada_w"][0])
    sh["ada_b"] = f(inputs["ada_b"][0][None])
    sh["g1"] = f(inputs["norm1_g"][0][None])
    sh["g2"] = f(inputs["norm2_g"][0][None])
    sh["fg"] = f(np.asarray(inputs["final_g"])[None])
    sh["w_in"] = f(inputs["w_in"][0])
    a_re = np.asarray(inputs["ssm_a_re"][0])
    a_im = np.asarray(inputs["ssm_a_im"][0])
    ldt = np.broadcast_to(np.asarray(inputs["ssm_log_dt"][0])[:, None], (32, 64))
    smf = lambda v: np.asarray(v).reshape(16, 2, 64).transpose(1, 2, 0).reshape(128, 16)
    sh["ssm_sm"] = f(np.stack([smf(a_re), smf(a_im), smf(ldt)], 1))
    sh["ssm_lam"] = f(np.stack([a_re.reshape(-1), a_im.reshape(-1), np.ascontiguousarray(ldt).reshape(-1)]))
    b = [np.asarray(inputs["ssm_b_re"][0]), np.asarray(inputs["ssm_b_im"][0])]
    c = [np.asarray(inputs["ssm_c_re"][0]), np.asarray(inputs["ssm_c_im"][0])]
    Bz = np.zeros((2, 128, 16, 128), np.float32)
    Cz = np.zeros((2, 128, 16, 128), np.float32)
    for k in range(16):
        for g2 in range(2):
            g = 2 * k + g2
            g8 = g % 8
            for ri in range(2):
                Bz[ri, g8 * 16:(g8 + 1) * 16, k, g2 * 64:(g2 + 1) * 64] = b[ri][g].T
                Cz[ri, g2 * 64:(g2 + 1) * 64, k, g8 * 16:(g8 + 1) * 16] = c[ri][g].T
    sh["ssm_B"] = Bz.reshape(2, 128, 2048)
    sh["ssm_C"] = Cz.reshape(2, 128, 2048)
    sh["ssm_d"] = f(np.asarray(inputs["ssm_d"][0]).reshape(4, 128).T)
    sh["glu_w"] = f(inputs["ssm_glu_w"][0])
    sh["glu_b"] = f(np.asarray(inputs["ssm_glu_b"][0]).reshape(4, 128).T)
    sh["wa"] = f(inputs["w_branch_a"][0])
    sh["wb"] = f(inputs["w_branch_b"][0])
    sh["wout"] = f(inputs["w_out"][0])
    sh["lng"] = f(inputs["gmlp_ln_g"][0][None])
    sh["lnb"] = f(inputs["gmlp_ln_b"][0][None])
    sh["wsT"] = f(np.asarray(inputs["gmlp_ws"][0]).transpose(2, 0, 1))
    sh["bs"] = f(np.asarray(inputs["gmlp_bs"][0]).reshape(1, 1024))
    sh["router_w"] = f(inputs["router_w"][0])
    sh["router_b"] = f(inputs["router_b"][0][None])
    sh["moe_w_in"] = f(inputs["moe_w_in"][0])
    sh["moe_b_in"] = f(np.asarray(inputs["moe_b_in"][0]).reshape(32, 16, 128).transpose(2, 0, 1))
    sh["moe_w_out"] = f(inputs["moe_w_out"][0])
    sh["moe_b_out"] = f(inputs["moe_b_out"][0])
    sh["ident"] = np.eye(128, dtype=np.float32)
    sh["tri"] = np.triu(np.ones((128, 128), np.float32))
    sh["stri"] = np.triu(np.ones((128, 128), np.float32), 1)
    sh["iota256"] = np.ascontiguousarray(np.broadcast_to(np.arange(256, dtype=np.float32)[None], (128, 256)))
    sh["eoff"] = np.ascontiguousarray(np.broadcast_to((2048.0 * np.arange(32, dtype=np.float32) + 1.0)[None], (128, 32)))
    sh["jj"] = np.ascontiguousarray(np.broadcast_to(np.arange(1, ST + 1, dtype=np.float32)[None], (128, ST)))
    return sh


def kernel(**inputs):
    sh = _prep(inputs)
    x = np.asarray(inputs["x"], dtype=np.float32)
    c = np.asarray(inputs["c"], dtype=np.float32)
    in_maps = []
    for b in range(8):
        m = dict(sh)
        m["x"] = np.ascontiguousarray(x[b])
        m["cT"] = np.ascontiguousarray(c[b].reshape(8, 128).T)
        in_maps.append(m)
    nc = build()
    res = run_bass_kernel_spmd(nc, in_maps, core_ids=list(range(8)))
    return np.stack([np.asarray(r["out"], dtype=np.float32) for r in res.results], 0)
```

```python
import numpy as np
from contextlib import ExitStack
import concourse.bass as bass
import concourse.mybir as mybir
from concourse.bass_utils import run_bass_kernel_spmd

F32 = mybir.dt.float32
BF16 = mybir.dt.bfloat16
I32 = mybir.dt.int32
AF = mybir.ActivationFunctionType
ALU = mybir.AluOpType

T = 2048
D = 1024
NT = 16
ST = 256
NST = T // ST
NE = 32
EPS = 1e-6
PI = float(np.pi)
TWO_PI = float(2 * np.pi)


class Op:
    __slots__ = ("eng", "emit", "deps", "is_dma", "slot", "val", "signal", "mark")


class Prog:
    ENGS = ("pe", "act", "dve", "pool", "sp")

    def __init__(self, nc, es, tag):
        self.nc = nc
        self.es = es
        self.tag = tag
        self.q = {e: [] for e in self.ENGS}
        self.lastw = {}
        self.readers = {}
        self.slot_ops = {}
        self.group_slots = set()

    def _deps(self, reads, writes, op):
        deps = []
        for r in reads:
            w = self.lastw.get(r)
            if w is not None:
                deps.append(w)
        for r in writes:
            w = self.lastw.get(r)
            if w is not None:
                deps.append(w)
            deps.extend(self.readers.get(r, ()))
        for r in reads:
            self.readers.setdefault(r, []).append(op)
        for r in writes:
            self.lastw[r] = op
            self.readers[r] = []
        return [d for d in deps if d is not op]

    def op(self, eng, emit, reads=(), writes=()):
        o = Op()
        o.eng, o.emit, o.is_dma, o.slot, o.signal = eng, emit, False, None, False
        o.mark = None
        o.deps = self._deps(reads, writes, o)
        self.q[eng].append(o)
        return o

    def marker(self, mark):
        for e in self.ENGS:
            o = Op()
            o.eng, o.emit, o.is_dma, o.slot, o.signal, o.mark, o.deps = e, None, False, None, False, mark, []
            self.q[e].append(o)

    def regload(self, regs, ap, reads):
        for e in self.ENGS:
            self.op(e, (lambda eng, e=e: eng.reg_load(regs[e], ap)), reads, ())

    def dma(self, eng, out, in_, reads=(), writes=(), slot=None, group=False, custom=None):
        o = Op()
        o.eng, o.is_dma, o.slot, o.signal = eng, True, slot, True
        o.mark = None
        o.emit = (lambda e: e.dma_start(out=out, in_=in_)) if custom is None else custom
        o.deps = self._deps(reads, writes, o)
        if group:
            o.deps = [d for d in o.deps if not (d.is_dma and d.slot == slot)]
            self.group_slots.add(slot)
        self.q[eng].append(o)
        self.slot_ops.setdefault(slot, []).append(o)
        return o

    def barrier(self):
        pend = []
        for e in self.ENGS:
            for o in reversed(self.q[e]):
                if not o.is_dma and o.emit is not None:
                    pend.append(o)
                    break
        for s, ops in self.slot_ops.items():
            pend.append(ops[-1])
        for e in self.ENGS:
            o = Op()
            o.eng, o.emit, o.is_dma, o.slot, o.signal = e, None, False, None, False
            o.mark = None
            o.deps = list(pend)
            self.q[e].append(o)
        self.lastw = {}
        self.readers = {}

    def emit(self):
        nc = self.nc
        for e in self.ENGS:
            for o in self.q[e]:
                for d in o.deps:
                    d.signal = True
        esem = {}
        for e in self.ENGS:
            esem[e] = self.es.enter_context(nc.semaphore("s%s_%s" % (self.tag, e)))
            c = 0
            for o in self.q[e]:
                if o.is_dma or o.emit is None:
                    continue
                if o.signal:
                    c += 1
                    o.val = c
        ssem = {}
        for i, (s, ops) in enumerate(self.slot_ops.items()):
            ssem[s] = self.es.enter_context(nc.semaphore("d%s_%d" % (self.tag, i)))
            if s in self.group_slots:
                for o in ops:
                    o.val = 16 * len(ops)
            else:
                for j, o in enumerate(ops):
                    o.val = 16 * (j + 1)

        regs = getattr(self, "regs", None)

        def emit_q(e, eng):
            seen = {}

            def emit_one(o):
                need = {}
                for d in o.deps:
                    if d.is_dma:
                        sem = ssem[d.slot]
                    else:
                        if d.emit is None or (d.eng == "pe" and e == "pe"):
                            continue
                        sem = esem[d.eng]
                    k = id(sem)
                    if need.get(k, (None, 0))[1] < d.val:
                        need[k] = (sem, d.val)
                for k, (sem, v) in need.items():
                    if seen.get(k, 0) < v:
                        eng.wait_ge(sem, v)
                        seen[k] = v
                if o.emit is None:
                    return
                ins = o.emit(eng)
                if o.is_dma:
                    ins.then_inc(ssem[o.slot], 16)
                elif o.signal:
                    ins.then_inc(esem[e], 1)

            def emit_range(ops):
                i = 0
                while i < len(ops):
                    o = ops[i]
                    if o.mark is not None and o.mark[0] == "rb":
                        depth = 1
                        j = i + 1
                        while True:
                            m = ops[j].mark
                            if m is not None and m[0] == "rb":
                                depth += 1
                            elif m is not None and m[0] == "re":
                                depth -= 1
                                if depth == 0:
                                    break
                            j += 1
                        body = ops[i + 1:j]
                        real = [b for b in body if b.emit is not None]
                        if real:
                            c = sum(1 for b in real if (not b.is_dma) and b.signal)
                            dcnt = {}
                            for b in real:
                                if b.is_dma:
                                    dcnt[b.slot] = dcnt.get(b.slot, 0) + 1
                            saved = dict(seen)
                            g = eng.If_lt(regs[e], o.mark[1])
                            g.__enter__()
                            if c > 0:
                                eng.sem_inc(esem[e], c)
                            for sl, n in dcnt.items():
                                eng.sem_inc(ssem[sl], 16 * n)
                            g.__exit__(None, None, None)
                            g2 = eng.Else()
                            g2.__enter__()
                            emit_range(body)
                            g2.__exit__(None, None, None)
                            seen.clear()
                            seen.update(saved)
                        i = j + 1
                        continue
                    if o.mark is None:
                        emit_one(o)
                    i += 1

            emit_range(self.q[e])

        with nc.Block() as block:

            @block.tensor
            def _(eng):
                emit_q("pe", eng)

            @block.scalar
            def _(eng):
                emit_q("act", eng)

            @block.vector
            def _(eng):
                emit_q("dve", eng)

            @block.gpsimd
            def _(eng):
                emit_q("pool", eng)

            @block.sync
            def _(eng):
                emit_q("sp", eng)

    def mm(self, out, pairs, r, w, start=True, stop=True):
        pairs = list(pairs)

        def emit(pe):
            n = len(pairs)
            ins = None
            for i, (l, rr) in enumerate(pairs):
                ins = pe.matmul(out, l, rr, start=(start and i == 0), stop=(stop and i == n - 1))
            return ins

        return self.op("pe", emit, r, w)

    def tr(self, out, in_, ident, r, w):
        return self.op("pe", lambda e: e.transpose(out, in_, ident), r, w)

    def tt(self, out, a, b, op, r, w, eng="dve"):
        return self.op(eng, lambda e: e.tensor_tensor(out, a, b, op), r, w)

    def ts(self, out, a, s1, op0, r, w, s2=None, op1=None, eng="dve"):
        if op1 is None:
            return self.op(eng, lambda e: e.tensor_scalar(out, a, s1, None, op0), r, w)
        return self.op(eng, lambda e: e.tensor_scalar(out, a, s1, s2, op0, op1), r, w)

    def stt(self, out, a, s, b, op0, op1, r, w, eng="dve"):
        return self.op(eng, lambda e: e.scalar_tensor_tensor(out, a, s, b, op0, op1), r, w)

    def cp(self, out, a, r, w, eng="dve"):
        return self.op(eng, lambda e: e.tensor_copy(out, a), r, w)

    def memset(self, out, v, w, eng="dve"):
        return self.op(eng, lambda e: e.memset(out, v), (), w)

    def act(self, out, a, func, r, w, bias=None, scale=None, accum=None):
        kw = {}
        if bias is not None:
            kw["bias"] = bias
        if scale is not None:
            kw["scale"] = scale
        if accum is not None:
            kw["accum_out"] = accum
        return self.op("act", lambda e: e.activation(out, a, func, **kw), r, w)

    def scan(self, out, d0, d1, init, r, w):
        return self.op("dve", lambda e: e.tensor_tensor_scan(out, d0, d1, init, ALU.mult, ALU.add), r, w)


def build(stage=None):
    nc = bass.Bass("TRN2", target_bir_lowering=False)
    top = ExitStack()

    def din(name, shape):
        return nc.dram_tensor(name, list(shape), F32, kind="ExternalInput").ap()

    x_d = din("x", [T, D])
    cT_d = din("cT", [128, 8])
    ada_w_d = din("ada_w", [D, 6 * D])
    ada_b_d = din("ada_b", [1, 6 * D])
    g1_d = din("g1", [1, D])
    g2_d = din("g2", [1, D])
    fg_d = din("fg", [1, D])
    w_in_d = din("w_in", [D, 3584])
    ssm_sm_d = din("ssm_sm", [128, 3, 16])
    ssm_lam_d = din("ssm_lam", [3, 2048])
    ssm_B_d = din("ssm_B", [2, 128, 2048])
    ssm_C_d = din("ssm_C", [2, 128, 2048])
    ssm_d_d = din("ssm_d", [128, 4])
    glu_w_d = din("glu_w", [512, 512])
    glu_b_d = din("glu_b", [128, 4])
    wa_d = din("wa", [512, D])
    wb_d = din("wb", [512, D])
    wout_d = din("wout", [D, D])
    lng_d = din("lng", [1, 512])
    lnb_d = din("lnb", [1, 512])
    wsT_d = din("wsT", [128, 8, 128])
    bs_d = din("bs", [1, 1024])
    rw_d = din("router_w", [D, NE])
    rb_d = din("router_b", [1, NE])
    mwi_d = din("moe_w_in", [NE, D, 2 * D])
    mbi_d = din("moe_b_in", [128, NE, 16])
    mwo_d = din("moe_w_out", [NE, D, D])
    mbo_d = din("moe_b_out", [NE, D])
    ident_d = din("ident", [128, 128])
    tri_d = din("tri", [128, 128])
    jj_d = din("jj", [128, ST])
    stri_d = din("stri", [128, 128])
    iota_d = din("iota256", [128, 256])
    eoff_d = din("eoff", [128, NE])
    out_d = nc.dram_tensor("out", [T, D], F32, kind="ExternalOutput").ap()
    x1s_d = nc.dram_tensor("x1s", [T, D], F32, kind=("ExternalOutput" if stage == "M" else "Internal")).ap()

    def dbg_out(Pg, name, ap, shape, dt=F32):
        o = nc.dram_tensor("dbg_" + name, list(shape), dt, kind="ExternalOutput").ap()
        Pg.dma("sp", o, ap, reads=[], writes=["dbg_" + name], slot="dbg_" + name)

    def sbuf(es, name, shape, dt=F32):
        return es.enter_context(nc.sbuf_tensor("sb_" + name, list(shape), dt))

    pst = [top.enter_context(nc.psum_tensor("pst%d" % i, [128, 1024], BF16)) for i in range(2)]
    psf = [top.enter_context(nc.psum_tensor("psf%d" % i, [128, 512], F32)) for i in range(6)]
    rot = {"f": 0, "t": 0, "n": 6}

    def nxt():
        i = rot["f"] % rot["n"]
        rot["f"] += 1
        return psf[i], "psf%d" % i

    def nxt_t():
        i = rot["t"] % 2
        rot["t"] += 1
        return pst[i], "pst%d" % i

    ident_bf = sbuf(top, "ident_bf", [128, 128], BF16)
    ident_f = sbuf(top, "ident_f", [128, 128], F32)
    ones_row = sbuf(top, "ones_row", [1, 128], F32)
    epsc = sbuf(top, "epsc", [128, 1], F32)
    cols = sbuf(top, "cols", [128, 32], F32)
    gate2b = sbuf(top, "gate2b", [128, D], F32)
    modscr_d = nc.dram_tensor("modscr", [2, D], F32, kind="Internal").ap()
    regs = {"pe": top.enter_context(nc.tensor.register("r_pe")), "act": top.enter_context(nc.scalar.register("r_act")),
            "dve": top.enter_context(nc.vector.register("r_dve")), "pool": top.enter_context(nc.gpsimd.register("r_pool")),
            "sp": top.enter_context(nc.sync.register("r_sp"))}
    a1c, s1c, a2c, s2c = (cols[:, 0:8], cols[:, 8:16], cols[:, 16:24], cols[:, 24:32])

    p1 = ExitStack()
    gate1b = sbuf(p1, "gate1b", [128, D], F32)
    BT_bf = sbuf(p1, "BT_bf", [128, 16, 2, 128], BF16)
    CT_bf = sbuf(p1, "CT_bf", [128, 16, 2, 128], BF16)
    cosT = sbuf(p1, "cosT", [128, 16, ST], BF16)
    sinT = sbuf(p1, "sinT", [128, 16, ST], BF16)
    rcol = sbuf(p1, "rcol", [128, 16], F32)
    carry = sbuf(p1, "carry", [128, 16, 2], F32)
    dcol = sbuf(p1, "dcol", [128, 4], F32)
    glubc = sbuf(p1, "glubc", [128, 4], F32)

    sA = ExitStack()
    PA = Prog(nc, top, "A")
    L3 = sbuf(sA, "L3", [128, 3, 2048], F32)
    Bz = sbuf(sA, "Bz", [128, 2, 2048], F32)
    tA = sbuf(sA, "tA", [128, 4096], F32)
    tB = sbuf(sA, "tB", [128, 4096], F32)
    tC = sbuf(sA, "tC", [128, 4096], F32)
    tI = sbuf(sA, "tI", [128, 4096], I32)
    sm = sbuf(sA, "sm", [128, 3, 16], F32)
    smw = sbuf(sA, "smw", [128, 2, 16], F32)
    jj = sbuf(sA, "jj", [128, ST], F32)
    idl = sbuf(sA, "idl", [128, 128], F32)
    hpic = sbuf(sA, "hpic", [128, 1], F32)

    PA.dma("sp", idl[:], ident_d, writes=["idl"], slot="ld", group=True)
    PA.dma("sp", ident_f[:], ident_d, writes=["ident_f"], slot="ld", group=True)
    for i in range(3):
        PA.dma("sp", L3[:, i, :], ssm_lam_d[i:i + 1, :].partition_broadcast(128), writes=["L3"], slot="ld", group=True)
    for i in range(2):
        PA.dma("sp", Bz[:, i, :], ssm_B_d[i], writes=["Bz"], slot="ld", group=True)
    PA.dma("sp", sm[:], ssm_sm_d, writes=["sm"], slot="ld", group=True)
    PA.dma("sp", jj[:], jj_d, writes=["jj"], slot="ld", group=True)
    PA.dma("sp", dcol[:], ssm_d_d, writes=["dcol"], slot="ld", group=True)
    PA.dma("sp", glubc[:], glu_b_d, writes=["glubc"], slot="ld", group=True)
    PA.cp(ident_bf[:], idl[:], ["idl"], ["ident_bf"])
    PA.memset(ones_row[:], 1.0, ["ones_row"])
    PA.memset(epsc[:], EPS, ["epsc"])
    PA.memset(hpic[:], PI / 2, ["hpic"])
    PA.memset(carry[:], 0.0, ["carry"])

    def range_reduce(Pg, y, x, shift, ntmp, itmp, rx, ry, rn, ri):
        Pg.ts(ntmp, x, 1.0 / TWO_PI, ALU.mult, [rx], [rn], s2=shift / TWO_PI, op1=ALU.add)
        Pg.cp(itmp, ntmp, [rn], [ri])
        Pg.cp(ntmp, itmp, [ri], [rn])
        Pg.ts(y, x, shift, ALU.add, [rx], [ry])
        Pg.stt(y, ntmp, -TWO_PI, y, ALU.mult, ALU.add, [rn, ry], [ry])
        Pg.op("dve", lambda e: e.tensor_single_scalar(ntmp, y, PI, ALU.is_gt), [ry], [rn])
        Pg.stt(y, ntmp, -TWO_PI, y, ALU.mult, ALU.add, [rn, ry], [ry])
        Pg.op("dve", lambda e: e.tensor_single_scalar(ntmp, y, -PI, ALU.is_lt), [ry], [rn])
        Pg.stt(y, ntmp, TWO_PI, y, ALU.mult, ALU.add, [rn, ry], [ry])

    A0, A1 = tA[:, 0:2048], tA[:, 2048:4096]
    B0, B1 = tB[:, 0:2048], tB[:, 2048:4096]
    C0, C1 = tC[:, 0:2048], tC[:, 2048:4096]
    I0 = tI[:, 0:2048]
    are, aim, ldt = L3[:, 0, :], L3[:, 1, :], L3[:, 2, :]
    PA.act(A0, ldt, AF.Exp, ["L3"], ["A0"])
    PA.tt(A1, aim, A0, ALU.mult, ["L3", "A0"], ["A1"])
    PA.tt(B0, are, A0, ALU.mult, ["L3", "A0"], ["B0"])
    PA.act(B0, B0, AF.Exp, ["B0"], ["B0"])
    range_reduce(PA, C0, A1, 0.0, C1, I0, "A1", "C0", "C1", "I0")
    PA.act(B1, C0, AF.Sin, ["C0"], ["B1"])
    PA.act(C1, C0, AF.Abs, ["C0"], ["C1"])
    PA.act(A0, C1, AF.Sin, ["C1", "A0"], ["A0"], bias=hpic[:, 0:1], scale=-1.0)
    PA.tt(A0, B0, A0, ALU.mult, ["B0", "A0"], ["A0"])
    PA.tt(B1, B0, B1, ALU.mult, ["B0", "B1"], ["B1"])
    PA.ts(A0, A0, -1.0, ALU.add, ["A0"], ["A0"])
    PA.tt(C0, are, are, ALU.mult, ["L3"], ["C0"])
    PA.tt(C1, aim, aim, ALU.mult, ["L3"], ["C1"])
    PA.tt(C0, C0, C1, ALU.add, ["C0", "C1"], ["C0"])
    PA.op("dve", lambda e: e.reciprocal(C0, C0), ["C0"], ["C0"])
    PA.tt(C1, A0, are, ALU.mult, ["A0", "L3"], ["C1"])
    PA.tt(B0, B1, aim, ALU.mult, ["B1", "L3"], ["B0"])
    PA.tt(C1, C1, B0, ALU.add, ["C1", "B0"], ["C1"])
    PA.tt(C1, C1, C0, ALU.mult, ["C1", "C0"], ["C1"])
    PA.tt(B0, B1, are, ALU.mult, ["B1", "L3"], ["B0"])
    PA.tt(A1, A0, aim, ALU.mult, ["A0", "L3"], ["A1"])
    PA.tt(B0, B0, A1, ALU.subtract, ["B0", "A1"], ["B0"])
    PA.tt(B0, B0, C0, ALU.mult, ["B0", "C0"], ["B0"])
    Bre, Bim = Bz[:, 0, :], Bz[:, 1, :]
    v3 = lambda ap: ap.rearrange("p (k s) -> p k s", k=16)
    PA.tt(A0, C1, Bre, ALU.mult, ["C1", "Bz"], ["A0"])
    PA.tt(A1, B0, Bim, ALU.mult, ["B0", "Bz"], ["A1"])
    PA.tt(BT_bf[:, :, 0, :], v3(A0), v3(A1), ALU.subtract, ["A0", "A1"], ["BT"])
    PA.tt(A0, C1, Bim, ALU.mult, ["C1", "Bz", "BT"], ["A0"])
    PA.tt(A1, B0, Bre, ALU.mult, ["B0", "Bz", "BT"], ["A1"])
    PA.tt(BT_bf[:, :, 1, :], v3(A0), v3(A1), ALU.add, ["A0", "A1"], ["BT"])
    PA.dma("sp", Bz[:, 0, :], ssm_C_d[0], reads=["BT"], writes=["Bz"], slot="ld2", group=True)
    PA.dma("sp", Bz[:, 1, :], ssm_C_d[1], reads=["BT"], writes=["Bz"], slot="ld2", group=True)
    PA.cp(CT_bf[:, :, 0, :], v3(Bz[:, 0, :]), ["Bz"], ["CT"])
    PA.ts(CT_bf[:, :, 1, :], v3(Bz[:, 1, :]), -1.0, ALU.mult, ["Bz"], ["CT"])
    PA.act(smw[:, 0, :], sm[:, 2, :], AF.Exp, ["sm"], ["smw0"])
    PA.tt(smw[:, 1, :], sm[:, 1, :], smw[:, 0, :], ALU.mult, ["sm", "smw0"], ["smw1"])
    PA.tt(rcol[:], sm[:, 0, :], smw[:, 0, :], ALU.mult, ["sm", "smw0"], ["rcol"])
    PA.act(rcol[:], rcol[:], AF.Exp, ["rcol"], ["rcol"])
    ang = tA[:, :].rearrange("p (k j) -> p k j", k=16)
    PA.tt(ang, jj[:].unsqueeze(1).to_broadcast([128, 16, ST]), smw[:, 1, :].unsqueeze(2).to_broadcast([128, 16, ST]),
          ALU.mult, ["jj", "smw1", "A0", "A1", "BT"], ["tA"])
    range_reduce(PA, tB[:, :], tA[:, :], 0.0, tC[:, :], tI[:, :], "tA", "tB", "tC", "tI")
    PA.act(sinT[:].rearrange("p k j -> p (k j)"), tB[:, :], AF.Sin, ["tB", "B0", "B1", "C0", "C1"], ["sinT"])
    PA.act(tC[:, :], tB[:, :], AF.Abs, ["tB"], ["tC"])
    PA.act(cosT[:].rearrange("p k j -> p (k j)"), tC[:, :], AF.Sin, ["tC"], ["cosT"], bias=hpic[:, 0:1], scale=-1.0)
    PA.barrier()
    if stage == "A":
        dbg_out(PA, "BT", BT_bf[:], [128, 16, 2, 128], BF16)
        dbg_out(PA, "CT", CT_bf[:], [128, 16, 2, 128], BF16)
        dbg_out(PA, "cosT", cosT[:], [128, 16, ST], BF16)
        dbg_out(PA, "sinT", sinT[:], [128, 16, ST], BF16)
        dbg_out(PA, "rcol", rcol[:], [128, 16], F32)
        PA.barrier()
        PA.emit()
        return nc
    PA.emit()
    nc.all_engine_barrier()
    sA.close()

    sB = ExitStack()
    PB = Prog(nc, top, "B")
    modrow = sbuf(sB, "modrow", [1, 6 * D], F32)
    grow = sbuf(sB, "grow", [1, 2 * D], F32)
    arow = sbuf(sB, "arow", [1, 2 * D], F32)
    cTt = sbuf(sB, "cTt", [128, 8], F32)
    adaR = [sbuf(sB, "adaR%d" % i, [128, 1536], F32) for i in range(3)]
    PB.dma("sp", modrow[:], ada_b_d, writes=["modrow"], slot="ld", group=True)
    PB.dma("sp", grow[:, 0:D], g1_d, writes=["grow"], slot="ld", group=True)
    PB.dma("sp", grow[:, D:2 * D], g2_d, writes=["grow"], slot="ld", group=True)
    PB.dma("sp", cTt[:], cT_d, writes=["cTt"], slot="ld", group=True)
    PB.act(cTt[:], cTt[:], AF.Silu, ["cTt"], ["cTt"])
    n_ada = 0
    for qd in range(4):
        banks = [nxt() for _ in range(3)]
        for kc in range(8):
            rb = n_ada % 3
            n_ada += 1
            PB.dma("sp", adaR[rb][:], ada_w_d[kc * 128:(kc + 1) * 128, qd * 1536:(qd + 1) * 1536],
                   writes=["adaR%d" % rb], slot="ada%d" % rb)
            for n in range(3):
                PB.mm(banks[n][0][0:1, :], [(cTt[:, kc:kc + 1], adaR[rb][:, n * 512:(n + 1) * 512])],
                      ["cTt", "adaR%d" % rb], [banks[n][1]], start=(kc == 0), stop=(kc == 7))
        for n in range(3):
            sl = modrow[:, qd * 1536 + n * 512: qd * 1536 + (n + 1) * 512]
            PB.tt(sl, banks[n][0][0:1, :], sl, ALU.add, [banks[n][1], "modrow"], ["modrow"])
    PB.stt(arow[:, 0:D], modrow[:, D:2 * D], 1.0, grow[:, 0:D], ALU.add, ALU.mult, ["modrow", "grow"], ["arow"])
    PB.stt(arow[:, D:2 * D], modrow[:, 4 * D:5 * D], 1.0, grow[:, D:2 * D], ALU.add, ALU.mult, ["modrow", "grow"], ["arow"])
    pc, pcn = nxt()
    vecs = [arow[:, 0:D], modrow[:, 0:D], arow[:, D:2 * D], modrow[:, 3 * D:4 * D]]
    for vi, vec in enumerate(vecs):
        for fc in range(8):
            PB.mm(pc[:, vi * 8 + fc: vi * 8 + fc + 1], [(vec[0:1, fc * 128:(fc + 1) * 128], ones_row[0:1, 0:1])],
                  ["arow", "modrow"], [pcn])
    PB.cp(cols[:], pc[:, 0:32], [pcn], ["cols"])
    for gi, (gsrc, gdst, gname) in enumerate([(modrow[:, 2 * D:3 * D], gate1b, "gate1b"), (modrow[:, 5 * D:6 * D], gate2b, "gate2b"),
                                              ]):
        for h in range(2):
            pb_, pbn = nxt()
            PB.mm(pb_[:, :], [(ones_row[0:1, :], gsrc[0:1, h * 512:(h + 1) * 512])], ["modrow", "arow"], [pbn])
            PB.cp(gdst[:, h * 512:(h + 1) * 512], pb_[:, :], [pbn], [gname])
    PB.dma("sp", modscr_d[0:1, :], arow[:, D:2 * D], reads=["arow"], writes=["modscr0"], slot="ms", group=True)
    PB.dma("sp", modscr_d[1:2, :], modrow[:, 3 * D:4 * D], reads=["modrow"], writes=["modscr1"], slot="ms", group=True)
    PB.barrier()
    if stage == "B":
        dbg_out(PB, "cols", cols[:], [128, 32], F32)
        dbg_out(PB, "gate1b", gate1b[:], [128, D], F32)
        dbg_out(PB, "gate2b", gate2b[:], [128, D], F32)
        PB.barrier()
        PB.emit()
        return nc
    PB.emit()
    nc.all_engine_barrier()
    sB.close()

    PM = Prog(nc, top, "M")
    w_in_bf = sbuf(p1, "w_in_bf", [128, 8, 3584], BF16)
    glu_bf = sbuf(p1, "glu_bf", [128, 4, 512], BF16)
    wa_bf = sbuf(p1, "wa_bf", [128, 4, D], BF16)
    wb_bf = sbuf(p1, "wb_bf", [128, 4, D], BF16)
    wout_bf = sbuf(p1, "wout_bf", [128, 8, D], BF16)
    wsT_bf = sbuf(p1, "wsT_bf", [128, 8, 128], BF16)
    lngb = sbuf(p1, "lngb", [128, 512], F32)
    lnbb = sbuf(p1, "lnbb", [128, 512], F32)
    bs_row = sbuf(p1, "bs_row", [1, 1024], F32)
    xt = [sbuf(p1, "xt%d" % i, [128, D], F32) for i in range(1)]
    xn = sbuf(p1, "xn", [128, D], BF16)
    st = sbuf(p1, "st", [128, 16], F32)
    hT = sbuf(p1, "hT", [128, 8, ST], BF16)
    us32 = sbuf(p1, "us32", [128, 4, ST], F32)
    usbf = sbuf(p1, "usbf", [128, 4, ST], BF16)
    guT = sbuf(p1, "guT", [128, 4, ST], BF16)
    gv = sbuf(p1, "gv", [128, 512], F32)
    gv2 = sbuf(p1, "gv2", [128, 512], F32)
    vn = sbuf(p1, "vn", [128, 2, 512], BF16)
    xf = sbuf(p1, "xf", [128, D], F32)
    sga = sbuf(p1, "sga", [128, 8, ST], BF16)
    sgb = sbuf(p1, "sgb", [128, 8, ST], BF16)
    zT = sbuf(p1, "zT", [128, 4, ST], BF16)
    sg = sbuf(p1, "sg", [128, 4, ST], BF16)
    oT = sbuf(p1, "oT", [128, 4, ST], BF16)
    pT = sbuf(p1, "pT", [128, 4, ST], BF16)
    ta = sbuf(p1, "ta", [128, 2, ST], F32)
    tb = sbuf(p1, "tb", [128, 2, ST], F32)
    mT = sbuf(p1, "mT", [128, 8, ST], BF16)
    t1 = sbuf(p1, "t1", [128, ST], F32)
    t2 = sbuf(p1, "t2", [128, ST], F32)
    wri = sbuf(p1, "wri", [128, 2, ST], F32)
    qri = sbuf(p1, "qri", [128, 2, ST], F32)
    s32 = sbuf(p1, "s32", [128, 2, ST], F32)
    sT = sbuf(p1, "sT", [128, 2, ST], BF16)
    yv = sbuf(p1, "yv", [128, ST], F32)

    for kc in range(8):
        PM.dma("pool", w_in_bf[:, kc, :], w_in_d[kc * 128:(kc + 1) * 128, :], writes=["w_in%d" % kc], slot="wl", group=True)
    PM.dma("pool", glu_bf[:], glu_w_d.rearrange("(c p) n -> p c n", p=128), writes=["glu"], slot="wl", group=True)
    PM.dma("pool", wa_bf[:], wa_d.rearrange("(c p) n -> p c n", p=128), writes=["wa"], slot="wl", group=True)
    PM.dma("pool", wb_bf[:], wb_d.rearrange("(c p) n -> p c n", p=128), writes=["wb"], slot="wl", group=True)
    PM.dma("pool", wout_bf[:], wout_d.rearrange("(c p) n -> p c n", p=128), writes=["wout"], slot="wl", group=True)
    PM.dma("sp", lngb[:], lng_d.partition_broadcast(128), writes=["lngb"], slot="wl2", group=True)
    PM.dma("sp", lnbb[:], lnb_d.partition_broadcast(128), writes=["lnbb"], slot="wl2", group=True)
    PM.dma("sp", bs_row[:], bs_d, writes=["bs_row"], slot="wl2", group=True)
    wsst = us32[:].rearrange("p a n -> p (a n)").rearrange("p (h t) -> p h t", h=8)
    PM.dma("sp", wsst, wsT_d, writes=["us32"], slot="wl2", group=True)
    PM.dma("sp", gv[:, 0:128], tri_d, writes=["gv"], slot="wl2", group=True)
    PM.tt(wsT_bf[:], wsst, gv[:, 0:128].unsqueeze(1).to_broadcast([128, 8, 128]), ALU.mult, ["us32", "gv"], ["wsT_bf"])
    W_IN = ["w_in%d" % kc for kc in range(8)]

    def pairview(ps_):
        return ps_[:, :].rearrange("p (a n) -> p a n", a=2)

    xq = [0]
    rot["n"] = 5
    for s in range(NST):
        def front_norm_tile(ss, i):
            if True:
                tix = 2 * ss + i
                xb = xf
                xbn = "xf"
                PM.dma("sp", xb[:], x_d[tix * 128:(tix + 1) * 128, :], writes=[xbn], slot=xbn)
                PM.memset(st[:, 0:1], 0.0, ["st0"])
                PM.act(xn[:], xb[:], AF.Square, [xbn, "st0"], ["xn", "st0"], accum=st[:, 0:1])
                PM.act(st[:, 1:2], st[:, 0:1], AF.Sqrt, ["st0"], ["st1"], bias=epsc[:, 0:1], scale=1.0 / D)
                PM.op("dve", lambda e: e.reciprocal(st[:, 2:3], st[:, 1:2]), ["st1"], ["st2"])
                PM.ts(xn[:], xb[:], st[:, 2:3], ALU.mult, [xbn, "st2"], ["xn"])
                pt_, ptn = nxt_t()
                for fc in range(8):
                    PM.tr(pt_[:, fc * 128:(fc + 1) * 128], xn[:, fc * 128:(fc + 1) * 128], ident_bf[:], ["xn"], [ptn])
                for fc in range(8):
                    PM.act(hT[:, fc, i * 128:(i + 1) * 128], pt_[:, fc * 128:(fc + 1) * 128], AF.Identity,
                           [ptn], ["hT"], bias=s1c[:, fc:fc + 1], scale=a1c[:, fc:fc + 1])

        def proj_u():
            for pr in range(2):
                ps_, psn = nxt()
                for a in range(2):
                    cc = 2 * pr + a
                    PM.mm(ps_[:, a * ST:(a + 1) * ST], [(w_in_bf[:, kc, cc * 128:(cc + 1) * 128], hT[:, kc, :]) for kc in range(8)],
                          W_IN + ["hT"], [psn])
                PM.act(us32[:, 2 * pr:2 * pr + 2, :], pairview(ps_), AF.Copy, [psn], ["us32"])
            PM.cp(usbf[:], us32[:], ["us32"], ["usbf"], eng="pool")

        def proj_zu():
            for pr in range(2):
                ps_, psn = nxt()
                for a in range(2):
                    cc = 2 * pr + a
                    PM.mm(ps_[:, a * ST:(a + 1) * ST], [(w_in_bf[:, kc, 512 + cc * 128:512 + (cc + 1) * 128], hT[:, kc, :]) for kc in range(8)],
                          W_IN + ["hT"], [psn])
                PM.act(guT[:, 2 * pr:2 * pr + 2, :], pairview(ps_), AF.Gelu_apprx_tanh, [psn], ["guT"])

        def proj_v(i):
            ps_, psn = nxt()
            PM.mm(ps_[:, :], [(hT[:, kc, i * 128:(i + 1) * 128], w_in_bf[:, kc, 1024:1536]) for kc in range(8)],
                  W_IN + ["hT"], [psn])
            PM.memset(st[:, 4:6], 0.0, ["st4"])
            PM.act(gv[:], ps_[:, :], AF.Gelu_apprx_tanh, [psn, "st4"], ["gv", "st4"], accum=st[:, 4:5])
            PM.act(gv2[:], gv[:], AF.Square, ["gv", "st4"], ["gv2", "st4"], accum=st[:, 5:6])
            PM.ts(st[:, 6:7], st[:, 4:5], 1.0 / 512, ALU.mult, ["st4"], ["st6"])
            PM.tt(st[:, 7:8], st[:, 6:7], st[:, 6:7], ALU.mult, ["st6"], ["st7"])
            PM.stt(st[:, 8:9], st[:, 5:6], 1.0 / 512, st[:, 7:8], ALU.mult, ALU.subtract, ["st4", "st7"], ["st8"])
            PM.act(st[:, 9:10], st[:, 8:9], AF.Sqrt, ["st8"], ["st9"], bias=epsc[:, 0:1], scale=1.0)
            PM.op("dve", lambda e: e.reciprocal(st[:, 10:11], st[:, 9:10]), ["st9"], ["st10"])
            PM.stt(st[:, 11:12], st[:, 6:7], -1.0, st[:, 10:11], ALU.mult, ALU.mult, ["st6", "st10"], ["st11"])
            PM.act(gv2[:], gv[:], AF.Identity, ["gv", "st10", "st11"], ["gv2"], bias=st[:, 11:12], scale=st[:, 10:11])
            PM.tt(gv[:], gv2[:], lngb[:], ALU.mult, ["gv2", "lngb"], ["gv"])
            PM.tt(vn[:, i, :], gv[:], lnbb[:], ALU.add, ["gv", "lnbb"], ["vn"])

        def mix(cc):
            ps_, psn = nxt()
            for i in range(2):
                for hh in range(2):
                    h = 2 * cc + hh
                    o_ = ps_[hh * 64:(hh + 1) * 64, i * 128:(i + 1) * 128]

                    def emit(pe, o_=o_, i=i, cc=cc, hh=hh, h=h):
                        pe.matmul(o_, vn[:, i, cc * 128 + hh * 64: cc * 128 + (hh + 1) * 64], wsT_bf[:, h, :], start=True, stop=False)
                        return pe.matmul(o_, ones_row[0:1, 0:64], bs_row[0:1, h * 128:(h + 1) * 128], start=False, stop=True)

                    PM.op("pe", emit, ["vn", "wsT_bf", "bs_row"], [psn])
            PM.tt(pT[:, cc, :], guT[:, cc, :], ps_[:, 0:ST], ALU.mult, ["guT", psn], ["pT"])

        def gates(pr):
            pga, pgan = nxt()
            pgb, pgbn = nxt()
            for a in range(2):
                dc = 2 * pr + a
                sl = slice(a * ST, (a + 1) * ST)
                PM.mm(pga[:, sl], [(w_in_bf[:, kc, 1536 + dc * 128:1536 + (dc + 1) * 128], hT[:, kc, :]) for kc in range(8)], W_IN + ["hT"], [pgan])
                PM.mm(pgb[:, sl], [(w_in_bf[:, kc, 2560 + dc * 128:2560 + (dc + 1) * 128], hT[:, kc, :]) for kc in range(8)], W_IN + ["hT"], [pgbn])
            PM.act(sga[:, 2 * pr:2 * pr + 2, :], pairview(pga), AF.Sigmoid, [pgan], ["sga"])
            PM.act(sgb[:, 2 * pr:2 * pr + 2, :], pairview(pgb), AF.Sigmoid, [pgbn], ["sgb"])

        bu_banks = {}

        def bu(k):
            cc = k // 4
            pb_, pbn = nxt()
            PM.mm(pb_[:, 0:ST], [(BT_bf[:, k, 0, :], usbf[:, cc, :])], ["usbf"], [pbn])
            PM.mm(pb_[:, ST:2 * ST], [(BT_bf[:, k, 1, :], usbf[:, cc, :])], ["usbf"], [pbn])
            bu_banks[k] = (pb_, pbn)

        def ssm_dve(k):
            pb_, pbn = bu_banks[k]
            bre, bim = pb_[:, 0:ST], pb_[:, ST:2 * ST]
            ck, sk = cosT[:, k, :], sinT[:, k, :]
            PM.tt(t1[:], bre, ck, ALU.mult, [pbn], ["t1"])
            PM.tt(t2[:], bim, sk, ALU.mult, [pbn], ["t2"])
            PM.tt(wri[:, 0, :], t1[:], t2[:], ALU.add, ["t1", "t2"], ["wri0"])
            PM.tt(t1[:], bim, ck, ALU.mult, [pbn], ["t1"])
            PM.tt(t2[:], bre, sk, ALU.mult, [pbn], ["t2"])
            PM.tt(wri[:, 1, :], t1[:], t2[:], ALU.subtract, ["t1", "t2"], ["wri1"])
            rb_ = rcol[:, k:k + 1].to_broadcast([128, ST])
            PM.scan(qri[:, 0, :], rb_, wri[:, 0, :], carry[:, k, 0:1], ["wri0", "carry%d" % k], ["qri0"])
            PM.scan(qri[:, 1, :], rb_, wri[:, 1, :], carry[:, k, 1:2], ["wri1", "carry%d" % k], ["qri1"])
            PM.tt(t1[:], qri[:, 0, :], ck, ALU.mult, ["qri0"], ["t1"])
            PM.tt(t2[:], qri[:, 1, :], sk, ALU.mult, ["qri1"], ["t2"])
            PM.tt(s32[:, 0, :], t1[:], t2[:], ALU.subtract, ["t1", "t2"], ["s32a"])
            PM.tt(t1[:], qri[:, 1, :], ck, ALU.mult, ["qri1"], ["t1"])
            PM.tt(t2[:], qri[:, 0, :], sk, ALU.mult, ["qri0"], ["t2"])
            PM.tt(s32[:, 1, :], t1[:], t2[:], ALU.add, ["t1", "t2"], ["s32b"])
            PM.act(sT[:], s32[:], AF.Copy, ["s32a", "s32b"], ["sT"])
            PM.cp(carry[:, k, :], s32[:, :, ST - 1], ["s32a", "s32b"], ["carry%d" % k], eng="pool")

        def ssm_c(k):
            cc = k // 4
            yps, ypn = psf[5], "psf5"
            PM.mm(yps[:, 0:ST], [(CT_bf[:, k, 0, :], sT[:, 0, :]), (CT_bf[:, k, 1, :], sT[:, 1, :])], ["sT"], [ypn],
                  start=(k % 4 == 0), stop=(k % 4 == 3))
            if k % 4 == 3:
                PM.stt(yv[:], us32[:, cc, :], dcol[:, cc:cc + 1], yps[:, 0:ST], ALU.mult, ALU.add, ["us32", ypn], ["yv"])
                PM.act(zT[:, cc, :], yv[:], AF.Gelu_apprx_tanh, ["yv"], ["zT"])

        extras = {0: proj_zu, 1: (lambda: proj_v(0)), 2: (lambda: proj_v(1)),
                  4: (lambda: mix(0)), 5: (lambda: mix(1)), 6: (lambda: mix(2)), 7: (lambda: mix(3)),
                  8: (lambda: gates(0)), 9: (lambda: gates(1)), 10: (lambda: gates(2)), 11: (lambda: gates(3))}
        if s == 0:
            front_norm_tile(0, 0)
            front_norm_tile(0, 1)
        if s + 1 < NST:
            extras[12] = (lambda: front_norm_tile(s + 1, 0))
            extras[13] = (lambda: front_norm_tile(s + 1, 1))
        proj_u()
        bu(0)
        for k in range(16):
            if k + 1 < 16:
                bu(k + 1)
            ssm_dve(k)
            if k in extras:
                extras[k]()
            ssm_c(k)
        for pr in range(2):
            ps_, psn = nxt()
            for a in range(2):
                n = 2 * pr + a
                PM.mm(ps_[:, a * ST:(a + 1) * ST], [(glu_bf[:, cc, n * 128:(n + 1) * 128], zT[:, cc, :]) for cc in range(4)],
                      ["glu", "zT"], [psn])
                PM.act(sg[:, n, :], ps_[:, a * ST:(a + 1) * ST], AF.Sigmoid, [psn], ["sg"], bias=glubc[:, n:n + 1], scale=1.0)
        PM.tt(oT[:], zT[:], sg[:], ALU.mult, ["zT", "sg"], ["oT"])
        for pr in range(4):
            pya, pyan = nxt()
            pyb, pybn = nxt()
            for a in range(2):
                dc = 2 * pr + a
                sl = slice(a * ST, (a + 1) * ST)
                PM.mm(pya[:, sl], [(wa_bf[:, cc, dc * 128:(dc + 1) * 128], oT[:, cc, :]) for cc in range(4)], ["wa", "oT"], [pyan])
                PM.mm(pyb[:, sl], [(wb_bf[:, cc, dc * 128:(dc + 1) * 128], pT[:, cc, :]) for cc in range(4)], ["wb", "pT"], [pybn])
            PM.tt(ta[:], sga[:, 2 * pr:2 * pr + 2, :], pairview(pya), ALU.mult, ["sga", pyan], ["ta"])
            PM.tt(tb[:], sgb[:, 2 * pr:2 * pr + 2, :], pairview(pyb), ALU.mult, ["sgb", pybn], ["tb"])
            PM.tt(mT[:, 2 * pr:2 * pr + 2, :], ta[:], tb[:], ALU.add, ["ta", "tb"], ["mT"])
        for i in range(2):
            tix = 2 * s + i
            xb = xt[0]
            xbn = "xt0"
            PM.dma("sp", xb[:], x_d[tix * 128:(tix + 1) * 128, :], writes=[xbn], slot=xbn)
            for dh in range(2):
                ps_, psn = nxt()
                PM.mm(ps_[:, :], [(mT[:, kc, i * 128:(i + 1) * 128], wout_bf[:, kc, dh * 512:(dh + 1) * 512]) for kc in range(8)],
                      ["mT", "wout"], [psn])
                PM.tt(gv2[:], ps_[:, :], gate1b[:, dh * 512:(dh + 1) * 512], ALU.mult, [psn], ["gv2"])
                PM.tt(xb[:, dh * 512:(dh + 1) * 512], gv2[:], xb[:, dh * 512:(dh + 1) * 512], ALU.add, ["gv2", xbn], [xbn])
            PM.dma("sp", x1s_d[tix * 128:(tix + 1) * 128, :], xb[:], reads=[xbn], writes=["x1s%d" % tix], slot=xbn)
    rot["n"] = 6
    PM.barrier()
    PM.emit()
    if stage == "M":
        return nc
    nc.all_engine_barrier()
    p1.close()

    p2 = ExitStack()
    PX = Prog(nc, top, "X")
    PX.regs = regs
    UW = 256
    NU = T // UW
    x1 = sbuf(p2, "x1", [128, NT, D], F32)
    h2tm = sbuf(p2, "h2tm", [128, NT, D], BF16)
    bgc = sbuf(p2, "bgc", [128, NE, 16], F32)
    posm = sbuf(p2, "posm", [128, NT, NE], F32)
    rwhl = sbuf(p2, "rwhl", [128, NT, NE, 2], BF16)
    iota = sbuf(p2, "iota", [128, UW], F32)
    ne_i = sbuf(p2, "ne_i", [1, NE], I32)
    p2r = ExitStack()
    rwb_bf = sbuf(p2r, "rwb_bf", [128, 8, NE], BF16)
    rbb = sbuf(p2r, "rbb", [128, NE], F32)
    bo_g = sbuf(p2r, "bo_g", [NE, D], F32)
    maskf = sbuf(p2r, "maskf", [128, NT, NE], F32)
    maskb = sbuf(p2r, "maskb", [128, NT, NE], BF16)
    rw = sbuf(p2r, "rw", [128, NT, NE], F32)
    stri_bf = sbuf(p2r, "stri_bf", [128, 128], BF16)
    ones_bf = sbuf(p2r, "ones_bf", [128, 128], BF16)
    cnt = sbuf(p2r, "cnt", [1, 3, NE], F32)
    acc2_ = [sbuf(p2r, "acc2_%d" % i, [128, D], F32) for i in range(2)]
    xn2_ = [sbuf(p2r, "xn2_%d" % i, [128, D], BF16) for i in range(2)]
    h2Tt_ = [sbuf(p2r, "h2Tt_%d" % i, [128, 8, 128], BF16) for i in range(2)]
    rwT_ = [sbuf(p2r, "rwT_%d" % i, [NE, 128], F32) for i in range(2)]
    rt_ = [sbuf(p2r, "rt_%d" % i, [128, 4, NE], F32) for i in range(2)]
    rs_ = [sbuf(p2r, "rs_%d" % i, [128, 24], F32) for i in range(2)]
    rtp = sbuf(p2r, "rtp", [128, 4, NE], F32)
    a2b = sbuf(p2r, "a2b", [128, D], F32)
    s2b = sbuf(p2r, "s2b", [128, D], F32)
    ldf = sbuf(p2r, "ldf", [128, 256], F32)

    PX.dma("sp", rbb[:], rb_d.partition_broadcast(128), writes=["rbb"], slot="ld", group=True)
    PX.dma("sp", a2b[:], modscr_d[0:1, :].partition_broadcast(128), writes=["a2b"], slot="ld", group=True)
    PX.dma("sp", s2b[:], modscr_d[1:2, :].partition_broadcast(128), writes=["s2b"], slot="ld", group=True)
    PX.dma("sp", bgc[:], mbi_d, writes=["bgc"], slot="ld", group=True)
    PX.dma("sp", bo_g[:], mbo_d, writes=["bo_g"], slot="ld", group=True)
    PX.dma("sp", iota[:], iota_d, writes=["iota"], slot="ld", group=True)
    PX.dma("sp", ldf[:, 0:128], stri_d, writes=["ldf"], slot="ld", group=True)
    PX.dma("pool", rwb_bf[:], rw_d.rearrange("(c p) e -> p c e", p=128), writes=["rwb"], slot="ldp")
    PX.cp(stri_bf[:], ldf[:, 0:128], ["ldf"], ["stri_bf"])
    PX.memset(ones_bf[:], 1.0, ["ones_bf"])
    PX.ts(bgc[:, :, 8:16], bgc[:, :, 8:16], 1.0, ALU.add, ["bgc"], ["bgc"])
    PX.tt(bo_g[:], bo_g[:], gate2b[0:NE, :], ALU.mult, ["bo_g"], ["bo_g"])

    for tix in range(NT):
        pb2 = tix % 2
        rs, rt, rwT, h2Tt, xn2, acc2 = rs_[pb2], rt_[pb2], rwT_[pb2], h2Tt_[pb2], xn2_[pb2], acc2_[pb2]
        xb = x1[:, tix, :]
        xbn = "x1_%d" % tix
        PX.dma("sp", xb, x1s_d[tix * 128:(tix + 1) * 128, :], writes=[xbn], slot="x1l%d" % tix)
        PX.memset(rs[:, 0:1], 0.0, ["rs0_%d" % pb2])
        PX.act(xn2[:], xb, AF.Square, [xbn, "rs0_%d" % pb2], ["xn2_%d" % pb2, "rs0_%d" % pb2], accum=rs[:, 0:1])
        PX.act(rs[:, 1:2], rs[:, 0:1], AF.Sqrt, ["rs0_%d" % pb2], ["rs1_%d" % pb2], bias=epsc[:, 0:1], scale=1.0 / D)
        PX.op("dve", lambda e, rs=rs: e.reciprocal(rs[:, 2:3], rs[:, 1:2]), ["rs1_%d" % pb2], ["rs2_%d" % pb2])
        PX.stt(acc2[:], xb, rs[:, 2:3], a2b[:], ALU.mult, ALU.mult, [xbn, "rs2_%d" % pb2], ["acc2_%d" % pb2])
        PX.tt(h2tm[:, tix, :], acc2[:], s2b[:], ALU.add, ["acc2_%d" % pb2], ["h2tm%d" % tix])
        pt_, ptn = nxt_t()
        for fc in range(8):
            PX.tr(pt_[:, fc * 128:(fc + 1) * 128], h2tm[:, tix, fc * 128:(fc + 1) * 128], ident_bf[:], ["h2tm%d" % tix], [ptn])
        PX.act(h2Tt[:].rearrange("p a b -> p (a b)"), pt_[:, :], AF.Copy, [ptn], ["h2Tt_%d" % pb2])
        pl, pln = nxt()
        PX.mm(pl[:, 0:NE], [(h2Tt[:, kc, :], rwb_bf[:, kc, :]) for kc in range(8)], ["h2Tt_%d" % pb2, "rwb"], [pln])
        lg, ex = rt[:, 0, :], rt[:, 2, :]
        PX.tt(lg, pl[:, 0:NE], rbb[:], ALU.add, [pln, "rbb"], ["lg_%d" % pb2])
        PX.op("dve", lambda e, lg=lg, rs=rs: e.max(rs[:, 4:12], lg), ["lg_%d" % pb2], ["rs4_%d" % pb2])
        PX.op("dve", lambda e, lg=lg, tix=tix, rs=rs: e.tensor_single_scalar(maskf[:, tix, :], lg, rs[:, 7:8], ALU.is_ge), ["lg_%d" % pb2, "rs4_%d" % pb2], ["maskf%d" % tix])
        PX.ts(rs[:, 12:13], rs[:, 4:5], -1.0, ALU.mult, ["rs4_%d" % pb2], ["rs12_%d" % pb2])
        PX.act(ex, lg, AF.Exp, ["lg_%d" % pb2, "rs12_%d" % pb2], ["ex_%d" % pb2], bias=rs[:, 12:13], scale=1.0)
        PX.tt(ex, ex, maskf[:, tix, :], ALU.mult, ["ex_%d" % pb2, "maskf%d" % tix], ["ex_%d" % pb2])
        PX.op("dve", lambda e, ex=ex, rs=rs: e.reduce_sum(rs[:, 13:14], ex, mybir.AxisListType.X), ["ex_%d" % pb2], ["rs13_%d" % pb2])
        PX.op("dve", lambda e, rs=rs: e.reciprocal(rs[:, 14:15], rs[:, 13:14]), ["rs13_%d" % pb2], ["rs14_%d" % pb2])
        PX.ts(rw[:, tix, :], ex, rs[:, 14:15], ALU.mult, ["ex_%d" % pb2, "rs14_%d" % pb2], ["rw%d" % tix])
        PX.cp(maskb[:, tix, :], maskf[:, tix, :], ["maskf%d" % tix], ["maskb%d" % tix])
        pr_, prn = nxt()
        PX.op("pe", lambda e, pr_=pr_, tix=tix: e.transpose(pr_[0:NE, 0:128], rw[:, tix, :], ident_f[:]), ["rw%d" % tix], [prn])
        PX.cp(rwT[:], pr_[0:NE, 0:128], [prn], ["rwT_%d" % pb2])
        for dh in range(2):
            pb_, pbn = nxt()
            PX.mm(pb_[:, :], [(rwT[:], bo_g[:, dh * 512:(dh + 1) * 512])], ["rwT_%d" % pb2, "bo_g"], [pbn])
            PX.tt(x1[:, tix, dh * 512:(dh + 1) * 512], pb_[:, :], x1[:, tix, dh * 512:(dh + 1) * 512], ALU.add, [pbn, xbn], [xbn])
        PX.cp(rwhl[:, tix, :, 0], rw[:, tix, :], ["rw%d" % tix], ["rwhl%d" % tix])
        PX.cp(rt[:, 1, :], rwhl[:, tix, :, 0], ["rwhl%d" % tix], ["rt1_%d" % pb2])
        PX.tt(rt[:, 1, :], rw[:, tix, :], rt[:, 1, :], ALU.subtract, ["rw%d" % tix, "rt1_%d" % pb2], ["rt1_%d" % pb2])
        PX.cp(rwhl[:, tix, :, 1], rt[:, 1, :], ["rt1_%d" % pb2], ["rwhl%d" % tix])

    rt = rtp

    MB = ["maskb%d" % t for t in range(NT)]
    for tix in range(NT):
        pp_, ppn = nxt()
        prs = [(ones_bf[:], maskb[:, t2, :]) for t2 in range(tix)] + [(stri_bf[:], maskb[:, tix, :])]
        PX.mm(pp_[:, 0:NE], prs, MB[:tix + 1] + ["ones_bf", "stri_bf"], [ppn])
        PX.ts(rt[:, 1, :], maskf[:, tix, :], -1.0, ALU.add, ["maskf%d" % tix], ["rt1"], s2=1.0e6, op1=ALU.mult)
        PX.tt(rt[:, 3, :], pp_[:, 0:NE], maskf[:, tix, :], ALU.mult, [ppn, "maskf%d" % tix], ["rt3"])
        PX.tt(posm[:, tix, :], rt[:, 3, :], rt[:, 1, :], ALU.add, ["rt3", "rt1"], ["posm"])
    pc_, pcn_ = nxt()
    PX.mm(pc_[0:1, 0:NE], [(ones_bf[:, 0:1], maskb[:, t2, :]) for t2 in range(NT)], MB + ["ones_bf"], [pcn_])
    PX.cp(cnt[:, 0, :], pc_[0:1, 0:NE], [pcn_], ["cnt0"])
    PX.memset(cnt[:, 1, :], 0.0, ["cnt1"])
    for u in range(NU):
        PX.stt(cnt[:, 1, :], cnt[:, 0, :], float(UW * u), cnt[:, 1, :], ALU.is_gt, ALU.add, ["cnt0", "cnt1"], ["cnt1"])
    PX.cp(ne_i[:], cnt[:, 1, :], ["cnt1"], ["ne_i"])
    PX.barrier()
    if stage == "XP":
        dbg_out(PX, "posm", posm[:], [128, NT, NE], F32)
        dbg_out(PX, "rw", rw[:], [128, NT, NE], F32)
        dbg_out(PX, "cnt", cnt[:], [1, 3, NE], F32)
        dbg_out(PX, "x1b", x1[:], [128, NT, D], F32)
        PX.barrier()
        PX.emit()
        return nc
    PX.emit()
    nc.all_engine_barrier()
    p2r.close()

    PX = Prog(nc, top, "E")
    PX.regs = regs
    p2e = ExitStack()
    NWP = 5
    wq = [sbuf(p2e, "wq%d" % i, [128, 8, 512], BF16) for i in range(NWP)]
    wo = sbuf(p2e, "wo", [128, 8, D], BF16)
    SelAll = sbuf(p2e, "SelAll", [128, NT, UW], BF16)
    SelT = sbuf(p2e, "SelT", [128, 2, T], BF16)
    pmu = sbuf(p2e, "pmu", [128, NT], F32)
    h2g = sbuf(p2e, "h2g", [128, 8, UW], BF16)
    actT = sbuf(p2e, "actT", [128, 8, UW], BF16)
    g_sb = sbuf(p2e, "g_sb", [128, 2, UW], BF16)
    sg2 = sbuf(p2e, "sg2", [128, 2, UW], BF16)
    u_sb = sbuf(p2e, "u_sb", [128, 2, UW], BF16)
    pp = sbuf(p2e, "pp", [128, 2, UW], BF16)
    yw = sbuf(p2e, "yw", [128, 2, D], BF16)
    rws = sbuf(p2e, "rws", [128, 2], F32)
    rws4 = sbuf(p2e, "rws4", [128, 4], F32)

    NEX = NE

    def piece_slot(e, q):
        return (4 * e + q) % NWP

    def load_piece(e, q):
        sl = piece_slot(e, q)
        src = mwi_d[e].rearrange("(c p) n -> p c n", p=128)
        PX.dma("pool", wq[sl][:, :, 0:256], src[:, :, 256 * q:256 * (q + 1)], writes=["wg%d" % sl], slot="wg%d" % sl)
        PX.dma("pool", wq[sl][:, :, 256:512], src[:, :, D + 256 * q:D + 256 * (q + 1)], writes=["wu%d" % sl], slot="wu%d" % sl)

    def load_wo(e):
        PX.dma("pool", wo[:], mwo_d[e].rearrange("(c p) n -> p c n", p=128), writes=["wo"], slot="wo")

    for q in range(4):
        load_piece(0, q)
    H2 = ["h2tm%d" % t for t in range(NT)]
    X1 = ["x1_%d" % t for t in range(NT)]
    for e in range(NEX):
        if e + 1 < NE:
            load_piece(e + 1, 0)
        load_wo(e)
        PX.regload(regs, ne_i[0:1, e:e + 1], ["ne_i"])
        for u in range(NU):
            PX.marker(("rb", u + 1))
            PX.ts(pmu[:], posm[:, :, e], float(-UW * u), ALU.add, ["posm"], ["pmu"])
            for t2 in range(NT):
                PX.op("dve", lambda eng, t2=t2: eng.tensor_single_scalar(SelAll[:, t2, :], iota[:], pmu[:, t2:t2 + 1], ALU.is_equal),
                      ["pmu", "iota"], ["Sel%d" % t2])
            SEL = ["Sel%d" % t2 for t2 in range(NT)]
            for fp in range(4):
                ps_, psn = nxt()
                for a in range(2):
                    fc = 2 * fp + a
                    PX.mm(ps_[:, a * UW:(a + 1) * UW], [(h2tm[:, t2, fc * 128:(fc + 1) * 128], SelAll[:, t2, :]) for t2 in range(NT)],
                          H2 + SEL, [psn])
                PX.act(h2g[:, 2 * fp:2 * fp + 2, :], ps_[:, :].rearrange("p (a n) -> p a n", a=2), AF.Copy, [psn], ["h2g"])
            prw, prwn = nxt()
            for st_ in range(2):
                PX.mm(prw[:, 2 * st_:2 * st_ + 2], [(SelAll[:, t2, st_ * 128:(st_ + 1) * 128], rwhl[:, t2, e, :]) for t2 in range(NT)],
                      SEL + ["rwhl"], [prwn])
            PX.act(rws4[:], prw[:, 0:4], AF.Copy, [prwn], ["rws4"])
            prv = rws4[:].rearrange("p (s h) -> p s h", s=2)
            PX.tt(rws[:], prv[:, :, 0], prv[:, :, 1], ALU.add, ["rws4"], ["rws"])
            for jp in range(4):
                sl = piece_slot(e, jp)
                pg_, pgn = nxt()
                pu_, pun = nxt()
                for a in range(2):
                    PX.mm(pg_[:, a * UW:(a + 1) * UW], [(wq[sl][:, kc, a * 128:(a + 1) * 128], h2g[:, kc, :]) for kc in range(8)], ["wg%d" % sl, "h2g"], [pgn])
                    PX.mm(pu_[:, a * UW:(a + 1) * UW], [(wq[sl][:, kc, 256 + a * 128:256 + (a + 1) * 128], h2g[:, kc, :]) for kc in range(8)], ["wu%d" % sl, "h2g"], [pun])
                for a in range(2):
                    j = 2 * jp + a
                    PX.ts(g_sb[:, a, :], pg_[:, a * UW:(a + 1) * UW], bgc[:, e, j:j + 1], ALU.add, [pgn, "bgc"], ["g_sb"], s2=7.0, op1=ALU.min)
                    PX.ts(u_sb[:, a, :], pu_[:, a * UW:(a + 1) * UW], bgc[:, e, 8 + j:9 + j], ALU.add, [pun, "bgc"], ["u_sb"], s2=8.0, op1=ALU.min)
                PX.act(sg2[:], g_sb[:], AF.Sigmoid, ["g_sb"], ["sg2"], scale=1.702)
                PX.tt(pp[:], g_sb[:], sg2[:], ALU.mult, ["g_sb", "sg2"], ["pp"])
                PX.stt(actT[:, 2 * jp:2 * jp + 2, :], u_sb[:], -6.0, pp[:], ALU.max, ALU.mult, ["u_sb", "pp"], ["actT%d" % jp])
            for st_ in range(2):
                for dh in range(2):
                    po, pon = nxt()
                    PX.mm(po[:, :], [(actT[:, jc, st_ * 128:(st_ + 1) * 128], wo[:, jc, dh * 512:(dh + 1) * 512]) for jc in range(8)],
                          ["actT%d" % jp for jp in range(4)] + ["wo"], [pon])
                    PX.stt(yw[:, st_, dh * 512:(dh + 1) * 512], po[:, :], rws[:, st_:st_ + 1], gate2b[:, dh * 512:(dh + 1) * 512],
                           ALU.mult, ALU.mult, [pon, "rws"], ["yw"])
            for st_ in range(2):
                for h8 in range(2):
                    pt_, ptn = nxt_t()
                    for i8 in range(8):
                        t2 = 8 * h8 + i8
                        PX.tr(pt_[:, i8 * 128:(i8 + 1) * 128], SelAll[:, t2, st_ * 128:(st_ + 1) * 128], ident_bf[:], ["Sel%d" % t2], [ptn])
                    PX.act(SelT[:, st_, h8 * 1024:(h8 + 1) * 1024], pt_[:, :], AF.Copy, [ptn], ["SelT"])
            for t2 in range(NT):
                for dh in range(2):
                    po, pon = nxt()
                    PX.mm(po[:, :], [(SelT[:, st_, t2 * 128:(t2 + 1) * 128], yw[:, st_, dh * 512:(dh + 1) * 512]) for st_ in range(2)],
                          ["SelT", "yw"], [pon])
                    PX.tt(x1[:, t2, dh * 512:(dh + 1) * 512], po[:, :], x1[:, t2, dh * 512:(dh + 1) * 512], ALU.add, [pon, X1[t2]], [X1[t2]])
        for u in range(NU):
            PX.marker(("re",))
        if e + 1 < NE:
            for q in range(1, 4):
                load_piece(e + 1, q)
    PX.barrier()
    if stage == "XE":
        dbg_out(PX, "x2", x1[:], [128, NT, D], F32)
        PX.barrier()
        PX.emit()
        return nc
    PX.emit()
    nc.all_engine_barrier()
    p2e.close()

    PX = Prog(nc, top, "F")
    fgb = sbuf(p2, "fgb", [128, D], F32)
    xn2 = sbuf(p2, "xn2f", [128, D], BF16)
    rs = sbuf(p2, "rsf", [128, 8], F32)
    PX.dma("sp", fgb[:], fg_d.partition_broadcast(128), writes=["fgb"], slot="ld", group=True)
    for tix in range(NT):
        r_ = "x1_%d" % tix
        PX.memset(rs[:, 0:1], 0.0, ["rs0"])
        PX.act(xn2[:], x1[:, tix, :], AF.Square, [r_, "rs0"], ["xn2", "rs0"], accum=rs[:, 0:1])
        PX.act(rs[:, 1:2], rs[:, 0:1], AF.Sqrt, ["rs0"], ["rs1"], bias=epsc[:, 0:1], scale=1.0 / D)
        PX.op("dve", lambda e: e.reciprocal(rs[:, 2:3], rs[:, 1:2]), ["rs1"], ["rs2"])
        PX.stt(x1[:, tix, :], x1[:, tix, :], rs[:, 2:3], fgb[:], ALU.mult, ALU.mult, [r_, "rs2", "fgb"], [r_])
        PX.dma("sp", out_d[tix * 128:(tix + 1) * 128, :], x1[:, tix, :], reads=[r_], writes=["out%d" % tix], slot="outs", group=True)
    PX.barrier()
    PX.emit()
    p2.close()
    top.close()
    return nc


def _prep(inputs):
    f = lambda a: np.ascontiguousarray(np.asarray(a), dtype=np.float32)
    sh = {}
    sh["ada_w"] = f(inputs["ada_w"][0])
    sh["ada_b"] = f(inputs["ada_b"][0][None])
    sh["g1"] = f(inputs["norm1_g"][0][None])
    sh["g2"] = f(inputs["norm2_g"][0][None])
    sh["fg"] = f(np.asarray(inputs["final_g"])[None])
    sh["w_in"] = f(inputs["w_in"][0])
    a_re = np.asarray(inputs["ssm_a_re"][0])
    a_im = np.asarray(inputs["ssm_a_im"][0])
    ldt = np.broadcast_to(np.asarray(inputs["ssm_log_dt"][0])[:, None], (32, 64))
    smf = lambda v: np.asarray(v).reshape(16, 2, 64).transpose(1, 2, 0).reshape(128, 16)
    sh["ssm_sm"] = f(np.stack([smf(a_re), smf(a_im), smf(ldt)], 1))
    sh["ssm_lam"] = f(np.stack([a_re.reshape(-1), a_im.reshape(-1), np.ascontiguousarray(ldt).reshape(-1)]))
    b = [np.asarray(inputs["ssm_b_re"][0]), np.asarray(inputs["ssm_b_im"][0])]
    c = [np.asarray(inputs["ssm_c_re"][0]), np.asarray(inputs["ssm_c_im"][0])]
    Bz = np.zeros((2, 128, 16, 128), np.float32)
    Cz = np.zeros((2, 128, 16, 128), np.float32)
    for k in range(16):
        for g2 in range(2):
            g = 2 * k + g2
            g8 = g % 8
            for ri in range(2):
                Bz[ri, g8 * 16:(g8 + 1) * 16, k, g2 * 64:(g2 + 1) * 64] = b[ri][g].T
                Cz[ri, g2 * 64:(g2 + 1) * 64, k, g8 * 16:(g8 + 1) * 16] = c[ri][g].T
    sh["ssm_B"] = Bz.reshape(2, 128, 2048)
    sh["ssm_C"] = Cz.reshape(2, 128, 2048)
    sh["ssm_d"] = f(np.asarray(inputs["ssm_d"][0]).reshape(4, 128).T)
    sh["glu_w"] = f(inputs["ssm_glu_w"][0])
    sh["glu_b"] = f(np.asarray(inputs["ssm_glu_b"][0]).reshape(4, 128).T)
    sh["wa"] = f(inputs["w_branch_a"][0])
    sh["wb"] = f(inputs["w_branch_b"][0])
    sh["wout"] = f(inputs["w_out"][0])
    sh["lng"] = f(inputs["gmlp_ln_g"][0][None])
    sh["lnb"] = f(inputs["gmlp_ln_b"][0][None])
    sh["wsT"] = f(np.asarray(inputs["gmlp_ws"][0]).transpose(2, 0, 1))
    sh["bs"] = f(np.asarray(inputs["gmlp_bs"][0]).reshape(1, 1024))
    sh["router_w"] = f(inputs["router_w"][0])
    sh["router_b"] = f(inputs["router_b"][0][None])
    sh["moe_w_in"] = f(inputs["moe_w_in"][0])
    sh["moe_b_in"] = f(np.asarray(inputs["moe_b_in"][0]).reshape(32, 16, 128).transpose(2, 0, 1))
    sh["moe_w_out"] = f(inputs["moe_w_out"][0])
    sh["moe_b_out"] = f(inputs["moe_b_out"][0])
    sh["ident"] = np.eye(128, dtype=np.float32)
    sh["tri"] = np.triu(np.ones((128, 128), np.float32))
    sh["stri"] = np.triu(np.ones((128, 128), np.float32), 1)
    sh["iota256"] = np.ascontiguousarray(np.broadcast_to(np.arange(256, dtype=np.float32)[None], (128, 256)))
    sh["eoff"] = np.ascontiguousarray(np.broadcast_to((2048.0 * np.arange(32, dtype=np.float32) + 1.0)[None], (128, 32)))
    sh["jj"] = np.ascontiguousarray(np.broadcast_to(np.arange(1, ST + 1, dtype=np.float32)[None], (128, ST)))
    return sh


def kernel(**inputs):
    sh = _prep(inputs)
    x = np.asarray(inputs["x"], dtype=np.float32)
    c = np.asarray(inputs["c"], dtype=np.float32)
    in_maps = []
    for b in range(8):
        m = dict(sh)
        m["x"] = np.ascontiguousarray(x[b])
        m["cT"] = np.ascontiguousarray(c[b].reshape(8, 128).T)
        in_maps.append(m)
    nc = build()
    res = run_bass_kernel_spmd(nc, in_maps, core_ids=list(range(8)))
    return np.stack([np.asarray(r["out"], dtype=np.float32) for r in res.results], 0)
```

```python
import numpy as np
from contextlib import ExitStack
import concourse.bass as bass
import concourse.mybir as mybir
from concourse.bass_utils import run_bass_kernel_spmd

F32 = mybir.dt.float32
BF16 = mybir.dt.bfloat16
I32 = mybir.dt.int32
AF = mybir.ActivationFunctionType
ALU = mybir.AluOpType

T = 2048
D = 1024
NT = 16
ST = 256
NST = T // ST
NE = 32
EPS = 1e-6
PI = float(np.pi)
TWO_PI = float(2 * np.pi)


class Op:
    __slots__ = ("eng", "emit", "deps", "is_dma", "slot", "val", "signal", "mark")


class Prog:
    ENGS = ("pe", "act", "dve", "pool", "sp")

    def __init__(self, nc, es, tag):
        self.nc = nc
        self.es = es
        self.tag = tag
        self.q = {e: [] for e in self.ENGS}
        self.lastw = {}
        self.readers = {}
        self.slot_ops = {}
        self.group_slots = set()

    def _deps(self, reads, writes, op):
        deps = []
        for r in reads:
            w = self.lastw.get(r)
            if w is not None:
                deps.append(w)
        for r in writes:
            w = self.lastw.get(r)
            if w is not None:
                deps.append(w)
            deps.extend(self.readers.get(r, ()))
        for r in reads:
            self.readers.setdefault(r, []).append(op)
        for r in writes:
            self.lastw[r] = op
            self.readers[r] = []
        return [d for d in deps if d is not op]

    def op(self, eng, emit, reads=(), writes=()):
        o = Op()
        o.eng, o.emit, o.is_dma, o.slot, o.signal = eng, emit, False, None, False
        o.mark = None
        o.deps = self._deps(reads, writes, o)
        self.q[eng].append(o)
        return o

    def marker(self, mark):
        for e in self.ENGS:
            o = Op()
            o.eng, o.emit, o.is_dma, o.slot, o.signal, o.mark, o.deps = e, None, False, None, False, mark, []
            self.q[e].append(o)

    def regload(self, regs, ap, reads):
        for e in self.ENGS:
            self.op(e, (lambda eng, e=e: eng.reg_load(regs[e], ap)), reads, ())

    def dma(self, eng, out, in_, reads=(), writes=(), slot=None, group=False, custom=None):
        o = Op()
        o.eng, o.is_dma, o.slot, o.signal = eng, True, slot, True
        o.mark = None
        o.emit = (lambda e: e.dma_start(out=out, in_=in_)) if custom is None else custom
        o.deps = self._deps(reads, writes, o)
        if group:
            o.deps = [d for d in o.deps if not (d.is_dma and d.slot == slot)]
            self.group_slots.add(slot)
        self.q[eng].append(o)
        self.slot_ops.setdefault(slot, []).append(o)
        return o

    def barrier(self):
        pend = []
        for e in self.ENGS:
            for o in reversed(self.q[e]):
                if not o.is_dma and o.emit is not None:
                    pend.append(o)
                    break
        for s, ops in self.slot_ops.items():
            pend.append(ops[-1])
        for e in self.ENGS:
            o = Op()
            o.eng, o.emit, o.is_dma, o.slot, o.signal = e, None, False, None, False
            o.mark = None
            o.deps = list(pend)
            self.q[e].append(o)
        self.lastw = {}
        self.readers = {}

    def emit(self):
        nc = self.nc
        for e in self.ENGS:
            for o in self.q[e]:
                for d in o.deps:
                    d.signal = True
        esem = {}
        for e in self.ENGS:
            esem[e] = self.es.enter_context(nc.semaphore("s%s_%s" % (self.tag, e)))
            c = 0
            for o in self.q[e]:
                if o.is_dma or o.emit is None:
                    continue
                if o.signal:
                    c += 1
                    o.val = c
        ssem = {}
        for i, (s, ops) in enumerate(self.slot_ops.items()):
            ssem[s] = self.es.enter_context(nc.semaphore("d%s_%d" % (self.tag, i)))
            if s in self.group_slots:
                for o in ops:
                    o.val = 16 * len(ops)
            else:
                for j, o in enumerate(ops):
                    o.val = 16 * (j + 1)

        regs = getattr(self, "regs", None)

        def emit_q(e, eng):
            seen = {}

            def emit_one(o):
                need = {}
                for d in o.deps:
                    if d.is_dma:
                        sem = ssem[d.slot]
                    else:
                        if d.emit is None or (d.eng == "pe" and e == "pe"):
                            continue
                        sem = esem[d.eng]
                    k = id(sem)
                    if need.get(k, (None, 0))[1] < d.val:
                        need[k] = (sem, d.val)
                for k, (sem, v) in need.items():
                    if seen.get(k, 0) < v:
                        eng.wait_ge(sem, v)
                        seen[k] = v
                if o.emit is None:
                    return
                ins = o.emit(eng)
                if o.is_dma:
                    ins.then_inc(ssem[o.slot], 16)
                elif o.signal:
                    ins.then_inc(esem[e], 1)
                    cur[0] = o.val

            cur = [0]

            def emit_range(ops):
                i = 0
                while i < len(ops):
                    o = ops[i]
                    if o.mark is not None and o.mark[0] == "rb":
                        depth = 1
                        j = i + 1
                        while True:
                            m = ops[j].mark
                            if m is not None and m[0] == "rb":
                                depth += 1
                            elif m is not None and m[0] == "re":
                                depth -= 1
                                if depth == 0:
                                    break
                            j += 1
                        body = ops[i + 1:j]
                        real = [b for b in body if b.emit is not None]
                        if real:
                            c = sum(1 for b in real if (not b.is_dma) and b.signal)
                            dcnt = {}
                            for b in real:
                                if b.is_dma:
                                    dcnt[b.slot] = dcnt.get(b.slot, 0) + 1
                            saved = dict(seen)
                            cur_before = cur[0]
                            g = eng.If_lt(regs[e], o.mark[1])
                            g.__enter__()
                            if c > 0:
                                if cur_before > 0:
                                    eng.wait_ge(esem[e], cur_before)
                                eng.sem_inc(esem[e], c)
                            for sl, n in dcnt.items():
                                eng.sem_inc(ssem[sl], 16 * n)
                            g.__exit__(None, None, None)
                            g2 = eng.Else()
                            g2.__enter__()
                            emit_range(body)
                            g2.__exit__(None, None, None)
                            seen.clear()
                            seen.update(saved)
                            cur[0] = cur_before + c
                        i = j + 1
                        continue
                    if o.mark is None:
                        emit_one(o)
                    i += 1

            emit_range(self.q[e])

        with nc.Block() as block:

            @block.tensor
            def _(eng):
                emit_q("pe", eng)

            @block.scalar
            def _(eng):
                emit_q("act", eng)

            @block.vector
            def _(eng):
                emit_q("dve", eng)

            @block.gpsimd
            def _(eng):
                emit_q("pool", eng)

            @block.sync
            def _(eng):
                emit_q("sp", eng)

    def mm(self, out, pairs, r, w, start=True, stop=True):
        pairs = list(pairs)

        def emit(pe):
            n = len(pairs)
            ins = None
            for i, (l, rr) in enumerate(pairs):
                ins = pe.matmul(out, l, rr, start=(start and i == 0), stop=(stop and i == n - 1))
            return ins

        return self.op("pe", emit, r, w)

    def tr(self, out, in_, ident, r, w):
        return self.op("pe", lambda e: e.transpose(out, in_, ident), r, w)

    def tt(self, out, a, b, op, r, w, eng="dve"):
        return self.op(eng, lambda e: e.tensor_tensor(out, a, b, op), r, w)

    def ts(self, out, a, s1, op0, r, w, s2=None, op1=None, eng="dve"):
        if op1 is None:
            return self.op(eng, lambda e: e.tensor_scalar(out, a, s1, None, op0), r, w)
        return self.op(eng, lambda e: e.tensor_scalar(out, a, s1, s2, op0, op1), r, w)

    def stt(self, out, a, s, b, op0, op1, r, w, eng="dve"):
        return self.op(eng, lambda e: e.scalar_tensor_tensor(out, a, s, b, op0, op1), r, w)

    def cp(self, out, a, r, w, eng="dve"):
        return self.op(eng, lambda e: e.tensor_copy(out, a), r, w)

    def memset(self, out, v, w, eng="dve"):
        return self.op(eng, lambda e: e.memset(out, v), (), w)

    def act(self, out, a, func, r, w, bias=None, scale=None, accum=None):
        kw = {}
        if bias is not None:
            kw["bias"] = bias
        if scale is not None:
            kw["scale"] = scale
        if accum is not None:
            kw["accum_out"] = accum
        return self.op("act", lambda e: e.activation(out, a, func, **kw), r, w)

    def scan(self, out, d0, d1, init, r, w):
        return self.op("dve", lambda e: e.tensor_tensor_scan(out, d0, d1, init, ALU.mult, ALU.add), r, w)


def build(stage=None):
    nc = bass.Bass("TRN2", target_bir_lowering=False)
    top = ExitStack()

    def din(name, shape):
        return nc.dram_tensor(name, list(shape), F32, kind="ExternalInput").ap()

    x_d = din("x", [T, D])
    cT_d = din("cT", [128, 8])
    ada_w_d = din("ada_w", [D, 6 * D])
    ada_b_d = din("ada_b", [1, 6 * D])
    g1_d = din("g1", [1, D])
    g2_d = din("g2", [1, D])
    fg_d = din("fg", [1, D])
    w_in_d = din("w_in", [D, 3584])
    ssm_sm_d = din("ssm_sm", [128, 3, 16])
    ssm_lam_d = din("ssm_lam", [3, 2048])
    ssm_B_d = din("ssm_B", [2, 128, 2048])
    ssm_C_d = din("ssm_C", [2, 128, 2048])
    ssm_d_d = din("ssm_d", [128, 4])
    glu_w_d = din("glu_w", [512, 512])
    glu_b_d = din("glu_b", [128, 4])
    wa_d = din("wa", [512, D])
    wb_d = din("wb", [512, D])
    wout_d = din("wout", [D, D])
    lng_d = din("lng", [1, 512])
    lnb_d = din("lnb", [1, 512])
    wsT_d = din("wsT", [128, 8, 128])
    bs_d = din("bs", [1, 1024])
    rw_d = din("router_w", [D, NE])
    rb_d = din("router_b", [1, NE])
    mwi_d = din("moe_w_in", [NE, D, 2 * D])
    mbi_d = din("moe_b_in", [128, NE, 16])
    mwo_d = din("moe_w_out", [NE, D, D])
    mbo_d = din("moe_b_out", [NE, D])
    ident_d = din("ident", [128, 128])
    tri_d = din("tri", [128, 128])
    jj_d = din("jj", [128, ST])
    stri_d = din("stri", [128, 128])
    iota_d = din("iota256", [128, 256])
    eoff_d = din("eoff", [128, NE])
    out_d = nc.dram_tensor("out", [T, D], F32, kind="ExternalOutput").ap()
    x1s_d = nc.dram_tensor("x1s", [T, D], F32, kind=("ExternalOutput" if stage == "M" else "Internal")).ap()

    def dbg_out(Pg, name, ap, shape, dt=F32):
        o = nc.dram_tensor("dbg_" + name, list(shape), dt, kind="ExternalOutput").ap()
        Pg.dma("sp", o, ap, reads=[], writes=["dbg_" + name], slot="dbg_" + name)

    def sbuf(es, name, shape, dt=F32):
        return es.enter_context(nc.sbuf_tensor("sb_" + name, list(shape), dt))

    pst = [top.enter_context(nc.psum_tensor("pst%d" % i, [128, 1024], BF16)) for i in range(2)]
    psf = [top.enter_context(nc.psum_tensor("psf%d" % i, [128, 512], F32)) for i in range(6)]
    rot = {"f": 0, "t": 0, "n": 6}

    def nxt():
        i = rot["f"] % rot["n"]
        rot["f"] += 1
        return psf[i], "psf%d" % i

    def nxt_t():
        i = rot["t"] % 2
        rot["t"] += 1
        return pst[i], "pst%d" % i

    ident_bf = sbuf(top, "ident_bf", [128, 128], BF16)
    ident_f = sbuf(top, "ident_f", [128, 128], F32)
    ones_row = sbuf(top, "ones_row", [1, 128], F32)
    epsc = sbuf(top, "epsc", [128, 1], F32)
    cols = sbuf(top, "cols", [128, 32], F32)
    gate2b = sbuf(top, "gate2b", [128, D], F32)
    modscr_d = nc.dram_tensor("modscr", [2, D], F32, kind="Internal").ap()
    regs = {"pe": top.enter_context(nc.tensor.register("r_pe")), "act": top.enter_context(nc.scalar.register("r_act")),
            "dve": top.enter_context(nc.vector.register("r_dve")), "pool": top.enter_context(nc.gpsimd.register("r_pool")),
            "sp": top.enter_context(nc.sync.register("r_sp"))}
    a1c, s1c, a2c, s2c = (cols[:, 0:8], cols[:, 8:16], cols[:, 16:24], cols[:, 24:32])

    p1 = ExitStack()
    gate1b = sbuf(p1, "gate1b", [128, D], F32)
    BT_bf = sbuf(p1, "BT_bf", [128, 16, 2, 128], BF16)
    CT_bf = sbuf(p1, "CT_bf", [128, 16, 2, 128], BF16)
    cosT = sbuf(p1, "cosT", [128, 16, ST], BF16)
    sinT = sbuf(p1, "sinT", [128, 16, ST], BF16)
    rcol = sbuf(p1, "rcol", [128, 16], F32)
    carry = sbuf(p1, "carry", [128, 16, 2], F32)
    dcol = sbuf(p1, "dcol", [128, 4], F32)
    glubc = sbuf(p1, "glubc", [128, 4], F32)

    sA = ExitStack()
    PA = Prog(nc, top, "A")
    L3 = sbuf(sA, "L3", [128, 3, 2048], F32)
    Bz = sbuf(sA, "Bz", [128, 2, 2048], F32)
    tA = sbuf(sA, "tA", [128, 4096], F32)
    tB = sbuf(sA, "tB", [128, 4096], F32)
    tC = sbuf(sA, "tC", [128, 4096], F32)
    tI = sbuf(sA, "tI", [128, 4096], I32)
    sm = sbuf(sA, "sm", [128, 3, 16], F32)
    smw = sbuf(sA, "smw", [128, 2, 16], F32)
    jj = sbuf(sA, "jj", [128, ST], F32)
    idl = sbuf(sA, "idl", [128, 128], F32)
    hpic = sbuf(sA, "hpic", [128, 1], F32)

    PA.dma("sp", idl[:], ident_d, writes=["idl"], slot="ld", group=True)
    PA.dma("sp", ident_f[:], ident_d, writes=["ident_f"], slot="ld", group=True)
    for i in range(3):
        PA.dma("sp", L3[:, i, :], ssm_lam_d[i:i + 1, :].partition_broadcast(128), writes=["L3"], slot="ld", group=True)
    for i in range(2):
        PA.dma("sp", Bz[:, i, :], ssm_B_d[i], writes=["Bz"], slot="ld", group=True)
    PA.dma("sp", sm[:], ssm_sm_d, writes=["sm"], slot="ld", group=True)
    PA.dma("sp", jj[:], jj_d, writes=["jj"], slot="ld", group=True)
    PA.dma("sp", dcol[:], ssm_d_d, writes=["dcol"], slot="ld", group=True)
    PA.dma("sp", glubc[:], glu_b_d, writes=["glubc"], slot="ld", group=True)
    PA.cp(ident_bf[:], idl[:], ["idl"], ["ident_bf"])
    PA.memset(ones_row[:], 1.0, ["ones_row"])
    PA.memset(epsc[:], EPS, ["epsc"])
    PA.memset(hpic[:], PI / 2, ["hpic"])
    PA.memset(carry[:], 0.0, ["carry"])

    def range_reduce(Pg, y, x, shift, ntmp, itmp, rx, ry, rn, ri):
        Pg.ts(ntmp, x, 1.0 / TWO_PI, ALU.mult, [rx], [rn], s2=shift / TWO_PI, op1=ALU.add)
        Pg.cp(itmp, ntmp, [rn], [ri])
        Pg.cp(ntmp, itmp, [ri], [rn])
        Pg.ts(y, x, shift, ALU.add, [rx], [ry])
        Pg.stt(y, ntmp, -TWO_PI, y, ALU.mult, ALU.add, [rn, ry], [ry])
        Pg.op("dve", lambda e: e.tensor_single_scalar(ntmp, y, PI, ALU.is_gt), [ry], [rn])
        Pg.stt(y, ntmp, -TWO_PI, y, ALU.mult, ALU.add, [rn, ry], [ry])
        Pg.op("dve", lambda e: e.tensor_single_scalar(ntmp, y, -PI, ALU.is_lt), [ry], [rn])
        Pg.stt(y, ntmp, TWO_PI, y, ALU.mult, ALU.add, [rn, ry], [ry])

    A0, A1 = tA[:, 0:2048], tA[:, 2048:4096]
    B0, B1 = tB[:, 0:2048], tB[:, 2048:4096]
    C0, C1 = tC[:, 0:2048], tC[:, 2048:4096]
    I0 = tI[:, 0:2048]
    are, aim, ldt = L3[:, 0, :], L3[:, 1, :], L3[:, 2, :]
    PA.act(A0, ldt, AF.Exp, ["L3"], ["A0"])
    PA.tt(A1, aim, A0, ALU.mult, ["L3", "A0"], ["A1"])
    PA.tt(B0, are, A0, ALU.mult, ["L3", "A0"], ["B0"])
    PA.act(B0, B0, AF.Exp, ["B0"], ["B0"])
    range_reduce(PA, C0, A1, 0.0, C1, I0, "A1", "C0", "C1", "I0")
    PA.act(B1, C0, AF.Sin, ["C0"], ["B1"])
    PA.act(C1, C0, AF.Abs, ["C0"], ["C1"])
    PA.act(A0, C1, AF.Sin, ["C1", "A0"], ["A0"], bias=hpic[:, 0:1], scale=-1.0)
    PA.tt(A0, B0, A0, ALU.mult, ["B0", "A0"], ["A0"])
    PA.tt(B1, B0, B1, ALU.mult, ["B0", "B1"], ["B1"])
    PA.ts(A0, A0, -1.0, ALU.add, ["A0"], ["A0"])
    PA.tt(C0, are, are, ALU.mult, ["L3"], ["C0"])
    PA.tt(C1, aim, aim, ALU.mult, ["L3"], ["C1"])
    PA.tt(C0, C0, C1, ALU.add, ["C0", "C1"], ["C0"])
    PA.op("dve", lambda e: e.reciprocal(C0, C0), ["C0"], ["C0"])
    PA.tt(C1, A0, are, ALU.mult, ["A0", "L3"], ["C1"])
    PA.tt(B0, B1, aim, ALU.mult, ["B1", "L3"], ["B0"])
    PA.tt(C1, C1, B0, ALU.add, ["C1", "B0"], ["C1"])
    PA.tt(C1, C1, C0, ALU.mult, ["C1", "C0"], ["C1"])
    PA.tt(B0, B1, are, ALU.mult, ["B1", "L3"], ["B0"])
    PA.tt(A1, A0, aim, ALU.mult, ["A0", "L3"], ["A1"])
    PA.tt(B0, B0, A1, ALU.subtract, ["B0", "A1"], ["B0"])
    PA.tt(B0, B0, C0, ALU.mult, ["B0", "C0"], ["B0"])
    Bre, Bim = Bz[:, 0, :], Bz[:, 1, :]
    v3 = lambda ap: ap.rearrange("p (k s) -> p k s", k=16)
    PA.tt(A0, C1, Bre, ALU.mult, ["C1", "Bz"], ["A0"])
    PA.tt(A1, B0, Bim, ALU.mult, ["B0", "Bz"], ["A1"])
    PA.tt(BT_bf[:, :, 0, :], v3(A0), v3(A1), ALU.subtract, ["A0", "A1"], ["BT"])
    PA.tt(A0, C1, Bim, ALU.mult, ["C1", "Bz", "BT"], ["A0"])
    PA.tt(A1, B0, Bre, ALU.mult, ["B0", "Bz", "BT"], ["A1"])
    PA.tt(BT_bf[:, :, 1, :], v3(A0), v3(A1), ALU.add, ["A0", "A1"], ["BT"])
    PA.dma("sp", Bz[:, 0, :], ssm_C_d[0], reads=["BT"], writes=["Bz"], slot="ld2", group=True)
    PA.dma("sp", Bz[:, 1, :], ssm_C_d[1], reads=["BT"], writes=["Bz"], slot="ld2", group=True)
    PA.cp(CT_bf[:, :, 0, :], v3(Bz[:, 0, :]), ["Bz"], ["CT"])
    PA.ts(CT_bf[:, :, 1, :], v3(Bz[:, 1, :]), -1.0, ALU.mult, ["Bz"], ["CT"])
    PA.act(smw[:, 0, :], sm[:, 2, :], AF.Exp, ["sm"], ["smw0"])
    PA.tt(smw[:, 1, :], sm[:, 1, :], smw[:, 0, :], ALU.mult, ["sm", "smw0"], ["smw1"])
    PA.tt(rcol[:], sm[:, 0, :], smw[:, 0, :], ALU.mult, ["sm", "smw0"], ["rcol"])
    PA.act(rcol[:], rcol[:], AF.Exp, ["rcol"], ["rcol"])
    ang = tA[:, :].rearrange("p (k j) -> p k j", k=16)
    PA.tt(ang, jj[:].unsqueeze(1).to_broadcast([128, 16, ST]), smw[:, 1, :].unsqueeze(2).to_broadcast([128, 16, ST]),
          ALU.mult, ["jj", "smw1", "A0", "A1", "BT"], ["tA"])
    range_reduce(PA, tB[:, :], tA[:, :], 0.0, tC[:, :], tI[:, :], "tA", "tB", "tC", "tI")
    PA.act(sinT[:].rearrange("p k j -> p (k j)"), tB[:, :], AF.Sin, ["tB", "B0", "B1", "C0", "C1"], ["sinT"])
    PA.act(tC[:, :], tB[:, :], AF.Abs, ["tB"], ["tC"])
    PA.act(cosT[:].rearrange("p k j -> p (k j)"), tC[:, :], AF.Sin, ["tC"], ["cosT"], bias=hpic[:, 0:1], scale=-1.0)
    PA.barrier()
    if stage == "A":
        dbg_out(PA, "BT", BT_bf[:], [128, 16, 2, 128], BF16)
        dbg_out(PA, "CT", CT_bf[:], [128, 16, 2, 128], BF16)
        dbg_out(PA, "cosT", cosT[:], [128, 16, ST], BF16)
        dbg_out(PA, "sinT", sinT[:], [128, 16, ST], BF16)
        dbg_out(PA, "rcol", rcol[:], [128, 16], F32)
        PA.barrier()
        PA.emit()
        return nc
    PA.emit()
    nc.all_engine_barrier()
    sA.close()

    sB = ExitStack()
    PB = Prog(nc, top, "B")
    modrow = sbuf(sB, "modrow", [1, 6 * D], F32)
    grow = sbuf(sB, "grow", [1, 2 * D], F32)
    arow = sbuf(sB, "arow", [1, 2 * D], F32)
    cTt = sbuf(sB, "cTt", [128, 8], F32)
    adaR = [sbuf(sB, "adaR%d" % i, [128, 1536], F32) for i in range(3)]
    PB.dma("sp", modrow[:], ada_b_d, writes=["modrow"], slot="ld", group=True)
    PB.dma("sp", grow[:, 0:D], g1_d, writes=["grow"], slot="ld", group=True)
    PB.dma("sp", grow[:, D:2 * D], g2_d, writes=["grow"], slot="ld", group=True)
    PB.dma("sp", cTt[:], cT_d, writes=["cTt"], slot="ld", group=True)
    PB.act(cTt[:], cTt[:], AF.Silu, ["cTt"], ["cTt"])
    n_ada = 0
    for qd in range(4):
        banks = [nxt() for _ in range(3)]
        for kc in range(8):
            rb = n_ada % 3
            n_ada += 1
            PB.dma("sp", adaR[rb][:], ada_w_d[kc * 128:(kc + 1) * 128, qd * 1536:(qd + 1) * 1536],
                   writes=["adaR%d" % rb], slot="ada%d" % rb)
            for n in range(3):
                PB.mm(banks[n][0][0:1, :], [(cTt[:, kc:kc + 1], adaR[rb][:, n * 512:(n + 1) * 512])],
                      ["cTt", "adaR%d" % rb], [banks[n][1]], start=(kc == 0), stop=(kc == 7))
        for n in range(3):
            sl = modrow[:, qd * 1536 + n * 512: qd * 1536 + (n + 1) * 512]
            PB.tt(sl, banks[n][0][0:1, :], sl, ALU.add, [banks[n][1], "modrow"], ["modrow"])
    PB.stt(arow[:, 0:D], modrow[:, D:2 * D], 1.0, grow[:, 0:D], ALU.add, ALU.mult, ["modrow", "grow"], ["arow"])
    PB.stt(arow[:, D:2 * D], modrow[:, 4 * D:5 * D], 1.0, grow[:, D:2 * D], ALU.add, ALU.mult, ["modrow", "grow"], ["arow"])
    pc, pcn = nxt()
    vecs = [arow[:, 0:D], modrow[:, 0:D], arow[:, D:2 * D], modrow[:, 3 * D:4 * D]]
    for vi, vec in enumerate(vecs):
        for fc in range(8):
            PB.mm(pc[:, vi * 8 + fc: vi * 8 + fc + 1], [(vec[0:1, fc * 128:(fc + 1) * 128], ones_row[0:1, 0:1])],
                  ["arow", "modrow"], [pcn])
    PB.cp(cols[:], pc[:, 0:32], [pcn], ["cols"])
    for gi, (gsrc, gdst, gname) in enumerate([(modrow[:, 2 * D:3 * D], gate1b, "gate1b"), (modrow[:, 5 * D:6 * D], gate2b, "gate2b"),
                                              ]):
        for h in range(2):
            pb_, pbn = nxt()
            PB.mm(pb_[:, :], [(ones_row[0:1, :], gsrc[0:1, h * 512:(h + 1) * 512])], ["modrow", "arow"], [pbn])
            PB.cp(gdst[:, h * 512:(h + 1) * 512], pb_[:, :], [pbn], [gname])
    PB.dma("sp", modscr_d[0:1, :], arow[:, D:2 * D], reads=["arow"], writes=["modscr0"], slot="ms", group=True)
    PB.dma("sp", modscr_d[1:2, :], modrow[:, 3 * D:4 * D], reads=["modrow"], writes=["modscr1"], slot="ms", group=True)
    PB.barrier()
    if stage == "B":
        dbg_out(PB, "cols", cols[:], [128, 32], F32)
        dbg_out(PB, "gate1b", gate1b[:], [128, D], F32)
        dbg_out(PB, "gate2b", gate2b[:], [128, D], F32)
        PB.barrier()
        PB.emit()
        return nc
    PB.emit()
    nc.all_engine_barrier()
    sB.close()

    PM = Prog(nc, top, "M")
    w_in_bf = sbuf(p1, "w_in_bf", [128, 8, 3584], BF16)
    glu_bf = sbuf(p1, "glu_bf", [128, 4, 512], BF16)
    wa_bf = sbuf(p1, "wa_bf", [128, 4, D], BF16)
    wb_bf = sbuf(p1, "wb_bf", [128, 4, D], BF16)
    wout_bf = sbuf(p1, "wout_bf", [128, 8, D], BF16)
    wsT_bf = sbuf(p1, "wsT_bf", [128, 8, 128], BF16)
    lngb = sbuf(p1, "lngb", [128, 512], F32)
    lnbb = sbuf(p1, "lnbb", [128, 512], F32)
    bs_row = sbuf(p1, "bs_row", [1, 1024], F32)
    xt = [sbuf(p1, "xt%d" % i, [128, D], F32) for i in range(1)]
    xn = sbuf(p1, "xn", [128, D], BF16)
    st = sbuf(p1, "st", [128, 16], F32)
    hT = sbuf(p1, "hT", [128, 8, ST], BF16)
    us32 = sbuf(p1, "us32", [128, 4, ST], F32)
    usbf = sbuf(p1, "usbf", [128, 4, ST], BF16)
    guT = sbuf(p1, "guT", [128, 4, ST], BF16)
    gv = sbuf(p1, "gv", [128, 512], F32)
    gv2 = sbuf(p1, "gv2", [128, 512], F32)
    vn = sbuf(p1, "vn", [128, 2, 512], BF16)
    xf = sbuf(p1, "xf", [128, D], F32)
    sga = sbuf(p1, "sga", [128, 8, ST], BF16)
    sgb = sbuf(p1, "sgb", [128, 8, ST], BF16)
    zT = sbuf(p1, "zT", [128, 4, ST], BF16)
    sg = sbuf(p1, "sg", [128, 4, ST], BF16)
    oT = sbuf(p1, "oT", [128, 4, ST], BF16)
    pT = sbuf(p1, "pT", [128, 4, ST], BF16)
    ta = sbuf(p1, "ta", [128, 2, ST], F32)
    tb = sbuf(p1, "tb", [128, 2, ST], F32)
    mT = sbuf(p1, "mT", [128, 8, ST], BF16)
    t1 = sbuf(p1, "t1", [128, ST], F32)
    t2 = sbuf(p1, "t2", [128, ST], F32)
    wri = sbuf(p1, "wri", [128, 2, ST], F32)
    qri = sbuf(p1, "qri", [128, 2, ST], F32)
    s32 = sbuf(p1, "s32", [128, 2, ST], F32)
    sT = sbuf(p1, "sT", [128, 2, ST], BF16)
    yv = sbuf(p1, "yv", [128, ST], F32)

    for kc in range(8):
        PM.dma("pool", w_in_bf[:, kc, :], w_in_d[kc * 128:(kc + 1) * 128, :], writes=["w_in%d" % kc], slot="wl", group=True)
    PM.dma("pool", glu_bf[:], glu_w_d.rearrange("(c p) n -> p c n", p=128), writes=["glu"], slot="wl", group=True)
    PM.dma("pool", wa_bf[:], wa_d.rearrange("(c p) n -> p c n", p=128), writes=["wa"], slot="wl", group=True)
    PM.dma("pool", wb_bf[:], wb_d.rearrange("(c p) n -> p c n", p=128), writes=["wb"], slot="wl", group=True)
    PM.dma("pool", wout_bf[:], wout_d.rearrange("(c p) n -> p c n", p=128), writes=["wout"], slot="wl", group=True)
    PM.dma("sp", lngb[:], lng_d.partition_broadcast(128), writes=["lngb"], slot="wl2", group=True)
    PM.dma("sp", lnbb[:], lnb_d.partition_broadcast(128), writes=["lnbb"], slot="wl2", group=True)
    PM.dma("sp", bs_row[:], bs_d, writes=["bs_row"], slot="wl2", group=True)
    wsst = us32[:].rearrange("p a n -> p (a n)").rearrange("p (h t) -> p h t", h=8)
    PM.dma("sp", wsst, wsT_d, writes=["us32"], slot="wl2", group=True)
    PM.dma("sp", gv[:, 0:128], tri_d, writes=["gv"], slot="wl2", group=True)
    PM.tt(wsT_bf[:], wsst, gv[:, 0:128].unsqueeze(1).to_broadcast([128, 8, 128]), ALU.mult, ["us32", "gv"], ["wsT_bf"])
    W_IN = ["w_in%d" % kc for kc in range(8)]

    def pairview(ps_):
        return ps_[:, :].rearrange("p (a n) -> p a n", a=2)

    xq = [0]
    rot["n"] = 5
    for s in range(NST):
        def front_norm_tile(ss, i):
            if True:
                tix = 2 * ss + i
                xb = xf
                xbn = "xf"
                PM.dma("sp", xb[:], x_d[tix * 128:(tix + 1) * 128, :], writes=[xbn], slot=xbn)
                PM.memset(st[:, 0:1], 0.0, ["st0"])
                PM.act(xn[:], xb[:], AF.Square, [xbn, "st0"], ["xn", "st0"], accum=st[:, 0:1])
                PM.act(st[:, 1:2], st[:, 0:1], AF.Sqrt, ["st0"], ["st1"], bias=epsc[:, 0:1], scale=1.0 / D)
                PM.op("dve", lambda e: e.reciprocal(st[:, 2:3], st[:, 1:2]), ["st1"], ["st2"])
                PM.ts(xn[:], xb[:], st[:, 2:3], ALU.mult, [xbn, "st2"], ["xn"])
                pt_, ptn = nxt_t()
                for fc in range(8):
                    PM.tr(pt_[:, fc * 128:(fc + 1) * 128], xn[:, fc * 128:(fc + 1) * 128], ident_bf[:], ["xn"], [ptn])
                for fc in range(8):
                    PM.act(hT[:, fc, i * 128:(i + 1) * 128], pt_[:, fc * 128:(fc + 1) * 128], AF.Identity,
                           [ptn], ["hT"], bias=s1c[:, fc:fc + 1], scale=a1c[:, fc:fc + 1])

        def proj_u():
            for pr in range(2):
                ps_, psn = nxt()
                for a in range(2):
                    cc = 2 * pr + a
                    PM.mm(ps_[:, a * ST:(a + 1) * ST], [(w_in_bf[:, kc, cc * 128:(cc + 1) * 128], hT[:, kc, :]) for kc in range(8)],
                          W_IN + ["hT"], [psn])
                PM.act(us32[:, 2 * pr:2 * pr + 2, :], pairview(ps_), AF.Copy, [psn], ["us32"])
            PM.cp(usbf[:], us32[:], ["us32"], ["usbf"], eng="pool")

        def proj_zu():
            for pr in range(2):
                ps_, psn = nxt()
                for a in range(2):
                    cc = 2 * pr + a
                    PM.mm(ps_[:, a * ST:(a + 1) * ST], [(w_in_bf[:, kc, 512 + cc * 128:512 + (cc + 1) * 128], hT[:, kc, :]) for kc in range(8)],
                          W_IN + ["hT"], [psn])
                PM.act(guT[:, 2 * pr:2 * pr + 2, :], pairview(ps_), AF.Gelu_apprx_tanh, [psn], ["guT"])

        def proj_v(i):
            ps_, psn = nxt()
            PM.mm(ps_[:, :], [(hT[:, kc, i * 128:(i + 1) * 128], w_in_bf[:, kc, 1024:1536]) for kc in range(8)],
                  W_IN + ["hT"], [psn])
            PM.memset(st[:, 4:6], 0.0, ["st4"])
            PM.act(gv[:], ps_[:, :], AF.Gelu_apprx_tanh, [psn, "st4"], ["gv", "st4"], accum=st[:, 4:5])
            PM.act(gv2[:], gv[:], AF.Square, ["gv", "st4"], ["gv2", "st4"], accum=st[:, 5:6])
            PM.ts(st[:, 6:7], st[:, 4:5], 1.0 / 512, ALU.mult, ["st4"], ["st6"])
            PM.tt(st[:, 7:8], st[:, 6:7], st[:, 6:7], ALU.mult, ["st6"], ["st7"])
            PM.stt(st[:, 8:9], st[:, 5:6], 1.0 / 512, st[:, 7:8], ALU.mult, ALU.subtract, ["st4", "st7"], ["st8"])
            PM.act(st[:, 9:10], st[:, 8:9], AF.Sqrt, ["st8"], ["st9"], bias=epsc[:, 0:1], scale=1.0)
            PM.op("dve", lambda e: e.reciprocal(st[:, 10:11], st[:, 9:10]), ["st9"], ["st10"])
            PM.stt(st[:, 11:12], st[:, 6:7], -1.0, st[:, 10:11], ALU.mult, ALU.mult, ["st6", "st10"], ["st11"])
            PM.act(gv2[:], gv[:], AF.Identity, ["gv", "st10", "st11"], ["gv2"], bias=st[:, 11:12], scale=st[:, 10:11])
            PM.tt(gv[:], gv2[:], lngb[:], ALU.mult, ["gv2", "lngb"], ["gv"])
            PM.tt(vn[:, i, :], gv[:], lnbb[:], ALU.add, ["gv", "lnbb"], ["vn"])

        def mix(cc):
            ps_, psn = nxt()
            for i in range(2):
                for hh in range(2):
                    h = 2 * cc + hh
                    o_ = ps_[hh * 64:(hh + 1) * 64, i * 128:(i + 1) * 128]

                    def emit(pe, o_=o_, i=i, cc=cc, hh=hh, h=h):
                        pe.matmul(o_, vn[:, i, cc * 128 + hh * 64: cc * 128 + (hh + 1) * 64], wsT_bf[:, h, :], start=True, stop=False)
                        return pe.matmul(o_, ones_row[0:1, 0:64], bs_row[0:1, h * 128:(h + 1) * 128], start=False, stop=True)

                    PM.op("pe", emit, ["vn", "wsT_bf", "bs_row"], [psn])
            PM.tt(pT[:, cc, :], guT[:, cc, :], ps_[:, 0:ST], ALU.mult, ["guT", psn], ["pT"])

        def gates(pr):
            pga, pgan = nxt()
            pgb, pgbn = nxt()
            for a in range(2):
                dc = 2 * pr + a
                sl = slice(a * ST, (a + 1) * ST)
                PM.mm(pga[:, sl], [(w_in_bf[:, kc, 1536 + dc * 128:1536 + (dc + 1) * 128], hT[:, kc, :]) for kc in range(8)], W_IN + ["hT"], [pgan])
                PM.mm(pgb[:, sl], [(w_in_bf[:, kc, 2560 + dc * 128:2560 + (dc + 1) * 128], hT[:, kc, :]) for kc in range(8)], W_IN + ["hT"], [pgbn])
            PM.act(sga[:, 2 * pr:2 * pr + 2, :], pairview(pga), AF.Sigmoid, [pgan], ["sga"])
            PM.act(sgb[:, 2 * pr:2 * pr + 2, :], pairview(pgb), AF.Sigmoid, [pgbn], ["sgb"])

        bu_banks = {}

        def bu(k):
            cc = k // 4
            pb_, pbn = nxt()
            PM.mm(pb_[:, 0:ST], [(BT_bf[:, k, 0, :], usbf[:, cc, :])], ["usbf"], [pbn])
            PM.mm(pb_[:, ST:2 * ST], [(BT_bf[:, k, 1, :], usbf[:, cc, :])], ["usbf"], [pbn])
            bu_banks[k] = (pb_, pbn)

        def ssm_dve(k):
            pb_, pbn = bu_banks[k]
            bre, bim = pb_[:, 0:ST], pb_[:, ST:2 * ST]
            ck, sk = cosT[:, k, :], sinT[:, k, :]
            PM.tt(t1[:], bre, ck, ALU.mult, [pbn], ["t1"])
            PM.tt(t2[:], bim, sk, ALU.mult, [pbn], ["t2"])
            PM.tt(wri[:, 0, :], t1[:], t2[:], ALU.add, ["t1", "t2"], ["wri0"])
            PM.tt(t1[:], bim, ck, ALU.mult, [pbn], ["t1"])
            PM.tt(t2[:], bre, sk, ALU.mult, [pbn], ["t2"])
            PM.tt(wri[:, 1, :], t1[:], t2[:], ALU.subtract, ["t1", "t2"], ["wri1"])
            rb_ = rcol[:, k:k + 1].to_broadcast([128, ST])
            PM.scan(qri[:, 0, :], rb_, wri[:, 0, :], carry[:, k, 0:1], ["wri0", "carry%d" % k], ["qri0"])
            PM.scan(qri[:, 1, :], rb_, wri[:, 1, :], carry[:, k, 1:2], ["wri1", "carry%d" % k], ["qri1"])
            PM.tt(t1[:], qri[:, 0, :], ck, ALU.mult, ["qri0"], ["t1"])
            PM.tt(t2[:], qri[:, 1, :], sk, ALU.mult, ["qri1"], ["t2"])
            PM.tt(s32[:, 0, :], t1[:], t2[:], ALU.subtract, ["t1", "t2"], ["s32a"])
            PM.tt(t1[:], qri[:, 1, :], ck, ALU.mult, ["qri1"], ["t1"])
            PM.tt(t2[:], qri[:, 0, :], sk, ALU.mult, ["qri0"], ["t2"])
            PM.tt(s32[:, 1, :], t1[:], t2[:], ALU.add, ["t1", "t2"], ["s32b"])
            PM.act(sT[:], s32[:], AF.Copy, ["s32a", "s32b"], ["sT"])
            PM.cp(carry[:, k, :], s32[:, :, ST - 1], ["s32a", "s32b"], ["carry%d" % k], eng="pool")

        def ssm_c(k):
            cc = k // 4
            yps, ypn = psf[5], "psf5"
            PM.mm(yps[:, 0:ST], [(CT_bf[:, k, 0, :], sT[:, 0, :]), (CT_bf[:, k, 1, :], sT[:, 1, :])], ["sT"], [ypn],
                  start=(k % 4 == 0), stop=(k % 4 == 3))
            if k % 4 == 3:
                PM.stt(yv[:], us32[:, cc, :], dcol[:, cc:cc + 1], yps[:, 0:ST], ALU.mult, ALU.add, ["us32", ypn], ["yv"])
                PM.act(zT[:, cc, :], yv[:], AF.Gelu_apprx_tanh, ["yv"], ["zT"])

        extras = {0: proj_zu, 1: (lambda: proj_v(0)), 2: (lambda: proj_v(1)),
                  4: (lambda: mix(0)), 5: (lambda: mix(1)), 6: (lambda: mix(2)), 7: (lambda: mix(3)),
                  8: (lambda: gates(0)), 9: (lambda: gates(1)), 10: (lambda: gates(2)), 11: (lambda: gates(3))}
        if s == 0:
            front_norm_tile(0, 0)
            front_norm_tile(0, 1)
        if s + 1 < NST:
            extras[12] = (lambda: front_norm_tile(s + 1, 0))
            extras[13] = (lambda: front_norm_tile(s + 1, 1))
        proj_u()
        bu(0)
        for k in range(16):
            if k + 1 < 16:
                bu(k + 1)
            ssm_dve(k)
            if k in extras:
                extras[k]()
            ssm_c(k)
        for pr in range(2):
            ps_, psn = nxt()
            for a in range(2):
                n = 2 * pr + a
                PM.mm(ps_[:, a * ST:(a + 1) * ST], [(glu_bf[:, cc, n * 128:(n + 1) * 128], zT[:, cc, :]) for cc in range(4)],
                      ["glu", "zT"], [psn])
                PM.act(sg[:, n, :], ps_[:, a * ST:(a + 1) * ST], AF.Sigmoid, [psn], ["sg"], bias=glubc[:, n:n + 1], scale=1.0)
        PM.tt(oT[:], zT[:], sg[:], ALU.mult, ["zT", "sg"], ["oT"])
        for pr in range(4):
            pya, pyan = nxt()
            pyb, pybn = nxt()
            for a in range(2):
                dc = 2 * pr + a
                sl = slice(a * ST, (a + 1) * ST)
                PM.mm(pya[:, sl], [(wa_bf[:, cc, dc * 128:(dc + 1) * 128], oT[:, cc, :]) for cc in range(4)], ["wa", "oT"], [pyan])
                PM.mm(pyb[:, sl], [(wb_bf[:, cc, dc * 128:(dc + 1) * 128], pT[:, cc, :]) for cc in range(4)], ["wb", "pT"], [pybn])
            PM.tt(ta[:], sga[:, 2 * pr:2 * pr + 2, :], pairview(pya), ALU.mult, ["sga", pyan], ["ta"])
            PM.tt(tb[:], sgb[:, 2 * pr:2 * pr + 2, :], pairview(pyb), ALU.mult, ["sgb", pybn], ["tb"])
            PM.tt(mT[:, 2 * pr:2 * pr + 2, :], ta[:], tb[:], ALU.add, ["ta", "tb"], ["mT"])
        for i in range(2):
            tix = 2 * s + i
            xb = xt[0]
            xbn = "xt0"
            PM.dma("sp", xb[:], x_d[tix * 128:(tix + 1) * 128, :], writes=[xbn], slot=xbn)
            for dh in range(2):
                ps_, psn = nxt()
                PM.mm(ps_[:, :], [(mT[:, kc, i * 128:(i + 1) * 128], wout_bf[:, kc, dh * 512:(dh + 1) * 512]) for kc in range(8)],
                      ["mT", "wout"], [psn])
                PM.tt(gv2[:], ps_[:, :], gate1b[:, dh * 512:(dh + 1) * 512], ALU.mult, [psn], ["gv2"])
                PM.tt(xb[:, dh * 512:(dh + 1) * 512], gv2[:], xb[:, dh * 512:(dh + 1) * 512], ALU.add, ["gv2", xbn], [xbn])
            PM.dma("sp", x1s_d[tix * 128:(tix + 1) * 128, :], xb[:], reads=[xbn], writes=["x1s%d" % tix], slot=xbn)
    rot["n"] = 6
    PM.barrier()
    PM.emit()
    if stage == "M":
        return nc
    nc.all_engine_barrier()
    p1.close()

    p2 = ExitStack()
    PX = Prog(nc, top, "X")
    PX.regs = regs
    UW = 256
    NU = T // UW
    x1 = sbuf(p2, "x1", [128, NT, D], F32)
    h2tm = sbuf(p2, "h2tm", [128, NT, D], BF16)
    bgc = sbuf(p2, "bgc", [128, NE, 16], F32)
    posm = sbuf(p2, "posm", [128, NT, NE], F32)
    rwhl = sbuf(p2, "rwhl", [128, NT, NE, 2], BF16)
    iota = sbuf(p2, "iota", [128, UW], F32)
    ne_i = sbuf(p2, "ne_i", [1, NE], I32)
    p2r = ExitStack()
    rwb_bf = sbuf(p2r, "rwb_bf", [128, 8, NE], BF16)
    rbb = sbuf(p2r, "rbb", [128, NE], F32)
    bo_g = sbuf(p2r, "bo_g", [NE, D], F32)
    maskf = sbuf(p2r, "maskf", [128, NT, NE], F32)
    maskb = sbuf(p2r, "maskb", [128, NT, NE], BF16)
    rw = sbuf(p2r, "rw", [128, NT, NE], F32)
    stri_bf = sbuf(p2r, "stri_bf", [128, 128], BF16)
    ones_bf = sbuf(p2r, "ones_bf", [128, 128], BF16)
    cnt = sbuf(p2r, "cnt", [1, 3, NE], F32)
    acc2_ = [sbuf(p2r, "acc2_%d" % i, [128, D], F32) for i in range(2)]
    xn2_ = [sbuf(p2r, "xn2_%d" % i, [128, D], BF16) for i in range(2)]
    h2Tt_ = [sbuf(p2r, "h2Tt_%d" % i, [128, 8, 128], BF16) for i in range(2)]
    rwT_ = [sbuf(p2r, "rwT_%d" % i, [NE, 128], F32) for i in range(2)]
    rt_ = [sbuf(p2r, "rt_%d" % i, [128, 4, NE], F32) for i in range(2)]
    rs_ = [sbuf(p2r, "rs_%d" % i, [128, 24], F32) for i in range(2)]
    rtp = sbuf(p2r, "rtp", [128, 4, NE], F32)
    a2b = sbuf(p2r, "a2b", [128, D], F32)
    s2b = sbuf(p2r, "s2b", [128, D], F32)
    ldf = sbuf(p2r, "ldf", [128, 256], F32)

    PX.dma("sp", rbb[:], rb_d.partition_broadcast(128), writes=["rbb"], slot="ld", group=True)
    PX.dma("sp", a2b[:], modscr_d[0:1, :].partition_broadcast(128), writes=["a2b"], slot="ld", group=True)
    PX.dma("sp", s2b[:], modscr_d[1:2, :].partition_broadcast(128), writes=["s2b"], slot="ld", group=True)
    PX.dma("sp", bgc[:], mbi_d, writes=["bgc"], slot="ld", group=True)
    PX.dma("sp", bo_g[:], mbo_d, writes=["bo_g"], slot="ld", group=True)
    PX.dma("sp", iota[:], iota_d, writes=["iota"], slot="ld", group=True)
    PX.dma("sp", ldf[:, 0:128], stri_d, writes=["ldf"], slot="ld", group=True)
    PX.dma("pool", rwb_bf[:], rw_d.rearrange("(c p) e -> p c e", p=128), writes=["rwb"], slot="ldp")
    PX.cp(stri_bf[:], ldf[:, 0:128], ["ldf"], ["stri_bf"])
    PX.memset(ones_bf[:], 1.0, ["ones_bf"])
    PX.ts(bgc[:, :, 8:16], bgc[:, :, 8:16], 1.0, ALU.add, ["bgc"], ["bgc"])
    PX.tt(bo_g[:], bo_g[:], gate2b[0:NE, :], ALU.mult, ["bo_g"], ["bo_g"])

    for tix in range(NT):
        pb2 = tix % 2
        rs, rt, rwT, h2Tt, xn2, acc2 = rs_[pb2], rt_[pb2], rwT_[pb2], h2Tt_[pb2], xn2_[pb2], acc2_[pb2]
        xb = x1[:, tix, :]
        xbn = "x1_%d" % tix
        PX.dma("sp", xb, x1s_d[tix * 128:(tix + 1) * 128, :], writes=[xbn], slot="x1l%d" % tix)
        PX.memset(rs[:, 0:1], 0.0, ["rs0_%d" % pb2])
        PX.act(xn2[:], xb, AF.Square, [xbn, "rs0_%d" % pb2], ["xn2_%d" % pb2, "rs0_%d" % pb2], accum=rs[:, 0:1])
        PX.act(rs[:, 1:2], rs[:, 0:1], AF.Sqrt, ["rs0_%d" % pb2], ["rs1_%d" % pb2], bias=epsc[:, 0:1], scale=1.0 / D)
        PX.op("dve", lambda e, rs=rs: e.reciprocal(rs[:, 2:3], rs[:, 1:2]), ["rs1_%d" % pb2], ["rs2_%d" % pb2])
        PX.stt(acc2[:], xb, rs[:, 2:3], a2b[:], ALU.mult, ALU.mult, [xbn, "rs2_%d" % pb2], ["acc2_%d" % pb2])
        PX.tt(h2tm[:, tix, :], acc2[:], s2b[:], ALU.add, ["acc2_%d" % pb2], ["h2tm%d" % tix])
        pt_, ptn = nxt_t()
        for fc in range(8):
            PX.tr(pt_[:, fc * 128:(fc + 1) * 128], h2tm[:, tix, fc * 128:(fc + 1) * 128], ident_bf[:], ["h2tm%d" % tix], [ptn])
        PX.act(h2Tt[:].rearrange("p a b -> p (a b)"), pt_[:, :], AF.Copy, [ptn], ["h2Tt_%d" % pb2])
        pl, pln = nxt()
        PX.mm(pl[:, 0:NE], [(h2Tt[:, kc, :], rwb_bf[:, kc, :]) for kc in range(8)], ["h2Tt_%d" % pb2, "rwb"], [pln])
        lg, ex = rt[:, 0, :], rt[:, 2, :]
        PX.tt(lg, pl[:, 0:NE], rbb[:], ALU.add, [pln, "rbb"], ["lg_%d" % pb2])
        PX.op("dve", lambda e, lg=lg, rs=rs: e.max(rs[:, 4:12], lg), ["lg_%d" % pb2], ["rs4_%d" % pb2])
        PX.op("dve", lambda e, lg=lg, tix=tix, rs=rs: e.tensor_single_scalar(maskf[:, tix, :], lg, rs[:, 7:8], ALU.is_ge), ["lg_%d" % pb2, "rs4_%d" % pb2], ["maskf%d" % tix])
        PX.ts(rs[:, 12:13], rs[:, 4:5], -1.0, ALU.mult, ["rs4_%d" % pb2], ["rs12_%d" % pb2])
        PX.act(ex, lg, AF.Exp, ["lg_%d" % pb2, "rs12_%d" % pb2], ["ex_%d" % pb2], bias=rs[:, 12:13], scale=1.0)
        PX.tt(ex, ex, maskf[:, tix, :], ALU.mult, ["ex_%d" % pb2, "maskf%d" % tix], ["ex_%d" % pb2])
        PX.op("dve", lambda e, ex=ex, rs=rs: e.reduce_sum(rs[:, 13:14], ex, mybir.AxisListType.X), ["ex_%d" % pb2], ["rs13_%d" % pb2])
        PX.op("dve", lambda e, rs=rs: e.reciprocal(rs[:, 14:15], rs[:, 13:14]), ["rs13_%d" % pb2], ["rs14_%d" % pb2])
        PX.ts(rw[:, tix, :], ex, rs[:, 14:15], ALU.mult, ["ex_%d" % pb2, "rs14_%d" % pb2], ["rw%d" % tix])
        PX.cp(maskb[:, tix, :], maskf[:, tix, :], ["maskf%d" % tix], ["maskb%d" % tix])
        pr_, prn = nxt()
        PX.op("pe", lambda e, pr_=pr_, tix=tix: e.transpose(pr_[0:NE, 0:128], rw[:, tix, :], ident_f[:]), ["rw%d" % tix], [prn])
        PX.cp(rwT[:], pr_[0:NE, 0:128], [prn], ["rwT_%d" % pb2])
        for dh in range(2):
            pb_, pbn = nxt()
            PX.mm(pb_[:, :], [(rwT[:], bo_g[:, dh * 512:(dh + 1) * 512])], ["rwT_%d" % pb2, "bo_g"], [pbn])
            PX.tt(x1[:, tix, dh * 512:(dh + 1) * 512], pb_[:, :], x1[:, tix, dh * 512:(dh + 1) * 512], ALU.add, [pbn, xbn], [xbn])
        PX.cp(rwhl[:, tix, :, 0], rw[:, tix, :], ["rw%d" % tix], ["rwhl%d" % tix])
        PX.cp(rt[:, 1, :], rwhl[:, tix, :, 0], ["rwhl%d" % tix], ["rt1_%d" % pb2])
        PX.tt(rt[:, 1, :], rw[:, tix, :], rt[:, 1, :], ALU.subtract, ["rw%d" % tix, "rt1_%d" % pb2], ["rt1_%d" % pb2])
        PX.cp(rwhl[:, tix, :, 1], rt[:, 1, :], ["rt1_%d" % pb2], ["rwhl%d" % tix])

    rt = rtp

    MB = ["maskb%d" % t for t in range(NT)]
    for tix in range(NT):
        pp_, ppn = nxt()
        prs = [(ones_bf[:], maskb[:, t2, :]) for t2 in range(tix)] + [(stri_bf[:], maskb[:, tix, :])]
        PX.mm(pp_[:, 0:NE], prs, MB[:tix + 1] + ["ones_bf", "stri_bf"], [ppn])
        PX.ts(rt[:, 1, :], maskf[:, tix, :], -1.0, ALU.add, ["maskf%d" % tix], ["rt1"], s2=1.0e6, op1=ALU.mult)
        PX.tt(rt[:, 3, :], pp_[:, 0:NE], maskf[:, tix, :], ALU.mult, [ppn, "maskf%d" % tix], ["rt3"])
        PX.tt(posm[:, tix, :], rt[:, 3, :], rt[:, 1, :], ALU.add, ["rt3", "rt1"], ["posm"])
    pc_, pcn_ = nxt()
    PX.mm(pc_[0:1, 0:NE], [(ones_bf[:, 0:1], maskb[:, t2, :]) for t2 in range(NT)], MB + ["ones_bf"], [pcn_])
    PX.cp(cnt[:, 0, :], pc_[0:1, 0:NE], [pcn_], ["cnt0"])
    PX.memset(cnt[:, 1, :], 0.0, ["cnt1"])
    for u in range(NU):
        PX.stt(cnt[:, 1, :], cnt[:, 0, :], float(UW * u), cnt[:, 1, :], ALU.is_gt, ALU.add, ["cnt0", "cnt1"], ["cnt1"])
    PX.cp(ne_i[:], cnt[:, 1, :], ["cnt1"], ["ne_i"])
    PX.barrier()
    if stage == "XP":
        dbg_out(PX, "posm", posm[:], [128, NT, NE], F32)
        dbg_out(PX, "rw", rw[:], [128, NT, NE], F32)
        dbg_out(PX, "cnt", cnt[:], [1, 3, NE], F32)
        dbg_out(PX, "x1b", x1[:], [128, NT, D], F32)
        PX.barrier()
        PX.emit()
        return nc
    PX.emit()
    nc.all_engine_barrier()
    p2r.close()

    PX = Prog(nc, top, "E")
    PX.regs = regs
    p2e = ExitStack()
    NWP = 5
    wq = [sbuf(p2e, "wq%d" % i, [128, 8, 512], BF16) for i in range(NWP)]
    wo = sbuf(p2e, "wo", [128, 8, D], BF16)
    SelAll = sbuf(p2e, "SelAll", [128, NT, UW], BF16)
    SelT = sbuf(p2e, "SelT", [128, 2, T], BF16)
    pmu = sbuf(p2e, "pmu", [128, NT], F32)
    h2g = sbuf(p2e, "h2g", [128, 8, UW], BF16)
    actT = sbuf(p2e, "actT", [128, 8, UW], BF16)
    g_sb = sbuf(p2e, "g_sb", [128, 2, UW], BF16)
    sg2 = sbuf(p2e, "sg2", [128, 2, UW], BF16)
    u_sb = sbuf(p2e, "u_sb", [128, 2, UW], BF16)
    pp = sbuf(p2e, "pp", [128, 2, UW], BF16)
    yw = sbuf(p2e, "yw", [128, 2, D], BF16)
    rws = sbuf(p2e, "rws", [128, 2], F32)
    rws4 = sbuf(p2e, "rws4", [128, 4], F32)

    NEX = NE

    def piece_slot(e, q):
        return (4 * e + q) % NWP

    def load_piece(e, q):
        sl = piece_slot(e, q)
        src = mwi_d[e].rearrange("(c p) n -> p c n", p=128)
        PX.dma("pool", wq[sl][:, :, 0:256], src[:, :, 256 * q:256 * (q + 1)], writes=["wg%d" % sl], slot="wg%d" % sl)
        PX.dma("pool", wq[sl][:, :, 256:512], src[:, :, D + 256 * q:D + 256 * (q + 1)], writes=["wu%d" % sl], slot="wu%d" % sl)

    def load_wo(e):
        PX.dma("pool", wo[:], mwo_d[e].rearrange("(c p) n -> p c n", p=128), writes=["wo"], slot="wo")

    for q in range(4):
        load_piece(0, q)
    H2 = ["h2tm%d" % t for t in range(NT)]
    X1 = ["x1_%d" % t for t in range(NT)]
    for e in range(NEX):
        if e + 1 < NE:
            load_piece(e + 1, 0)
        load_wo(e)
        PX.regload(regs, ne_i[0:1, e:e + 1], ["ne_i"])
        for u in range(NU):
            PX.marker(("rb", u + 1))
            PX.ts(pmu[:], posm[:, :, e], float(-UW * u), ALU.add, ["posm"], ["pmu"])
            for t2 in range(NT):
                PX.op("dve", lambda eng, t2=t2: eng.tensor_single_scalar(SelAll[:, t2, :], iota[:], pmu[:, t2:t2 + 1], ALU.is_equal),
                      ["pmu", "iota"], ["Sel%d" % t2])
            SEL = ["Sel%d" % t2 for t2 in range(NT)]
            for fp in range(4):
                ps_, psn = nxt()
                for a in range(2):
                    fc = 2 * fp + a
                    PX.mm(ps_[:, a * UW:(a + 1) * UW], [(h2tm[:, t2, fc * 128:(fc + 1) * 128], SelAll[:, t2, :]) for t2 in range(NT)],
                          H2 + SEL, [psn])
                PX.act(h2g[:, 2 * fp:2 * fp + 2, :], ps_[:, :].rearrange("p (a n) -> p a n", a=2), AF.Copy, [psn], ["h2g"])
            for st_ in range(2):
                for h8 in range(2):
                    pt_, ptn = nxt_t()
                    for i8 in range(8):
                        t2 = 8 * h8 + i8
                        PX.tr(pt_[:, i8 * 128:(i8 + 1) * 128], SelAll[:, t2, st_ * 128:(st_ + 1) * 128], ident_bf[:], ["Sel%d" % t2], [ptn])
                    PX.act(SelT[:, st_, h8 * 1024:(h8 + 1) * 1024], pt_[:, :], AF.Copy, [ptn], ["SelT"])
            prw, prwn = nxt()
            for st_ in range(2):
                PX.mm(prw[:, 2 * st_:2 * st_ + 2], [(SelAll[:, t2, st_ * 128:(st_ + 1) * 128], rwhl[:, t2, e, :]) for t2 in range(NT)],
                      SEL + ["rwhl"], [prwn])
            PX.act(rws4[:], prw[:, 0:4], AF.Copy, [prwn], ["rws4"])
            prv = rws4[:].rearrange("p (s h) -> p s h", s=2)
            PX.tt(rws[:], prv[:, :, 0], prv[:, :, 1], ALU.add, ["rws4"], ["rws"])
            for jp in range(4):
                sl = piece_slot(e, jp)
                pg_, pgn = nxt()
                pu_, pun = nxt()
                for a in range(2):
                    PX.mm(pg_[:, a * UW:(a + 1) * UW], [(wq[sl][:, kc, a * 128:(a + 1) * 128], h2g[:, kc, :]) for kc in range(8)], ["wg%d" % sl, "h2g"], [pgn])
                    PX.mm(pu_[:, a * UW:(a + 1) * UW], [(wq[sl][:, kc, 256 + a * 128:256 + (a + 1) * 128], h2g[:, kc, :]) for kc in range(8)], ["wu%d" % sl, "h2g"], [pun])
                for a in range(2):
                    j = 2 * jp + a
                    PX.ts(g_sb[:, a, :], pg_[:, a * UW:(a + 1) * UW], bgc[:, e, j:j + 1], ALU.add, [pgn, "bgc"], ["g_sb"], s2=7.0, op1=ALU.min)
                    PX.ts(u_sb[:, a, :], pu_[:, a * UW:(a + 1) * UW], bgc[:, e, 8 + j:9 + j], ALU.add, [pun, "bgc"], ["u_sb"], s2=8.0, op1=ALU.min)
                PX.act(sg2[:], g_sb[:], AF.Sigmoid, ["g_sb"], ["sg2"], scale=1.702)
                PX.tt(pp[:], g_sb[:], sg2[:], ALU.mult, ["g_sb", "sg2"], ["pp"])
                PX.stt(actT[:, 2 * jp:2 * jp + 2, :], u_sb[:], -6.0, pp[:], ALU.max, ALU.mult, ["u_sb", "pp"], ["actT%d" % jp])
            for st_ in range(2):
                for dh in range(2):
                    po, pon = nxt()
                    PX.mm(po[:, :], [(actT[:, jc, st_ * 128:(st_ + 1) * 128], wo[:, jc, dh * 512:(dh + 1) * 512]) for jc in range(8)],
                          ["actT%d" % jp for jp in range(4)] + ["wo"], [pon])
                    PX.stt(yw[:, st_, dh * 512:(dh + 1) * 512], po[:, :], rws[:, st_:st_ + 1], gate2b[:, dh * 512:(dh + 1) * 512],
                           ALU.mult, ALU.mult, [pon, "rws"], ["yw"])
            for t2 in range(NT):
                for dh in range(2):
                    po, pon = nxt()
                    PX.mm(po[:, :], [(SelT[:, st_, t2 * 128:(t2 + 1) * 128], yw[:, st_, dh * 512:(dh + 1) * 512]) for st_ in range(2)],
                          ["SelT", "yw"], [pon])
                    PX.tt(x1[:, t2, dh * 512:(dh + 1) * 512], po[:, :], x1[:, t2, dh * 512:(dh + 1) * 512], ALU.add, [pon, X1[t2]], [X1[t2]])
        for u in range(NU):
            PX.marker(("re",))
        if e + 1 < NE:
            for q in range(1, 4):
                load_piece(e + 1, q)
    PX.barrier()
    if stage == "XE":
        dbg_out(PX, "x2", x1[:], [128, NT, D], F32)
        PX.barrier()
        PX.emit()
        return nc
    PX.emit()
    nc.all_engine_barrier()
    p2e.close()

    PX = Prog(nc, top, "F")
    fgb = sbuf(p2, "fgb", [128, D], F32)
    xn2 = sbuf(p2, "xn2f", [128, D], BF16)
    rs = sbuf(p2, "rsf", [128, 8], F32)
    PX.dma("sp", fgb[:], fg_d.partition_broadcast(128), writes=["fgb"], slot="ld", group=True)
    for tix in range(NT):
        r_ = "x1_%d" % tix
        PX.memset(rs[:, 0:1], 0.0, ["rs0"])
        PX.act(xn2[:], x1[:, tix, :], AF.Square, [r_, "rs0"], ["xn2", "rs0"], accum=rs[:, 0:1])
        PX.act(rs[:, 1:2], rs[:, 0:1], AF.Sqrt, ["rs0"], ["rs1"], bias=epsc[:, 0:1], scale=1.0 / D)
        PX.op("dve", lambda e: e.reciprocal(rs[:, 2:3], rs[:, 1:2]), ["rs1"], ["rs2"])
        PX.stt(x1[:, tix, :], x1[:, tix, :], rs[:, 2:3], fgb[:], ALU.mult, ALU.mult, [r_, "rs2", "fgb"], [r_])
        PX.dma("sp", out_d[tix * 128:(tix + 1) * 128, :], x1[:, tix, :], reads=[r_], writes=["out%d" % tix], slot="outs", group=True)
    PX.barrier()
    PX.emit()
    p2.close()
    top.close()
    return nc


def _prep(inputs):
    f = lambda a: np.ascontiguousarray(np.asarray(a), dtype=np.float32)
    sh = {}
    sh["ada_w"] = f(inputs["ada_w"][0])
    sh["ada_b"] = f(inputs["ada_b"][0][None])
    sh["g1"] = f(inputs["norm1_g"][0][None])
    sh["g2"] = f(inputs["norm2_g"][0][None])
    sh["fg"] = f(np.asarray(inputs["final_g"])[None])
    sh["w_in"] = f(inputs["w_in"][0])
    a_re = np.asarray(inputs["ssm_a_re"][0])
    a_im = np.asarray(inputs["ssm_a_im"][0])
    ldt = np.broadcast_to(np.asarray(inputs["ssm_log_dt"][0])[:, None], (32, 64))
    smf = lambda v: np.asarray(v).reshape(16, 2, 64).transpose(1, 2, 0).reshape(128, 16)
    sh["ssm_sm"] = f(np.stack([smf(a_re), smf(a_im), smf(ldt)], 1))
    sh["ssm_lam"] = f(np.stack([a_re.reshape(-1), a_im.reshape(-1), np.ascontiguousarray(ldt).reshape(-1)]))
    b = [np.asarray(inputs["ssm_b_re"][0]), np.asarray(inputs["ssm_b_im"][0])]
    c = [np.asarray(inputs["ssm_c_re"][0]), np.asarray(inputs["ssm_c_im"][0])]
    Bz = np.zeros((2, 128, 16, 128), np.float32)
    Cz = np.zeros((2, 128, 16, 128), np.float32)
    for k in range(16):
        for g2 in range(2):
            g = 2 * k + g2
            g8 = g % 8
            for ri in range(2):
                Bz[ri, g8 * 16:(g8 + 1) * 16, k, g2 * 64:(g2 + 1) * 64] = b[ri][g].T
                Cz[ri, g2 * 64:(g2 + 1) * 64, k, g8 * 16:(g8 + 1) * 16] = c[ri][g].T
    sh["ssm_B"] = Bz.reshape(2, 128, 2048)
    sh["ssm_C"] = Cz.reshape(2, 128, 2048)
    sh["ssm_d"] = f(np.asarray(inputs["ssm_d"][0]).reshape(4, 128).T)
    sh["glu_w"] = f(inputs["ssm_glu_w"][0])
    sh["glu_b"] = f(np.asarray(inputs["ssm_glu_b"][0]).reshape(4, 128).T)
    sh["wa"] = f(inputs["w_branch_a"][0])
    sh["wb"] = f(inputs["w_branch_b"][0])
    sh["wout"] = f(inputs["w_out"][0])
    sh["lng"] = f(inputs["gmlp_ln_g"][0][None])
    sh["lnb"] = f(inputs["gmlp_ln_b"][0][None])
    sh["wsT"] = f(np.asarray(inputs["gmlp_ws"][0]).transpose(2, 0, 1))
    sh["bs"] = f(np.asarray(inputs["gmlp_bs"][0]).reshape(1, 1024))
    sh["router_w"] = f(inputs["router_w"][0])
    sh["router_b"] = f(inputs["router_b"][0][None])
    sh["moe_w_in"] = f(inputs["moe_w_in"][0])
    sh["moe_b_in"] = f(np.asarray(inputs["moe_b_in"][0]).reshape(32, 16, 128).transpose(2, 0, 1))
    sh["moe_w_out"] = f(inputs["moe_w_out"][0])
    sh["moe_b_out"] = f(inputs["moe_b_out"][0])
    sh["ident"] = np.eye(128, dtype=np.float32)
    sh["tri"] = np.triu(np.ones((128, 128), np.float32))
    sh["stri"] = np.triu(np.ones((128, 128), np.float32), 1)
    sh["iota256"] = np.ascontiguousarray(np.broadcast_to(np.arange(256, dtype=np.float32)[None], (128, 256)))
    sh["eoff"] = np.ascontiguousarray(np.broadcast_to((2048.0 * np.arange(32, dtype=np.float32) + 1.0)[None], (128, 32)))
    sh["jj"] = np.ascontiguousarray(np.broadcast_to(np.arange(1, ST + 1, dtype=np.float32)[None], (128, ST)))
    return sh


def kernel(**inputs):
    sh = _prep(inputs)
    x = np.asarray(inputs["x"], dtype=np.float32)
    c = np.asarray(inputs["c"], dtype=np.float32)
    in_maps = []
    for b in range(8):
        m = dict(sh)
        m["x"] = np.ascontiguousarray(x[b])
        m["cT"] = np.ascontiguousarray(c[b].reshape(8, 128).T)
        in_maps.append(m)
    nc = build()
    res = run_bass_kernel_spmd(nc, in_maps, core_ids=list(range(8)))
    return np.stack([np.asarray(r["out"], dtype=np.float32) for r in res.results], 0)
```

```python
import numpy as np
from contextlib import ExitStack
import concourse.bass as bass
import concourse.mybir as mybir
from concourse.bass_utils import run_bass_kernel_spmd

F32 = mybir.dt.float32
BF16 = mybir.dt.bfloat16
I32 = mybir.dt.int32
AF = mybir.ActivationFunctionType
ALU = mybir.AluOpType

T = 2048
D = 1024
NT = 16
ST = 256
NST = T // ST
NE = 32
EPS = 1e-6
PI = float(np.pi)
TWO_PI = float(2 * np.pi)


class Op:
    __slots__ = ("eng", "emit", "deps", "is_dma", "slot", "val", "signal", "mark")


class Prog:
    ENGS = ("pe", "act", "dve", "pool", "sp")

    def __init__(self, nc, es, tag):
        self.nc = nc
        self.es = es
        self.tag = tag
        self.q = {e: [] for e in self.ENGS}
        self.lastw = {}
        self.readers = {}
        self.slot_ops = {}
        self.group_slots = set()

    def _deps(self, reads, writes, op):
        deps = []
        for r in reads:
            w = self.lastw.get(r)
            if w is not None:
                deps.append(w)
        for r in writes:
            w = self.lastw.get(r)
            if w is not None:
                deps.append(w)
            deps.extend(self.readers.get(r, ()))
        for r in reads:
            self.readers.setdefault(r, []).append(op)
        for r in writes:
            self.lastw[r] = op
            self.readers[r] = []
        return [d for d in deps if d is not op]

    def op(self, eng, emit, reads=(), writes=()):
        o = Op()
        o.eng, o.emit, o.is_dma, o.slot, o.signal = eng, emit, False, None, False
        o.mark = None
        o.deps = self._deps(reads, writes, o)
        self.q[eng].append(o)
        return o

    def marker(self, mark):
        for e in self.ENGS:
            o = Op()
            o.eng, o.emit, o.is_dma, o.slot, o.signal, o.mark, o.deps = e, None, False, None, False, mark, []
            self.q[e].append(o)

    def regload(self, regs, ap, reads):
        for e in self.ENGS:
            self.op(e, (lambda eng, e=e: eng.reg_load(regs[e], ap)), reads, ())

    def dma(self, eng, out, in_, reads=(), writes=(), slot=None, group=False, custom=None):
        o = Op()
        o.eng, o.is_dma, o.slot, o.signal = eng, True, slot, True
        o.mark = None
        o.emit = (lambda e: e.dma_start(out=out, in_=in_)) if custom is None else custom
        o.deps = self._deps(reads, writes, o)
        if group:
            o.deps = [d for d in o.deps if not (d.is_dma and d.slot == slot)]
            self.group_slots.add(slot)
        self.q[eng].append(o)
        self.slot_ops.setdefault(slot, []).append(o)
        return o

    def barrier(self):
        pend = []
        for e in self.ENGS:
            for o in reversed(self.q[e]):
                if not o.is_dma and o.emit is not None:
                    pend.append(o)
                    break
        for s, ops in self.slot_ops.items():
            pend.append(ops[-1])
        for e in self.ENGS:
            o = Op()
            o.eng, o.emit, o.is_dma, o.slot, o.signal = e, None, False, None, False
            o.mark = None
            o.deps = list(pend)
            self.q[e].append(o)
        self.lastw = {}
        self.readers = {}

    def emit(self):
        nc = self.nc
        for e in self.ENGS:
            for o in self.q[e]:
                for d in o.deps:
                    d.signal = True
        esem = {}
        for e in self.ENGS:
            esem[e] = self.es.enter_context(nc.semaphore("s%s_%s" % (self.tag, e)))
            c = 0
            for o in self.q[e]:
                if o.is_dma or o.emit is None:
                    continue
                if o.signal:
                    c += 1
                    o.val = c
        ssem = {}
        for i, (s, ops) in enumerate(self.slot_ops.items()):
            ssem[s] = self.es.enter_context(nc.semaphore("d%s_%d" % (self.tag, i)))
            if s in self.group_slots:
                for o in ops:
                    o.val = 16 * len(ops)
            else:
                for j, o in enumerate(ops):
                    o.val = 16 * (j + 1)

        regs = getattr(self, "regs", None)

        def emit_q(e, eng):
            seen = {}

            def emit_one(o):
                need = {}
                for d in o.deps:
                    if d.is_dma:
                        sem = ssem[d.slot]
                    else:
                        if d.emit is None or (d.eng == "pe" and e == "pe"):
                            continue
                        sem = esem[d.eng]
                    k = id(sem)
                    if need.get(k, (None, 0))[1] < d.val:
                        need[k] = (sem, d.val)
                for k, (sem, v) in need.items():
                    if seen.get(k, 0) < v:
                        eng.wait_ge(sem, v)
                        seen[k] = v
                if o.emit is None:
                    return
                ins = o.emit(eng)
                if o.is_dma:
                    ins.then_inc(ssem[o.slot], 16)
                elif o.signal:
                    ins.then_inc(esem[e], 1)
                    cur[0] = o.val

            cur = [0]

            def emit_range(ops):
                i = 0
                while i < len(ops):
                    o = ops[i]
                    if o.mark is not None and o.mark[0] == "rb":
                        depth = 1
                        j = i + 1
                        while True:
                            m = ops[j].mark
                            if m is not None and m[0] == "rb":
                                depth += 1
                            elif m is not None and m[0] == "re":
                                depth -= 1
                                if depth == 0:
                                    break
                            j += 1
                        body = ops[i + 1:j]
                        real = [b for b in body if b.emit is not None]
                        if real:
                            c = sum(1 for b in real if (not b.is_dma) and b.signal)
                            dcnt = {}
                            for b in real:
                                if b.is_dma:
                                    dcnt[b.slot] = dcnt.get(b.slot, 0) + 1
                            saved = dict(seen)
                            cur_before = cur[0]
                            g = eng.If_lt(regs[e], o.mark[1])
                            g.__enter__()
                            if c > 0:
                                if cur_before > 0:
                                    eng.wait_ge(esem[e], cur_before)
                                eng.sem_inc(esem[e], c)
                            for sl, n in dcnt.items():
                                eng.sem_inc(ssem[sl], 16 * n)
                            g.__exit__(None, None, None)
                            g2 = eng.Else()
                            g2.__enter__()
                            emit_range(body)
                            g2.__exit__(None, None, None)
                            seen.clear()
                            seen.update(saved)
                            cur[0] = cur_before + c
                        i = j + 1
                        continue
                    if o.mark is None:
                        emit_one(o)
                    i += 1

            emit_range(self.q[e])

        with nc.Block() as block:

            @block.tensor
            def _(eng):
                emit_q("pe", eng)

            @block.scalar
            def _(eng):
                emit_q("act", eng)

            @block.vector
            def _(eng):
                emit_q("dve", eng)

            @block.gpsimd
            def _(eng):
                emit_q("pool", eng)

            @block.sync
            def _(eng):
                emit_q("sp", eng)

    def mm(self, out, pairs, r, w, start=True, stop=True):
        pairs = list(pairs)

        def emit(pe):
            n = len(pairs)
            ins = None
            for i, (l, rr) in enumerate(pairs):
                ins = pe.matmul(out, l, rr, start=(start and i == 0), stop=(stop and i == n - 1))
            return ins

        return self.op("pe", emit, r, w)

    def tr(self, out, in_, ident, r, w):
        return self.op("pe", lambda e: e.transpose(out, in_, ident), r, w)

    def tt(self, out, a, b, op, r, w, eng="dve"):
        return self.op(eng, lambda e: e.tensor_tensor(out, a, b, op), r, w)

    def ts(self, out, a, s1, op0, r, w, s2=None, op1=None, eng="dve"):
        if op1 is None:
            return self.op(eng, lambda e: e.tensor_scalar(out, a, s1, None, op0), r, w)
        return self.op(eng, lambda e: e.tensor_scalar(out, a, s1, s2, op0, op1), r, w)

    def stt(self, out, a, s, b, op0, op1, r, w, eng="dve"):
        return self.op(eng, lambda e: e.scalar_tensor_tensor(out, a, s, b, op0, op1), r, w)

    def cp(self, out, a, r, w, eng="dve"):
        return self.op(eng, lambda e: e.tensor_copy(out, a), r, w)

    def memset(self, out, v, w, eng="dve"):
        return self.op(eng, lambda e: e.memset(out, v), (), w)

    def act(self, out, a, func, r, w, bias=None, scale=None, accum=None):
        kw = {}
        if bias is not None:
            kw["bias"] = bias
        if scale is not None:
            kw["scale"] = scale
        if accum is not None:
            kw["accum_out"] = accum
        return self.op("act", lambda e: e.activation(out, a, func, **kw), r, w)

    def scan(self, out, d0, d1, init, r, w):
        return self.op("dve", lambda e: e.tensor_tensor_scan(out, d0, d1, init, ALU.mult, ALU.add), r, w)


def build(stage=None):
    nc = bass.Bass("TRN2", target_bir_lowering=False)
    top = ExitStack()

    def din(name, shape):
        return nc.dram_tensor(name, list(shape), F32, kind="ExternalInput").ap()

    x_d = din("x", [T, D])
    cT_d = din("cT", [128, 8])
    ada_w_d = din("ada_w", [D, 6 * D])
    ada_b_d = din("ada_b", [1, 6 * D])
    g1_d = din("g1", [1, D])
    g2_d = din("g2", [1, D])
    fg_d = din("fg", [1, D])
    w_in_d = din("w_in", [D, 3584])
    ssm_sm_d = din("ssm_sm", [128, 3, 16])
    ssm_lam_d = din("ssm_lam", [3, 2048])
    ssm_B_d = din("ssm_B", [2, 128, 2048])
    ssm_C_d = din("ssm_C", [2, 128, 2048])
    ssm_d_d = din("ssm_d", [128, 4])
    glu_w_d = din("glu_w", [512, 512])
    glu_b_d = din("glu_b", [128, 4])
    wa_d = din("wa", [512, D])
    wb_d = din("wb", [512, D])
    wout_d = din("wout", [D, D])
    lng_d = din("lng", [1, 512])
    lnb_d = din("lnb", [1, 512])
    wsT_d = din("wsT", [128, 8, 128])
    bs_d = din("bs", [1, 1024])
    rw_d = din("router_w", [D, NE])
    rb_d = din("router_b", [1, NE])
    mwi_d = din("moe_w_in", [NE, D, 2 * D])
    mbi_d = din("moe_b_in", [128, NE, 16])
    mwo_d = din("moe_w_out", [NE, D, D])
    mbo_d = din("moe_b_out", [NE, D])
    ident_d = din("ident", [128, 128])
    tri_d = din("tri", [128, 128])
    jj_d = din("jj", [128, ST])
    stri_d = din("stri", [128, 128])
    iota_d = din("iota256", [128, 256])
    eoff_d = din("eoff", [128, NE])
    out_d = nc.dram_tensor("out", [T, D], F32, kind="ExternalOutput").ap()
    x1s_d = nc.dram_tensor("x1s", [T, D], F32, kind=("ExternalOutput" if stage == "M" else "Internal")).ap()

    def dbg_out(Pg, name, ap, shape, dt=F32):
        o = nc.dram_tensor("dbg_" + name, list(shape), dt, kind="ExternalOutput").ap()
        Pg.dma("sp", o, ap, reads=[], writes=["dbg_" + name], slot="dbg_" + name)

    def sbuf(es, name, shape, dt=F32):
        return es.enter_context(nc.sbuf_tensor("sb_" + name, list(shape), dt))

    pst = [top.enter_context(nc.psum_tensor("pst%d" % i, [128, 1024], BF16)) for i in range(2)]
    psf = [top.enter_context(nc.psum_tensor("psf%d" % i, [128, 512], F32)) for i in range(6)]
    rot = {"f": 0, "t": 0, "n": 6}

    def nxt():
        i = rot["f"] % rot["n"]
        rot["f"] += 1
        return psf[i], "psf%d" % i

    def nxt_t():
        i = rot["t"] % 2
        rot["t"] += 1
        return pst[i], "pst%d" % i

    ident_bf = sbuf(top, "ident_bf", [128, 128], BF16)
    ident_f = sbuf(top, "ident_f", [128, 128], F32)
    ones_row = sbuf(top, "ones_row", [1, 128], F32)
    epsc = sbuf(top, "epsc", [128, 1], F32)
    cols = sbuf(top, "cols", [128, 32], F32)
    gate2b = sbuf(top, "gate2b", [128, D], F32)
    modscr_d = nc.dram_tensor("modscr", [2, D], F32, kind="Internal").ap()
    regs = {"pe": top.enter_context(nc.tensor.register("r_pe")), "act": top.enter_context(nc.scalar.register("r_act")),
            "dve": top.enter_context(nc.vector.register("r_dve")), "pool": top.enter_context(nc.gpsimd.register("r_pool")),
            "sp": top.enter_context(nc.sync.register("r_sp"))}
    a1c, s1c, a2c, s2c = (cols[:, 0:8], cols[:, 8:16], cols[:, 16:24], cols[:, 24:32])

    p1 = ExitStack()
    gate1b = sbuf(p1, "gate1b", [128, D], F32)
    BT_bf = sbuf(p1, "BT_bf", [128, 16, 2, 128], BF16)
    CT_bf = sbuf(p1, "CT_bf", [128, 16, 2, 128], BF16)
    cosT = sbuf(p1, "cosT", [128, 16, ST], BF16)
    sinT = sbuf(p1, "sinT", [128, 16, ST], BF16)
    rcol = sbuf(p1, "rcol", [128, 16], F32)
    carry = sbuf(p1, "carry", [128, 16, 2], F32)
    dcol = sbuf(p1, "dcol", [128, 4], F32)
    glubc = sbuf(p1, "glubc", [128, 4], F32)

    sA = ExitStack()
    PA = Prog(nc, top, "A")
    L3 = sbuf(sA, "L3", [128, 3, 2048], F32)
    Bz = sbuf(sA, "Bz", [128, 2, 2048], F32)
    tA = sbuf(sA, "tA", [128, 4096], F32)
    tB = sbuf(sA, "tB", [128, 4096], F32)
    tC = sbuf(sA, "tC", [128, 4096], F32)
    tI = sbuf(sA, "tI", [128, 4096], I32)
    sm = sbuf(sA, "sm", [128, 3, 16], F32)
    smw = sbuf(sA, "smw", [128, 2, 16], F32)
    jj = sbuf(sA, "jj", [128, ST], F32)
    idl = sbuf(sA, "idl", [128, 128], F32)
    hpic = sbuf(sA, "hpic", [128, 1], F32)

    PA.dma("sp", idl[:], ident_d, writes=["idl"], slot="ld", group=True)
    PA.dma("sp", ident_f[:], ident_d, writes=["ident_f"], slot="ld", group=True)
    for i in range(3):
        PA.dma("sp", L3[:, i, :], ssm_lam_d[i:i + 1, :].partition_broadcast(128), writes=["L3"], slot="ld", group=True)
    for i in range(2):
        PA.dma("sp", Bz[:, i, :], ssm_B_d[i], writes=["Bz"], slot="ld", group=True)
    PA.dma("sp", sm[:], ssm_sm_d, writes=["sm"], slot="ld", group=True)
    PA.dma("sp", jj[:], jj_d, writes=["jj"], slot="ld", group=True)
    PA.dma("sp", dcol[:], ssm_d_d, writes=["dcol"], slot="ld", group=True)
    PA.dma("sp", glubc[:], glu_b_d, writes=["glubc"], slot="ld", group=True)
    PA.cp(ident_bf[:], idl[:], ["idl"], ["ident_bf"])
    PA.memset(ones_row[:], 1.0, ["ones_row"])
    PA.memset(epsc[:], EPS, ["epsc"])
    PA.memset(hpic[:], PI / 2, ["hpic"])
    PA.memset(carry[:], 0.0, ["carry"])

    def range_reduce(Pg, y, x, shift, ntmp, itmp, rx, ry, rn, ri):
        Pg.ts(ntmp, x, 1.0 / TWO_PI, ALU.mult, [rx], [rn], s2=shift / TWO_PI, op1=ALU.add)
        Pg.cp(itmp, ntmp, [rn], [ri])
        Pg.cp(ntmp, itmp, [ri], [rn])
        Pg.ts(y, x, shift, ALU.add, [rx], [ry])
        Pg.stt(y, ntmp, -TWO_PI, y, ALU.mult, ALU.add, [rn, ry], [ry])
        Pg.op("dve", lambda e: e.tensor_single_scalar(ntmp, y, PI, ALU.is_gt), [ry], [rn])
        Pg.stt(y, ntmp, -TWO_PI, y, ALU.mult, ALU.add, [rn, ry], [ry])
        Pg.op("dve", lambda e: e.tensor_single_scalar(ntmp, y, -PI, ALU.is_lt), [ry], [rn])
        Pg.stt(y, ntmp, TWO_PI, y, ALU.mult, ALU.add, [rn, ry], [ry])

    A0, A1 = tA[:, 0:2048], tA[:, 2048:4096]
    B0, B1 = tB[:, 0:2048], tB[:, 2048:4096]
    C0, C1 = tC[:, 0:2048], tC[:, 2048:4096]
    I0 = tI[:, 0:2048]
    are, aim, ldt = L3[:, 0, :], L3[:, 1, :], L3[:, 2, :]
    PA.act(A0, ldt, AF.Exp, ["L3"], ["A0"])
    PA.tt(A1, aim, A0, ALU.mult, ["L3", "A0"], ["A1"])
    PA.tt(B0, are, A0, ALU.mult, ["L3", "A0"], ["B0"])
    PA.act(B0, B0, AF.Exp, ["B0"], ["B0"])
    range_reduce(PA, C0, A1, 0.0, C1, I0, "A1", "C0", "C1", "I0")
    PA.act(B1, C0, AF.Sin, ["C0"], ["B1"])
    PA.act(C1, C0, AF.Abs, ["C0"], ["C1"])
    PA.act(A0, C1, AF.Sin, ["C1", "A0"], ["A0"], bias=hpic[:, 0:1], scale=-1.0)
    PA.tt(A0, B0, A0, ALU.mult, ["B0", "A0"], ["A0"])
    PA.tt(B1, B0, B1, ALU.mult, ["B0", "B1"], ["B1"])
    PA.ts(A0, A0, -1.0, ALU.add, ["A0"], ["A0"])
    PA.tt(C0, are, are, ALU.mult, ["L3"], ["C0"])
    PA.tt(C1, aim, aim, ALU.mult, ["L3"], ["C1"])
    PA.tt(C0, C0, C1, ALU.add, ["C0", "C1"], ["C0"])
    PA.op("dve", lambda e: e.reciprocal(C0, C0), ["C0"], ["C0"])
    PA.tt(C1, A0, are, ALU.mult, ["A0", "L3"], ["C1"])
    PA.tt(B0, B1, aim, ALU.mult, ["B1", "L3"], ["B0"])
    PA.tt(C1, C1, B0, ALU.add, ["C1", "B0"], ["C1"])
    PA.tt(C1, C1, C0, ALU.mult, ["C1", "C0"], ["C1"])
    PA.tt(B0, B1, are, ALU.mult, ["B1", "L3"], ["B0"])
    PA.tt(A1, A0, aim, ALU.mult, ["A0", "L3"], ["A1"])
    PA.tt(B0, B0, A1, ALU.subtract, ["B0", "A1"], ["B0"])
    PA.tt(B0, B0, C0, ALU.mult, ["B0", "C0"], ["B0"])
    Bre, Bim = Bz[:, 0, :], Bz[:, 1, :]
    v3 = lambda ap: ap.rearrange("p (k s) -> p k s", k=16)
    PA.tt(A0, C1, Bre, ALU.mult, ["C1", "Bz"], ["A0"])
    PA.tt(A1, B0, Bim, ALU.mult, ["B0", "Bz"], ["A1"])
    PA.tt(BT_bf[:, :, 0, :], v3(A0), v3(A1), ALU.subtract, ["A0", "A1"], ["BT"])
    PA.tt(A0, C1, Bim, ALU.mult, ["C1", "Bz", "BT"], ["A0"])
    PA.tt(A1, B0, Bre, ALU.mult, ["B0", "Bz", "BT"], ["A1"])
    PA.tt(BT_bf[:, :, 1, :], v3(A0), v3(A1), ALU.add, ["A0", "A1"], ["BT"])
    PA.dma("sp", Bz[:, 0, :], ssm_C_d[0], reads=["BT"], writes=["Bz"], slot="ld2", group=True)
    PA.dma("sp", Bz[:, 1, :], ssm_C_d[1], reads=["BT"], writes=["Bz"], slot="ld2", group=True)
    PA.cp(CT_bf[:, :, 0, :], v3(Bz[:, 0, :]), ["Bz"], ["CT"])
    PA.ts(CT_bf[:, :, 1, :], v3(Bz[:, 1, :]), -1.0, ALU.mult, ["Bz"], ["CT"])
    PA.act(smw[:, 0, :], sm[:, 2, :], AF.Exp, ["sm"], ["smw0"])
    PA.tt(smw[:, 1, :], sm[:, 1, :], smw[:, 0, :], ALU.mult, ["sm", "smw0"], ["smw1"])
    PA.tt(rcol[:], sm[:, 0, :], smw[:, 0, :], ALU.mult, ["sm", "smw0"], ["rcol"])
    PA.act(rcol[:], rcol[:], AF.Exp, ["rcol"], ["rcol"])
    ang = tA[:, :].rearrange("p (k j) -> p k j", k=16)
    PA.tt(ang, jj[:].unsqueeze(1).to_broadcast([128, 16, ST]), smw[:, 1, :].unsqueeze(2).to_broadcast([128, 16, ST]),
          ALU.mult, ["jj", "smw1", "A0", "A1", "BT"], ["tA"])
    range_reduce(PA, tB[:, :], tA[:, :], 0.0, tC[:, :], tI[:, :], "tA", "tB", "tC", "tI")
    PA.act(sinT[:].rearrange("p k j -> p (k j)"), tB[:, :], AF.Sin, ["tB", "B0", "B1", "C0", "C1"], ["sinT"])
    PA.act(tC[:, :], tB[:, :], AF.Abs, ["tB"], ["tC"])
    PA.act(cosT[:].rearrange("p k j -> p (k j)"), tC[:, :], AF.Sin, ["tC"], ["cosT"], bias=hpic[:, 0:1], scale=-1.0)
    PA.barrier()
    if stage == "A":
        dbg_out(PA, "BT", BT_bf[:], [128, 16, 2, 128], BF16)
        dbg_out(PA, "CT", CT_bf[:], [128, 16, 2, 128], BF16)
        dbg_out(PA, "cosT", cosT[:], [128, 16, ST], BF16)
        dbg_out(PA, "sinT", sinT[:], [128, 16, ST], BF16)
        dbg_out(PA, "rcol", rcol[:], [128, 16], F32)
        PA.barrier()
        PA.emit()
        return nc
    PA.emit()
    nc.all_engine_barrier()
    sA.close()

    sB = ExitStack()
    PB = Prog(nc, top, "B")
    modrow = sbuf(sB, "modrow", [1, 6 * D], F32)
    grow = sbuf(sB, "grow", [1, 2 * D], F32)
    arow = sbuf(sB, "arow", [1, 2 * D], F32)
    cTt = sbuf(sB, "cTt", [128, 8], F32)
    adaR = [sbuf(sB, "adaR%d" % i, [128, 1536], F32) for i in range(3)]
    PB.dma("sp", modrow[:], ada_b_d, writes=["modrow"], slot="ld", group=True)
    PB.dma("sp", grow[:, 0:D], g1_d, writes=["grow"], slot="ld", group=True)
    PB.dma("sp", grow[:, D:2 * D], g2_d, writes=["grow"], slot="ld", group=True)
    PB.dma("sp", cTt[:], cT_d, writes=["cTt"], slot="ld", group=True)
    PB.act(cTt[:], cTt[:], AF.Silu, ["cTt"], ["cTt"])
    n_ada = 0
    for qd in range(4):
        banks = [nxt() for _ in range(3)]
        for kc in range(8):
            rb = n_ada % 3
            n_ada += 1
            PB.dma("sp", adaR[rb][:], ada_w_d[kc * 128:(kc + 1) * 128, qd * 1536:(qd + 1) * 1536],
                   writes=["adaR%d" % rb], slot="ada%d" % rb)
            for n in range(3):
                PB.mm(banks[n][0][0:1, :], [(cTt[:, kc:kc + 1], adaR[rb][:, n * 512:(n + 1) * 512])],
                      ["cTt", "adaR%d" % rb], [banks[n][1]], start=(kc == 0), stop=(kc == 7))
        for n in range(3):
            sl = modrow[:, qd * 1536 + n * 512: qd * 1536 + (n + 1) * 512]
            PB.tt(sl, banks[n][0][0:1, :], sl, ALU.add, [banks[n][1], "modrow"], ["modrow"])
    PB.stt(arow[:, 0:D], modrow[:, D:2 * D], 1.0, grow[:, 0:D], ALU.add, ALU.mult, ["modrow", "grow"], ["arow"])
    PB.stt(arow[:, D:2 * D], modrow[:, 4 * D:5 * D], 1.0, grow[:, D:2 * D], ALU.add, ALU.mult, ["modrow", "grow"], ["arow"])
    pc, pcn = nxt()
    vecs = [arow[:, 0:D], modrow[:, 0:D], arow[:, D:2 * D], modrow[:, 3 * D:4 * D]]
    for vi, vec in enumerate(vecs):
        for fc in range(8):
            PB.mm(pc[:, vi * 8 + fc: vi * 8 + fc + 1], [(vec[0:1, fc * 128:(fc + 1) * 128], ones_row[0:1, 0:1])],
                  ["arow", "modrow"], [pcn])
    PB.cp(cols[:], pc[:, 0:32], [pcn], ["cols"])
    for gi, (gsrc, gdst, gname) in enumerate([(modrow[:, 2 * D:3 * D], gate1b, "gate1b"), (modrow[:, 5 * D:6 * D], gate2b, "gate2b"),
                                              ]):
        for h in range(2):
            pb_, pbn = nxt()
            PB.mm(pb_[:, :], [(ones_row[0:1, :], gsrc[0:1, h * 512:(h + 1) * 512])], ["modrow", "arow"], [pbn])
            PB.cp(gdst[:, h * 512:(h + 1) * 512], pb_[:, :], [pbn], [gname])
    PB.dma("sp", modscr_d[0:1, :], arow[:, D:2 * D], reads=["arow"], writes=["modscr0"], slot="ms", group=True)
    PB.dma("sp", modscr_d[1:2, :], modrow[:, 3 * D:4 * D], reads=["modrow"], writes=["modscr1"], slot="ms", group=True)
    PB.barrier()
    if stage == "B":
        dbg_out(PB, "cols", cols[:], [128, 32], F32)
        dbg_out(PB, "gate1b", gate1b[:], [128, D], F32)
        dbg_out(PB, "gate2b", gate2b[:], [128, D], F32)
        PB.barrier()
        PB.emit()
        return nc
    PB.emit()
    nc.all_engine_barrier()
    sB.close()

    PM = Prog(nc, top, "M")
    w_in_bf = sbuf(p1, "w_in_bf", [128, 8, 3584], BF16)
    glu_bf = sbuf(p1, "glu_bf", [128, 4, 512], BF16)
    wa_bf = sbuf(p1, "wa_bf", [128, 4, D], BF16)
    wb_bf = sbuf(p1, "wb_bf", [128, 4, D], BF16)
    wout_bf = sbuf(p1, "wout_bf", [128, 8, D], BF16)
    wsT_bf = sbuf(p1, "wsT_bf", [128, 8, 128], BF16)
    lngb = sbuf(p1, "lngb", [128, 512], F32)
    lnbb = sbuf(p1, "lnbb", [128, 512], F32)
    bs_row = sbuf(p1, "bs_row", [1, 1024], F32)
    xt = [sbuf(p1, "xt%d" % i, [128, D], F32) for i in range(1)]
    xn = sbuf(p1, "xn", [128, D], BF16)
    st = sbuf(p1, "st", [128, 16], F32)
    hT = sbuf(p1, "hT", [128, 8, ST], BF16)
    us32 = sbuf(p1, "us32", [128, 4, ST], F32)
    usbf = sbuf(p1, "usbf", [128, 4, ST], BF16)
    guT = sbuf(p1, "guT", [128, 4, ST], BF16)
    gv = sbuf(p1, "gv", [128, 512], F32)
    gv2 = sbuf(p1, "gv2", [128, 512], F32)
    vn = sbuf(p1, "vn", [128, 2, 512], BF16)
    xf = sbuf(p1, "xf", [128, D], F32)
    sga = sbuf(p1, "sga", [128, 8, ST], BF16)
    sgb = sbuf(p1, "sgb", [128, 8, ST], BF16)
    zT = sbuf(p1, "zT", [128, 4, ST], BF16)
    sg = sbuf(p1, "sg", [128, 4, ST], BF16)
    oT = sbuf(p1, "oT", [128, 4, ST], BF16)
    pT = sbuf(p1, "pT", [128, 4, ST], BF16)
    ta = sbuf(p1, "ta", [128, 2, ST], F32)
    tb = sbuf(p1, "tb", [128, 2, ST], F32)
    mT = sbuf(p1, "mT", [128, 8, ST], BF16)
    t1 = sbuf(p1, "t1", [128, ST], F32)
    t2 = sbuf(p1, "t2", [128, ST], F32)
    wri = sbuf(p1, "wri", [128, 2, ST], F32)
    qri = sbuf(p1, "qri", [128, 2, ST], F32)
    s32 = sbuf(p1, "s32", [128, 2, ST], F32)
    sT = sbuf(p1, "sT", [128, 2, ST], BF16)
    yv = sbuf(p1, "yv", [128, ST], F32)

    for kc in range(8):
        PM.dma("pool", w_in_bf[:, kc, :], w_in_d[kc * 128:(kc + 1) * 128, :], writes=["w_in%d" % kc], slot="wl", group=True)
    PM.dma("pool", glu_bf[:], glu_w_d.rearrange("(c p) n -> p c n", p=128), writes=["glu"], slot="wl", group=True)
    PM.dma("pool", wa_bf[:], wa_d.rearrange("(c p) n -> p c n", p=128), writes=["wa"], slot="wl", group=True)
    PM.dma("pool", wb_bf[:], wb_d.rearrange("(c p) n -> p c n", p=128), writes=["wb"], slot="wl", group=True)
    PM.dma("pool", wout_bf[:], wout_d.rearrange("(c p) n -> p c n", p=128), writes=["wout"], slot="wl", group=True)
    PM.dma("sp", lngb[:], lng_d.partition_broadcast(128), writes=["lngb"], slot="wl2", group=True)
    PM.dma("sp", lnbb[:], lnb_d.partition_broadcast(128), writes=["lnbb"], slot="wl2", group=True)
    PM.dma("sp", bs_row[:], bs_d, writes=["bs_row"], slot="wl2", group=True)
    wsst = us32[:].rearrange("p a n -> p (a n)").rearrange("p (h t) -> p h t", h=8)
    PM.dma("sp", wsst, wsT_d, writes=["us32"], slot="wl2", group=True)
    PM.dma("sp", gv[:, 0:128], tri_d, writes=["gv"], slot="wl2", group=True)
    PM.tt(wsT_bf[:], wsst, gv[:, 0:128].unsqueeze(1).to_broadcast([128, 8, 128]), ALU.mult, ["us32", "gv"], ["wsT_bf"])
    W_IN = ["w_in%d" % kc for kc in range(8)]

    def pairview(ps_):
        return ps_[:, :].rearrange("p (a n) -> p a n", a=2)

    xq = [0]
    rot["n"] = 5
    for s in range(NST):
        def front_norm_tile(ss, i):
            if True:
                tix = 2 * ss + i
                xb = xf
                xbn = "xf"
                PM.dma("sp", xb[:], x_d[tix * 128:(tix + 1) * 128, :], writes=[xbn], slot=xbn)
                PM.memset(st[:, 0:1], 0.0, ["st0"])
                PM.act(xn[:], xb[:], AF.Square, [xbn, "st0"], ["xn", "st0"], accum=st[:, 0:1])
                PM.act(st[:, 1:2], st[:, 0:1], AF.Sqrt, ["st0"], ["st1"], bias=epsc[:, 0:1], scale=1.0 / D)
                PM.op("dve", lambda e: e.reciprocal(st[:, 2:3], st[:, 1:2]), ["st1"], ["st2"])
                PM.ts(xn[:], xb[:], st[:, 2:3], ALU.mult, [xbn, "st2"], ["xn"])
                pt_, ptn = nxt_t()
                for fc in range(8):
                    PM.tr(pt_[:, fc * 128:(fc + 1) * 128], xn[:, fc * 128:(fc + 1) * 128], ident_bf[:], ["xn"], [ptn])
                for fc in range(8):
                    PM.act(hT[:, fc, i * 128:(i + 1) * 128], pt_[:, fc * 128:(fc + 1) * 128], AF.Identity,
                           [ptn], ["hT"], bias=s1c[:, fc:fc + 1], scale=a1c[:, fc:fc + 1])

        def proj_u():
            for pr in range(2):
                ps_, psn = nxt()
                for a in range(2):
                    cc = 2 * pr + a
                    PM.mm(ps_[:, a * ST:(a + 1) * ST], [(w_in_bf[:, kc, cc * 128:(cc + 1) * 128], hT[:, kc, :]) for kc in range(8)],
                          W_IN + ["hT"], [psn])
                PM.act(us32[:, 2 * pr:2 * pr + 2, :], pairview(ps_), AF.Copy, [psn], ["us32"])
            PM.cp(usbf[:], us32[:], ["us32"], ["usbf"], eng="pool")

        def proj_zu():
            for pr in range(2):
                ps_, psn = nxt()
                for a in range(2):
                    cc = 2 * pr + a
                    PM.mm(ps_[:, a * ST:(a + 1) * ST], [(w_in_bf[:, kc, 512 + cc * 128:512 + (cc + 1) * 128], hT[:, kc, :]) for kc in range(8)],
                          W_IN + ["hT"], [psn])
                PM.act(guT[:, 2 * pr:2 * pr + 2, :], pairview(ps_), AF.Gelu_apprx_tanh, [psn], ["guT"])

        def proj_v(i):
            ps_, psn = nxt()
            PM.mm(ps_[:, :], [(hT[:, kc, i * 128:(i + 1) * 128], w_in_bf[:, kc, 1024:1536]) for kc in range(8)],
                  W_IN + ["hT"], [psn])
            PM.memset(st[:, 4:6], 0.0, ["st4"])
            PM.act(gv[:], ps_[:, :], AF.Gelu_apprx_tanh, [psn, "st4"], ["gv", "st4"], accum=st[:, 4:5])
            PM.act(gv2[:], gv[:], AF.Square, ["gv", "st4"], ["gv2", "st4"], accum=st[:, 5:6])
            PM.ts(st[:, 6:7], st[:, 4:5], 1.0 / 512, ALU.mult, ["st4"], ["st6"])
            PM.tt(st[:, 7:8], st[:, 6:7], st[:, 6:7], ALU.mult, ["st6"], ["st7"])
            PM.stt(st[:, 8:9], st[:, 5:6], 1.0 / 512, st[:, 7:8], ALU.mult, ALU.subtract, ["st4", "st7"], ["st8"])
            PM.act(st[:, 9:10], st[:, 8:9], AF.Sqrt, ["st8"], ["st9"], bias=epsc[:, 0:1], scale=1.0)
            PM.op("dve", lambda e: e.reciprocal(st[:, 10:11], st[:, 9:10]), ["st9"], ["st10"])
            PM.stt(st[:, 11:12], st[:, 6:7], -1.0, st[:, 10:11], ALU.mult, ALU.mult, ["st6", "st10"], ["st11"])
            PM.act(gv2[:], gv[:], AF.Identity, ["gv", "st10", "st11"], ["gv2"], bias=st[:, 11:12], scale=st[:, 10:11])
            PM.tt(gv[:], gv2[:], lngb[:], ALU.mult, ["gv2", "lngb"], ["gv"])
            PM.tt(vn[:, i, :], gv[:], lnbb[:], ALU.add, ["gv", "lnbb"], ["vn"])

        def mix(cc):
            ps_, psn = nxt()
            for i in range(2):
                for hh in range(2):
                    h = 2 * cc + hh
                    o_ = ps_[hh * 64:(hh + 1) * 64, i * 128:(i + 1) * 128]

                    def emit(pe, o_=o_, i=i, cc=cc, hh=hh, h=h):
                        pe.matmul(o_, vn[:, i, cc * 128 + hh * 64: cc * 128 + (hh + 1) * 64], wsT_bf[:, h, :], start=True, stop=False)
                        return pe.matmul(o_, ones_row[0:1, 0:64], bs_row[0:1, h * 128:(h + 1) * 128], start=False, stop=True)

                    PM.op("pe", emit, ["vn", "wsT_bf", "bs_row"], [psn])
            PM.tt(pT[:, cc, :], guT[:, cc, :], ps_[:, 0:ST], ALU.mult, ["guT", psn], ["pT"])

        def gates(pr):
            pga, pgan = nxt()
            pgb, pgbn = nxt()
            for a in range(2):
                dc = 2 * pr + a
                sl = slice(a * ST, (a + 1) * ST)
                PM.mm(pga[:, sl], [(w_in_bf[:, kc, 1536 + dc * 128:1536 + (dc + 1) * 128], hT[:, kc, :]) for kc in range(8)], W_IN + ["hT"], [pgan])
                PM.mm(pgb[:, sl], [(w_in_bf[:, kc, 2560 + dc * 128:2560 + (dc + 1) * 128], hT[:, kc, :]) for kc in range(8)], W_IN + ["hT"], [pgbn])
            PM.act(sga[:, 2 * pr:2 * pr + 2, :], pairview(pga), AF.Sigmoid, [pgan], ["sga"])
            PM.act(sgb[:, 2 * pr:2 * pr + 2, :], pairview(pgb), AF.Sigmoid, [pgbn], ["sgb"])

        bu_banks = {}

        def bu(k):
            cc = k // 4
            pb_, pbn = nxt()
            PM.mm(pb_[:, 0:ST], [(BT_bf[:, k, 0, :], usbf[:, cc, :])], ["usbf"], [pbn])
            PM.mm(pb_[:, ST:2 * ST], [(BT_bf[:, k, 1, :], usbf[:, cc, :])], ["usbf"], [pbn])
            bu_banks[k] = (pb_, pbn)

        def ssm_dve(k):
            pb_, pbn = bu_banks[k]
            bre, bim = pb_[:, 0:ST], pb_[:, ST:2 * ST]
            ck, sk = cosT[:, k, :], sinT[:, k, :]
            PM.tt(t1[:], bre, ck, ALU.mult, [pbn], ["t1"])
            PM.tt(t2[:], bim, sk, ALU.mult, [pbn], ["t2"])
            PM.tt(wri[:, 0, :], t1[:], t2[:], ALU.add, ["t1", "t2"], ["wri0"])
            PM.tt(t1[:], bim, ck, ALU.mult, [pbn], ["t1"])
            PM.tt(t2[:], bre, sk, ALU.mult, [pbn], ["t2"])
            PM.tt(wri[:, 1, :], t1[:], t2[:], ALU.subtract, ["t1", "t2"], ["wri1"])
            rb_ = rcol[:, k:k + 1].to_broadcast([128, ST])
            PM.scan(qri[:, 0, :], rb_, wri[:, 0, :], carry[:, k, 0:1], ["wri0", "carry%d" % k], ["qri0"])
            PM.scan(qri[:, 1, :], rb_, wri[:, 1, :], carry[:, k, 1:2], ["wri1", "carry%d" % k], ["qri1"])
            PM.tt(t1[:], qri[:, 0, :], ck, ALU.mult, ["qri0"], ["t1"])
            PM.tt(t2[:], qri[:, 1, :], sk, ALU.mult, ["qri1"], ["t2"])
            PM.tt(s32[:, 0, :], t1[:], t2[:], ALU.subtract, ["t1", "t2"], ["s32a"])
            PM.tt(t1[:], qri[:, 1, :], ck, ALU.mult, ["qri1"], ["t1"])
            PM.tt(t2[:], qri[:, 0, :], sk, ALU.mult, ["qri0"], ["t2"])
            PM.tt(s32[:, 1, :], t1[:], t2[:], ALU.add, ["t1", "t2"], ["s32b"])
            PM.act(sT[:], s32[:], AF.Copy, ["s32a", "s32b"], ["sT"])
            PM.cp(carry[:, k, :], s32[:, :, ST - 1], ["s32a", "s32b"], ["carry%d" % k], eng="pool")

        def ssm_c(k):
            cc = k // 4
            yps, ypn = psf[5], "psf5"
            PM.mm(yps[:, 0:ST], [(CT_bf[:, k, 0, :], sT[:, 0, :]), (CT_bf[:, k, 1, :], sT[:, 1, :])], ["sT"], [ypn],
                  start=(k % 4 == 0), stop=(k % 4 == 3))
            if k % 4 == 3:
                PM.stt(yv[:], us32[:, cc, :], dcol[:, cc:cc + 1], yps[:, 0:ST], ALU.mult, ALU.add, ["us32", ypn], ["yv"])
                PM.act(zT[:, cc, :], yv[:], AF.Gelu_apprx_tanh, ["yv"], ["zT"])

        extras = {0: proj_zu, 1: (lambda: proj_v(0)), 2: (lambda: proj_v(1)),
                  4: (lambda: mix(0)), 5: (lambda: mix(1)), 6: (lambda: mix(2)), 7: (lambda: mix(3)),
                  8: (lambda: gates(0)), 9: (lambda: gates(1)), 10: (lambda: gates(2)), 11: (lambda: gates(3))}
        if s == 0:
            front_norm_tile(0, 0)
            front_norm_tile(0, 1)
        if s + 1 < NST:
            extras[12] = (lambda: front_norm_tile(s + 1, 0))
            extras[13] = (lambda: front_norm_tile(s + 1, 1))
        proj_u()
        bu(0)
        for k in range(16):
            if k + 1 < 16:
                bu(k + 1)
            ssm_dve(k)
            if k in extras:
                extras[k]()
            ssm_c(k)
        for pr in range(2):
            ps_, psn = nxt()
            for a in range(2):
                n = 2 * pr + a
                PM.mm(ps_[:, a * ST:(a + 1) * ST], [(glu_bf[:, cc, n * 128:(n + 1) * 128], zT[:, cc, :]) for cc in range(4)],
                      ["glu", "zT"], [psn])
                PM.act(sg[:, n, :], ps_[:, a * ST:(a + 1) * ST], AF.Sigmoid, [psn], ["sg"], bias=glubc[:, n:n + 1], scale=1.0)
        PM.tt(oT[:], zT[:], sg[:], ALU.mult, ["zT", "sg"], ["oT"])
        for pr in range(4):
            pya, pyan = nxt()
            pyb, pybn = nxt()
            for a in range(2):
                dc = 2 * pr + a
                sl = slice(a * ST, (a + 1) * ST)
                PM.mm(pya[:, sl], [(wa_bf[:, cc, dc * 128:(dc + 1) * 128], oT[:, cc, :]) for cc in range(4)], ["wa", "oT"], [pyan])
                PM.mm(pyb[:, sl], [(wb_bf[:, cc, dc * 128:(dc + 1) * 128], pT[:, cc, :]) for cc in range(4)], ["wb", "pT"], [pybn])
            PM.tt(ta[:], sga[:, 2 * pr:2 * pr + 2, :], pairview(pya), ALU.mult, ["sga", pyan], ["ta"])
            PM.tt(tb[:], sgb[:, 2 * pr:2 * pr + 2, :], pairview(pyb), ALU.mult, ["sgb", pybn], ["tb"])
            PM.tt(mT[:, 2 * pr:2 * pr + 2, :], ta[:], tb[:], ALU.add, ["ta", "tb"], ["mT"])
        for i in range(2):
            tix = 2 * s + i
            xb = xt[0]
            xbn = "xt0"
            PM.dma("sp", xb[:], x_d[tix * 128:(tix + 1) * 128, :], writes=[xbn], slot=xbn)
            for dh in range(2):
                ps_, psn = nxt()
                PM.mm(ps_[:, :], [(mT[:, kc, i * 128:(i + 1) * 128], wout_bf[:, kc, dh * 512:(dh + 1) * 512]) for kc in range(8)],
                      ["mT", "wout"], [psn])
                PM.tt(gv2[:], ps_[:, :], gate1b[:, dh * 512:(dh + 1) * 512], ALU.mult, [psn], ["gv2"])
                PM.tt(xb[:, dh * 512:(dh + 1) * 512], gv2[:], xb[:, dh * 512:(dh + 1) * 512], ALU.add, ["gv2", xbn], [xbn])
            PM.dma("sp", x1s_d[tix * 128:(tix + 1) * 128, :], xb[:], reads=[xbn], writes=["x1s%d" % tix], slot=xbn)
    rot["n"] = 6
    PM.barrier()
    PM.emit()
    if stage == "M":
        return nc
    nc.all_engine_barrier()
    p1.close()

    p2 = ExitStack()
    PX = Prog(nc, top, "X")
    PX.regs = regs
    UW = 256
    NU = T // UW
    x1 = sbuf(p2, "x1", [128, NT, D], F32)
    h2tm = sbuf(p2, "h2tm", [128, NT, D], BF16)
    bgc = sbuf(p2, "bgc", [128, NE, 16], F32)
    posm = sbuf(p2, "posm", [128, NT, NE], F32)
    rwhl = sbuf(p2, "rwhl", [128, NT, NE, 2], BF16)
    iota = sbuf(p2, "iota", [128, UW], F32)
    ne_i = sbuf(p2, "ne_i", [1, NE], I32)
    p2r = ExitStack()
    rwb_bf = sbuf(p2r, "rwb_bf", [128, 8, NE], BF16)
    rbb = sbuf(p2r, "rbb", [128, NE], F32)
    bo_g = sbuf(p2r, "bo_g", [NE, D], F32)
    maskf = sbuf(p2r, "maskf", [128, NT, NE], F32)
    maskb = sbuf(p2r, "maskb", [128, NT, NE], BF16)
    rw = sbuf(p2r, "rw", [128, NT, NE], F32)
    stri_bf = sbuf(p2r, "stri_bf", [128, 128], BF16)
    ones_bf = sbuf(p2r, "ones_bf", [128, 128], BF16)
    cnt = sbuf(p2r, "cnt", [1, 3, NE], F32)
    acc2_ = [sbuf(p2r, "acc2_%d" % i, [128, D], F32) for i in range(2)]
    xn2_ = [sbuf(p2r, "xn2_%d" % i, [128, D], BF16) for i in range(2)]
    h2Tt_ = [sbuf(p2r, "h2Tt_%d" % i, [128, 8, 128], BF16) for i in range(2)]
    rwT_ = [sbuf(p2r, "rwT_%d" % i, [NE, 128], F32) for i in range(2)]
    rt_ = [sbuf(p2r, "rt_%d" % i, [128, 4, NE], F32) for i in range(2)]
    rs_ = [sbuf(p2r, "rs_%d" % i, [128, 24], F32) for i in range(2)]
    rtp = sbuf(p2r, "rtp", [128, 4, NE], F32)
    a2b = sbuf(p2r, "a2b", [128, D], F32)
    s2b = sbuf(p2r, "s2b", [128, D], F32)
    ldf = sbuf(p2r, "ldf", [128, 256], F32)

    PX.dma("sp", rbb[:], rb_d.partition_broadcast(128), writes=["rbb"], slot="ld", group=True)
    PX.dma("sp", a2b[:], modscr_d[0:1, :].partition_broadcast(128), writes=["a2b"], slot="ld", group=True)
    PX.dma("sp", s2b[:], modscr_d[1:2, :].partition_broadcast(128), writes=["s2b"], slot="ld", group=True)
    PX.dma("sp", bgc[:], mbi_d, writes=["bgc"], slot="ld", group=True)
    PX.dma("sp", bo_g[:], mbo_d, writes=["bo_g"], slot="ld", group=True)
    PX.dma("sp", iota[:], iota_d, writes=["iota"], slot="ld", group=True)
    PX.dma("sp", ldf[:, 0:128], stri_d, writes=["ldf"], slot="ld", group=True)
    PX.dma("pool", rwb_bf[:], rw_d.rearrange("(c p) e -> p c e", p=128), writes=["rwb"], slot="ldp")
    PX.cp(stri_bf[:], ldf[:, 0:128], ["ldf"], ["stri_bf"])
    PX.memset(ones_bf[:], 1.0, ["ones_bf"])
    PX.ts(bgc[:, :, 8:16], bgc[:, :, 8:16], 1.0, ALU.add, ["bgc"], ["bgc"])
    PX.tt(bo_g[:], bo_g[:], gate2b[0:NE, :], ALU.mult, ["bo_g"], ["bo_g"])

    for tix in range(NT):
        pb2 = tix % 2
        rs, rt, rwT, h2Tt, xn2, acc2 = rs_[pb2], rt_[pb2], rwT_[pb2], h2Tt_[pb2], xn2_[pb2], acc2_[pb2]
        xb = x1[:, tix, :]
        xbn = "x1_%d" % tix
        PX.dma("sp", xb, x1s_d[tix * 128:(tix + 1) * 128, :], writes=[xbn], slot="x1l%d" % tix)
        PX.memset(rs[:, 0:1], 0.0, ["rs0_%d" % pb2])
        PX.act(xn2[:], xb, AF.Square, [xbn, "rs0_%d" % pb2], ["xn2_%d" % pb2, "rs0_%d" % pb2], accum=rs[:, 0:1])
        PX.act(rs[:, 1:2], rs[:, 0:1], AF.Sqrt, ["rs0_%d" % pb2], ["rs1_%d" % pb2], bias=epsc[:, 0:1], scale=1.0 / D)
        PX.op("dve", lambda e, rs=rs: e.reciprocal(rs[:, 2:3], rs[:, 1:2]), ["rs1_%d" % pb2], ["rs2_%d" % pb2])
        PX.stt(acc2[:], xb, rs[:, 2:3], a2b[:], ALU.mult, ALU.mult, [xbn, "rs2_%d" % pb2], ["acc2_%d" % pb2])
        PX.tt(h2tm[:, tix, :], acc2[:], s2b[:], ALU.add, ["acc2_%d" % pb2], ["h2tm%d" % tix])
        pt_, ptn = nxt_t()
        for fc in range(8):
            PX.tr(pt_[:, fc * 128:(fc + 1) * 128], h2tm[:, tix, fc * 128:(fc + 1) * 128], ident_bf[:], ["h2tm%d" % tix], [ptn])
        PX.act(h2Tt[:].rearrange("p a b -> p (a b)"), pt_[:, :], AF.Copy, [ptn], ["h2Tt_%d" % pb2])
        pl, pln = nxt()
        PX.mm(pl[:, 0:NE], [(h2Tt[:, kc, :], rwb_bf[:, kc, :]) for kc in range(8)], ["h2Tt_%d" % pb2, "rwb"], [pln])
        lg, ex = rt[:, 0, :], rt[:, 2, :]
        PX.tt(lg, pl[:, 0:NE], rbb[:], ALU.add, [pln, "rbb"], ["lg_%d" % pb2])
        PX.op("dve", lambda e, lg=lg, rs=rs: e.max(rs[:, 4:12], lg), ["lg_%d" % pb2], ["rs4_%d" % pb2])
        PX.op("dve", lambda e, lg=lg, tix=tix, rs=rs: e.tensor_single_scalar(maskf[:, tix, :], lg, rs[:, 7:8], ALU.is_ge), ["lg_%d" % pb2, "rs4_%d" % pb2], ["maskf%d" % tix])
        PX.ts(rs[:, 12:13], rs[:, 4:5], -1.0, ALU.mult, ["rs4_%d" % pb2], ["rs12_%d" % pb2])
        PX.act(ex, lg, AF.Exp, ["lg_%d" % pb2, "rs12_%d" % pb2], ["ex_%d" % pb2], bias=rs[:, 12:13], scale=1.0)
        PX.tt(ex, ex, maskf[:, tix, :], ALU.mult, ["ex_%d" % pb2, "maskf%d" % tix], ["ex_%d" % pb2])
        PX.op("dve", lambda e, ex=ex, rs=rs: e.reduce_sum(rs[:, 13:14], ex, mybir.AxisListType.X), ["ex_%d" % pb2], ["rs13_%d" % pb2])
        PX.op("dve", lambda e, rs=rs: e.reciprocal(rs[:, 14:15], rs[:, 13:14]), ["rs13_%d" % pb2], ["rs14_%d" % pb2])
        PX.ts(rw[:, tix, :], ex, rs[:, 14:15], ALU.mult, ["ex_%d" % pb2, "rs14_%d" % pb2], ["rw%d" % tix])
        PX.cp(maskb[:, tix, :], maskf[:, tix, :], ["maskf%d" % tix], ["maskb%d" % tix])
        pr_, prn = nxt()
        PX.op("pe", lambda e, pr_=pr_, tix=tix: e.transpose(pr_[0:NE, 0:128], rw[:, tix, :], ident_f[:]), ["rw%d" % tix], [prn])
        PX.cp(rwT[:], pr_[0:NE, 0:128], [prn], ["rwT_%d" % pb2])
        for dh in range(2):
            pb_, pbn = nxt()
            PX.mm(pb_[:, :], [(rwT[:], bo_g[:, dh * 512:(dh + 1) * 512])], ["rwT_%d" % pb2, "bo_g"], [pbn])
            PX.tt(x1[:, tix, dh * 512:(dh + 1) * 512], pb_[:, :], x1[:, tix, dh * 512:(dh + 1) * 512], ALU.add, [pbn, xbn], [xbn])
        PX.cp(rwhl[:, tix, :, 0], rw[:, tix, :], ["rw%d" % tix], ["rwhl%d" % tix])
        PX.cp(rt[:, 1, :], rwhl[:, tix, :, 0], ["rwhl%d" % tix], ["rt1_%d" % pb2])
        PX.tt(rt[:, 1, :], rw[:, tix, :], rt[:, 1, :], ALU.subtract, ["rw%d" % tix, "rt1_%d" % pb2], ["rt1_%d" % pb2])
        PX.cp(rwhl[:, tix, :, 1], rt[:, 1, :], ["rt1_%d" % pb2], ["rwhl%d" % tix])

    rt = rtp

    MB = ["maskb%d" % t for t in range(NT)]
    for tix in range(NT):
        pp_, ppn = nxt()
        prs = [(ones_bf[:], maskb[:, t2, :]) for t2 in range(tix)] + [(stri_bf[:], maskb[:, tix, :])]
        PX.mm(pp_[:, 0:NE], prs, MB[:tix + 1] + ["ones_bf", "stri_bf"], [ppn])
        PX.ts(rt[:, 1, :], maskf[:, tix, :], -1.0, ALU.add, ["maskf%d" % tix], ["rt1"], s2=1.0e6, op1=ALU.mult)
        PX.tt(rt[:, 3, :], pp_[:, 0:NE], maskf[:, tix, :], ALU.mult, [ppn, "maskf%d" % tix], ["rt3"])
        PX.tt(posm[:, tix, :], rt[:, 3, :], rt[:, 1, :], ALU.add, ["rt3", "rt1"], ["posm"])
    pc_, pcn_ = nxt()
    PX.mm(pc_[0:1, 0:NE], [(ones_bf[:, 0:1], maskb[:, t2, :]) for t2 in range(NT)], MB + ["ones_bf"], [pcn_])
    PX.cp(cnt[:, 0, :], pc_[0:1, 0:NE], [pcn_], ["cnt0"])
    PX.memset(cnt[:, 1, :], 0.0, ["cnt1"])
    for u in range(NU):
        PX.stt(cnt[:, 1, :], cnt[:, 0, :], float(UW * u), cnt[:, 1, :], ALU.is_gt, ALU.add, ["cnt0", "cnt1"], ["cnt1"])
    PX.cp(ne_i[:], cnt[:, 1, :], ["cnt1"], ["ne_i"])
    PX.barrier()
    if stage == "XP":
        dbg_out(PX, "posm", posm[:], [128, NT, NE], F32)
        dbg_out(PX, "rw", rw[:], [128, NT, NE], F32)
        dbg_out(PX, "cnt", cnt[:], [1, 3, NE], F32)
        dbg_out(PX, "x1b", x1[:], [128, NT, D], F32)
        PX.barrier()
        PX.emit()
        return nc
    PX.emit()
    nc.all_engine_barrier()
    p2r.close()

    PX = Prog(nc, top, "E")
    PX.regs = regs
    p2e = ExitStack()
    NWP = 5
    wq = [sbuf(p2e, "wq%d" % i, [128, 8, 512], BF16) for i in range(NWP)]
    wo = sbuf(p2e, "wo", [128, 8, D], BF16)
    SelAll = sbuf(p2e, "SelAll", [128, NT, UW], BF16)
    SelT = sbuf(p2e, "SelT", [128, 2, T], BF16)
    pmu = sbuf(p2e, "pmu", [128, NT], F32)
    h2g = sbuf(p2e, "h2g", [128, 8, UW], BF16)
    actT = sbuf(p2e, "actT", [128, 8, UW], BF16)
    g_sb = sbuf(p2e, "g_sb", [128, 2, UW], BF16)
    sg2 = sbuf(p2e, "sg2", [128, 2, UW], BF16)
    u_sb = sbuf(p2e, "u_sb", [128, 2, UW], BF16)
    pp = sbuf(p2e, "pp", [128, 2, UW], BF16)
    yw = sbuf(p2e, "yw", [128, 2, D], BF16)
    rws = sbuf(p2e, "rws", [128, 2], F32)
    rws4 = sbuf(p2e, "rws4", [128, 4], F32)

    NEX = NE

    def piece_slot(e, q):
        return (4 * e + q) % NWP

    def load_piece(e, q):
        sl = piece_slot(e, q)
        src = mwi_d[e].rearrange("(c p) n -> p c n", p=128)
        PX.dma("pool", wq[sl][:, :, 0:256], src[:, :, 256 * q:256 * (q + 1)], writes=["wg%d" % sl], slot="wg%d" % sl)
        PX.dma("pool", wq[sl][:, :, 256:512], src[:, :, D + 256 * q:D + 256 * (q + 1)], writes=["wu%d" % sl], slot="wu%d" % sl)

    def load_wo(e):
        PX.dma("pool", wo[:], mwo_d[e].rearrange("(c p) n -> p c n", p=128), writes=["wo"], slot="wo")

    for q in range(4):
        load_piece(0, q)
    H2 = ["h2tm%d" % t for t in range(NT)]
    X1 = ["x1_%d" % t for t in range(NT)]
    for e in range(NEX):
        if e + 1 < NE:
            load_piece(e + 1, 0)
        load_wo(e)
        PX.regload(regs, ne_i[0:1, e:e + 1], ["ne_i"])
        for u in range(NU):
            PX.marker(("rb", u + 1))
            PX.ts(pmu[:], posm[:, :, e], float(-UW * u), ALU.add, ["posm"], ["pmu"])
            for t2 in range(NT):
                PX.op("dve", lambda eng, t2=t2: eng.tensor_single_scalar(SelAll[:, t2, :], iota[:], pmu[:, t2:t2 + 1], ALU.is_equal),
                      ["pmu", "iota"], ["Sel%d" % t2])
            SEL = ["Sel%d" % t2 for t2 in range(NT)]
            for fp in range(4):
                ps_, psn = nxt()
                for a in range(2):
                    fc = 2 * fp + a
                    PX.mm(ps_[:, a * UW:(a + 1) * UW], [(h2tm[:, t2, fc * 128:(fc + 1) * 128], SelAll[:, t2, :]) for t2 in range(NT)],
                          H2 + SEL, [psn])
                PX.act(h2g[:, 2 * fp:2 * fp + 2, :], ps_[:, :].rearrange("p (a n) -> p a n", a=2), AF.Copy, [psn], ["h2g"])
            for st_ in range(2):
                for h8 in range(2):
                    pt_, ptn = nxt_t()
                    for i8 in range(8):
                        t2 = 8 * h8 + i8
                        PX.tr(pt_[:, i8 * 128:(i8 + 1) * 128], SelAll[:, t2, st_ * 128:(st_ + 1) * 128], ident_bf[:], ["Sel%d" % t2], [ptn])
                    PX.act(SelT[:, st_, h8 * 1024:(h8 + 1) * 1024], pt_[:, :], AF.Copy, [ptn], ["SelT"])
            for jp in range(4):
                sl = piece_slot(e, jp)
                pg_, pgn = nxt()
                pu_, pun = nxt()
                for a in range(2):
                    PX.mm(pg_[:, a * UW:(a + 1) * UW], [(wq[sl][:, kc, a * 128:(a + 1) * 128], h2g[:, kc, :]) for kc in range(8)], ["wg%d" % sl, "h2g"], [pgn])
                    PX.mm(pu_[:, a * UW:(a + 1) * UW], [(wq[sl][:, kc, 256 + a * 128:256 + (a + 1) * 128], h2g[:, kc, :]) for kc in range(8)], ["wu%d" % sl, "h2g"], [pun])
                for a in range(2):
                    j = 2 * jp + a
                    PX.ts(g_sb[:, a, :], pg_[:, a * UW:(a + 1) * UW], bgc[:, e, j:j + 1], ALU.add, [pgn, "bgc"], ["g_sb"], s2=7.0, op1=ALU.min)
                    PX.ts(u_sb[:, a, :], pu_[:, a * UW:(a + 1) * UW], bgc[:, e, 8 + j:9 + j], ALU.add, [pun, "bgc"], ["u_sb"], s2=8.0, op1=ALU.min)
                PX.act(sg2[:], g_sb[:], AF.Sigmoid, ["g_sb"], ["sg2"], scale=1.702)
                PX.tt(pp[:], g_sb[:], sg2[:], ALU.mult, ["g_sb", "sg2"], ["pp"])
                PX.stt(actT[:, 2 * jp:2 * jp + 2, :], u_sb[:], -6.0, pp[:], ALU.max, ALU.mult, ["u_sb", "pp"], ["actT%d" % jp])
            prw, prwn = nxt()
            for st_ in range(2):
                PX.mm(prw[:, 2 * st_:2 * st_ + 2], [(SelAll[:, t2, st_ * 128:(st_ + 1) * 128], rwhl[:, t2, e, :]) for t2 in range(NT)],
                      SEL + ["rwhl"], [prwn])
            PX.act(rws4[:], prw[:, 0:4], AF.Copy, [prwn], ["rws4"])
            prv = rws4[:].rearrange("p (s h) -> p s h", s=2)
            PX.tt(rws[:], prv[:, :, 0], prv[:, :, 1], ALU.add, ["rws4"], ["rws"])
            for st_ in range(2):
                for dh in range(2):
                    po, pon = nxt()
                    PX.mm(po[:, :], [(actT[:, jc, st_ * 128:(st_ + 1) * 128], wo[:, jc, dh * 512:(dh + 1) * 512]) for jc in range(8)],
                          ["actT%d" % jp for jp in range(4)] + ["wo"], [pon])
                    PX.stt(yw[:, st_, dh * 512:(dh + 1) * 512], po[:, :], rws[:, st_:st_ + 1], gate2b[:, dh * 512:(dh + 1) * 512],
                           ALU.mult, ALU.mult, [pon, "rws"], ["yw"])
            for t2 in range(NT):
                for dh in range(2):
                    po, pon = nxt()
                    PX.mm(po[:, :], [(SelT[:, st_, t2 * 128:(t2 + 1) * 128], yw[:, st_, dh * 512:(dh + 1) * 512]) for st_ in range(2)],
                          ["SelT", "yw"], [pon])
                    PX.tt(x1[:, t2, dh * 512:(dh + 1) * 512], po[:, :], x1[:, t2, dh * 512:(dh + 1) * 512], ALU.add, [pon, X1[t2]], [X1[t2]])
        for u in range(NU):
            PX.marker(("re",))
        if e + 1 < NE:
            for q in range(1, 4):
                load_piece(e + 1, q)
    PX.barrier()
    if stage == "XE":
        dbg_out(PX, "x2", x1[:], [128, NT, D], F32)
        PX.barrier()
        PX.emit()
        return nc
    PX.emit()
    nc.all_engine_barrier()
    p2e.close()

    PX = Prog(nc, top, "F")
    fgb = sbuf(p2, "fgb", [128, D], F32)
    xn2 = sbuf(p2, "xn2f", [128, D], BF16)
    rs = sbuf(p2, "rsf", [128, 8], F32)
    PX.dma("sp", fgb[:], fg_d.partition_broadcast(128), writes=["fgb"], slot="ld", group=True)
    for tix in range(NT):
        r_ = "x1_%d" % tix
        PX.memset(rs[:, 0:1], 0.0, ["rs0"])
        PX.act(xn2[:], x1[:, tix, :], AF.Square, [r_, "rs0"], ["xn2", "rs0"], accum=rs[:, 0:1])
        PX.act(rs[:, 1:2], rs[:, 0:1], AF.Sqrt, ["rs0"], ["rs1"], bias=epsc[:, 0:1], scale=1.0 / D)
        PX.op("dve", lambda e: e.reciprocal(rs[:, 2:3], rs[:, 1:2]), ["rs1"], ["rs2"])
        PX.stt(x1[:, tix, :], x1[:, tix, :], rs[:, 2:3], fgb[:], ALU.mult, ALU.mult, [r_, "rs2", "fgb"], [r_])
        PX.dma("sp", out_d[tix * 128:(tix + 1) * 128, :], x1[:, tix, :], reads=[r_], writes=["out%d" % tix], slot="outs", group=True)
    PX.barrier()
    PX.emit()
    p2.close()
    top.close()
    return nc


def _prep(inputs):
    f = lambda a: np.ascontiguousarray(np.asarray(a), dtype=np.float32)
    sh = {}
    sh["ada_w"] = f(inputs["ada_w"][0])
    sh["ada_b"] = f(inputs["ada_b"][0][None])
    sh["g1"] = f(inputs["norm1_g"][0][None])
    sh["g2"] = f(inputs["norm2_g"][0][None])
    sh["fg"] = f(np.asarray(inputs["final_g"])[None])
    sh["w_in"] = f(inputs["w_in"][0])
    a_re = np.asarray(inputs["ssm_a_re"][0])
    a_im = np.asarray(inputs["ssm_a_im"][0])
    ldt = np.broadcast_to(np.asarray(inputs["ssm_log_dt"][0])[:, None], (32, 64))
    smf = lambda v: np.asarray(v).reshape(16, 2, 64).transpose(1, 2, 0).reshape(128, 16)
    sh["ssm_sm"] = f(np.stack([smf(a_re), smf(a_im), smf(ldt)], 1))
    sh["ssm_lam"] = f(np.stack([a_re.reshape(-1), a_im.reshape(-1), np.ascontiguousarray(ldt).reshape(-1)]))
    b = [np.asarray(inputs["ssm_b_re"][0]), np.asarray(inputs["ssm_b_im"][0])]
    c = [np.asarray(inputs["ssm_c_re"][0]), np.asarray(inputs["ssm_c_im"][0])]
    Bz = np.zeros((2, 128, 16, 128), np.float32)
    Cz = np.zeros((2, 128, 16, 128), np.float32)
    for k in range(16):
        for g2 in range(2):
            g = 2 * k + g2
            g8 = g % 8
            for ri in range(2):
                Bz[ri, g8 * 16:(g8 + 1) * 16, k, g2 * 64:(g2 + 1) * 64] = b[ri][g].T
                Cz[ri, g2 * 64:(g2 + 1) * 64, k, g8 * 16:(g8 + 1) * 16] = c[ri][g].T
    sh["ssm_B"] = Bz.reshape(2, 128, 2048)
    sh["ssm_C"] = Cz.reshape(2, 128, 2048)
    sh["ssm_d"] = f(np.asarray(inputs["ssm_d"][0]).reshape(4, 128).T)
    sh["glu_w"] = f(inputs["ssm_glu_w"][0])
    sh["glu_b"] = f(np.asarray(inputs["ssm_glu_b"][0]).reshape(4, 128).T)
    sh["wa"] = f(inputs["w_branch_a"][0])
    sh["wb"] = f(inputs["w_branch_b"][0])
    sh["wout"] = f(inputs["w_out"][0])
    sh["lng"] = f(inputs["gmlp_ln_g"][0][None])
    sh["lnb"] = f(inputs["gmlp_ln_b"][0][None])
    sh["wsT"] = f(np.asarray(inputs["gmlp_ws"][0]).transpose(2, 0, 1))
    sh["bs"] = f(np.asarray(inputs["gmlp_bs"][0]).reshape(1, 1024))
    sh["router_w"] = f(inputs["router_w"][0])
    sh["router_b"] = f(inputs["router_b"][0][None])
    sh["moe_w_in"] = f(inputs["moe_w_in"][0])
    sh["moe_b_in"] = f(np.asarray(inputs["moe_b_in"][0]).reshape(32, 16, 128).transpose(2, 0, 1))
    sh["moe_w_out"] = f(inputs["moe_w_out"][0])
    sh["moe_b_out"] = f(inputs["moe_b_out"][0])
    sh["ident"] = np.eye(128, dtype=np.float32)
    sh["tri"] = np.triu(np.ones((128, 128), np.float32))
    sh["stri"] = np.triu(np.ones((128, 128), np.float32), 1)
    sh["iota256"] = np.ascontiguousarray(np.broadcast_to(np.arange(256, dtype=np.float32)[None], (128, 256)))
    sh["eoff"] = np.ascontiguousarray(np.broadcast_to((2048.0 * np.arange(32, dtype=np.float32) + 1.0)[None], (128, 32)))
    sh["jj"] = np.ascontiguousarray(np.broadcast_to(np.arange(1, ST + 1, dtype=np.float32)[None], (128, ST)))
    return sh


def kernel(**inputs):
    sh = _prep(inputs)
    x = np.asarray(inputs["x"], dtype=np.float32)
    c = np.asarray(inputs["c"], dtype=np.float32)
    in_maps = []
    for b in range(8):
        m = dict(sh)
        m["x"] = np.ascontiguousarray(x[b])
        m["cT"] = np.ascontiguousarray(c[b].reshape(8, 128).T)
        in_maps.append(m)
    nc = build()
    res = run_bass_kernel_spmd(nc, in_maps, core_ids=list(range(8)))
    return np.stack([np.asarray(r["out"], dtype=np.float32) for r in res.results], 0)
```

```python
import numpy as np
from contextlib import ExitStack
import concourse.bass as bass
import concourse.mybir as mybir
from concourse.bass_utils import run_bass_kernel_spmd

F32 = mybir.dt.float32
BF16 = mybir.dt.bfloat16
I32 = mybir.dt.int32
AF = mybir.ActivationFunctionType
ALU = mybir.AluOpType

T = 2048
D = 1024
NT = 16
ST = 256
NST = T // ST
NE = 32
EPS = 1e-6
PI = float(np.pi)
TWO_PI = float(2 * np.pi)


class Op:
    __slots__ = ("eng", "emit", "deps", "is_dma", "slot", "val", "signal", "mark")


class Prog:
    ENGS = ("pe", "act", "dve", "pool", "sp")

    def __init__(self, nc, es, tag):
        self.nc = nc
        self.es = es
        self.tag = tag
        self.q = {e: [] for e in self.ENGS}
        self.lastw = {}
        self.readers = {}
        self.slot_ops = {}
        self.group_slots = set()

    def _deps(self, reads, writes, op):
        deps = []
        for r in reads:
            w = self.lastw.get(r)
            if w is not None:
                deps.append(w)
        for r in writes:
            w = self.lastw.get(r)
            if w is not None:
                deps.append(w)
            deps.extend(self.readers.get(r, ()))
        for r in reads:
            self.readers.setdefault(r, []).append(op)
        for r in writes:
            self.lastw[r] = op
            self.readers[r] = []
        return [d for d in deps if d is not op]

    def op(self, eng, emit, reads=(), writes=()):
        o = Op()
        o.eng, o.emit, o.is_dma, o.slot, o.signal = eng, emit, False, None, False
        o.mark = None
        o.deps = self._deps(reads, writes, o)
        self.q[eng].append(o)
        return o

    def marker(self, mark):
        for e in self.ENGS:
            o = Op()
            o.eng, o.emit, o.is_dma, o.slot, o.signal, o.mark, o.deps = e, None, False, None, False, mark, []
            self.q[e].append(o)

    def regload(self, regs, ap, reads):
        for e in self.ENGS:
            self.op(e, (lambda eng, e=e: eng.reg_load(regs[e], ap)), reads, ())

    def dma(self, eng, out, in_, reads=(), writes=(), slot=None, group=False, custom=None):
        o = Op()
        o.eng, o.is_dma, o.slot, o.signal = eng, True, slot, True
        o.mark = None
        o.emit = (lambda e: e.dma_start(out=out, in_=in_)) if custom is None else custom
        o.deps = self._deps(reads, writes, o)
        if group:
            o.deps = [d for d in o.deps if not (d.is_dma and d.slot == slot)]
            self.group_slots.add(slot)
        self.q[eng].append(o)
        self.slot_ops.setdefault(slot, []).append(o)
        return o

    def barrier(self):
        pend = []
        for e in self.ENGS:
            for o in reversed(self.q[e]):
                if not o.is_dma and o.emit is not None:
                    pend.append(o)
                    break
        for s, ops in self.slot_ops.items():
            pend.append(ops[-1])
        for e in self.ENGS:
            o = Op()
            o.eng, o.emit, o.is_dma, o.slot, o.signal = e, None, False, None, False
            o.mark = None
            o.deps = list(pend)
            self.q[e].append(o)
        self.lastw = {}
        self.readers = {}

    def emit(self):
        nc = self.nc
        for e in self.ENGS:
            for o in self.q[e]:
                for d in o.deps:
                    d.signal = True
        esem = {}
        for e in self.ENGS:
            esem[e] = self.es.enter_context(nc.semaphore("s%s_%s" % (self.tag, e)))
            c = 0
            for o in self.q[e]:
                if o.is_dma or o.emit is None:
                    continue
                if o.signal:
                    c += 1
                    o.val = c
        ssem = {}
        for i, (s, ops) in enumerate(self.slot_ops.items()):
            ssem[s] = self.es.enter_context(nc.semaphore("d%s_%d" % (self.tag, i)))
            if s in self.group_slots:
                for o in ops:
                    o.val = 16 * len(ops)
            else:
                for j, o in enumerate(ops):
                    o.val = 16 * (j + 1)

        regs = getattr(self, "regs", None)

        def emit_q(e, eng):
            seen = {}

            def emit_one(o):
                need = {}
                for d in o.deps:
                    if d.is_dma:
                        sem = ssem[d.slot]
                    else:
                        if d.emit is None or (d.eng == "pe" and e == "pe"):
                            continue
                        sem = esem[d.eng]
                    k = id(sem)
                    if need.get(k, (None, 0))[1] < d.val:
                        need[k] = (sem, d.val)
                for k, (sem, v) in need.items():
                    if seen.get(k, 0) < v:
                        eng.wait_ge(sem, v)
                        seen[k] = v
                if o.emit is None:
                    return
                ins = o.emit(eng)
                if o.is_dma:
                    ins.then_inc(ssem[o.slot], 16)
                elif o.signal:
                    ins.then_inc(esem[e], 1)
                    cur[0] = o.val

            cur = [0]

            def emit_range(ops):
                i = 0
                while i < len(ops):
                    o = ops[i]
                    if o.mark is not None and o.mark[0] == "rb":
                        depth = 1
                        j = i + 1
                        while True:
                            m = ops[j].mark
                            if m is not None and m[0] == "rb":
                                depth += 1
                            elif m is not None and m[0] == "re":
                                depth -= 1
                                if depth == 0:
                                    break
                            j += 1
                        body = ops[i + 1:j]
                        real = [b for b in body if b.emit is not None]
                        if real:
                            c = sum(1 for b in real if (not b.is_dma) and b.signal)
                            dcnt = {}
                            for b in real:
                                if b.is_dma:
                                    dcnt[b.slot] = dcnt.get(b.slot, 0) + 1
                            saved = dict(seen)
                            cur_before = cur[0]
                            g = eng.If_lt(regs[e], o.mark[1])
                            g.__enter__()
                            if c > 0:
                                if cur_before > 0:
                                    eng.wait_ge(esem[e], cur_before)
                                eng.sem_inc(esem[e], c)
                            for sl, n in dcnt.items():
                                eng.sem_inc(ssem[sl], 16 * n)
                            g.__exit__(None, None, None)
                            g2 = eng.Else()
                            g2.__enter__()
                            emit_range(body)
                            g2.__exit__(None, None, None)
                            seen.clear()
                            seen.update(saved)
                            cur[0] = cur_before + c
                        i = j + 1
                        continue
                    if o.mark is None:
                        emit_one(o)
                    i += 1

            emit_range(self.q[e])

        with nc.Block() as block:

            @block.tensor
            def _(eng):
                emit_q("pe", eng)

            @block.scalar
            def _(eng):
                emit_q("act", eng)

            @block.vector
            def _(eng):
                emit_q("dve", eng)

            @block.gpsimd
            def _(eng):
                emit_q("pool", eng)

            @block.sync
            def _(eng):
                emit_q("sp", eng)

    def mm(self, out, pairs, r, w, start=True, stop=True):
        pairs = list(pairs)

        def emit(pe):
            n = len(pairs)
            ins = None
            for i, (l, rr) in enumerate(pairs):
                ins = pe.matmul(out, l, rr, start=(start and i == 0), stop=(stop and i == n - 1))
            return ins

        return self.op("pe", emit, r, w)

    def tr(self, out, in_, ident, r, w):
        return self.op("pe", lambda e: e.transpose(out, in_, ident), r, w)

    def tt(self, out, a, b, op, r, w, eng="dve"):
        return self.op(eng, lambda e: e.tensor_tensor(out, a, b, op), r, w)

    def ts(self, out, a, s1, op0, r, w, s2=None, op1=None, eng="dve"):
        if op1 is None:
            return self.op(eng, lambda e: e.tensor_scalar(out, a, s1, None, op0), r, w)
        return self.op(eng, lambda e: e.tensor_scalar(out, a, s1, s2, op0, op1), r, w)

    def stt(self, out, a, s, b, op0, op1, r, w, eng="dve"):
        return self.op(eng, lambda e: e.scalar_tensor_tensor(out, a, s, b, op0, op1), r, w)

    def cp(self, out, a, r, w, eng="dve"):
        return self.op(eng, lambda e: e.tensor_copy(out, a), r, w)

    def memset(self, out, v, w, eng="dve"):
        return self.op(eng, lambda e: e.memset(out, v), (), w)

    def act(self, out, a, func, r, w, bias=None, scale=None, accum=None):
        kw = {}
        if bias is not None:
            kw["bias"] = bias
        if scale is not None:
            kw["scale"] = scale
        if accum is not None:
            kw["accum_out"] = accum
        return self.op("act", lambda e: e.activation(out, a, func, **kw), r, w)

    def scan(self, out, d0, d1, init, r, w):
        return self.op("dve", lambda e: e.tensor_tensor_scan(out, d0, d1, init, ALU.mult, ALU.add), r, w)


def build(stage=None):
    nc = bass.Bass("TRN2", target_bir_lowering=False)
    top = ExitStack()

    def din(name, shape):
        return nc.dram_tensor(name, list(shape), F32, kind="ExternalInput").ap()

    x_d = din("x", [T, D])
    cT_d = din("cT", [128, 8])
    ada_w_d = din("ada_w", [D, 6 * D])
    ada_b_d = din("ada_b", [1, 6 * D])
    g1_d = din("g1", [1, D])
    g2_d = din("g2", [1, D])
    fg_d = din("fg", [1, D])
    w_in_d = din("w_in", [D, 3584])
    ssm_sm_d = din("ssm_sm", [128, 3, 16])
    ssm_lam_d = din("ssm_lam", [3, 2048])
    ssm_B_d = din("ssm_B", [2, 128, 2048])
    ssm_C_d = din("ssm_C", [2, 128, 2048])
    ssm_d_d = din("ssm_d", [128, 4])
    glu_w_d = din("glu_w", [512, 512])
    glu_b_d = din("glu_b", [128, 4])
    wa_d = din("wa", [512, D])
    wb_d = din("wb", [512, D])
    wout_d = din("wout", [D, D])
    lng_d = din("lng", [1, 512])
    lnb_d = din("lnb", [1, 512])
    wsT_d = din("wsT", [128, 8, 128])
    bs_d = din("bs", [1, 1024])
    rw_d = din("router_w", [D, NE])
    rb_d = din("router_b", [1, NE])
    mwi_d = din("moe_w_in", [NE, D, 2 * D])
    mbi_d = din("moe_b_in", [128, NE, 16])
    mwo_d = din("moe_w_out", [NE, D, D])
    mbo_d = din("moe_b_out", [NE, D])
    ident_d = din("ident", [128, 128])
    tri_d = din("tri", [128, 128])
    jj_d = din("jj", [128, ST])
    stri_d = din("stri", [128, 128])
    iota_d = din("iota256", [128, 256])
    eoff_d = din("eoff", [128, NE])
    out_d = nc.dram_tensor("out", [T, D], F32, kind="ExternalOutput").ap()
    x1s_d = nc.dram_tensor("x1s", [T, D], F32, kind=("ExternalOutput" if stage == "M" else "Internal")).ap()

    def dbg_out(Pg, name, ap, shape, dt=F32):
        o = nc.dram_tensor("dbg_" + name, list(shape), dt, kind="ExternalOutput").ap()
        Pg.dma("sp", o, ap, reads=[], writes=["dbg_" + name], slot="dbg_" + name)

    def sbuf(es, name, shape, dt=F32):
        return es.enter_context(nc.sbuf_tensor("sb_" + name, list(shape), dt))

    pst = [top.enter_context(nc.psum_tensor("pst%d" % i, [128, 1024], BF16)) for i in range(2)]
    psf = [top.enter_context(nc.psum_tensor("psf%d" % i, [128, 512], F32)) for i in range(6)]
    rot = {"f": 0, "t": 0, "n": 6}

    def nxt():
        i = rot["f"] % rot["n"]
        rot["f"] += 1
        return psf[i], "psf%d" % i

    def nxt_t():
        i = rot["t"] % 2
        rot["t"] += 1
        return pst[i], "pst%d" % i

    ident_bf = sbuf(top, "ident_bf", [128, 128], BF16)
    ident_f = sbuf(top, "ident_f", [128, 128], F32)
    ones_row = sbuf(top, "ones_row", [1, 128], F32)
    epsc = sbuf(top, "epsc", [128, 1], F32)
    cols = sbuf(top, "cols", [128, 32], F32)
    gate2b = sbuf(top, "gate2b", [128, D], F32)
    modscr_d = nc.dram_tensor("modscr", [2, D], F32, kind="Internal").ap()
    regs = {"pe": top.enter_context(nc.tensor.register("r_pe")), "act": top.enter_context(nc.scalar.register("r_act")),
            "dve": top.enter_context(nc.vector.register("r_dve")), "pool": top.enter_context(nc.gpsimd.register("r_pool")),
            "sp": top.enter_context(nc.sync.register("r_sp"))}
    a1c, s1c, a2c, s2c = (cols[:, 0:8], cols[:, 8:16], cols[:, 16:24], cols[:, 24:32])

    p1 = ExitStack()
    gate1b = sbuf(p1, "gate1b", [128, D], F32)
    BT_bf = sbuf(p1, "BT_bf", [128, 16, 2, 128], BF16)
    CT_bf = sbuf(p1, "CT_bf", [128, 16, 2, 128], BF16)
    cosT = sbuf(p1, "cosT", [128, 16, ST], BF16)
    sinT = sbuf(p1, "sinT", [128, 16, ST], BF16)
    rcol = sbuf(p1, "rcol", [128, 16], F32)
    carry = sbuf(p1, "carry", [128, 16, 2], F32)
    dcol = sbuf(p1, "dcol", [128, 4], F32)
    glubc = sbuf(p1, "glubc", [128, 4], F32)

    sA = ExitStack()
    PA = Prog(nc, top, "A")
    L3 = sbuf(sA, "L3", [128, 3, 2048], F32)
    Bz = sbuf(sA, "Bz", [128, 2, 2048], F32)
    tA = sbuf(sA, "tA", [128, 4096], F32)
    tB = sbuf(sA, "tB", [128, 4096], F32)
    tC = sbuf(sA, "tC", [128, 4096], F32)
    tI = sbuf(sA, "tI", [128, 4096], I32)
    sm = sbuf(sA, "sm", [128, 3, 16], F32)
    smw = sbuf(sA, "smw", [128, 2, 16], F32)
    jj = sbuf(sA, "jj", [128, ST], F32)
    idl = sbuf(sA, "idl", [128, 128], F32)
    hpic = sbuf(sA, "hpic", [128, 1], F32)

    PA.dma("sp", idl[:], ident_d, writes=["idl"], slot="ld", group=True)
    PA.dma("sp", ident_f[:], ident_d, writes=["ident_f"], slot="ld", group=True)
    for i in range(3):
        PA.dma("sp", L3[:, i, :], ssm_lam_d[i:i + 1, :].partition_broadcast(128), writes=["L3"], slot="ld", group=True)
    for i in range(2):
        PA.dma("sp", Bz[:, i, :], ssm_B_d[i], writes=["Bz"], slot="ld", group=True)
    PA.dma("sp", sm[:], ssm_sm_d, writes=["sm"], slot="ld", group=True)
    PA.dma("sp", jj[:], jj_d, writes=["jj"], slot="ld", group=True)
    PA.dma("sp", dcol[:], ssm_d_d, writes=["dcol"], slot="ld", group=True)
    PA.dma("sp", glubc[:], glu_b_d, writes=["glubc"], slot="ld", group=True)
    PA.cp(ident_bf[:], idl[:], ["idl"], ["ident_bf"])
    PA.memset(ones_row[:], 1.0, ["ones_row"])
    PA.memset(epsc[:], EPS, ["epsc"])
    PA.memset(hpic[:], PI / 2, ["hpic"])
    PA.memset(carry[:], 0.0, ["carry"])

    def range_reduce(Pg, y, x, shift, ntmp, itmp, rx, ry, rn, ri):
        Pg.ts(ntmp, x, 1.0 / TWO_PI, ALU.mult, [rx], [rn], s2=shift / TWO_PI, op1=ALU.add)
        Pg.cp(itmp, ntmp, [rn], [ri])
        Pg.cp(ntmp, itmp, [ri], [rn])
        Pg.ts(y, x, shift, ALU.add, [rx], [ry])
        Pg.stt(y, ntmp, -TWO_PI, y, ALU.mult, ALU.add, [rn, ry], [ry])
        Pg.op("dve", lambda e: e.tensor_single_scalar(ntmp, y, PI, ALU.is_gt), [ry], [rn])
        Pg.stt(y, ntmp, -TWO_PI, y, ALU.mult, ALU.add, [rn, ry], [ry])
        Pg.op("dve", lambda e: e.tensor_single_scalar(ntmp, y, -PI, ALU.is_lt), [ry], [rn])
        Pg.stt(y, ntmp, TWO_PI, y, ALU.mult, ALU.add, [rn, ry], [ry])

    A0, A1 = tA[:, 0:2048], tA[:, 2048:4096]
    B0, B1 = tB[:, 0:2048], tB[:, 2048:4096]
    C0, C1 = tC[:, 0:2048], tC[:, 2048:4096]
    I0 = tI[:, 0:2048]
    are, aim, ldt = L3[:, 0, :], L3[:, 1, :], L3[:, 2, :]
    PA.act(A0, ldt, AF.Exp, ["L3"], ["A0"])
    PA.tt(A1, aim, A0, ALU.mult, ["L3", "A0"], ["A1"])
    PA.tt(B0, are, A0, ALU.mult, ["L3", "A0"], ["B0"])
    PA.act(B0, B0, AF.Exp, ["B0"], ["B0"])
    range_reduce(PA, C0, A1, 0.0, C1, I0, "A1", "C0", "C1", "I0")
    PA.act(B1, C0, AF.Sin, ["C0"], ["B1"])
    PA.act(C1, C0, AF.Abs, ["C0"], ["C1"])
    PA.act(A0, C1, AF.Sin, ["C1", "A0"], ["A0"], bias=hpic[:, 0:1], scale=-1.0)
    PA.tt(A0, B0, A0, ALU.mult, ["B0", "A0"], ["A0"])
    PA.tt(B1, B0, B1, ALU.mult, ["B0", "B1"], ["B1"])
    PA.ts(A0, A0, -1.0, ALU.add, ["A0"], ["A0"])
    PA.tt(C0, are, are, ALU.mult, ["L3"], ["C0"])
    PA.tt(C1, aim, aim, ALU.mult, ["L3"], ["C1"])
    PA.tt(C0, C0, C1, ALU.add, ["C0", "C1"], ["C0"])
    PA.op("dve", lambda e: e.reciprocal(C0, C0), ["C0"], ["C0"])
    PA.tt(C1, A0, are, ALU.mult, ["A0", "L3"], ["C1"])
    PA.tt(B0, B1, aim, ALU.mult, ["B1", "L3"], ["B0"])
    PA.tt(C1, C1, B0, ALU.add, ["C1", "B0"], ["C1"])
    PA.tt(C1, C1, C0, ALU.mult, ["C1", "C0"], ["C1"])
    PA.tt(B0, B1, are, ALU.mult, ["B1", "L3"], ["B0"])
    PA.tt(A1, A0, aim, ALU.mult, ["A0", "L3"], ["A1"])
    PA.tt(B0, B0, A1, ALU.subtract, ["B0", "A1"], ["B0"])
    PA.tt(B0, B0, C0, ALU.mult, ["B0", "C0"], ["B0"])
    Bre, Bim = Bz[:, 0, :], Bz[:, 1, :]
    v3 = lambda ap: ap.rearrange("p (k s) -> p k s", k=16)
    PA.tt(A0, C1, Bre, ALU.mult, ["C1", "Bz"], ["A0"])
    PA.tt(A1, B0, Bim, ALU.mult, ["B0", "Bz"], ["A1"])
    PA.tt(BT_bf[:, :, 0, :], v3(A0), v3(A1), ALU.subtract, ["A0", "A1"], ["BT"])
    PA.tt(A0, C1, Bim, ALU.mult, ["C1", "Bz", "BT"], ["A0"])
    PA.tt(A1, B0, Bre, ALU.mult, ["B0", "Bz", "BT"], ["A1"])
    PA.tt(BT_bf[:, :, 1, :], v3(A0), v3(A1), ALU.add, ["A0", "A1"], ["BT"])
    PA.dma("sp", Bz[:, 0, :], ssm_C_d[0], reads=["BT"], writes=["Bz"], slot="ld2", group=True)
    PA.dma("sp", Bz[:, 1, :], ssm_C_d[1], reads=["BT"], writes=["Bz"], slot="ld2", group=True)
    PA.cp(CT_bf[:, :, 0, :], v3(Bz[:, 0, :]), ["Bz"], ["CT"])
    PA.ts(CT_bf[:, :, 1, :], v3(Bz[:, 1, :]), -1.0, ALU.mult, ["Bz"], ["CT"])
    PA.act(smw[:, 0, :], sm[:, 2, :], AF.Exp, ["sm"], ["smw0"])
    PA.tt(smw[:, 1, :], sm[:, 1, :], smw[:, 0, :], ALU.mult, ["sm", "smw0"], ["smw1"])
    PA.tt(rcol[:], sm[:, 0, :], smw[:, 0, :], ALU.mult, ["sm", "smw0"], ["rcol"])
    PA.act(rcol[:], rcol[:], AF.Exp, ["rcol"], ["rcol"])
    ang = tA[:, :].rearrange("p (k j) -> p k j", k=16)
    PA.tt(ang, jj[:].unsqueeze(1).to_broadcast([128, 16, ST]), smw[:, 1, :].unsqueeze(2).to_broadcast([128, 16, ST]),
          ALU.mult, ["jj", "smw1", "A0", "A1", "BT"], ["tA"])
    range_reduce(PA, tB[:, :], tA[:, :], 0.0, tC[:, :], tI[:, :], "tA", "tB", "tC", "tI")
    PA.act(sinT[:].rearrange("p k j -> p (k j)"), tB[:, :], AF.Sin, ["tB", "B0", "B1", "C0", "C1"], ["sinT"])
    PA.act(tC[:, :], tB[:, :], AF.Abs, ["tB"], ["tC"])
    PA.act(cosT[:].rearrange("p k j -> p (k j)"), tC[:, :], AF.Sin, ["tC"], ["cosT"], bias=hpic[:, 0:1], scale=-1.0)
    PA.barrier()
    if stage == "A":
        dbg_out(PA, "BT", BT_bf[:], [128, 16, 2, 128], BF16)
        dbg_out(PA, "CT", CT_bf[:], [128, 16, 2, 128], BF16)
        dbg_out(PA, "cosT", cosT[:], [128, 16, ST], BF16)
        dbg_out(PA, "sinT", sinT[:], [128, 16, ST], BF16)
        dbg_out(PA, "rcol", rcol[:], [128, 16], F32)
        PA.barrier()
        PA.emit()
        return nc
    PA.emit()
    nc.all_engine_barrier()
    sA.close()

    sB = ExitStack()
    PB = Prog(nc, top, "B")
    modrow = sbuf(sB, "modrow", [1, 6 * D], F32)
    grow = sbuf(sB, "grow", [1, 2 * D], F32)
    arow = sbuf(sB, "arow", [1, 2 * D], F32)
    cTt = sbuf(sB, "cTt", [128, 8], F32)
    adaR = [sbuf(sB, "adaR%d" % i, [128, 1536], F32) for i in range(3)]
    PB.dma("sp", modrow[:], ada_b_d, writes=["modrow"], slot="ld", group=True)
    PB.dma("sp", grow[:, 0:D], g1_d, writes=["grow"], slot="ld", group=True)
    PB.dma("sp", grow[:, D:2 * D], g2_d, writes=["grow"], slot="ld", group=True)
    PB.dma("sp", cTt[:], cT_d, writes=["cTt"], slot="ld", group=True)
    PB.act(cTt[:], cTt[:], AF.Silu, ["cTt"], ["cTt"])
    n_ada = 0
    for qd in range(4):
        banks = [nxt() for _ in range(3)]
        for kc in range(8):
            rb = n_ada % 3
            n_ada += 1
            PB.dma("sp", adaR[rb][:], ada_w_d[kc * 128:(kc + 1) * 128, qd * 1536:(qd + 1) * 1536],
                   writes=["adaR%d" % rb], slot="ada%d" % rb)
            for n in range(3):
                PB.mm(banks[n][0][0:1, :], [(cTt[:, kc:kc + 1], adaR[rb][:, n * 512:(n + 1) * 512])],
                      ["cTt", "adaR%d" % rb], [banks[n][1]], start=(kc == 0), stop=(kc == 7))
        for n in range(3):
            sl = modrow[:, qd * 1536 + n * 512: qd * 1536 + (n + 1) * 512]
            PB.tt(sl, banks[n][0][0:1, :], sl, ALU.add, [banks[n][1], "modrow"], ["modrow"])
    PB.stt(arow[:, 0:D], modrow[:, D:2 * D], 1.0, grow[:, 0:D], ALU.add, ALU.mult, ["modrow", "grow"], ["arow"])
    PB.stt(arow[:, D:2 * D], modrow[:, 4 * D:5 * D], 1.0, grow[:, D:2 * D], ALU.add, ALU.mult, ["modrow", "grow"], ["arow"])
    pc, pcn = nxt()
    vecs = [arow[:, 0:D], modrow[:, 0:D], arow[:, D:2 * D], modrow[:, 3 * D:4 * D]]
    for vi, vec in enumerate(vecs):
        for fc in range(8):
            PB.mm(pc[:, vi * 8 + fc: vi * 8 + fc + 1], [(vec[0:1, fc * 128:(fc + 1) * 128], ones_row[0:1, 0:1])],
                  ["arow", "modrow"], [pcn])
    PB.cp(cols[:], pc[:, 0:32], [pcn], ["cols"])
    for gi, (gsrc, gdst, gname) in enumerate([(modrow[:, 2 * D:3 * D], gate1b, "gate1b"), (modrow[:, 5 * D:6 * D], gate2b, "gate2b"),
                                              ]):
        for h in range(2):
            pb_, pbn = nxt()
            PB.mm(pb_[:, :], [(ones_row[0:1, :], gsrc[0:1, h * 512:(h + 1) * 512])], ["modrow", "arow"], [pbn])
            PB.cp(gdst[:, h * 512:(h + 1) * 512], pb_[:, :], [pbn], [gname])
    PB.dma("sp", modscr_d[0:1, :], arow[:, D:2 * D], reads=["arow"], writes=["modscr0"], slot="ms", group=True)
    PB.dma("sp", modscr_d[1:2, :], modrow[:, 3 * D:4 * D], reads=["modrow"], writes=["modscr1"], slot="ms", group=True)
    PB.barrier()
    if stage == "B":
        dbg_out(PB, "cols", cols[:], [128, 32], F32)
        dbg_out(PB, "gate1b", gate1b[:], [128, D], F32)
        dbg_out(PB, "gate2b", gate2b[:], [128, D], F32)
        PB.barrier()
        PB.emit()
        return nc
    PB.emit()
    nc.all_engine_barrier()
    sB.close()

    PM = Prog(nc, top, "M")
    w_in_bf = sbuf(p1, "w_in_bf", [128, 8, 3584], BF16)
    glu_bf = sbuf(p1, "glu_bf", [128, 4, 512], BF16)
    wa_bf = sbuf(p1, "wa_bf", [128, 4, D], BF16)
    wb_bf = sbuf(p1, "wb_bf", [128, 4, D], BF16)
    wout_bf = sbuf(p1, "wout_bf", [128, 8, D], BF16)
    wsT_bf = sbuf(p1, "wsT_bf", [128, 8, 128], BF16)
    lngb = sbuf(p1, "lngb", [128, 512], F32)
    lnbb = sbuf(p1, "lnbb", [128, 512], F32)
    bs_row = sbuf(p1, "bs_row", [1, 1024], F32)
    xt = [sbuf(p1, "xt%d" % i, [128, D], F32) for i in range(1)]
    xn = sbuf(p1, "xn", [128, D], BF16)
    st = sbuf(p1, "st", [128, 16], F32)
    hT = sbuf(p1, "hT", [128, 8, ST], BF16)
    us32 = sbuf(p1, "us32", [128, 4, ST], F32)
    usbf = sbuf(p1, "usbf", [128, 4, ST], BF16)
    guT = sbuf(p1, "guT", [128, 4, ST], BF16)
    gv = sbuf(p1, "gv", [128, 512], F32)
    gv2 = sbuf(p1, "gv2", [128, 512], F32)
    vn = sbuf(p1, "vn", [128, 2, 512], BF16)
    xf = sbuf(p1, "xf", [128, D], F32)
    sga = sbuf(p1, "sga", [128, 8, ST], BF16)
    sgb = sbuf(p1, "sgb", [128, 8, ST], BF16)
    zT = sbuf(p1, "zT", [128, 4, ST], BF16)
    sg = sbuf(p1, "sg", [128, 4, ST], BF16)
    oT = sbuf(p1, "oT", [128, 4, ST], BF16)
    pT = sbuf(p1, "pT", [128, 4, ST], BF16)
    ta = sbuf(p1, "ta", [128, 2, ST], F32)
    tb = sbuf(p1, "tb", [128, 2, ST], F32)
    mT = sbuf(p1, "mT", [128, 8, ST], BF16)
    t1 = sbuf(p1, "t1", [128, ST], F32)
    t2 = sbuf(p1, "t2", [128, ST], F32)
    wri = sbuf(p1, "wri", [128, 2, ST], F32)
    qri = sbuf(p1, "qri", [128, 2, ST], F32)
    s32 = sbuf(p1, "s32", [128, 2, ST], F32)
    sT = sbuf(p1, "sT", [128, 2, ST], BF16)
    yv = sbuf(p1, "yv", [128, ST], F32)

    for kc in range(8):
        PM.dma("pool", w_in_bf[:, kc, :], w_in_d[kc * 128:(kc + 1) * 128, :], writes=["w_in%d" % kc], slot="wl", group=True)
    PM.dma("pool", glu_bf[:], glu_w_d.rearrange("(c p) n -> p c n", p=128), writes=["glu"], slot="wl", group=True)
    PM.dma("pool", wa_bf[:], wa_d.rearrange("(c p) n -> p c n", p=128), writes=["wa"], slot="wl", group=True)
    PM.dma("pool", wb_bf[:], wb_d.rearrange("(c p) n -> p c n", p=128), writes=["wb"], slot="wl", group=True)
    PM.dma("pool", wout_bf[:], wout_d.rearrange("(c p) n -> p c n", p=128), writes=["wout"], slot="wl", group=True)
    PM.dma("sp", lngb[:], lng_d.partition_broadcast(128), writes=["lngb"], slot="wl2", group=True)
    PM.dma("sp", lnbb[:], lnb_d.partition_broadcast(128), writes=["lnbb"], slot="wl2", group=True)
    PM.dma("sp", bs_row[:], bs_d, writes=["bs_row"], slot="wl2", group=True)
    wsst = us32[:].rearrange("p a n -> p (a n)").rearrange("p (h t) -> p h t", h=8)
    PM.dma("sp", wsst, wsT_d, writes=["us32"], slot="wl2", group=True)
    PM.dma("sp", gv[:, 0:128], tri_d, writes=["gv"], slot="wl2", group=True)
    PM.tt(wsT_bf[:], wsst, gv[:, 0:128].unsqueeze(1).to_broadcast([128, 8, 128]), ALU.mult, ["us32", "gv"], ["wsT_bf"])
    W_IN = ["w_in%d" % kc for kc in range(8)]

    def pairview(ps_):
        return ps_[:, :].rearrange("p (a n) -> p a n", a=2)

    xq = [0]
    rot["n"] = 5
    for s in range(NST):
        def front_norm_tile(ss, i):
            if True:
                tix = 2 * ss + i
                xb = xf
                xbn = "xf"
                PM.dma("sp", xb[:], x_d[tix * 128:(tix + 1) * 128, :], writes=[xbn], slot=xbn)
                PM.memset(st[:, 0:1], 0.0, ["st0"])
                PM.act(xn[:], xb[:], AF.Square, [xbn, "st0"], ["xn", "st0"], accum=st[:, 0:1])
                PM.act(st[:, 1:2], st[:, 0:1], AF.Sqrt, ["st0"], ["st1"], bias=epsc[:, 0:1], scale=1.0 / D)
                PM.op("dve", lambda e: e.reciprocal(st[:, 2:3], st[:, 1:2]), ["st1"], ["st2"])
                PM.ts(xn[:], xb[:], st[:, 2:3], ALU.mult, [xbn, "st2"], ["xn"])
                pt_, ptn = nxt_t()
                for fc in range(8):
                    PM.tr(pt_[:, fc * 128:(fc + 1) * 128], xn[:, fc * 128:(fc + 1) * 128], ident_bf[:], ["xn"], [ptn])
                for fc in range(8):
                    PM.act(hT[:, fc, i * 128:(i + 1) * 128], pt_[:, fc * 128:(fc + 1) * 128], AF.Identity,
                           [ptn], ["hT"], bias=s1c[:, fc:fc + 1], scale=a1c[:, fc:fc + 1])

        def proj_u():
            for pr in range(2):
                ps_, psn = nxt()
                for a in range(2):
                    cc = 2 * pr + a
                    PM.mm(ps_[:, a * ST:(a + 1) * ST], [(w_in_bf[:, kc, cc * 128:(cc + 1) * 128], hT[:, kc, :]) for kc in range(8)],
                          W_IN + ["hT"], [psn])
                PM.act(us32[:, 2 * pr:2 * pr + 2, :], pairview(ps_), AF.Copy, [psn], ["us32"])
            PM.cp(usbf[:], us32[:], ["us32"], ["usbf"], eng="pool")

        def proj_zu():
            for pr in range(2):
                ps_, psn = nxt()
                for a in range(2):
                    cc = 2 * pr + a
                    PM.mm(ps_[:, a * ST:(a + 1) * ST], [(w_in_bf[:, kc, 512 + cc * 128:512 + (cc + 1) * 128], hT[:, kc, :]) for kc in range(8)],
                          W_IN + ["hT"], [psn])
                PM.act(guT[:, 2 * pr:2 * pr + 2, :], pairview(ps_), AF.Gelu_apprx_tanh, [psn], ["guT"])

        def proj_v(i):
            ps_, psn = nxt()
            PM.mm(ps_[:, :], [(hT[:, kc, i * 128:(i + 1) * 128], w_in_bf[:, kc, 1024:1536]) for kc in range(8)],
                  W_IN + ["hT"], [psn])
            PM.memset(st[:, 4:6], 0.0, ["st4"])
            PM.act(gv[:], ps_[:, :], AF.Gelu_apprx_tanh, [psn, "st4"], ["gv", "st4"], accum=st[:, 4:5])
            PM.act(gv2[:], gv[:], AF.Square, ["gv", "st4"], ["gv2", "st4"], accum=st[:, 5:6])
            PM.ts(st[:, 6:7], st[:, 4:5], 1.0 / 512, ALU.mult, ["st4"], ["st6"])
            PM.tt(st[:, 7:8], st[:, 6:7], st[:, 6:7], ALU.mult, ["st6"], ["st7"])
            PM.stt(st[:, 8:9], st[:, 5:6], 1.0 / 512, st[:, 7:8], ALU.mult, ALU.subtract, ["st4", "st7"], ["st8"])
            PM.act(st[:, 9:10], st[:, 8:9], AF.Sqrt, ["st8"], ["st9"], bias=epsc[:, 0:1], scale=1.0)
            PM.op("dve", lambda e: e.reciprocal(st[:, 10:11], st[:, 9:10]), ["st9"], ["st10"])
            PM.stt(st[:, 11:12], st[:, 6:7], -1.0, st[:, 10:11], ALU.mult, ALU.mult, ["st6", "st10"], ["st11"])
            PM.act(gv2[:], gv[:], AF.Identity, ["gv", "st10", "st11"], ["gv2"], bias=st[:, 11:12], scale=st[:, 10:11])
            PM.tt(gv[:], gv2[:], lngb[:], ALU.mult, ["gv2", "lngb"], ["gv"])
            PM.tt(vn[:, i, :], gv[:], lnbb[:], ALU.add, ["gv", "lnbb"], ["vn"])

        def mix(cc):
            ps_, psn = nxt()
            for i in range(2):
                for hh in range(2):
                    h = 2 * cc + hh
                    o_ = ps_[hh * 64:(hh + 1) * 64, i * 128:(i + 1) * 128]

                    def emit(pe, o_=o_, i=i, cc=cc, hh=hh, h=h):
                        pe.matmul(o_, vn[:, i, cc * 128 + hh * 64: cc * 128 + (hh + 1) * 64], wsT_bf[:, h, :], start=True, stop=False)
                        return pe.matmul(o_, ones_row[0:1, 0:64], bs_row[0:1, h * 128:(h + 1) * 128], start=False, stop=True)

                    PM.op("pe", emit, ["vn", "wsT_bf", "bs_row"], [psn])
            PM.tt(pT[:, cc, :], guT[:, cc, :], ps_[:, 0:ST], ALU.mult, ["guT", psn], ["pT"])

        def gates(pr):
            pga, pgan = nxt()
            pgb, pgbn = nxt()
            for a in range(2):
                dc = 2 * pr + a
                sl = slice(a * ST, (a + 1) * ST)
                PM.mm(pga[:, sl], [(w_in_bf[:, kc, 1536 + dc * 128:1536 + (dc + 1) * 128], hT[:, kc, :]) for kc in range(8)], W_IN + ["hT"], [pgan])
                PM.mm(pgb[:, sl], [(w_in_bf[:, kc, 2560 + dc * 128:2560 + (dc + 1) * 128], hT[:, kc, :]) for kc in range(8)], W_IN + ["hT"], [pgbn])
            PM.act(sga[:, 2 * pr:2 * pr + 2, :], pairview(pga), AF.Sigmoid, [pgan], ["sga"])
            PM.act(sgb[:, 2 * pr:2 * pr + 2, :], pairview(pgb), AF.Sigmoid, [pgbn], ["sgb"])

        bu_banks = {}

        def bu(k):
            cc = k // 4
            pb_, pbn = nxt()
            PM.mm(pb_[:, 0:ST], [(BT_bf[:, k, 0, :], usbf[:, cc, :])], ["usbf"], [pbn])
            PM.mm(pb_[:, ST:2 * ST], [(BT_bf[:, k, 1, :], usbf[:, cc, :])], ["usbf"], [pbn])
            bu_banks[k] = (pb_, pbn)

        def ssm_dve(k):
            pb_, pbn = bu_banks[k]
            bre, bim = pb_[:, 0:ST], pb_[:, ST:2 * ST]
            ck, sk = cosT[:, k, :], sinT[:, k, :]
            PM.tt(t1[:], bre, ck, ALU.mult, [pbn], ["t1"])
            PM.tt(t2[:], bim, sk, ALU.mult, [pbn], ["t2"])
            PM.tt(wri[:, 0, :], t1[:], t2[:], ALU.add, ["t1", "t2"], ["wri0"])
            PM.tt(t1[:], bim, ck, ALU.mult, [pbn], ["t1"])
            PM.tt(t2[:], bre, sk, ALU.mult, [pbn], ["t2"])
            PM.tt(wri[:, 1, :], t1[:], t2[:], ALU.subtract, ["t1", "t2"], ["wri1"])
            rb_ = rcol[:, k:k + 1].to_broadcast([128, ST])
            PM.scan(qri[:, 0, :], rb_, wri[:, 0, :], carry[:, k, 0:1], ["wri0", "carry%d" % k], ["qri0"])
            PM.scan(qri[:, 1, :], rb_, wri[:, 1, :], carry[:, k, 1:2], ["wri1", "carry%d" % k], ["qri1"])
            PM.tt(t1[:], qri[:, 0, :], ck, ALU.mult, ["qri0"], ["t1"])
            PM.tt(t2[:], qri[:, 1, :], sk, ALU.mult, ["qri1"], ["t2"])
            PM.tt(s32[:, 0, :], t1[:], t2[:], ALU.subtract, ["t1", "t2"], ["s32a"])
            PM.tt(t1[:], qri[:, 1, :], ck, ALU.mult, ["qri1"], ["t1"])
            PM.tt(t2[:], qri[:, 0, :], sk, ALU.mult, ["qri0"], ["t2"])
            PM.tt(s32[:, 1, :], t1[:], t2[:], ALU.add, ["t1", "t2"], ["s32b"])
            PM.act(sT[:], s32[:], AF.Copy, ["s32a", "s32b"], ["sT"])
            PM.cp(carry[:, k, :], s32[:, :, ST - 1], ["s32a", "s32b"], ["carry%d" % k], eng="pool")

        def ssm_c(k):
            cc = k // 4
            yps, ypn = psf[5], "psf5"
            PM.mm(yps[:, 0:ST], [(CT_bf[:, k, 0, :], sT[:, 0, :]), (CT_bf[:, k, 1, :], sT[:, 1, :])], ["sT"], [ypn],
                  start=(k % 4 == 0), stop=(k % 4 == 3))
            if k % 4 == 3:
                PM.stt(yv[:], us32[:, cc, :], dcol[:, cc:cc + 1], yps[:, 0:ST], ALU.mult, ALU.add, ["us32", ypn], ["yv"])
                PM.act(zT[:, cc, :], yv[:], AF.Gelu_apprx_tanh, ["yv"], ["zT"])

        extras = {0: proj_zu, 1: (lambda: proj_v(0)), 2: (lambda: proj_v(1)),
                  4: (lambda: mix(0)), 5: (lambda: mix(1)), 6: (lambda: mix(2)), 7: (lambda: mix(3)),
                  8: (lambda: gates(0)), 9: (lambda: gates(1)), 10: (lambda: gates(2)), 11: (lambda: gates(3))}
        if s == 0:
            front_norm_tile(0, 0)
            front_norm_tile(0, 1)
        if s + 1 < NST:
            extras[12] = (lambda: front_norm_tile(s + 1, 0))
            extras[13] = (lambda: front_norm_tile(s + 1, 1))
        proj_u()
        bu(0)
        for k in range(16):
            if k + 1 < 16:
                bu(k + 1)
            ssm_dve(k)
            if k in extras:
                extras[k]()
            ssm_c(k)
        for pr in range(2):
            ps_, psn = nxt()
            for a in range(2):
                n = 2 * pr + a
                PM.mm(ps_[:, a * ST:(a + 1) * ST], [(glu_bf[:, cc, n * 128:(n + 1) * 128], zT[:, cc, :]) for cc in range(4)],
                      ["glu", "zT"], [psn])
                PM.act(sg[:, n, :], ps_[:, a * ST:(a + 1) * ST], AF.Sigmoid, [psn], ["sg"], bias=glubc[:, n:n + 1], scale=1.0)
        PM.tt(oT[:], zT[:], sg[:], ALU.mult, ["zT", "sg"], ["oT"])
        for pr in range(4):
            pya, pyan = nxt()
            pyb, pybn = nxt()
            for a in range(2):
                dc = 2 * pr + a
                sl = slice(a * ST, (a + 1) * ST)
                PM.mm(pya[:, sl], [(wa_bf[:, cc, dc * 128:(dc + 1) * 128], oT[:, cc, :]) for cc in range(4)], ["wa", "oT"], [pyan])
                PM.mm(pyb[:, sl], [(wb_bf[:, cc, dc * 128:(dc + 1) * 128], pT[:, cc, :]) for cc in range(4)], ["wb", "pT"], [pybn])
            PM.tt(ta[:], sga[:, 2 * pr:2 * pr + 2, :], pairview(pya), ALU.mult, ["sga", pyan], ["ta"])
            PM.tt(tb[:], sgb[:, 2 * pr:2 * pr + 2, :], pairview(pyb), ALU.mult, ["sgb", pybn], ["tb"])
            PM.tt(mT[:, 2 * pr:2 * pr + 2, :], ta[:], tb[:], ALU.add, ["ta", "tb"], ["mT"])
        for i in range(2):
            tix = 2 * s + i
            xb = xt[0]
            xbn = "xt0"
            PM.dma("sp", xb[:], x_d[tix * 128:(tix + 1) * 128, :], writes=[xbn], slot=xbn)
            for dh in range(2):
                ps_, psn = nxt()
                PM.mm(ps_[:, :], [(mT[:, kc, i * 128:(i + 1) * 128], wout_bf[:, kc, dh * 512:(dh + 1) * 512]) for kc in range(8)],
                      ["mT", "wout"], [psn])
                PM.tt(gv2[:], ps_[:, :], gate1b[:, dh * 512:(dh + 1) * 512], ALU.mult, [psn], ["gv2"])
                PM.tt(xb[:, dh * 512:(dh + 1) * 512], gv2[:], xb[:, dh * 512:(dh + 1) * 512], ALU.add, ["gv2", xbn], [xbn])
            PM.dma("sp", x1s_d[tix * 128:(tix + 1) * 128, :], xb[:], reads=[xbn], writes=["x1s%d" % tix], slot=xbn)
    rot["n"] = 6
    PM.barrier()
    PM.emit()
    if stage == "M":
        return nc
    nc.all_engine_barrier()
    p1.close()

    p2 = ExitStack()
    PX = Prog(nc, top, "X")
    PX.regs = regs
    UW = 256
    NU = T // UW
    x1 = sbuf(p2, "x1", [128, NT, D], F32)
    h2tm = sbuf(p2, "h2tm", [128, NT, D], BF16)
    bgc = sbuf(p2, "bgc", [128, NE, 16], F32)
    posm = sbuf(p2, "posm", [128, NT, NE], F32)
    rwhl = sbuf(p2, "rwhl", [128, NT, NE, 2], BF16)
    iota = sbuf(p2, "iota", [128, UW], F32)
    ne_i = sbuf(p2, "ne_i", [1, NE], I32)
    p2r = ExitStack()
    rwb_bf = sbuf(p2r, "rwb_bf", [128, 8, NE], BF16)
    rbb = sbuf(p2r, "rbb", [128, NE], F32)
    bo_g = sbuf(p2r, "bo_g", [NE, D], F32)
    maskf = sbuf(p2r, "maskf", [128, NT, NE], F32)
    maskb = sbuf(p2r, "maskb", [128, NT, NE], BF16)
    rw = sbuf(p2r, "rw", [128, NT, NE], F32)
    stri_bf = sbuf(p2r, "stri_bf", [128, 128], BF16)
    ones_bf = sbuf(p2r, "ones_bf", [128, 128], BF16)
    cnt = sbuf(p2r, "cnt", [1, 3, NE], F32)
    acc2_ = [sbuf(p2r, "acc2_%d" % i, [128, D], F32) for i in range(2)]
    xn2_ = [sbuf(p2r, "xn2_%d" % i, [128, D], BF16) for i in range(2)]
    h2Tt_ = [sbuf(p2r, "h2Tt_%d" % i, [128, 8, 128], BF16) for i in range(2)]
    rwT_ = [sbuf(p2r, "rwT_%d" % i, [NE, 128], F32) for i in range(2)]
    rt_ = [sbuf(p2r, "rt_%d" % i, [128, 4, NE], F32) for i in range(2)]
    rs_ = [sbuf(p2r, "rs_%d" % i, [128, 24], F32) for i in range(2)]
    rtp = sbuf(p2r, "rtp", [128, 4, NE], F32)
    a2b = sbuf(p2r, "a2b", [128, D], F32)
    s2b = sbuf(p2r, "s2b", [128, D], F32)
    ldf = sbuf(p2r, "ldf", [128, 256], F32)

    PX.dma("sp", rbb[:], rb_d.partition_broadcast(128), writes=["rbb"], slot="ld", group=True)
    PX.dma("sp", a2b[:], modscr_d[0:1, :].partition_broadcast(128), writes=["a2b"], slot="ld", group=True)
    PX.dma("sp", s2b[:], modscr_d[1:2, :].partition_broadcast(128), writes=["s2b"], slot="ld", group=True)
    PX.dma("sp", bgc[:], mbi_d, writes=["bgc"], slot="ld", group=True)
    PX.dma("sp", bo_g[:], mbo_d, writes=["bo_g"], slot="ld", group=True)
    PX.dma("sp", iota[:], iota_d, writes=["iota"], slot="ld", group=True)
    PX.dma("sp", ldf[:, 0:128], stri_d, writes=["ldf"], slot="ld", group=True)
    PX.dma("pool", rwb_bf[:], rw_d.rearrange("(c p) e -> p c e", p=128), writes=["rwb"], slot="ldp")
    PX.cp(stri_bf[:], ldf[:, 0:128], ["ldf"], ["stri_bf"])
    PX.memset(ones_bf[:], 1.0, ["ones_bf"])
    PX.ts(bgc[:, :, 8:16], bgc[:, :, 8:16], 1.0, ALU.add, ["bgc"], ["bgc"])
    PX.tt(bo_g[:], bo_g[:], gate2b[0:NE, :], ALU.mult, ["bo_g"], ["bo_g"])

    for tix in range(NT):
        pb2 = tix % 2
        rs, rt, rwT, h2Tt, xn2, acc2 = rs_[pb2], rt_[pb2], rwT_[pb2], h2Tt_[pb2], xn2_[pb2], acc2_[pb2]
        xb = x1[:, tix, :]
        xbn = "x1_%d" % tix
        PX.dma("sp", xb, x1s_d[tix * 128:(tix + 1) * 128, :], writes=[xbn], slot="x1l%d" % tix)
        PX.memset(rs[:, 0:1], 0.0, ["rs0_%d" % pb2])
        PX.act(xn2[:], xb, AF.Square, [xbn, "rs0_%d" % pb2], ["xn2_%d" % pb2, "rs0_%d" % pb2], accum=rs[:, 0:1])
        PX.act(rs[:, 1:2], rs[:, 0:1], AF.Sqrt, ["rs0_%d" % pb2], ["rs1_%d" % pb2], bias=epsc[:, 0:1], scale=1.0 / D)
        PX.op("dve", lambda e, rs=rs: e.reciprocal(rs[:, 2:3], rs[:, 1:2]), ["rs1_%d" % pb2], ["rs2_%d" % pb2])
        PX.stt(acc2[:], xb, rs[:, 2:3], a2b[:], ALU.mult, ALU.mult, [xbn, "rs2_%d" % pb2], ["acc2_%d" % pb2])
        PX.tt(h2tm[:, tix, :], acc2[:], s2b[:], ALU.add, ["acc2_%d" % pb2], ["h2tm%d" % tix])
        pt_, ptn = nxt_t()
        for fc in range(8):
            PX.tr(pt_[:, fc * 128:(fc + 1) * 128], h2tm[:, tix, fc * 128:(fc + 1) * 128], ident_bf[:], ["h2tm%d" % tix], [ptn])
        PX.act(h2Tt[:].rearrange("p a b -> p (a b)"), pt_[:, :], AF.Copy, [ptn], ["h2Tt_%d" % pb2])
        pl, pln = nxt()
        PX.mm(pl[:, 0:NE], [(h2Tt[:, kc, :], rwb_bf[:, kc, :]) for kc in range(8)], ["h2Tt_%d" % pb2, "rwb"], [pln])
        lg, ex = rt[:, 0, :], rt[:, 2, :]
        PX.tt(lg, pl[:, 0:NE], rbb[:], ALU.add, [pln, "rbb"], ["lg_%d" % pb2])
        PX.op("dve", lambda e, lg=lg, rs=rs: e.max(rs[:, 4:12], lg), ["lg_%d" % pb2], ["rs4_%d" % pb2])
        PX.op("dve", lambda e, lg=lg, tix=tix, rs=rs: e.tensor_single_scalar(maskf[:, tix, :], lg, rs[:, 7:8], ALU.is_ge), ["lg_%d" % pb2, "rs4_%d" % pb2], ["maskf%d" % tix])
        PX.ts(rs[:, 12:13], rs[:, 4:5], -1.0, ALU.mult, ["rs4_%d" % pb2], ["rs12_%d" % pb2])
        PX.act(ex, lg, AF.Exp, ["lg_%d" % pb2, "rs12_%d" % pb2], ["ex_%d" % pb2], bias=rs[:, 12:13], scale=1.0)
        PX.tt(ex, ex, maskf[:, tix, :], ALU.mult, ["ex_%d" % pb2, "maskf%d" % tix], ["ex_%d" % pb2])
        PX.op("dve", lambda e, ex=ex, rs=rs: e.reduce_sum(rs[:, 13:14], ex, mybir.AxisListType.X), ["ex_%d" % pb2], ["rs13_%d" % pb2])
        PX.op("dve", lambda e, rs=rs: e.reciprocal(rs[:, 14:15], rs[:, 13:14]), ["rs13_%d" % pb2], ["rs14_%d" % pb2])
        PX.ts(rw[:, tix, :], ex, rs[:, 14:15], ALU.mult, ["ex_%d" % pb2, "rs14_%d" % pb2], ["rw%d" % tix])
        PX.cp(maskb[:, tix, :], maskf[:, tix, :], ["maskf%d" % tix], ["maskb%d" % tix])
        pr_, prn = nxt()
        PX.op("pe", lambda e, pr_=pr_, tix=tix: e.transpose(pr_[0:NE, 0:128], rw[:, tix, :], ident_f[:]), ["rw%d" % tix], [prn])
        PX.cp(rwT[:], pr_[0:NE, 0:128], [prn], ["rwT_%d" % pb2])
        for dh in range(2):
            pb_, pbn = nxt()
            PX.mm(pb_[:, :], [(rwT[:], bo_g[:, dh * 512:(dh + 1) * 512])], ["rwT_%d" % pb2, "bo_g"], [pbn])
            PX.tt(x1[:, tix, dh * 512:(dh + 1) * 512], pb_[:, :], x1[:, tix, dh * 512:(dh + 1) * 512], ALU.add, [pbn, xbn], [xbn])
        PX.cp(rwhl[:, tix, :, 0], rw[:, tix, :], ["rw%d" % tix], ["rwhl%d" % tix])
        PX.cp(rt[:, 1, :], rwhl[:, tix, :, 0], ["rwhl%d" % tix], ["rt1_%d" % pb2])
        PX.tt(rt[:, 1, :], rw[:, tix, :], rt[:, 1, :], ALU.subtract, ["rw%d" % tix, "rt1_%d" % pb2], ["rt1_%d" % pb2])
        PX.cp(rwhl[:, tix, :, 1], rt[:, 1, :], ["rt1_%d" % pb2], ["rwhl%d" % tix])

    rt = rtp

    MB = ["maskb%d" % t for t in range(NT)]
    for tix in range(NT):
        pp_, ppn = nxt()
        prs = [(ones_bf[:], maskb[:, t2, :]) for t2 in range(tix)] + [(stri_bf[:], maskb[:, tix, :])]
        PX.mm(pp_[:, 0:NE], prs, MB[:tix + 1] + ["ones_bf", "stri_bf"], [ppn])
        PX.ts(rt[:, 1, :], maskf[:, tix, :], -1.0, ALU.add, ["maskf%d" % tix], ["rt1"], s2=1.0e6, op1=ALU.mult)
        PX.tt(rt[:, 3, :], pp_[:, 0:NE], maskf[:, tix, :], ALU.mult, [ppn, "maskf%d" % tix], ["rt3"])
        PX.tt(posm[:, tix, :], rt[:, 3, :], rt[:, 1, :], ALU.add, ["rt3", "rt1"], ["posm"])
    pc_, pcn_ = nxt()
    PX.mm(pc_[0:1, 0:NE], [(ones_bf[:, 0:1], maskb[:, t2, :]) for t2 in range(NT)], MB + ["ones_bf"], [pcn_])
    PX.cp(cnt[:, 0, :], pc_[0:1, 0:NE], [pcn_], ["cnt0"])
    PX.memset(cnt[:, 1, :], 0.0, ["cnt1"])
    for u in range(NU):
        PX.stt(cnt[:, 1, :], cnt[:, 0, :], float(UW * u), cnt[:, 1, :], ALU.is_gt, ALU.add, ["cnt0", "cnt1"], ["cnt1"])
    PX.cp(ne_i[:], cnt[:, 1, :], ["cnt1"], ["ne_i"])
    PX.barrier()
    if stage == "XP":
        dbg_out(PX, "posm", posm[:], [128, NT, NE], F32)
        dbg_out(PX, "rw", rw[:], [128, NT, NE], F32)
        dbg_out(PX, "cnt", cnt[:], [1, 3, NE], F32)
        dbg_out(PX, "x1b", x1[:], [128, NT, D], F32)
        PX.barrier()
        PX.emit()
        return nc
    PX.emit()
    nc.all_engine_barrier()
    p2r.close()

    PX = Prog(nc, top, "E")
    PX.regs = regs
    p2e = ExitStack()
    NWP = 5
    wq = [sbuf(p2e, "wq%d" % i, [128, 8, 512], BF16) for i in range(NWP)]
    wo = sbuf(p2e, "wo", [128, 8, D], BF16)
    SelAllb = [sbuf(p2e, "SelAll%d" % i, [128, NT, UW], BF16) for i in range(2)]
    SelT = sbuf(p2e, "SelT", [128, 2, T], BF16)
    pmub = [sbuf(p2e, "pmu%d" % i, [128, NT], F32) for i in range(2)]
    h2g = sbuf(p2e, "h2g", [128, 8, UW], BF16)
    actT = sbuf(p2e, "actT", [128, 8, UW], BF16)
    g_sb = sbuf(p2e, "g_sb", [128, 2, UW], BF16)
    sg2 = sbuf(p2e, "sg2", [128, 2, UW], BF16)
    u_sb = sbuf(p2e, "u_sb", [128, 2, UW], BF16)
    pp = sbuf(p2e, "pp", [128, 2, UW], BF16)
    yw = sbuf(p2e, "yw", [128, 2, D], BF16)
    rws = sbuf(p2e, "rws", [128, 2], F32)
    rws4 = sbuf(p2e, "rws4", [128, 4], F32)

    NEX = NE

    def piece_slot(e, q):
        return (4 * e + q) % NWP

    def load_piece(e, q):
        sl = piece_slot(e, q)
        src = mwi_d[e].rearrange("(c p) n -> p c n", p=128)
        PX.dma("pool", wq[sl][:, :, 0:256], src[:, :, 256 * q:256 * (q + 1)], writes=["wg%d" % sl], slot="wg%d" % sl)
        PX.dma("pool", wq[sl][:, :, 256:512], src[:, :, D + 256 * q:D + 256 * (q + 1)], writes=["wu%d" % sl], slot="wu%d" % sl)

    def load_wo(e):
        PX.dma("pool", wo[:], mwo_d[e].rearrange("(c p) n -> p c n", p=128), writes=["wo"], slot="wo")

    for q in range(4):
        load_piece(0, q)

    def build_sel(e, u):
        pb = e % 2
        PX.ts(pmub[pb][:], posm[:, :, e], float(-UW * u), ALU.add, ["posm"], ["pmu%d" % pb])
        for t2 in range(NT):
            PX.op("dve", lambda eng, t2=t2, pb=pb: eng.tensor_single_scalar(SelAllb[pb][:, t2, :], iota[:], pmub[pb][:, t2:t2 + 1], ALU.is_equal),
                  ["pmu%d" % pb, "iota"], ["Sel%d_%d" % (pb, t2)])

    build_sel(0, 0)
    H2 = ["h2tm%d" % t for t in range(NT)]
    X1 = ["x1_%d" % t for t in range(NT)]
    for e in range(NEX):
        if e + 1 < NE:
            load_piece(e + 1, 0)
        load_wo(e)
        PX.regload(regs, ne_i[0:1, e:e + 1], ["ne_i"])
        if e + 1 < NEX:
            build_sel(e + 1, 0)
        for u in range(NU):
            PX.marker(("rb", u + 1))
            SelAll = SelAllb[e % 2]
            if u >= 1:
                build_sel(e, u)
            SEL = ["Sel%d_%d" % (e % 2, t2) for t2 in range(NT)]
            for fp in range(4):
                ps_, psn = nxt()
                for a in range(2):
                    fc = 2 * fp + a
                    PX.mm(ps_[:, a * UW:(a + 1) * UW], [(h2tm[:, t2, fc * 128:(fc + 1) * 128], SelAll[:, t2, :]) for t2 in range(NT)],
                          H2 + SEL, [psn])
                PX.act(h2g[:, 2 * fp:2 * fp + 2, :], ps_[:, :].rearrange("p (a n) -> p a n", a=2), AF.Copy, [psn], ["h2g"])
            for st_ in range(2):
                for h8 in range(2):
                    pt_, ptn = nxt_t()
                    for i8 in range(8):
                        t2 = 8 * h8 + i8
                        PX.tr(pt_[:, i8 * 128:(i8 + 1) * 128], SelAll[:, t2, st_ * 128:(st_ + 1) * 128], ident_bf[:], ["Sel%d_%d" % (e % 2, t2)], [ptn])
                    PX.act(SelT[:, st_, h8 * 1024:(h8 + 1) * 1024], pt_[:, :], AF.Copy, [ptn], ["SelT"])
            for jp in range(4):
                sl = piece_slot(e, jp)
                pg_, pgn = nxt()
                pu_, pun = nxt()
                for a in range(2):
                    PX.mm(pg_[:, a * UW:(a + 1) * UW], [(wq[sl][:, kc, a * 128:(a + 1) * 128], h2g[:, kc, :]) for kc in range(8)], ["wg%d" % sl, "h2g"], [pgn])
                    PX.mm(pu_[:, a * UW:(a + 1) * UW], [(wq[sl][:, kc, 256 + a * 128:256 + (a + 1) * 128], h2g[:, kc, :]) for kc in range(8)], ["wu%d" % sl, "h2g"], [pun])
                for a in range(2):
                    j = 2 * jp + a
                    PX.ts(g_sb[:, a, :], pg_[:, a * UW:(a + 1) * UW], bgc[:, e, j:j + 1], ALU.add, [pgn, "bgc"], ["g_sb"], s2=7.0, op1=ALU.min)
                    PX.ts(u_sb[:, a, :], pu_[:, a * UW:(a + 1) * UW], bgc[:, e, 8 + j:9 + j], ALU.add, [pun, "bgc"], ["u_sb"], s2=8.0, op1=ALU.min)
                PX.act(sg2[:], g_sb[:], AF.Sigmoid, ["g_sb"], ["sg2"], scale=1.702)
                PX.tt(pp[:], g_sb[:], sg2[:], ALU.mult, ["g_sb", "sg2"], ["pp"])
                PX.stt(actT[:, 2 * jp:2 * jp + 2, :], u_sb[:], -6.0, pp[:], ALU.max, ALU.mult, ["u_sb", "pp"], ["actT%d" % jp])
            prw, prwn = nxt()
            for st_ in range(2):
                PX.mm(prw[:, 2 * st_:2 * st_ + 2], [(SelAll[:, t2, st_ * 128:(st_ + 1) * 128], rwhl[:, t2, e, :]) for t2 in range(NT)],
                      SEL + ["rwhl"], [prwn])
            PX.act(rws4[:], prw[:, 0:4], AF.Copy, [prwn], ["rws4"])
            prv = rws4[:].rearrange("p (s h) -> p s h", s=2)
            PX.tt(rws[:], prv[:, :, 0], prv[:, :, 1], ALU.add, ["rws4"], ["rws"])
            for st_ in range(2):
                for dh in range(2):
                    po, pon = nxt()
                    PX.mm(po[:, :], [(actT[:, jc, st_ * 128:(st_ + 1) * 128], wo[:, jc, dh * 512:(dh + 1) * 512]) for jc in range(8)],
                          ["actT%d" % jp for jp in range(4)] + ["wo"], [pon])
                    PX.stt(yw[:, st_, dh * 512:(dh + 1) * 512], po[:, :], rws[:, st_:st_ + 1], gate2b[:, dh * 512:(dh + 1) * 512],
                           ALU.mult, ALU.mult, [pon, "rws"], ["yw"])
            for t2 in range(NT):
                for dh in range(2):
                    po, pon = nxt()
                    PX.mm(po[:, :], [(SelT[:, st_, t2 * 128:(t2 + 1) * 128], yw[:, st_, dh * 512:(dh + 1) * 512]) for st_ in range(2)],
                          ["SelT", "yw"], [pon])
                    PX.tt(x1[:, t2, dh * 512:(dh + 1) * 512], po[:, :], x1[:, t2, dh * 512:(dh + 1) * 512], ALU.add, [pon, X1[t2]], [X1[t2]])
        for u in range(NU):
            PX.marker(("re",))
        if e + 1 < NE:
            for q in range(1, 4):
                load_piece(e + 1, q)
    PX.barrier()
    if stage == "XE":
        dbg_out(PX, "x2", x1[:], [128, NT, D], F32)
        PX.barrier()
        PX.emit()
        return nc
    PX.emit()
    nc.all_engine_barrier()
    p2e.close()

    PX = Prog(nc, top, "F")
    fgb = sbuf(p2, "fgb", [128, D], F32)
    xn2 = sbuf(p2, "xn2f", [128, D], BF16)
    rs = sbuf(p2, "rsf", [128, 8], F32)
    PX.dma("sp", fgb[:], fg_d.partition_broadcast(128), writes=["fgb"], slot="ld", group=True)
    for tix in range(NT):
        r_ = "x1_%d" % tix
        PX.memset(rs[:, 0:1], 0.0, ["rs0"])
        PX.act(xn2[:], x1[:, tix, :], AF.Square, [r_, "rs0"], ["xn2", "rs0"], accum=rs[:, 0:1])
        PX.act(rs[:, 1:2], rs[:, 0:1], AF.Sqrt, ["rs0"], ["rs1"], bias=epsc[:, 0:1], scale=1.0 / D)
        PX.op("dve", lambda e: e.reciprocal(rs[:, 2:3], rs[:, 1:2]), ["rs1"], ["rs2"])
        PX.stt(x1[:, tix, :], x1[:, tix, :], rs[:, 2:3], fgb[:], ALU.mult, ALU.mult, [r_, "rs2", "fgb"], [r_])
        PX.dma("sp", out_d[tix * 128:(tix + 1) * 128, :], x1[:, tix, :], reads=[r_], writes=["out%d" % tix], slot="outs", group=True)
    PX.barrier()
    PX.emit()
    p2.close()
    top.close()
    return nc


def _prep(inputs):
    f = lambda a: np.ascontiguousarray(np.asarray(a), dtype=np.float32)
    sh = {}
    sh["ada_w"] = f(inputs["ada_w"][0])
    sh["ada_b"] = f(inputs["ada_b"][0][None])
    sh["g1"] = f(inputs["norm1_g"][0][None])
    sh["g2"] = f(inputs["norm2_g"][0][None])
    sh["fg"] = f(np.asarray(inputs["final_g"])[None])
    sh["w_in"] = f(inputs["w_in"][0])
    a_re = np.asarray(inputs["ssm_a_re"][0])
    a_im = np.asarray(inputs["ssm_a_im"][0])
    ldt = np.broadcast_to(np.asarray(inputs["ssm_log_dt"][0])[:, None], (32, 64))
    smf = lambda v: np.asarray(v).reshape(16, 2, 64).transpose(1, 2, 0).reshape(128, 16)
    sh["ssm_sm"] = f(np.stack([smf(a_re), smf(a_im), smf(ldt)], 1))
    sh["ssm_lam"] = f(np.stack([a_re.reshape(-1), a_im.reshape(-1), np.ascontiguousarray(ldt).reshape(-1)]))
    b = [np.asarray(inputs["ssm_b_re"][0]), np.asarray(inputs["ssm_b_im"][0])]
    c = [np.asarray(inputs["ssm_c_re"][0]), np.asarray(inputs["ssm_c_im"][0])]
    Bz = np.zeros((2, 128, 16, 128), np.float32)
    Cz = np.zeros((2, 128, 16, 128), np.float32)
    for k in range(16):
        for g2 in range(2):
            g = 2 * k + g2
            g8 = g % 8
            for ri in range(2):
                Bz[ri, g8 * 16:(g8 + 1) * 16, k, g2 * 64:(g2 + 1) * 64] = b[ri][g].T
                Cz[ri, g2 * 64:(g2 + 1) * 64, k, g8 * 16:(g8 + 1) * 16] = c[ri][g].T
    sh["ssm_B"] = Bz.reshape(2, 128, 2048)
    sh["ssm_C"] = Cz.reshape(2, 128, 2048)
    sh["ssm_d"] = f(np.asarray(inputs["ssm_d"][0]).reshape(4, 128).T)
    sh["glu_w"] = f(inputs["ssm_glu_w"][0])
    sh["glu_b"] = f(np.asarray(inputs["ssm_glu_b"][0]).reshape(4, 128).T)
    sh["wa"] = f(inputs["w_branch_a"][0])
    sh["wb"] = f(inputs["w_branch_b"][0])
    sh["wout"] = f(inputs["w_out"][0])
    sh["lng"] = f(inputs["gmlp_ln_g"][0][None])
    sh["lnb"] = f(inputs["gmlp_ln_b"][0][None])
    sh["wsT"] = f(np.asarray(inputs["gmlp_ws"][0]).transpose(2, 0, 1))
    sh["bs"] = f(np.asarray(inputs["gmlp_bs"][0]).reshape(1, 1024))
    sh["router_w"] = f(inputs["router_w"][0])
    sh["router_b"] = f(inputs["router_b"][0][None])
    sh["moe_w_in"] = f(inputs["moe_w_in"][0])
    sh["moe_b_in"] = f(np.asarray(inputs["moe_b_in"][0]).reshape(32, 16, 128).transpose(2, 0, 1))
    sh["moe_w_out"] = f(inputs["moe_w_out"][0])
    sh["moe_b_out"] = f(inputs["moe_b_out"][0])
    sh["ident"] = np.eye(128, dtype=np.float32)
    sh["tri"] = np.triu(np.ones((128, 128), np.float32))
    sh["stri"] = np.triu(np.ones((128, 128), np.float32), 1)
    sh["iota256"] = np.ascontiguousarray(np.broadcast_to(np.arange(256, dtype=np.float32)[None], (128, 256)))
    sh["eoff"] = np.ascontiguousarray(np.broadcast_to((2048.0 * np.arange(32, dtype=np.float32) + 1.0)[None], (128, 32)))
    sh["jj"] = np.ascontiguousarray(np.broadcast_to(np.arange(1, ST + 1, dtype=np.float32)[None], (128, ST)))
    return sh


def kernel(**inputs):
    sh = _prep(inputs)
    x = np.asarray(inputs["x"], dtype=np.float32)
    c = np.asarray(inputs["c"], dtype=np.float32)
    in_maps = []
    for b in range(8):
        m = dict(sh)
        m["x"] = np.ascontiguousarray(x[b])
        m["cT"] = np.ascontiguousarray(c[b].reshape(8, 128).T)
        in_maps.append(m)
    nc = build()
    res = run_bass_kernel_spmd(nc, in_maps, core_ids=list(range(8)))
    return np.stack([np.asarray(r["out"], dtype=np.float32) for r in res.results], 0)
```

```python
import numpy as np
from contextlib import ExitStack
import concourse.bass as bass
import concourse.mybir as mybir
from concourse.bass_utils import run_bass_kernel_spmd

F32 = mybir.dt.float32
BF16 = mybir.dt.bfloat16
I32 = mybir.dt.int32
AF = mybir.ActivationFunctionType
ALU = mybir.AluOpType

T = 2048
D = 1024
NT = 16
ST = 256
NST = T // ST
NE = 32
EPS = 1e-6
PI = float(np.pi)
TWO_PI = float(2 * np.pi)


class Op:
    __slots__ = ("eng", "emit", "deps", "is_dma", "slot", "val", "signal", "mark")


class Prog:
    ENGS = ("pe", "act", "dve", "pool", "sp")

    def __init__(self, nc, es, tag):
        self.nc = nc
        self.es = es
        self.tag = tag
        self.q = {e: [] for e in self.ENGS}
        self.lastw = {}
        self.readers = {}
        self.slot_ops = {}
        self.group_slots = set()

    def _deps(self, reads, writes, op):
        deps = []
        for r in reads:
            w = self.lastw.get(r)
            if w is not None:
                deps.append(w)
        for r in writes:
            w = self.lastw.get(r)
            if w is not None:
                deps.append(w)
            deps.extend(self.readers.get(r, ()))
        for r in reads:
            self.readers.setdefault(r, []).append(op)
        for r in writes:
            self.lastw[r] = op
            self.readers[r] = []
        return [d for d in deps if d is not op]

    def op(self, eng, emit, reads=(), writes=()):
        o = Op()
        o.eng, o.emit, o.is_dma, o.slot, o.signal = eng, emit, False, None, False
        o.mark = None
        o.deps = self._deps(reads, writes, o)
        self.q[eng].append(o)
        return o

    def marker(self, mark):
        for e in self.ENGS:
            o = Op()
            o.eng, o.emit, o.is_dma, o.slot, o.signal, o.mark, o.deps = e, None, False, None, False, mark, []
            self.q[e].append(o)

    def regload(self, regs, ap, reads):
        for e in self.ENGS:
            self.op(e, (lambda eng, e=e: eng.reg_load(regs[e], ap)), reads, ())

    def dma(self, eng, out, in_, reads=(), writes=(), slot=None, group=False, custom=None):
        o = Op()
        o.eng, o.is_dma, o.slot, o.signal = eng, True, slot, True
        o.mark = None
        o.emit = (lambda e: e.dma_start(out=out, in_=in_)) if custom is None else custom
        o.deps = self._deps(reads, writes, o)
        if group:
            o.deps = [d for d in o.deps if not (d.is_dma and d.slot == slot)]
            self.group_slots.add(slot)
        self.q[eng].append(o)
        self.slot_ops.setdefault(slot, []).append(o)
        return o

    def barrier(self):
        pend = []
        for e in self.ENGS:
            for o in reversed(self.q[e]):
                if not o.is_dma and o.emit is not None:
                    pend.append(o)
                    break
        for s, ops in self.slot_ops.items():
            pend.append(ops[-1])
        for e in self.ENGS:
            o = Op()
            o.eng, o.emit, o.is_dma, o.slot, o.signal = e, None, False, None, False
            o.mark = None
            o.deps = list(pend)
            self.q[e].append(o)
        self.lastw = {}
        self.readers = {}

    def emit(self):
        nc = self.nc
        for e in self.ENGS:
            for o in self.q[e]:
                for d in o.deps:
                    d.signal = True
        esem = {}
        for e in self.ENGS:
            esem[e] = self.es.enter_context(nc.semaphore("s%s_%s" % (self.tag, e)))
            c = 0
            for o in self.q[e]:
                if o.is_dma or o.emit is None:
                    continue
                if o.signal:
                    c += 1
                    o.val = c
        ssem = {}
        for i, (s, ops) in enumerate(self.slot_ops.items()):
            ssem[s] = self.es.enter_context(nc.semaphore("d%s_%d" % (self.tag, i)))
            if s in self.group_slots:
                for o in ops:
                    o.val = 16 * len(ops)
            else:
                for j, o in enumerate(ops):
                    o.val = 16 * (j + 1)

        regs = getattr(self, "regs", None)

        def emit_q(e, eng):
            seen = {}

            def emit_one(o):
                need = {}
                for d in o.deps:
                    if d.is_dma:
                        sem = ssem[d.slot]
                    else:
                        if d.emit is None or (d.eng == "pe" and e == "pe"):
                            continue
                        sem = esem[d.eng]
                    k = id(sem)
                    if need.get(k, (None, 0))[1] < d.val:
                        need[k] = (sem, d.val)
                for k, (sem, v) in need.items():
                    if seen.get(k, 0) < v:
                        eng.wait_ge(sem, v)
                        seen[k] = v
                if o.emit is None:
                    return
                ins = o.emit(eng)
                if o.is_dma:
                    ins.then_inc(ssem[o.slot], 16)
                elif o.signal:
                    ins.then_inc(esem[e], 1)
                    cur[0] = o.val

            cur = [0]

            def emit_range(ops):
                i = 0
                while i < len(ops):
                    o = ops[i]
                    if o.mark is not None and o.mark[0] == "rb":
                        depth = 1
                        j = i + 1
                        while True:
                            m = ops[j].mark
                            if m is not None and m[0] == "rb":
                                depth += 1
                            elif m is not None and m[0] == "re":
                                depth -= 1
                                if depth == 0:
                                    break
                            j += 1
                        body = ops[i + 1:j]
                        real = [b for b in body if b.emit is not None]
                        if real:
                            c = sum(1 for b in real if (not b.is_dma) and b.signal)
                            dcnt = {}
                            for b in real:
                                if b.is_dma:
                                    dcnt[b.slot] = dcnt.get(b.slot, 0) + 1
                            saved = dict(seen)
                            cur_before = cur[0]
                            g = eng.If_lt(regs[e], o.mark[1])
                            g.__enter__()
                            if c > 0:
                                if cur_before > 0:
                                    eng.wait_ge(esem[e], cur_before)
                                eng.sem_inc(esem[e], c)
                            for sl, n in dcnt.items():
                                eng.sem_inc(ssem[sl], 16 * n)
                            g.__exit__(None, None, None)
                            g2 = eng.Else()
                            g2.__enter__()
                            emit_range(body)
                            g2.__exit__(None, None, None)
                            seen.clear()
                            seen.update(saved)
                            cur[0] = cur_before + c
                        i = j + 1
                        continue
                    if o.mark is None:
                        emit_one(o)
                    i += 1

            emit_range(self.q[e])

        with nc.Block() as block:

            @block.tensor
            def _(eng):
                emit_q("pe", eng)

            @block.scalar
            def _(eng):
                emit_q("act", eng)

            @block.vector
            def _(eng):
                emit_q("dve", eng)

            @block.gpsimd
            def _(eng):
                emit_q("pool", eng)

            @block.sync
            def _(eng):
                emit_q("sp", eng)

    def mm(self, out, pairs, r, w, start=True, stop=True):
        pairs = list(pairs)

        def emit(pe):
            n = len(pairs)
            ins = None
            for i, (l, rr) in enumerate(pairs):
                ins = pe.matmul(out, l, rr, start=(start and i == 0), stop=(stop and i == n - 1))
            return ins

        return self.op("pe", emit, r, w)

    def tr(self, out, in_, ident, r, w):
        return self.op("pe", lambda e: e.transpose(out, in_, ident), r, w)

    def tt(self, out, a, b, op, r, w, eng="dve"):
        return self.op(eng, lambda e: e.tensor_tensor(out, a, b, op), r, w)

    def ts(self, out, a, s1, op0, r, w, s2=None, op1=None, eng="dve"):
        if op1 is None:
            return self.op(eng, lambda e: e.tensor_scalar(out, a, s1, None, op0), r, w)
        return self.op(eng, lambda e: e.tensor_scalar(out, a, s1, s2, op0, op1), r, w)

    def stt(self, out, a, s, b, op0, op1, r, w, eng="dve"):
        return self.op(eng, lambda e: e.scalar_tensor_tensor(out, a, s, b, op0, op1), r, w)

    def cp(self, out, a, r, w, eng="dve"):
        return self.op(eng, lambda e: e.tensor_copy(out, a), r, w)

    def memset(self, out, v, w, eng="dve"):
        return self.op(eng, lambda e: e.memset(out, v), (), w)

    def act(self, out, a, func, r, w, bias=None, scale=None, accum=None):
        kw = {}
        if bias is not None:
            kw["bias"] = bias
        if scale is not None:
            kw["scale"] = scale
        if accum is not None:
            kw["accum_out"] = accum
        return self.op("act", lambda e: e.activation(out, a, func, **kw), r, w)

    def scan(self, out, d0, d1, init, r, w):
        return self.op("dve", lambda e: e.tensor_tensor_scan(out, d0, d1, init, ALU.mult, ALU.add), r, w)


def build(stage=None):
    nc = bass.Bass("TRN2", target_bir_lowering=False)
    top = ExitStack()

    def din(name, shape):
        return nc.dram_tensor(name, list(shape), F32, kind="ExternalInput").ap()

    x_d = din("x", [T, D])
    cT_d = din("cT", [128, 8])
    ada_w_d = din("ada_w", [D, 6 * D])
    ada_b_d = din("ada_b", [1, 6 * D])
    g1_d = din("g1", [1, D])
    g2_d = din("g2", [1, D])
    fg_d = din("fg", [1, D])
    w_in_d = din("w_in", [D, 3584])
    ssm_sm_d = din("ssm_sm", [128, 3, 16])
    ssm_lam_d = din("ssm_lam", [3, 2048])
    ssm_B_d = din("ssm_B", [2, 128, 2048])
    ssm_C_d = din("ssm_C", [2, 128, 2048])
    ssm_d_d = din("ssm_d", [128, 4])
    glu_w_d = din("glu_w", [512, 512])
    glu_b_d = din("glu_b", [128, 4])
    wa_d = din("wa", [512, D])
    wb_d = din("wb", [512, D])
    wout_d = din("wout", [D, D])
    lng_d = din("lng", [1, 512])
    lnb_d = din("lnb", [1, 512])
    wsT_d = din("wsT", [128, 8, 128])
    bs_d = din("bs", [1, 1024])
    rw_d = din("router_w", [D, NE])
    rb_d = din("router_b", [1, NE])
    mwi_d = din("moe_w_in", [NE, D, 2 * D])
    mbi_d = din("moe_b_in", [128, NE, 16])
    mwo_d = din("moe_w_out", [NE, D, D])
    mbo_d = din("moe_b_out", [NE, D])
    ident_d = din("ident", [128, 128])
    tri_d = din("tri", [128, 128])
    jj_d = din("jj", [128, ST])
    stri_d = din("stri", [128, 128])
    iota_d = din("iota256", [128, 256])
    eoff_d = din("eoff", [128, NE])
    out_d = nc.dram_tensor("out", [T, D], F32, kind="ExternalOutput").ap()
    x1s_d = nc.dram_tensor("x1s", [T, D], F32, kind=("ExternalOutput" if stage == "M" else "Internal")).ap()

    def dbg_out(Pg, name, ap, shape, dt=F32):
        o = nc.dram_tensor("dbg_" + name, list(shape), dt, kind="ExternalOutput").ap()
        Pg.dma("sp", o, ap, reads=[], writes=["dbg_" + name], slot="dbg_" + name)

    def sbuf(es, name, shape, dt=F32):
        return es.enter_context(nc.sbuf_tensor("sb_" + name, list(shape), dt))

    pst = [top.enter_context(nc.psum_tensor("pst%d" % i, [128, 1024], BF16)) for i in range(2)]
    psf = [top.enter_context(nc.psum_tensor("psf%d" % i, [128, 512], F32)) for i in range(6)]
    rot = {"f": 0, "t": 0, "n": 6}

    def nxt():
        i = rot["f"] % rot["n"]
        rot["f"] += 1
        return psf[i], "psf%d" % i

    def nxt_t():
        i = rot["t"] % 2
        rot["t"] += 1
        return pst[i], "pst%d" % i

    ident_bf = sbuf(top, "ident_bf", [128, 128], BF16)
    ident_f = sbuf(top, "ident_f", [128, 128], F32)
    ones_row = sbuf(top, "ones_row", [1, 128], F32)
    epsc = sbuf(top, "epsc", [128, 1], F32)
    cols = sbuf(top, "cols", [128, 32], F32)
    gate2b = sbuf(top, "gate2b", [128, D], F32)
    modscr_d = nc.dram_tensor("modscr", [2, D], F32, kind="Internal").ap()
    regs = {"pe": top.enter_context(nc.tensor.register("r_pe")), "act": top.enter_context(nc.scalar.register("r_act")),
            "dve": top.enter_context(nc.vector.register("r_dve")), "pool": top.enter_context(nc.gpsimd.register("r_pool")),
            "sp": top.enter_context(nc.sync.register("r_sp"))}
    a1c, s1c, a2c, s2c = (cols[:, 0:8], cols[:, 8:16], cols[:, 16:24], cols[:, 24:32])

    p1 = ExitStack()
    gate1b = sbuf(p1, "gate1b", [128, D], F32)
    BT_bf = sbuf(p1, "BT_bf", [128, 16, 2, 128], BF16)
    CT_bf = sbuf(p1, "CT_bf", [128, 16, 2, 128], BF16)
    cosT = sbuf(p1, "cosT", [128, 16, ST], BF16)
    sinT = sbuf(p1, "sinT", [128, 16, ST], BF16)
    rcol = sbuf(p1, "rcol", [128, 16], F32)
    carry = sbuf(p1, "carry", [128, 16, 2], F32)
    dcol = sbuf(p1, "dcol", [128, 4], F32)
    glubc = sbuf(p1, "glubc", [128, 4], F32)

    sA = ExitStack()
    PA = Prog(nc, top, "A")
    L3 = sbuf(sA, "L3", [128, 3, 2048], F32)
    Bz = sbuf(sA, "Bz", [128, 2, 2048], F32)
    tA = sbuf(sA, "tA", [128, 4096], F32)
    tB = sbuf(sA, "tB", [128, 4096], F32)
    tC = sbuf(sA, "tC", [128, 4096], F32)
    tI = sbuf(sA, "tI", [128, 4096], I32)
    sm = sbuf(sA, "sm", [128, 3, 16], F32)
    smw = sbuf(sA, "smw", [128, 2, 16], F32)
    jj = sbuf(sA, "jj", [128, ST], F32)
    idl = sbuf(sA, "idl", [128, 128], F32)
    hpic = sbuf(sA, "hpic", [128, 1], F32)

    PA.dma("sp", idl[:], ident_d, writes=["idl"], slot="ld", group=True)
    PA.dma("sp", ident_f[:], ident_d, writes=["ident_f"], slot="ld", group=True)
    for i in range(3):
        PA.dma("sp", L3[:, i, :], ssm_lam_d[i:i + 1, :].partition_broadcast(128), writes=["L3"], slot="ld", group=True)
    for i in range(2):
        PA.dma("sp", Bz[:, i, :], ssm_B_d[i], writes=["Bz"], slot="ld", group=True)
    PA.dma("sp", sm[:], ssm_sm_d, writes=["sm"], slot="ld", group=True)
    PA.dma("sp", jj[:], jj_d, writes=["jj"], slot="ld", group=True)
    PA.dma("sp", dcol[:], ssm_d_d, writes=["dcol"], slot="ld", group=True)
    PA.dma("sp", glubc[:], glu_b_d, writes=["glubc"], slot="ld", group=True)
    PA.cp(ident_bf[:], idl[:], ["idl"], ["ident_bf"])
    PA.memset(ones_row[:], 1.0, ["ones_row"])
    PA.memset(epsc[:], EPS, ["epsc"])
    PA.memset(hpic[:], PI / 2, ["hpic"])
    PA.memset(carry[:], 0.0, ["carry"])

    def range_reduce(Pg, y, x, shift, ntmp, itmp, rx, ry, rn, ri):
        Pg.ts(ntmp, x, 1.0 / TWO_PI, ALU.mult, [rx], [rn], s2=shift / TWO_PI, op1=ALU.add)
        Pg.cp(itmp, ntmp, [rn], [ri])
        Pg.cp(ntmp, itmp, [ri], [rn])
        Pg.ts(y, x, shift, ALU.add, [rx], [ry])
        Pg.stt(y, ntmp, -TWO_PI, y, ALU.mult, ALU.add, [rn, ry], [ry])
        Pg.op("dve", lambda e: e.tensor_single_scalar(ntmp, y, PI, ALU.is_gt), [ry], [rn])
        Pg.stt(y, ntmp, -TWO_PI, y, ALU.mult, ALU.add, [rn, ry], [ry])
        Pg.op("dve", lambda e: e.tensor_single_scalar(ntmp, y, -PI, ALU.is_lt), [ry], [rn])
        Pg.stt(y, ntmp, TWO_PI, y, ALU.mult, ALU.add, [rn, ry], [ry])

    A0, A1 = tA[:, 0:2048], tA[:, 2048:4096]
    B0, B1 = tB[:, 0:2048], tB[:, 2048:4096]
    C0, C1 = tC[:, 0:2048], tC[:, 2048:4096]
    I0 = tI[:, 0:2048]
    are, aim, ldt = L3[:, 0, :], L3[:, 1, :], L3[:, 2, :]
    PA.act(A0, ldt, AF.Exp, ["L3"], ["A0"])
    PA.tt(A1, aim, A0, ALU.mult, ["L3", "A0"], ["A1"])
    PA.tt(B0, are, A0, ALU.mult, ["L3", "A0"], ["B0"])
    PA.act(B0, B0, AF.Exp, ["B0"], ["B0"])
    range_reduce(PA, C0, A1, 0.0, C1, I0, "A1", "C0", "C1", "I0")
    PA.act(B1, C0, AF.Sin, ["C0"], ["B1"])
    PA.act(C1, C0, AF.Abs, ["C0"], ["C1"])
    PA.act(A0, C1, AF.Sin, ["C1", "A0"], ["A0"], bias=hpic[:, 0:1], scale=-1.0)
    PA.tt(A0, B0, A0, ALU.mult, ["B0", "A0"], ["A0"])
    PA.tt(B1, B0, B1, ALU.mult, ["B0", "B1"], ["B1"])
    PA.ts(A0, A0, -1.0, ALU.add, ["A0"], ["A0"])
    PA.tt(C0, are, are, ALU.mult, ["L3"], ["C0"])
    PA.tt(C1, aim, aim, ALU.mult, ["L3"], ["C1"])
    PA.tt(C0, C0, C1, ALU.add, ["C0", "C1"], ["C0"])
    PA.op("dve", lambda e: e.reciprocal(C0, C0), ["C0"], ["C0"])
    PA.tt(C1, A0, are, ALU.mult, ["A0", "L3"], ["C1"])
    PA.tt(B0, B1, aim, ALU.mult, ["B1", "L3"], ["B0"])
    PA.tt(C1, C1, B0, ALU.add, ["C1", "B0"], ["C1"])
    PA.tt(C1, C1, C0, ALU.mult, ["C1", "C0"], ["C1"])
    PA.tt(B0, B1, are, ALU.mult, ["B1", "L3"], ["B0"])
    PA.tt(A1, A0, aim, ALU.mult, ["A0", "L3"], ["A1"])
    PA.tt(B0, B0, A1, ALU.subtract, ["B0", "A1"], ["B0"])
    PA.tt(B0, B0, C0, ALU.mult, ["B0", "C0"], ["B0"])
    Bre, Bim = Bz[:, 0, :], Bz[:, 1, :]
    v3 = lambda ap: ap.rearrange("p (k s) -> p k s", k=16)
    PA.tt(A0, C1, Bre, ALU.mult, ["C1", "Bz"], ["A0"])
    PA.tt(A1, B0, Bim, ALU.mult, ["B0", "Bz"], ["A1"])
    PA.tt(BT_bf[:, :, 0, :], v3(A0), v3(A1), ALU.subtract, ["A0", "A1"], ["BT"])
    PA.tt(A0, C1, Bim, ALU.mult, ["C1", "Bz", "BT"], ["A0"])
    PA.tt(A1, B0, Bre, ALU.mult, ["B0", "Bz", "BT"], ["A1"])
    PA.tt(BT_bf[:, :, 1, :], v3(A0), v3(A1), ALU.add, ["A0", "A1"], ["BT"])
    PA.dma("sp", Bz[:, 0, :], ssm_C_d[0], reads=["BT"], writes=["Bz"], slot="ld2", group=True)
    PA.dma("sp", Bz[:, 1, :], ssm_C_d[1], reads=["BT"], writes=["Bz"], slot="ld2", group=True)
    PA.cp(CT_bf[:, :, 0, :], v3(Bz[:, 0, :]), ["Bz"], ["CT"])
    PA.ts(CT_bf[:, :, 1, :], v3(Bz[:, 1, :]), -1.0, ALU.mult, ["Bz"], ["CT"])
    PA.act(smw[:, 0, :], sm[:, 2, :], AF.Exp, ["sm"], ["smw0"])
    PA.tt(smw[:, 1, :], sm[:, 1, :], smw[:, 0, :], ALU.mult, ["sm", "smw0"], ["smw1"])
    PA.tt(rcol[:], sm[:, 0, :], smw[:, 0, :], ALU.mult, ["sm", "smw0"], ["rcol"])
    PA.act(rcol[:], rcol[:], AF.Exp, ["rcol"], ["rcol"])
    ang = tA[:, :].rearrange("p (k j) -> p k j", k=16)
    PA.tt(ang, jj[:].unsqueeze(1).to_broadcast([128, 16, ST]), smw[:, 1, :].unsqueeze(2).to_broadcast([128, 16, ST]),
          ALU.mult, ["jj", "smw1", "A0", "A1", "BT"], ["tA"])
    range_reduce(PA, tB[:, :], tA[:, :], 0.0, tC[:, :], tI[:, :], "tA", "tB", "tC", "tI")
    PA.act(sinT[:].rearrange("p k j -> p (k j)"), tB[:, :], AF.Sin, ["tB", "B0", "B1", "C0", "C1"], ["sinT"])
    PA.act(tC[:, :], tB[:, :], AF.Abs, ["tB"], ["tC"])
    PA.act(cosT[:].rearrange("p k j -> p (k j)"), tC[:, :], AF.Sin, ["tC"], ["cosT"], bias=hpic[:, 0:1], scale=-1.0)
    PA.barrier()
    if stage == "A":
        dbg_out(PA, "BT", BT_bf[:], [128, 16, 2, 128], BF16)
        dbg_out(PA, "CT", CT_bf[:], [128, 16, 2, 128], BF16)
        dbg_out(PA, "cosT", cosT[:], [128, 16, ST], BF16)
        dbg_out(PA, "sinT", sinT[:], [128, 16, ST], BF16)
        dbg_out(PA, "rcol", rcol[:], [128, 16], F32)
        PA.barrier()
        PA.emit()
        return nc
    PA.emit()
    nc.all_engine_barrier()
    sA.close()

    sB = ExitStack()
    PB = Prog(nc, top, "B")
    modrow = sbuf(sB, "modrow", [1, 6 * D], F32)
    grow = sbuf(sB, "grow", [1, 2 * D], F32)
    arow = sbuf(sB, "arow", [1, 2 * D], F32)
    cTt = sbuf(sB, "cTt", [128, 8], F32)
    adaR = [sbuf(sB, "adaR%d" % i, [128, 1536], F32) for i in range(3)]
    PB.dma("sp", modrow[:], ada_b_d, writes=["modrow"], slot="ld", group=True)
    PB.dma("sp", grow[:, 0:D], g1_d, writes=["grow"], slot="ld", group=True)
    PB.dma("sp", grow[:, D:2 * D], g2_d, writes=["grow"], slot="ld", group=True)
    PB.dma("sp", cTt[:], cT_d, writes=["cTt"], slot="ld", group=True)
    PB.act(cTt[:], cTt[:], AF.Silu, ["cTt"], ["cTt"])
    n_ada = 0
    for qd in range(4):
        banks = [nxt() for _ in range(3)]
        for kc in range(8):
            rb = n_ada % 3
            n_ada += 1
            PB.dma("sp", adaR[rb][:], ada_w_d[kc * 128:(kc + 1) * 128, qd * 1536:(qd + 1) * 1536],
                   writes=["adaR%d" % rb], slot="ada%d" % rb)
            for n in range(3):
                PB.mm(banks[n][0][0:1, :], [(cTt[:, kc:kc + 1], adaR[rb][:, n * 512:(n + 1) * 512])],
                      ["cTt", "adaR%d" % rb], [banks[n][1]], start=(kc == 0), stop=(kc == 7))
        for n in range(3):
            sl = modrow[:, qd * 1536 + n * 512: qd * 1536 + (n + 1) * 512]
            PB.tt(sl, banks[n][0][0:1, :], sl, ALU.add, [banks[n][1], "modrow"], ["modrow"])
    PB.stt(arow[:, 0:D], modrow[:, D:2 * D], 1.0, grow[:, 0:D], ALU.add, ALU.mult, ["modrow", "grow"], ["arow"])
    PB.stt(arow[:, D:2 * D], modrow[:, 4 * D:5 * D], 1.0, grow[:, D:2 * D], ALU.add, ALU.mult, ["modrow", "grow"], ["arow"])
    pc, pcn = nxt()
    vecs = [arow[:, 0:D], modrow[:, 0:D], arow[:, D:2 * D], modrow[:, 3 * D:4 * D]]
    for vi, vec in enumerate(vecs):
        for fc in range(8):
            PB.mm(pc[:, vi * 8 + fc: vi * 8 + fc + 1], [(vec[0:1, fc * 128:(fc + 1) * 128], ones_row[0:1, 0:1])],
                  ["arow", "modrow"], [pcn])
    PB.cp(cols[:], pc[:, 0:32], [pcn], ["cols"])
    for gi, (gsrc, gdst, gname) in enumerate([(modrow[:, 2 * D:3 * D], gate1b, "gate1b"), (modrow[:, 5 * D:6 * D], gate2b, "gate2b"),
                                              ]):
        for h in range(2):
            pb_, pbn = nxt()
            PB.mm(pb_[:, :], [(ones_row[0:1, :], gsrc[0:1, h * 512:(h + 1) * 512])], ["modrow", "arow"], [pbn])
            PB.cp(gdst[:, h * 512:(h + 1) * 512], pb_[:, :], [pbn], [gname])
    PB.dma("sp", modscr_d[0:1, :], arow[:, D:2 * D], reads=["arow"], writes=["modscr0"], slot="ms", group=True)
    PB.dma("sp", modscr_d[1:2, :], modrow[:, 3 * D:4 * D], reads=["modrow"], writes=["modscr1"], slot="ms", group=True)
    PB.barrier()
    if stage == "B":
        dbg_out(PB, "cols", cols[:], [128, 32], F32)
        dbg_out(PB, "gate1b", gate1b[:], [128, D], F32)
        dbg_out(PB, "gate2b", gate2b[:], [128, D], F32)
        PB.barrier()
        PB.emit()
        return nc
    PB.emit()
    nc.all_engine_barrier()
    sB.close()

    PM = Prog(nc, top, "M")
    w_in_bf = sbuf(p1, "w_in_bf", [128, 8, 3584], BF16)
    glu_bf = sbuf(p1, "glu_bf", [128, 4, 512], BF16)
    wa_bf = sbuf(p1, "wa_bf", [128, 4, D], BF16)
    wb_bf = sbuf(p1, "wb_bf", [128, 4, D], BF16)
    wout_bf = sbuf(p1, "wout_bf", [128, 8, D], BF16)
    wsT_bf = sbuf(p1, "wsT_bf", [128, 8, 128], BF16)
    lngb = sbuf(p1, "lngb", [128, 512], F32)
    lnbb = sbuf(p1, "lnbb", [128, 512], F32)
    bs_row = sbuf(p1, "bs_row", [1, 1024], F32)
    xt = [sbuf(p1, "xt%d" % i, [128, D], F32) for i in range(1)]
    xn = sbuf(p1, "xn", [128, D], BF16)
    st = sbuf(p1, "st", [128, 16], F32)
    hT = sbuf(p1, "hT", [128, 8, ST], BF16)
    us32 = sbuf(p1, "us32", [128, 4, ST], F32)
    usbf = sbuf(p1, "usbf", [128, 4, ST], BF16)
    guT = sbuf(p1, "guT", [128, 4, ST], BF16)
    gv = sbuf(p1, "gv", [128, 512], F32)
    gv2 = sbuf(p1, "gv2", [128, 512], F32)
    vn = sbuf(p1, "vn", [128, 2, 512], BF16)
    xf = sbuf(p1, "xf", [128, D], F32)
    sga = sbuf(p1, "sga", [128, 8, ST], BF16)
    sgb = sbuf(p1, "sgb", [128, 8, ST], BF16)
    zT = sbuf(p1, "zT", [128, 4, ST], BF16)
    sg = sbuf(p1, "sg", [128, 4, ST], BF16)
    oT = sbuf(p1, "oT", [128, 4, ST], BF16)
    pT = sbuf(p1, "pT", [128, 4, ST], BF16)
    ta = sbuf(p1, "ta", [128, 2, ST], F32)
    tb = sbuf(p1, "tb", [128, 2, ST], F32)
    mT = sbuf(p1, "mT", [128, 8, ST], BF16)
    t1 = sbuf(p1, "t1", [128, 2, ST], F32)
    t2 = sbuf(p1, "t2", [128, 2, ST], F32)
    wri = sbuf(p1, "wri", [128, 2, ST], F32)
    qri = sbuf(p1, "qri", [128, 2, ST], F32)
    s32 = sbuf(p1, "s32", [128, 2, ST], F32)
    sT = sbuf(p1, "sT", [128, 2, ST], BF16)
    yv = sbuf(p1, "yv", [128, ST], F32)

    for kc in range(8):
        PM.dma("pool", w_in_bf[:, kc, :], w_in_d[kc * 128:(kc + 1) * 128, :], writes=["w_in%d" % kc], slot="wl", group=True)
    PM.dma("pool", glu_bf[:], glu_w_d.rearrange("(c p) n -> p c n", p=128), writes=["glu"], slot="wl", group=True)
    PM.dma("pool", wa_bf[:], wa_d.rearrange("(c p) n -> p c n", p=128), writes=["wa"], slot="wl", group=True)
    PM.dma("pool", wb_bf[:], wb_d.rearrange("(c p) n -> p c n", p=128), writes=["wb"], slot="wl", group=True)
    PM.dma("pool", wout_bf[:], wout_d.rearrange("(c p) n -> p c n", p=128), writes=["wout"], slot="wl", group=True)
    PM.dma("sp", lngb[:], lng_d.partition_broadcast(128), writes=["lngb"], slot="wl2", group=True)
    PM.dma("sp", lnbb[:], lnb_d.partition_broadcast(128), writes=["lnbb"], slot="wl2", group=True)
    PM.dma("sp", bs_row[:], bs_d, writes=["bs_row"], slot="wl2", group=True)
    wsst = us32[:].rearrange("p a n -> p (a n)").rearrange("p (h t) -> p h t", h=8)
    PM.dma("sp", wsst, wsT_d, writes=["us32"], slot="wl2", group=True)
    PM.dma("sp", gv[:, 0:128], tri_d, writes=["gv"], slot="wl2", group=True)
    PM.tt(wsT_bf[:], wsst, gv[:, 0:128].unsqueeze(1).to_broadcast([128, 8, 128]), ALU.mult, ["us32", "gv"], ["wsT_bf"])
    W_IN = ["w_in%d" % kc for kc in range(8)]

    def pairview(ps_):
        return ps_[:, :].rearrange("p (a n) -> p a n", a=2)

    xq = [0]
    rot["n"] = 5
    for s in range(NST):
        def front_norm_tile(ss, i):
            if True:
                tix = 2 * ss + i
                xb = xf
                xbn = "xf"
                PM.dma("sp", xb[:], x_d[tix * 128:(tix + 1) * 128, :], writes=[xbn], slot=xbn)
                PM.memset(st[:, 0:1], 0.0, ["st0"])
                PM.act(xn[:], xb[:], AF.Square, [xbn, "st0"], ["xn", "st0"], accum=st[:, 0:1])
                PM.act(st[:, 1:2], st[:, 0:1], AF.Sqrt, ["st0"], ["st1"], bias=epsc[:, 0:1], scale=1.0 / D)
                PM.op("dve", lambda e: e.reciprocal(st[:, 2:3], st[:, 1:2]), ["st1"], ["st2"])
                PM.ts(xn[:], xb[:], st[:, 2:3], ALU.mult, [xbn, "st2"], ["xn"])
                pt_, ptn = nxt_t()
                for fc in range(8):
                    PM.tr(pt_[:, fc * 128:(fc + 1) * 128], xn[:, fc * 128:(fc + 1) * 128], ident_bf[:], ["xn"], [ptn])
                for fc in range(8):
                    PM.act(hT[:, fc, i * 128:(i + 1) * 128], pt_[:, fc * 128:(fc + 1) * 128], AF.Identity,
                           [ptn], ["hT"], bias=s1c[:, fc:fc + 1], scale=a1c[:, fc:fc + 1])

        def proj_u():
            for pr in range(2):
                ps_, psn = nxt()
                for a in range(2):
                    cc = 2 * pr + a
                    PM.mm(ps_[:, a * ST:(a + 1) * ST], [(w_in_bf[:, kc, cc * 128:(cc + 1) * 128], hT[:, kc, :]) for kc in range(8)],
                          W_IN + ["hT"], [psn])
                PM.act(us32[:, 2 * pr:2 * pr + 2, :], pairview(ps_), AF.Copy, [psn], ["us32"])
            PM.cp(usbf[:], us32[:], ["us32"], ["usbf"], eng="pool")

        def proj_zu():
            for pr in range(2):
                ps_, psn = nxt()
                for a in range(2):
                    cc = 2 * pr + a
                    PM.mm(ps_[:, a * ST:(a + 1) * ST], [(w_in_bf[:, kc, 512 + cc * 128:512 + (cc + 1) * 128], hT[:, kc, :]) for kc in range(8)],
                          W_IN + ["hT"], [psn])
                PM.act(guT[:, 2 * pr:2 * pr + 2, :], pairview(ps_), AF.Gelu_apprx_tanh, [psn], ["guT"])

        def proj_v(i):
            ps_, psn = nxt()
            PM.mm(ps_[:, :], [(hT[:, kc, i * 128:(i + 1) * 128], w_in_bf[:, kc, 1024:1536]) for kc in range(8)],
                  W_IN + ["hT"], [psn])
            PM.memset(st[:, 4:6], 0.0, ["st4"])
            PM.act(gv[:], ps_[:, :], AF.Gelu_apprx_tanh, [psn, "st4"], ["gv", "st4"], accum=st[:, 4:5])
            PM.act(gv2[:], gv[:], AF.Square, ["gv", "st4"], ["gv2", "st4"], accum=st[:, 5:6])
            PM.ts(st[:, 6:7], st[:, 4:5], 1.0 / 512, ALU.mult, ["st4"], ["st6"])
            PM.tt(st[:, 7:8], st[:, 6:7], st[:, 6:7], ALU.mult, ["st6"], ["st7"])
            PM.stt(st[:, 8:9], st[:, 5:6], 1.0 / 512, st[:, 7:8], ALU.mult, ALU.subtract, ["st4", "st7"], ["st8"])
            PM.act(st[:, 9:10], st[:, 8:9], AF.Sqrt, ["st8"], ["st9"], bias=epsc[:, 0:1], scale=1.0)
            PM.op("dve", lambda e: e.reciprocal(st[:, 10:11], st[:, 9:10]), ["st9"], ["st10"])
            PM.stt(st[:, 11:12], st[:, 6:7], -1.0, st[:, 10:11], ALU.mult, ALU.mult, ["st6", "st10"], ["st11"])
            PM.act(gv2[:], gv[:], AF.Identity, ["gv", "st10", "st11"], ["gv2"], bias=st[:, 11:12], scale=st[:, 10:11])
            PM.tt(gv[:], gv2[:], lngb[:], ALU.mult, ["gv2", "lngb"], ["gv"])
            PM.tt(vn[:, i, :], gv[:], lnbb[:], ALU.add, ["gv", "lnbb"], ["vn"])

        def mix(cc):
            ps_, psn = nxt()
            for i in range(2):
                for hh in range(2):
                    h = 2 * cc + hh
                    o_ = ps_[hh * 64:(hh + 1) * 64, i * 128:(i + 1) * 128]

                    def emit(pe, o_=o_, i=i, cc=cc, hh=hh, h=h):
                        pe.matmul(o_, vn[:, i, cc * 128 + hh * 64: cc * 128 + (hh + 1) * 64], wsT_bf[:, h, :], start=True, stop=False)
                        return pe.matmul(o_, ones_row[0:1, 0:64], bs_row[0:1, h * 128:(h + 1) * 128], start=False, stop=True)

                    PM.op("pe", emit, ["vn", "wsT_bf", "bs_row"], [psn])
            PM.tt(pT[:, cc, :], guT[:, cc, :], ps_[:, 0:ST], ALU.mult, ["guT", psn], ["pT"])

        def gates(pr):
            pga, pgan = nxt()
            pgb, pgbn = nxt()
            for a in range(2):
                dc = 2 * pr + a
                sl = slice(a * ST, (a + 1) * ST)
                PM.mm(pga[:, sl], [(w_in_bf[:, kc, 1536 + dc * 128:1536 + (dc + 1) * 128], hT[:, kc, :]) for kc in range(8)], W_IN + ["hT"], [pgan])
                PM.mm(pgb[:, sl], [(w_in_bf[:, kc, 2560 + dc * 128:2560 + (dc + 1) * 128], hT[:, kc, :]) for kc in range(8)], W_IN + ["hT"], [pgbn])
            PM.act(sga[:, 2 * pr:2 * pr + 2, :], pairview(pga), AF.Sigmoid, [pgan], ["sga"])
            PM.act(sgb[:, 2 * pr:2 * pr + 2, :], pairview(pgb), AF.Sigmoid, [pgbn], ["sgb"])

        bu_banks = {}

        def bu(k):
            cc = k // 4
            pb_, pbn = nxt()
            PM.mm(pb_[:, 0:ST], [(BT_bf[:, k, 0, :], usbf[:, cc, :])], ["usbf"], [pbn])
            PM.mm(pb_[:, ST:2 * ST], [(BT_bf[:, k, 1, :], usbf[:, cc, :])], ["usbf"], [pbn])
            bu_banks[k] = (pb_, pbn)

        def ssm_dve(k):
            pb_, pbn = bu_banks[k]
            bv = pb_[:, :].rearrange("p (a n) -> p a n", a=2)
            ckb = cosT[:, k, :].unsqueeze(1).to_broadcast([128, 2, ST])
            skb = sinT[:, k, :].unsqueeze(1).to_broadcast([128, 2, ST])
            PM.tt(t1[:], bv, ckb, ALU.mult, [pbn], ["t1"])
            PM.tt(t2[:], bv, skb, ALU.mult, [pbn], ["t2"])
            PM.tt(wri[:, 0, :], t1[:, 0, :], t2[:, 1, :], ALU.add, ["t1", "t2"], ["wri0"])
            PM.tt(wri[:, 1, :], t1[:, 1, :], t2[:, 0, :], ALU.subtract, ["t1", "t2"], ["wri1"])
            rb_ = rcol[:, k:k + 1].to_broadcast([128, ST])
            PM.scan(qri[:, 0, :], rb_, wri[:, 0, :], carry[:, k, 0:1], ["wri0", "carry%d" % k], ["qri0"])
            PM.scan(qri[:, 1, :], rb_, wri[:, 1, :], carry[:, k, 1:2], ["wri1", "carry%d" % k], ["qri1"])
            PM.tt(t1[:], qri[:], ckb, ALU.mult, ["qri0", "qri1"], ["t1"])
            PM.tt(t2[:], qri[:], skb, ALU.mult, ["qri0", "qri1"], ["t2"])
            PM.tt(s32[:, 0, :], t1[:, 0, :], t2[:, 1, :], ALU.subtract, ["t1", "t2"], ["s32a"])
            PM.tt(s32[:, 1, :], t1[:, 1, :], t2[:, 0, :], ALU.add, ["t1", "t2"], ["s32b"])
            PM.act(sT[:], s32[:], AF.Copy, ["s32a", "s32b"], ["sT"])
            PM.cp(carry[:, k, :], s32[:, :, ST - 1], ["s32a", "s32b"], ["carry%d" % k], eng="pool")

        def ssm_c(k):
            cc = k // 4
            yps, ypn = psf[5], "psf5"
            PM.mm(yps[:, 0:ST], [(CT_bf[:, k, 0, :], sT[:, 0, :]), (CT_bf[:, k, 1, :], sT[:, 1, :])], ["sT"], [ypn],
                  start=(k % 4 == 0), stop=(k % 4 == 3))
            if k % 4 == 3:
                PM.stt(yv[:], us32[:, cc, :], dcol[:, cc:cc + 1], yps[:, 0:ST], ALU.mult, ALU.add, ["us32", ypn], ["yv"])
                PM.act(zT[:, cc, :], yv[:], AF.Gelu_apprx_tanh, ["yv"], ["zT"])

        extras = {0: proj_zu, 1: (lambda: proj_v(0)), 2: (lambda: proj_v(1)),
                  4: (lambda: mix(0)), 5: (lambda: mix(1)), 6: (lambda: mix(2)), 7: (lambda: mix(3)),
                  8: (lambda: gates(0)), 9: (lambda: gates(1)), 10: (lambda: gates(2)), 11: (lambda: gates(3))}
        if s == 0:
            front_norm_tile(0, 0)
            front_norm_tile(0, 1)
        if s + 1 < NST:
            extras[12] = (lambda: front_norm_tile(s + 1, 0))
            extras[13] = (lambda: front_norm_tile(s + 1, 1))
        proj_u()
        bu(0)
        for k in range(16):
            if k + 1 < 16:
                bu(k + 1)
            ssm_dve(k)
            if k in extras:
                extras[k]()
            ssm_c(k)
        for pr in range(2):
            ps_, psn = nxt()
            for a in range(2):
                n = 2 * pr + a
                PM.mm(ps_[:, a * ST:(a + 1) * ST], [(glu_bf[:, cc, n * 128:(n + 1) * 128], zT[:, cc, :]) for cc in range(4)],
                      ["glu", "zT"], [psn])
                PM.act(sg[:, n, :], ps_[:, a * ST:(a + 1) * ST], AF.Sigmoid, [psn], ["sg"], bias=glubc[:, n:n + 1], scale=1.0)
        PM.tt(oT[:], zT[:], sg[:], ALU.mult, ["zT", "sg"], ["oT"])
        for pr in range(4):
            pya, pyan = nxt()
            pyb, pybn = nxt()
            for a in range(2):
                dc = 2 * pr + a
                sl = slice(a * ST, (a + 1) * ST)
                PM.mm(pya[:, sl], [(wa_bf[:, cc, dc * 128:(dc + 1) * 128], oT[:, cc, :]) for cc in range(4)], ["wa", "oT"], [pyan])
                PM.mm(pyb[:, sl], [(wb_bf[:, cc, dc * 128:(dc + 1) * 128], pT[:, cc, :]) for cc in range(4)], ["wb", "pT"], [pybn])
            PM.tt(ta[:], sga[:, 2 * pr:2 * pr + 2, :], pairview(pya), ALU.mult, ["sga", pyan], ["ta"])
            PM.tt(tb[:], sgb[:, 2 * pr:2 * pr + 2, :], pairview(pyb), ALU.mult, ["sgb", pybn], ["tb"])
            PM.tt(mT[:, 2 * pr:2 * pr + 2, :], ta[:], tb[:], ALU.add, ["ta", "tb"], ["mT"])
        for i in range(2):
            tix = 2 * s + i
            xb = xt[0]
            xbn = "xt0"
            PM.dma("sp", xb[:], x_d[tix * 128:(tix + 1) * 128, :], writes=[xbn], slot=xbn)
            for dh in range(2):
                ps_, psn = nxt()
                PM.mm(ps_[:, :], [(mT[:, kc, i * 128:(i + 1) * 128], wout_bf[:, kc, dh * 512:(dh + 1) * 512]) for kc in range(8)],
                      ["mT", "wout"], [psn])
                PM.tt(gv2[:], ps_[:, :], gate1b[:, dh * 512:(dh + 1) * 512], ALU.mult, [psn], ["gv2"])
                PM.tt(xb[:, dh * 512:(dh + 1) * 512], gv2[:], xb[:, dh * 512:(dh + 1) * 512], ALU.add, ["gv2", xbn], [xbn])
            PM.dma("sp", x1s_d[tix * 128:(tix + 1) * 128, :], xb[:], reads=[xbn], writes=["x1s%d" % tix], slot=xbn)
    rot["n"] = 6
    PM.barrier()
    PM.emit()
    if stage == "M":
        return nc
    nc.all_engine_barrier()
    p1.close()

    p2 = ExitStack()
    PX = Prog(nc, top, "X")
    PX.regs = regs
    UW = 256
    NU = T // UW
    x1 = sbuf(p2, "x1", [128, NT, D], F32)
    h2tm = sbuf(p2, "h2tm", [128, NT, D], BF16)
    bgc = sbuf(p2, "bgc", [128, NE, 16], F32)
    posm = sbuf(p2, "posm", [128, NT, NE], F32)
    rwhl = sbuf(p2, "rwhl", [128, NT, NE, 2], BF16)
    iota = sbuf(p2, "iota", [128, UW], F32)
    ne_i = sbuf(p2, "ne_i", [1, NE], I32)
    p2r = ExitStack()
    rwb_bf = sbuf(p2r, "rwb_bf", [128, 8, NE], BF16)
    rbb = sbuf(p2r, "rbb", [128, NE], F32)
    bo_g = sbuf(p2r, "bo_g", [NE, D], F32)
    maskf = sbuf(p2r, "maskf", [128, NT, NE], F32)
    maskb = sbuf(p2r, "maskb", [128, NT, NE], BF16)
    rw = sbuf(p2r, "rw", [128, NT, NE], F32)
    stri_bf = sbuf(p2r, "stri_bf", [128, 128], BF16)
    ones_bf = sbuf(p2r, "ones_bf", [128, 128], BF16)
    cnt = sbuf(p2r, "cnt", [1, 3, NE], F32)
    acc2_ = [sbuf(p2r, "acc2_%d" % i, [128, D], F32) for i in range(2)]
    xn2_ = [sbuf(p2r, "xn2_%d" % i, [128, D], BF16) for i in range(2)]
    h2Tt_ = [sbuf(p2r, "h2Tt_%d" % i, [128, 8, 128], BF16) for i in range(2)]
    rwT_ = [sbuf(p2r, "rwT_%d" % i, [NE, 128], F32) for i in range(2)]
    rt_ = [sbuf(p2r, "rt_%d" % i, [128, 4, NE], F32) for i in range(2)]
    rs_ = [sbuf(p2r, "rs_%d" % i, [128, 24], F32) for i in range(2)]
    rtp = sbuf(p2r, "rtp", [128, 4, NE], F32)
    a2b = sbuf(p2r, "a2b", [128, D], F32)
    s2b = sbuf(p2r, "s2b", [128, D], F32)
    ldf = sbuf(p2r, "ldf", [128, 256], F32)

    PX.dma("sp", rbb[:], rb_d.partition_broadcast(128), writes=["rbb"], slot="ld", group=True)
    PX.dma("sp", a2b[:], modscr_d[0:1, :].partition_broadcast(128), writes=["a2b"], slot="ld", group=True)
    PX.dma("sp", s2b[:], modscr_d[1:2, :].partition_broadcast(128), writes=["s2b"], slot="ld", group=True)
    PX.dma("sp", bgc[:], mbi_d, writes=["bgc"], slot="ld", group=True)
    PX.dma("sp", bo_g[:], mbo_d, writes=["bo_g"], slot="ld", group=True)
    PX.dma("sp", iota[:], iota_d, writes=["iota"], slot="ld", group=True)
    PX.dma("sp", ldf[:, 0:128], stri_d, writes=["ldf"], slot="ld", group=True)
    PX.dma("pool", rwb_bf[:], rw_d.rearrange("(c p) e -> p c e", p=128), writes=["rwb"], slot="ldp")
    PX.cp(stri_bf[:], ldf[:, 0:128], ["ldf"], ["stri_bf"])
    PX.memset(ones_bf[:], 1.0, ["ones_bf"])
    PX.ts(bgc[:, :, 8:16], bgc[:, :, 8:16], 1.0, ALU.add, ["bgc"], ["bgc"])
    PX.tt(bo_g[:], bo_g[:], gate2b[0:NE, :], ALU.mult, ["bo_g"], ["bo_g"])

    for tix in range(NT):
        pb2 = tix % 2
        rs, rt, rwT, h2Tt, xn2, acc2 = rs_[pb2], rt_[pb2], rwT_[pb2], h2Tt_[pb2], xn2_[pb2], acc2_[pb2]
        xb = x1[:, tix, :]
        xbn = "x1_%d" % tix
        PX.dma("sp", xb, x1s_d[tix * 128:(tix + 1) * 128, :], writes=[xbn], slot="x1l%d" % tix)
        PX.memset(rs[:, 0:1], 0.0, ["rs0_%d" % pb2])
        PX.act(xn2[:], xb, AF.Square, [xbn, "rs0_%d" % pb2], ["xn2_%d" % pb2, "rs0_%d" % pb2], accum=rs[:, 0:1])
        PX.act(rs[:, 1:2], rs[:, 0:1], AF.Sqrt, ["rs0_%d" % pb2], ["rs1_%d" % pb2], bias=epsc[:, 0:1], scale=1.0 / D)
        PX.op("dve", lambda e, rs=rs: e.reciprocal(rs[:, 2:3], rs[:, 1:2]), ["rs1_%d" % pb2], ["rs2_%d" % pb2])
        PX.stt(acc2[:], xb, rs[:, 2:3], a2b[:], ALU.mult, ALU.mult, [xbn, "rs2_%d" % pb2], ["acc2_%d" % pb2])
        PX.tt(h2tm[:, tix, :], acc2[:], s2b[:], ALU.add, ["acc2_%d" % pb2], ["h2tm%d" % tix])
        pt_, ptn = nxt_t()
        for fc in range(8):
            PX.tr(pt_[:, fc * 128:(fc + 1) * 128], h2tm[:, tix, fc * 128:(fc + 1) * 128], ident_bf[:], ["h2tm%d" % tix], [ptn])
        PX.act(h2Tt[:].rearrange("p a b -> p (a b)"), pt_[:, :], AF.Copy, [ptn], ["h2Tt_%d" % pb2])
        pl, pln = nxt()
        PX.mm(pl[:, 0:NE], [(h2Tt[:, kc, :], rwb_bf[:, kc, :]) for kc in range(8)], ["h2Tt_%d" % pb2, "rwb"], [pln])
        lg, ex = rt[:, 0, :], rt[:, 2, :]
        PX.tt(lg, pl[:, 0:NE], rbb[:], ALU.add, [pln, "rbb"], ["lg_%d" % pb2])
        PX.op("dve", lambda e, lg=lg, rs=rs: e.max(rs[:, 4:12], lg), ["lg_%d" % pb2], ["rs4_%d" % pb2])
        PX.op("dve", lambda e, lg=lg, tix=tix, rs=rs: e.tensor_single_scalar(maskf[:, tix, :], lg, rs[:, 7:8], ALU.is_ge), ["lg_%d" % pb2, "rs4_%d" % pb2], ["maskf%d" % tix])
        PX.ts(rs[:, 12:13], rs[:, 4:5], -1.0, ALU.mult, ["rs4_%d" % pb2], ["rs12_%d" % pb2])
        PX.act(ex, lg, AF.Exp, ["lg_%d" % pb2, "rs12_%d" % pb2], ["ex_%d" % pb2], bias=rs[:, 12:13], scale=1.0)
        PX.tt(ex, ex, maskf[:, tix, :], ALU.mult, ["ex_%d" % pb2, "maskf%d" % tix], ["ex_%d" % pb2])
        PX.op("dve", lambda e, ex=ex, rs=rs: e.reduce_sum(rs[:, 13:14], ex, mybir.AxisListType.X), ["ex_%d" % pb2], ["rs13_%d" % pb2])
        PX.op("dve", lambda e, rs=rs: e.reciprocal(rs[:, 14:15], rs[:, 13:14]), ["rs13_%d" % pb2], ["rs14_%d" % pb2])
        PX.ts(rw[:, tix, :], ex, rs[:, 14:15], ALU.mult, ["ex_%d" % pb2, "rs14_%d" % pb2], ["rw%d" % tix])
        PX.cp(maskb[:, tix, :], maskf[:, tix, :], ["maskf%d" % tix], ["maskb%d" % tix])
        pr_, prn = nxt()
        PX.op("pe", lambda e, pr_=pr_, tix=tix: e.transpose(pr_[0:NE, 0:128], rw[:, tix, :], ident_f[:]), ["rw%d" % tix], [prn])
        PX.cp(rwT[:], pr_[0:NE, 0:128], [prn], ["rwT_%d" % pb2])
        for dh in range(2):
            pb_, pbn = nxt()
            PX.mm(pb_[:, :], [(rwT[:], bo_g[:, dh * 512:(dh + 1) * 512])], ["rwT_%d" % pb2, "bo_g"], [pbn])
            PX.tt(x1[:, tix, dh * 512:(dh + 1) * 512], pb_[:, :], x1[:, tix, dh * 512:(dh + 1) * 512], ALU.add, [pbn, xbn], [xbn])
        PX.cp(rwhl[:, tix, :, 0], rw[:, tix, :], ["rw%d" % tix], ["rwhl%d" % tix])
        PX.cp(rt[:, 1, :], rwhl[:, tix, :, 0], ["rwhl%d" % tix], ["rt1_%d" % pb2])
        PX.tt(rt[:, 1, :], rw[:, tix, :], rt[:, 1, :], ALU.subtract, ["rw%d" % tix, "rt1_%d" % pb2], ["rt1_%d" % pb2])
        PX.cp(rwhl[:, tix, :, 1], rt[:, 1, :], ["rt1_%d" % pb2], ["rwhl%d" % tix])

    rt = rtp

    MB = ["maskb%d" % t for t in range(NT)]
    for tix in range(NT):
        pp_, ppn = nxt()
        prs = [(ones_bf[:], maskb[:, t2, :]) for t2 in range(tix)] + [(stri_bf[:], maskb[:, tix, :])]
        PX.mm(pp_[:, 0:NE], prs, MB[:tix + 1] + ["ones_bf", "stri_bf"], [ppn])
        PX.ts(rt[:, 1, :], maskf[:, tix, :], -1.0, ALU.add, ["maskf%d" % tix], ["rt1"], s2=1.0e6, op1=ALU.mult)
        PX.tt(rt[:, 3, :], pp_[:, 0:NE], maskf[:, tix, :], ALU.mult, [ppn, "maskf%d" % tix], ["rt3"])
        PX.tt(posm[:, tix, :], rt[:, 3, :], rt[:, 1, :], ALU.add, ["rt3", "rt1"], ["posm"])
    pc_, pcn_ = nxt()
    PX.mm(pc_[0:1, 0:NE], [(ones_bf[:, 0:1], maskb[:, t2, :]) for t2 in range(NT)], MB + ["ones_bf"], [pcn_])
    PX.cp(cnt[:, 0, :], pc_[0:1, 0:NE], [pcn_], ["cnt0"])
    PX.memset(cnt[:, 1, :], 0.0, ["cnt1"])
    for u in range(NU):
        PX.stt(cnt[:, 1, :], cnt[:, 0, :], float(UW * u), cnt[:, 1, :], ALU.is_gt, ALU.add, ["cnt0", "cnt1"], ["cnt1"])
    PX.cp(ne_i[:], cnt[:, 1, :], ["cnt1"], ["ne_i"])
    PX.barrier()
    if stage == "XP":
        dbg_out(PX, "posm", posm[:], [128, NT, NE], F32)
        dbg_out(PX, "rw", rw[:], [128, NT, NE], F32)
        dbg_out(PX, "cnt", cnt[:], [1, 3, NE], F32)
        dbg_out(PX, "x1b", x1[:], [128, NT, D], F32)
        PX.barrier()
        PX.emit()
        return nc
    PX.emit()
    nc.all_engine_barrier()
    p2r.close()

    PX = Prog(nc, top, "E")
    PX.regs = regs
    p2e = ExitStack()
    NWP = 5
    wq = [sbuf(p2e, "wq%d" % i, [128, 8, 512], BF16) for i in range(NWP)]
    wo = sbuf(p2e, "wo", [128, 8, D], BF16)
    SelAllb = [sbuf(p2e, "SelAll%d" % i, [128, NT, UW], BF16) for i in range(2)]
    SelT = sbuf(p2e, "SelT", [128, 2, T], BF16)
    pmub = [sbuf(p2e, "pmu%d" % i, [128, NT], F32) for i in range(2)]
    h2g = sbuf(p2e, "h2g", [128, 8, UW], BF16)
    actT = sbuf(p2e, "actT", [128, 8, UW], BF16)
    g_sb = sbuf(p2e, "g_sb", [128, 2, UW], BF16)
    sg2 = sbuf(p2e, "sg2", [128, 2, UW], BF16)
    u_sb = sbuf(p2e, "u_sb", [128, 2, UW], BF16)
    pp = sbuf(p2e, "pp", [128, 2, UW], BF16)
    yw = sbuf(p2e, "yw", [128, 2, D], BF16)
    rws = sbuf(p2e, "rws", [128, 2], F32)
    rws4 = sbuf(p2e, "rws4", [128, 4], F32)

    NEX = NE

    def piece_slot(e, q):
        return (4 * e + q) % NWP

    def load_piece(e, q):
        sl = piece_slot(e, q)
        src = mwi_d[e].rearrange("(c p) n -> p c n", p=128)
        PX.dma("pool", wq[sl][:, :, 0:256], src[:, :, 256 * q:256 * (q + 1)], writes=["wg%d" % sl], slot="wg%d" % sl)
        PX.dma("pool", wq[sl][:, :, 256:512], src[:, :, D + 256 * q:D + 256 * (q + 1)], writes=["wu%d" % sl], slot="wu%d" % sl)

    def load_wo(e):
        PX.dma("pool", wo[:], mwo_d[e].rearrange("(c p) n -> p c n", p=128), writes=["wo"], slot="wo")

    for q in range(4):
        load_piece(0, q)

    def build_sel(e, u):
        pb = e % 2
        PX.ts(pmub[pb][:], posm[:, :, e], float(-UW * u), ALU.add, ["posm"], ["pmu%d" % pb])
        for t2 in range(NT):
            PX.op("dve", lambda eng, t2=t2, pb=pb: eng.tensor_single_scalar(SelAllb[pb][:, t2, :], iota[:], pmub[pb][:, t2:t2 + 1], ALU.is_equal),
                  ["pmu%d" % pb, "iota"], ["Sel%d_%d" % (pb, t2)])

    build_sel(0, 0)
    H2 = ["h2tm%d" % t for t in range(NT)]
    X1 = ["x1_%d" % t for t in range(NT)]
    for e in range(NEX):
        if e + 1 < NE:
            load_piece(e + 1, 0)
        load_wo(e)
        PX.regload(regs, ne_i[0:1, e:e + 1], ["ne_i"])
        if e + 1 < NEX:
            build_sel(e + 1, 0)
        for u in range(NU):
            PX.marker(("rb", u + 1))
            SelAll = SelAllb[e % 2]
            SEL = ["Sel%d_%d" % (e % 2, t2) for t2 in range(NT)]
            for fp in range(4):
                ps_, psn = nxt()
                for a in range(2):
                    fc = 2 * fp + a
                    PX.mm(ps_[:, a * UW:(a + 1) * UW], [(h2tm[:, t2, fc * 128:(fc + 1) * 128], SelAll[:, t2, :]) for t2 in range(NT)],
                          H2 + SEL, [psn])
                PX.act(h2g[:, 2 * fp:2 * fp + 2, :], ps_[:, :].rearrange("p (a n) -> p a n", a=2), AF.Copy, [psn], ["h2g"])
            for st_ in range(2):
                for h8 in range(2):
                    pt_, ptn = nxt_t()
                    for i8 in range(8):
                        t2 = 8 * h8 + i8
                        PX.tr(pt_[:, i8 * 128:(i8 + 1) * 128], SelAll[:, t2, st_ * 128:(st_ + 1) * 128], ident_bf[:], ["Sel%d_%d" % (e % 2, t2)], [ptn])
                    PX.act(SelT[:, st_, h8 * 1024:(h8 + 1) * 1024], pt_[:, :], AF.Copy, [ptn], ["SelT"])
            for jp in range(4):
                sl = piece_slot(e, jp)
                pg_, pgn = nxt()
                pu_, pun = nxt()
                for a in range(2):
                    PX.mm(pg_[:, a * UW:(a + 1) * UW], [(wq[sl][:, kc, a * 128:(a + 1) * 128], h2g[:, kc, :]) for kc in range(8)], ["wg%d" % sl, "h2g"], [pgn])
                    PX.mm(pu_[:, a * UW:(a + 1) * UW], [(wq[sl][:, kc, 256 + a * 128:256 + (a + 1) * 128], h2g[:, kc, :]) for kc in range(8)], ["wu%d" % sl, "h2g"], [pun])
                for a in range(2):
                    j = 2 * jp + a
                    PX.ts(g_sb[:, a, :], pg_[:, a * UW:(a + 1) * UW], bgc[:, e, j:j + 1], ALU.add, [pgn, "bgc"], ["g_sb"], s2=7.0, op1=ALU.min)
                    PX.ts(u_sb[:, a, :], pu_[:, a * UW:(a + 1) * UW], bgc[:, e, 8 + j:9 + j], ALU.add, [pun, "bgc"], ["u_sb"], s2=8.0, op1=ALU.min)
                PX.act(sg2[:], g_sb[:], AF.Sigmoid, ["g_sb"], ["sg2"], scale=1.702)
                PX.tt(pp[:], g_sb[:], sg2[:], ALU.mult, ["g_sb", "sg2"], ["pp"])
                PX.stt(actT[:, 2 * jp:2 * jp + 2, :], u_sb[:], -6.0, pp[:], ALU.max, ALU.mult, ["u_sb", "pp"], ["actT%d" % jp])
            prw, prwn = nxt()
            for st_ in range(2):
                PX.mm(prw[:, 2 * st_:2 * st_ + 2], [(SelAll[:, t2, st_ * 128:(st_ + 1) * 128], rwhl[:, t2, e, :]) for t2 in range(NT)],
                      SEL + ["rwhl"], [prwn])
            PX.act(rws4[:], prw[:, 0:4], AF.Copy, [prwn], ["rws4"])
            prv = rws4[:].rearrange("p (s h) -> p s h", s=2)
            PX.tt(rws[:], prv[:, :, 0], prv[:, :, 1], ALU.add, ["rws4"], ["rws"])
            for st_ in range(2):
                for dh in range(2):
                    po, pon = nxt()
                    PX.mm(po[:, :], [(actT[:, jc, st_ * 128:(st_ + 1) * 128], wo[:, jc, dh * 512:(dh + 1) * 512]) for jc in range(8)],
                          ["actT%d" % jp for jp in range(4)] + ["wo"], [pon])
                    PX.stt(yw[:, st_, dh * 512:(dh + 1) * 512], po[:, :], rws[:, st_:st_ + 1], gate2b[:, dh * 512:(dh + 1) * 512],
                           ALU.mult, ALU.mult, [pon, "rws"], ["yw"])
            if u + 1 < NU:
                build_sel(e, u + 1)
            for t2 in range(NT):
                for dh in range(2):
                    po, pon = nxt()
                    PX.mm(po[:, :], [(SelT[:, st_, t2 * 128:(t2 + 1) * 128], yw[:, st_, dh * 512:(dh + 1) * 512]) for st_ in range(2)],
                          ["SelT", "yw"], [pon])
                    PX.tt(x1[:, t2, dh * 512:(dh + 1) * 512], po[:, :], x1[:, t2, dh * 512:(dh + 1) * 512], ALU.add, [pon, X1[t2]], [X1[t2]])
        for u in range(NU):
            PX.marker(("re",))
        if e + 1 < NE:
            for q in range(1, 4):
                load_piece(e + 1, q)
    PX.barrier()
    if stage == "XE":
        dbg_out(PX, "x2", x1[:], [128, NT, D], F32)
        PX.barrier()
        PX.emit()
        return nc
    PX.emit()
    nc.all_engine_barrier()
    p2e.close()

    PX = Prog(nc, top, "F")
    fgb = sbuf(p2, "fgb", [128, D], F32)
    xn2 = sbuf(p2, "xn2f", [128, D], BF16)
    rs = sbuf(p2, "rsf", [128, 8], F32)
    PX.dma("sp", fgb[:], fg_d.partition_broadcast(128), writes=["fgb"], slot="ld", group=True)
    for tix in range(NT):
        r_ = "x1_%d" % tix
        PX.memset(rs[:, 0:1], 0.0, ["rs0"])
        PX.act(xn2[:], x1[:, tix, :], AF.Square, [r_, "rs0"], ["xn2", "rs0"], accum=rs[:, 0:1])
        PX.act(rs[:, 1:2], rs[:, 0:1], AF.Sqrt, ["rs0"], ["rs1"], bias=epsc[:, 0:1], scale=1.0 / D)
        PX.op("dve", lambda e: e.reciprocal(rs[:, 2:3], rs[:, 1:2]), ["rs1"], ["rs2"])
        PX.stt(x1[:, tix, :], x1[:, tix, :], rs[:, 2:3], fgb[:], ALU.mult, ALU.mult, [r_, "rs2", "fgb"], [r_])
        PX.dma("sp", out_d[tix * 128:(tix + 1) * 128, :], x1[:, tix, :], reads=[r_], writes=["out%d" % tix], slot="outs", group=True)
    PX.barrier()
    PX.emit()
    p2.close()
    top.close()
    return nc


def _prep(inputs):
    f = lambda a: np.ascontiguousarray(np.asarray(a), dtype=np.float32)
    sh = {}
    sh["ada_w"] = f(inputs["ada_w"][0])
    sh["ada_b"] = f(inputs["ada_b"][0][None])
    sh["g1"] = f(inputs["norm1_g"][0][None])
    sh["g2"] = f(inputs["norm2_g"][0][None])
    sh["fg"] = f(np.asarray(inputs["final_g"])[None])
    sh["w_in"] = f(inputs["w_in"][0])
    a_re = np.asarray(inputs["ssm_a_re"][0])
    a_im = np.asarray(inputs["ssm_a_im"][0])
    ldt = np.broadcast_to(np.asarray(inputs["ssm_log_dt"][0])[:, None], (32, 64))
    smf = lambda v: np.asarray(v).reshape(16, 2, 64).transpose(1, 2, 0).reshape(128, 16)
    sh["ssm_sm"] = f(np.stack([smf(a_re), smf(a_im), smf(ldt)], 1))
    sh["ssm_lam"] = f(np.stack([a_re.reshape(-1), a_im.reshape(-1), np.ascontiguousarray(ldt).reshape(-1)]))
    b = [np.asarray(inputs["ssm_b_re"][0]), np.asarray(inputs["ssm_b_im"][0])]
    c = [np.asarray(inputs["ssm_c_re"][0]), np.asarray(inputs["ssm_c_im"][0])]
    Bz = np.zeros((2, 128, 16, 128), np.float32)
    Cz = np.zeros((2, 128, 16, 128), np.float32)
    for k in range(16):
        for g2 in range(2):
            g = 2 * k + g2
            g8 = g % 8
            for ri in range(2):
                Bz[ri, g8 * 16:(g8 + 1) * 16, k, g2 * 64:(g2 + 1) * 64] = b[ri][g].T
                Cz[ri, g2 * 64:(g2 + 1) * 64, k, g8 * 16:(g8 + 1) * 16] = c[ri][g].T
    sh["ssm_B"] = Bz.reshape(2, 128, 2048)
    sh["ssm_C"] = Cz.reshape(2, 128, 2048)
    sh["ssm_d"] = f(np.asarray(inputs["ssm_d"][0]).reshape(4, 128).T)
    sh["glu_w"] = f(inputs["ssm_glu_w"][0])
    sh["glu_b"] = f(np.asarray(inputs["ssm_glu_b"][0]).reshape(4, 128).T)
    sh["wa"] = f(inputs["w_branch_a"][0])
    sh["wb"] = f(inputs["w_branch_b"][0])
    sh["wout"] = f(inputs["w_out"][0])
    sh["lng"] = f(inputs["gmlp_ln_g"][0][None])
    sh["lnb"] = f(inputs["gmlp_ln_b"][0][None])
    sh["wsT"] = f(np.asarray(inputs["gmlp_ws"][0]).transpose(2, 0, 1))
    sh["bs"] = f(np.asarray(inputs["gmlp_bs"][0]).reshape(1, 1024))
    sh["router_w"] = f(inputs["router_w"][0])
    sh["router_b"] = f(inputs["router_b"][0][None])
    sh["moe_w_in"] = f(inputs["moe_w_in"][0])
    sh["moe_b_in"] = f(np.asarray(inputs["moe_b_in"][0]).reshape(32, 16, 128).transpose(2, 0, 1))
    sh["moe_w_out"] = f(inputs["moe_w_out"][0])
    sh["moe_b_out"] = f(inputs["moe_b_out"][0])
    sh["ident"] = np.eye(128, dtype=np.float32)
    sh["tri"] = np.triu(np.ones((128, 128), np.float32))
    sh["stri"] = np.triu(np.ones((128, 128), np.float32), 1)
    sh["iota256"] = np.ascontiguousarray(np.broadcast_to(np.arange(256, dtype=np.float32)[None], (128, 256)))
    sh["eoff"] = np.ascontiguousarray(np.broadcast_to((2048.0 * np.arange(32, dtype=np.float32) + 1.0)[None], (128, 32)))
    sh["jj"] = np.ascontiguousarray(np.broadcast_to(np.arange(1, ST + 1, dtype=np.float32)[None], (128, ST)))
    return sh


def kernel(**inputs):
    sh = _prep(inputs)
    x = np.asarray(inputs["x"], dtype=np.float32)
    c = np.asarray(inputs["c"], dtype=np.float32)
    in_maps = []
    for b in range(8):
        m = dict(sh)
        m["x"] = np.ascontiguousarray(x[b])
        m["cT"] = np.ascontiguousarray(c[b].reshape(8, 128).T)
        in_maps.append(m)
    nc = build()
    res = run_bass_kernel_spmd(nc, in_maps, core_ids=list(range(8)))
    return np.stack([np.asarray(r["out"], dtype=np.float32) for r in res.results], 0)
```
